# Optimizing a Trainium2 kernel written in Bass

```python
import math
import jax
import jax.numpy as jnp
from jax import lax
import numpy as np

D_MODEL = 1024
BATCH = 4
SEQ = 4096
DEPTH = 4

SSD_HEADS = 16
SSD_HEAD_DIM = 64
SSD_WIDTH = SSD_HEADS * SSD_HEAD_DIM
SSD_GROUPS = 2
SSD_STATE = 128
SSD_BC = SSD_GROUPS * SSD_STATE
SSD_XBC = SSD_WIDTH + 2 * SSD_BC
SSD_CONV = 4
SSD_CHUNK = 128
GDN_HEADS = 4
GDN_HEAD_DIM = 128
GDN_WIDTH = GDN_HEADS * GDN_HEAD_DIM
GDN_CONV = 4
GDN_CHUNK = 64
HG_HEADS = 4
HG_HEAD_DIM = 128
HG_WIDTH = HG_HEADS * HG_HEAD_DIM
HG_CHUNK = 16
D_MIX = SSD_WIDTH + GDN_WIDTH + HG_WIDTH
IN_COLS = SSD_WIDTH + SSD_XBC + SSD_HEADS + 4 * GDN_WIDTH + 2 * GDN_HEADS + 4 * HG_WIDTH
MOE_GROUPS = 4
EXPERTS_PER_GROUP = 4
N_EXPERTS = MOE_GROUPS * EXPERTS_PER_GROUP
MOE_TOP_K = 2
D_EXPERT = 256
DN_ALPHA = (2 * DEPTH) ** 0.25
DN_BETA = (8 * DEPTH) ** -0.25

kernel_name = 'hymba_style_ssd_gdn_hgrn2_hmoe_deepnorm'


def _in_proj_splits():
    sizes = (SSD_WIDTH, SSD_XBC, SSD_HEADS, 3 * GDN_WIDTH, GDN_WIDTH, GDN_HEADS, GDN_HEADS,
             HG_WIDTH, HG_WIDTH, HG_WIDTH, HG_WIDTH)
    pts, acc = [], 0
    for s in sizes[:-1]:
        acc += s
        pts.append(acc)
    return pts


def _layer_norm(x, g, b, eps=1e-5):
    xf = x.astype(jnp.float32)
    mu = jnp.mean(xf, axis=-1, keepdims=True)
    var = jnp.mean(jnp.square(xf - mu), axis=-1, keepdims=True)
    return ((xf - mu) * lax.rsqrt(var + eps) * g.astype(jnp.float32) + b.astype(jnp.float32)).astype(x.dtype)


def _rms_normalize(x, eps=1e-6):
    xf = x.astype(jnp.float32)
    return xf * lax.rsqrt(jnp.mean(xf * xf, axis=-1, keepdims=True) + eps)


def _l2_normalize(x, eps=1e-6):
    return x * lax.rsqrt(jnp.sum(x * x, axis=-1, keepdims=True) + eps)


def _causal_conv(x, w):
    k = w.shape[0]
    seq = x.shape[1]
    xp = jnp.pad(x, ((0, 0), (k - 1, 0), (0, 0)))
    out = xp[:, 0:seq, :] * w[0]
    for i in range(1, k):
        out = out + xp[:, i:i + seq, :] * w[i]
    return out


def ssd_mixer(z, xbc, dt_raw, conv_w, conv_b, dt_bias, a_log, d_skip, norm_w):
    f32 = jnp.float32
    bsz, seq, _ = z.shape
    nc, cl = seq // SSD_CHUNK, SSD_CHUNK
    g, hg, p, n = SSD_GROUPS, SSD_HEADS // SSD_GROUPS, SSD_HEAD_DIM, SSD_STATE
    xbc = jax.nn.silu(_causal_conv(xbc, conv_w) + conv_b).astype(f32)
    xs, bm, cm = jnp.split(xbc, [SSD_WIDTH, SSD_WIDTH + SSD_BC], axis=-1)
    xs = xs.reshape(bsz, nc, cl, g, hg, p)
    bm = bm.reshape(bsz, nc, cl, g, n)
    cm = cm.reshape(bsz, nc, cl, g, n)
    dt = jax.nn.softplus(dt_raw.astype(f32) + dt_bias.astype(f32)).reshape(bsz, nc, cl, g, hg)
    a = -jnp.exp(a_log.astype(f32)).reshape(g, hg)
    acum = jnp.cumsum(dt * a, axis=2)
    xdt = xs * dt[..., None]
    causal = jnp.tril(jnp.ones((cl, cl), dtype=bool))[:, :, None, None]
    seg = acum[:, :, :, None] - acum[:, :, None, :]
    decay = jnp.exp(jnp.where(causal, seg, -jnp.inf))
    cb = jnp.einsum('bclgn,bcsgn->bclsg', cm, bm)
    y_diag = jnp.einsum('bclsgh,bcsghp->bclghp', cb[..., None] * decay, xdt)
    to_end = jnp.exp(acum[:, :, -1:] - acum)
    chunk_states = jnp.einsum('bclgn,bclghp->bcghpn', bm, xdt * to_end[..., None])
    chunk_decay = jnp.exp(acum[:, :, -1])

    def step(state, inp):
        cs, cd = inp
        return state * cd[..., None, None] + cs, state

    init = jnp.zeros((bsz, g, hg, p, n), f32)
    _, prev = lax.scan(step, init, (jnp.moveaxis(chunk_states, 1, 0), jnp.moveaxis(chunk_decay, 1, 0)))
    prev = jnp.moveaxis(prev, 0, 1)
    y_off = jnp.einsum('bclgn,bcghpn->bclghp', cm, prev) * jnp.exp(acum)[..., None]
    y = y_diag + y_off + xs * d_skip.astype(f32).reshape(g, hg, 1)
    y = y.reshape(bsz, seq, SSD_WIDTH) * jax.nn.silu(z.astype(f32))
    y = _rms_normalize(y.reshape(bsz, seq, g, SSD_WIDTH // g)).reshape(bsz, seq, SSD_WIDTH)
    return (y * norm_w.astype(f32)).astype(z.dtype)


def gdn_mixer(qkv, gate, b_raw, a_raw, conv_w, dt_bias, a_log, norm_w):
    f32 = jnp.float32
    bsz, seq, _ = qkv.shape
    nh, dh, cl = GDN_HEADS, GDN_HEAD_DIM, GDN_CHUNK
    nc = seq // cl
    qkv = jax.nn.silu(_causal_conv(qkv, conv_w)).astype(f32)
    q, k, v = jnp.split(qkv, 3, axis=-1)

    def heads(t):
        return t.reshape(bsz, nc, cl, nh, dh).transpose(0, 3, 1, 2, 4)

    q = _l2_normalize(heads(q)) * (dh ** -0.5)
    k = _l2_normalize(heads(k))
    v = heads(v)
    beta = jax.nn.sigmoid(b_raw.astype(f32)).reshape(bsz, nc, cl, nh).transpose(0, 3, 1, 2)
    log_a = -jnp.exp(a_log.astype(f32)) * jax.nn.softplus(a_raw.astype(f32) + dt_bias.astype(f32))
    gcum = jnp.cumsum(log_a.reshape(bsz, nc, cl, nh).transpose(0, 3, 1, 2), axis=-1)
    incl = jnp.tril(jnp.ones((cl, cl), dtype=bool))
    strict = jnp.tril(jnp.ones((cl, cl), dtype=bool), -1)
    decay = jnp.exp(jnp.where(incl, gcum[..., :, None] - gcum[..., None, :], -jnp.inf))
    kb = k * beta[..., None]
    m = jnp.where(strict, jnp.einsum('bhnid,bhnjd->bhnij', kb, k) * decay, 0.0)
    a_mat = m + jnp.eye(cl, dtype=f32)
    rhs = jnp.concatenate([v * beta[..., None], kb * jnp.exp(gcum)[..., None]], axis=-1)
    sol = lax.linalg.triangular_solve(a_mat, rhs, left_side=True, lower=True, unit_diagonal=True)
    u, w = jnp.split(sol, 2, axis=-1)
    qk = jnp.einsum('bhnid,bhnjd->bhnij', q, k) * decay
    qg = q * jnp.exp(gcum)[..., None]
    kd = k * jnp.exp(gcum[..., -1:] - gcum)[..., None]
    gl = jnp.exp(gcum[..., -1])

    def step(state, inp):
        u_c, w_c, qk_c, qg_c, kd_c, gl_c = inp
        v_new = u_c - jnp.einsum('bhcd,bhde->bhce', w_c, state)
        o = jnp.einsum('bhcd,bhde->bhce', qg_c, state) + jnp.einsum('bhij,bhje->bhie', qk_c, v_new)
        state = state * gl_c[..., None, None] + jnp.einsum('bhcd,bhce->bhde', kd_c, v_new)
        return state, o

    init = jnp.zeros((bsz, nh, dh, dh), f32)
    _, o = lax.scan(step, init, tuple(jnp.moveaxis(t, 2, 0) for t in (u, w, qk, qg, kd, gl)))
    o = jnp.moveaxis(o, 0, 2).transpose(0, 2, 3, 1, 4).reshape(bsz, seq, nh, dh)
    o = _rms_normalize(o) * norm_w.astype(f32) * jax.nn.silu(gate.astype(f32).reshape(bsz, seq, nh, dh))
    return o.reshape(bsz, seq, GDN_WIDTH).astype(qkv.dtype)


def hgrn2_mixer(q_raw, f_raw, i_raw, gate, lb, norm_w):
    f32 = jnp.float32
    bsz, seq, _ = q_raw.shape
    nh, dk, cl = HG_HEADS, HG_HEAD_DIM, HG_CHUNK
    nc = seq // cl
    f_raw = f_raw.astype(f32)
    lb = lb.astype(f32)
    log_f = jnp.logaddexp(jnp.log(lb), jnp.log1p(-lb) + jax.nn.log_sigmoid(f_raw))
    key_in = (1.0 - lb) * jax.nn.sigmoid(-f_raw)

    def heads(t):
        return t.astype(f32).reshape(bsz, nc, cl, nh, dk).transpose(0, 3, 1, 2, 4)

    q = heads(jax.nn.silu(q_raw.astype(f32)))
    k = heads(key_in)
    v = heads(i_raw)
    bcum = jnp.cumsum(heads(log_f), axis=3)
    causal = jnp.tril(jnp.ones((cl, cl), dtype=bool))[:, :, None]
    seg = bcum[:, :, :, :, None, :] - bcum[:, :, :, None, :, :]
    decay = jnp.exp(jnp.where(causal, seg, -jnp.inf))
    scores = jnp.einsum('bhntsd,bhnsd->bhnts', q[:, :, :, :, None, :] * decay, k)
    o_intra = jnp.einsum('bhnts,bhnse->bhnte', scores, v)
    qg = q * jnp.exp(bcum)
    kd = k * jnp.exp(bcum[:, :, :, -1:] - bcum)
    gl = jnp.exp(bcum[:, :, :, -1])

    def step(state, inp):
        qg_c, kd_c, v_c, gl_c = inp
        o = jnp.einsum('bhtd,bhde->bhte', qg_c, state)
        state = state * gl_c[..., None] + jnp.einsum('bhsd,bhse->bhde', kd_c, v_c)
        return state, o

    init = jnp.zeros((bsz, nh, dk, dk), f32)
    _, o_inter = lax.scan(step, init, tuple(jnp.moveaxis(t, 2, 0) for t in (qg, kd, v, gl)))
    o = o_intra + jnp.moveaxis(o_inter, 0, 2)
    o = o.transpose(0, 2, 3, 1, 4).reshape(bsz, seq, nh, dk)
    o = _rms_normalize(o) * norm_w.astype(f32) * jax.nn.silu(gate.astype(f32).reshape(bsz, seq, nh, dk))
    return o.reshape(bsz, seq, HG_WIDTH).astype(q_raw.dtype)


def hier_moe(x, w_rg, b_rg, w_re, b_re, w_gu, w_dn):
    f32 = jnp.float32
    bsz, seq, d = x.shape
    t = x.reshape(-1, d)
    pg = jax.nn.softmax((t @ w_rg).astype(f32) + b_rg.astype(f32), axis=-1)
    g_p, g_idx = lax.top_k(pg, 1)
    le = ((t @ w_re).astype(f32) + b_re.astype(f32)).reshape(-1, MOE_GROUPS, EXPERTS_PER_GROUP)
    le = jnp.take_along_axis(le, g_idx[:, :, None], axis=1)[:, 0]
    pe = jax.nn.softmax(le, axis=-1)
    e_p, e_idx = lax.top_k(pe, MOE_TOP_K)
    wts = g_p * e_p / jnp.sum(e_p, axis=-1, keepdims=True)
    eid = g_idx * EXPERTS_PER_GROUP + e_idx
    combine = jnp.sum(jax.nn.one_hot(eid, N_EXPERTS, dtype=f32) * wts[..., None], axis=1)
    gu = jnp.einsum('td,edf->tef', t, w_gu)
    g_half, u_half = jnp.split(gu, 2, axis=-1)
    h = jax.nn.silu(g_half) * u_half * combine[..., None].astype(t.dtype)
    y = jnp.einsum('tef,efd->td', h, w_dn)
    return y.reshape(bsz, seq, d)


def setup_inputs(seed: int = 0) -> dict:
    key = jax.random.key(seed)
    ks = jax.random.split(key, 32)
    f32 = jnp.float32

    def nrm(k, shape, s):
        return jax.random.normal(k, shape, f32) * s

    def dt_bias_init(k, n_heads):
        u = jax.random.uniform(k, (DEPTH, n_heads), f32)
        dt = jnp.exp(u * (math.log(0.1) - math.log(0.001)) + math.log(0.001))
        dt = jnp.maximum(dt, 1e-4)
        return dt + jnp.log(-jnp.expm1(-dt))

    return {
        'x': nrm(ks[0], (BATCH, SEQ, D_MODEL), 1.0),
        'w_in': nrm(ks[1], (DEPTH, D_MODEL, IN_COLS), D_MODEL ** -0.5),
        'ssd_conv_w': nrm(ks[2], (DEPTH, SSD_CONV, SSD_XBC), SSD_CONV ** -0.5),
        'ssd_conv_b': nrm(ks[3], (DEPTH, SSD_XBC), 0.02),
        'ssd_dt_bias': dt_bias_init(ks[4], SSD_HEADS),
        'ssd_a_log': jnp.log(jax.random.uniform(ks[5], (DEPTH, SSD_HEADS), f32, 1.0, 16.0)),
        'ssd_d': 1.0 + nrm(ks[6], (DEPTH, SSD_HEADS), 0.02),
        'ssd_norm_w': 1.0 + nrm(ks[7], (DEPTH, SSD_WIDTH), 0.02),
        'gdn_conv_w': nrm(ks[8], (DEPTH, GDN_CONV, 3 * GDN_WIDTH), GDN_CONV ** -0.5),
        'gdn_dt_bias': dt_bias_init(ks[9], GDN_HEADS),
        'gdn_a_log': jnp.log(jax.random.uniform(ks[10], (DEPTH, GDN_HEADS), f32, 1.0, 16.0)),
        'gdn_norm_w': 1.0 + nrm(ks[11], (DEPTH, GDN_HEAD_DIM), 0.02),
        'hg_lb_logits': nrm(ks[12], (DEPTH, HG_WIDTH), 0.1),
        'hg_norm_w': 1.0 + nrm(ks[13], (DEPTH, HG_HEAD_DIM), 0.02),
        'w_out': nrm(ks[14], (DEPTH, D_MIX, D_MODEL), DN_BETA * D_MIX ** -0.5),
        'ln1_g': 1.0 + nrm(ks[15], (DEPTH, D_MODEL), 0.02),
        'ln1_b': nrm(ks[16], (DEPTH, D_MODEL), 0.02),
        'w_router_group': nrm(ks[17], (DEPTH, D_MODEL, MOE_GROUPS), D_MODEL ** -0.5),
        'b_router_group': nrm(ks[18], (DEPTH, MOE_GROUPS), 0.01),
        'w_router_expert': nrm(ks[19], (DEPTH, D_MODEL, N_EXPERTS), D_MODEL ** -0.5),
        'b_router_expert': nrm(ks[20], (DEPTH, N_EXPERTS), 0.01),
        'w_expert_gate_up': nrm(ks[21], (DEPTH, N_EXPERTS, D_MODEL, 2 * D_EXPERT), D_MODEL ** -0.5),
        'w_expert_down': nrm(ks[22], (DEPTH, N_EXPERTS, D_EXPERT, D_MODEL), DN_BETA * D_EXPERT ** -0.5),
        'ln2_g': 1.0 + nrm(ks[23], (DEPTH, D_MODEL), 0.02),
        'ln2_b': nrm(ks[24], (DEPTH, D_MODEL), 0.02),
    }


def reference(x, w_in, ssd_conv_w, ssd_conv_b, ssd_dt_bias, ssd_a_log, ssd_d, ssd_norm_w,
              gdn_conv_w, gdn_dt_bias, gdn_a_log, gdn_norm_w, hg_lb_logits, hg_norm_w, w_out,
              ln1_g, ln1_b, w_router_group, b_router_group, w_router_expert, b_router_expert,
              w_expert_gate_up, w_expert_down, ln2_g, ln2_b):
    splits = _in_proj_splits()
    lb_cum = jnp.cumsum(jax.nn.softmax(hg_lb_logits.astype(jnp.float32), axis=0), axis=0)
    lb_all = lb_cum - lb_cum[0:1]
    h = x
    for l in range(DEPTH):
        proj = jnp.einsum('bld,dc->blc', h, w_in[l])
        (z, xbc, dt_raw, qkv, gdn_gate, gdn_b, gdn_a,
         hg_q, hg_f, hg_i, hg_gate) = jnp.split(proj, splits, axis=-1)
        y_ssd = ssd_mixer(z, xbc, dt_raw, ssd_conv_w[l], ssd_conv_b[l], ssd_dt_bias[l],
                          ssd_a_log[l], ssd_d[l], ssd_norm_w[l])
        y_gdn = gdn_mixer(qkv, gdn_gate, gdn_b, gdn_a, gdn_conv_w[l], gdn_dt_bias[l],
                          gdn_a_log[l], gdn_norm_w[l])
        y_hg = hgrn2_mixer(hg_q, hg_f, hg_i, hg_gate, lb_all[l], hg_norm_w[l])
        mix = jnp.einsum('blc,cd->bld', jnp.concatenate([y_ssd, y_gdn, y_hg], axis=-1), w_out[l])
        h = _layer_norm(DN_ALPHA * h + mix, ln1_g[l], ln1_b[l])
        ffn = hier_moe(h, w_router_group[l], b_router_group[l], w_router_expert[l],
                       b_router_expert[l], w_expert_gate_up[l], w_expert_down[l])
        h = _layer_norm(DN_ALPHA * h + ffn, ln2_g[l], ln2_b[l])
    return h
```

```python
import numpy as np
from contextlib import ExitStack
import concourse.bass as bass
import concourse.mybir as mybir
from concourse.bass_utils import run_bass_kernel_spmd

F32 = mybir.dt.float32
BF16 = mybir.dt.bfloat16
AF = mybir.ActivationFunctionType
ALU = mybir.AluOpType
AX = mybir.AxisListType

COMPUTE = ('pe', 'dve', 'act', 'pool')
ALLENG = ('pe', 'dve', 'act', 'pool', 'sp')
QUEUES = ('sp', 'act', 'pool')
NDMA = 8

D_MODEL = 1024
IN_COLS = 6680
DEPTH = 4
DN_ALPHA = (2 * DEPTH) ** 0.25


class Buf:
    def __init__(self, name, t=None, parent=None):
        self.name = name
        self.t = t
        self.parent = parent
        self.children = []
        self.last_write = None
        self.reads = []
        self.excl = False

    def sub(self, key):
        c = Buf(f"{self.name}.{key}", self.t, self)
        self.children.append(c)
        return c

    def __getitem__(self, k):
        return self.t[k]


class Prog:
    def __init__(self, nc, stack):
        self.nc = nc
        self.stack = stack
        self.ops = {e: [] for e in ALLENG}
        self.sem = {e: stack.enter_context(nc.semaphore(f"s_{e}")) for e in COMPUTE}
        self.cnt = {e: 0 for e in COMPUTE}
        self.known = {e: {} for e in ALLENG}
        self.dsem = {q: [stack.enter_context(nc.semaphore(f"d_{q}_{i}")) for i in range(NDMA)] for q in QUEUES}
        self.dcnt = {q: [0] * NDMA for q in QUEUES}
        self.dnext = {q: 0 for q in QUEUES}
        self.semobj = {}
        for e in COMPUTE:
            self.semobj[('c', e)] = self.sem[e]
        for q in QUEUES:
            for i in range(NDMA):
                self.semobj[('d', q, i)] = self.dsem[q][i]
        self.nops = 0
        self.nwaits = 0

    def sb(self, name, shape, dtype=F32):
        self.nalloc = getattr(self, 'nalloc', 0) + 1
        t = self.stack.enter_context(self.nc.sbuf_tensor(f"sb{self.nalloc}_{name}", list(shape), dtype))
        return Buf(name, t)

    def _collect(self, b, write):
        toks = []

        def add(x):
            if x.last_write is not None:
                toks.append(x.last_write)
            if write:
                toks.extend(x.reads)
        add(b)
        p = b.parent
        while p is not None:
            add(p)
            p = p.parent

        def rec(x):
            for c in x.children:
                add(c)
                rec(c)
        rec(b)
        return toks

    def _wait(self, eng, tok):
        key, val = tok
        if self.known[eng].get(key, 0) >= val:
            return
        self.known[eng][key] = val
        sem = self.semobj[key]
        self.ops[eng].append(lambda e, sem=sem, val=val: e.wait_ge(sem, val))
        self.nwaits += 1

    def _deps(self, eng, reads, writes, is_dma):
        own = ('c', eng) if (eng in COMPUTE and not is_dma) else None
        for b in reads:
            for tok in self._collect(b, False):
                self._wait(eng, tok)
            if b.excl:
                for tok in b.reads:
                    if own is not None and tok[0] == own:
                        continue
                    self._wait(eng, tok)
        for b in writes:
            for tok in self._collect(b, True):
                if own is not None and tok[0] == own:
                    continue
                self._wait(eng, tok)

    def _record(self, tok, reads, writes):
        for b in reads:
            b.reads.append(tok)
            if len(b.reads) > 64:
                b.reads = b.reads[-64:] if False else b.reads
        for b in writes:
            b.last_write = tok
            b.reads = []

            def rec(x):
                for c in x.children:
                    c.last_write = None
                    c.reads = []
                    rec(c)
            rec(b)

    def op(self, eng, fn, reads=(), writes=()):
        self._deps(eng, reads, writes, False)
        self.cnt[eng] += 1
        n = self.cnt[eng]
        sem = self.sem[eng]
        self.ops[eng].append(lambda e, fn=fn, sem=sem: fn(e).then_inc(sem, 1))
        tok = (('c', eng), n)
        self._record(tok, reads, writes)
        self.nops += 1
        return tok

    def dma(self, q, out, in_, reads=(), writes=(), **kw):
        self._deps(q, reads, writes, True)
        i = self.dnext[q]
        self.dnext[q] = (i + 1) % NDMA
        key = ('d', q, i)
        if self.dcnt[q][i] > 0:
            self._wait(q, (key, self.dcnt[q][i]))
        self.dcnt[q][i] += 16
        val = self.dcnt[q][i]
        sem = self.dsem[q][i]
        self.ops[q].append(lambda e, out=out, in_=in_, sem=sem, kw=kw:
                           e.dma_start(out=out, in_=in_, **kw).then_inc(sem, 16))
        tok = (key, val)
        self._record(tok, reads, writes)
        self.nops += 1
        return tok

    def barrier(self):
        for e in ALLENG:
            for c in COMPUTE:
                if self.cnt[c] > 0 and c != e:
                    self._wait(e, (('c', c), self.cnt[c]))
            for q in QUEUES:
                for i in range(NDMA):
                    if self.dcnt[q][i] > 0:
                        self._wait(e, (('d', q, i), self.dcnt[q][i]))

    def flush(self):
        self.barrier()
        nc = self.nc
        ops = self.ops
        self.ops = {e: [] for e in ALLENG}
        with nc.Block() as block:
            @block.sync
            def _(e):
                for f in ops['sp']:
                    f(e)

            @block.tensor
            def _(e):
                for f in ops['pe']:
                    f(e)

            @block.vector
            def _(e):
                for f in ops['dve']:
                    f(e)

            @block.scalar
            def _(e):
                for f in ops['act']:
                    f(e)

            @block.gpsimd
            def _(e):
                for f in ops['pool']:
                    f(e)


C_I, C_LE, C_GT, C_ONES, C_LE64, C_GT64, C_LT64, C_SAME64, C_SEL0, C_SEL1 = range(10)
NCONST = 10


def make_consts():
    k = np.arange(128)[:, None]
    l = np.arange(128)[None, :]
    c = np.zeros((128, NCONST, 128), np.float32)
    c[:, C_I] = (k == l)
    c[:, C_LE] = (k <= l)
    c[:, C_GT] = (k > l)
    c[:, C_ONES] = 1.0
    same64 = (k // 64) == (l // 64)
    c[:, C_LE64] = (k <= l) & same64
    c[:, C_GT64] = (k > l) & same64
    c[:, C_LT64] = (k < l) & same64
    c[:, C_SAME64] = same64
    c[:, C_SEL0] = (k < 64) & (l >= 0)
    c[:, C_SEL1] = (k >= 64) & (l >= 0)
    return c


class K:
    pass


STAGE = 99


class StopPass(Exception):
    pass


def chk(n):
    if STAGE == n:
        raise StopPass()


def mm(P, out, lhsT, rhs, start, stop, reads, writes):
    return P.op('pe', lambda e: e.matmul(out, lhsT=lhsT, rhs=rhs, start=start, stop=stop), reads, writes)


def tr(P, out, in_, ident, reads, writes):
    return P.op('pe', lambda e: e.transpose(out, in_, ident), reads, writes)


def act(P, out, in_, func, reads, writes, **kw):
    return P.op('act', lambda e: e.activation(out=out, in_=in_, func=func, **kw), reads, writes)


def tt(P, out, in0, in1, op, reads, writes):
    return P.op('dve', lambda e: e.tensor_tensor(out=out, in0=in0, in1=in1, op=op), reads, writes)


def ts(P, out, in0, s1, s2, op0, op1, reads, writes, **kw):
    if op1 is None:
        return P.op('dve', lambda e: e.tensor_scalar(out=out, in0=in0, scalar1=s1, scalar2=None, op0=op0, **kw), reads, writes)
    return P.op('dve', lambda e: e.tensor_scalar(out=out, in0=in0, scalar1=s1, scalar2=s2, op0=op0, op1=op1, **kw), reads, writes)


def stt(P, out, in0, scalar, in1, op0, op1, reads, writes):
    return P.op('dve', lambda e: e.scalar_tensor_tensor(out=out, in0=in0, scalar=scalar, in1=in1, op0=op0, op1=op1), reads, writes)


def load_weights(P, dst, dst_ap_fn, src_rows_fn, ncols, nk):
    toks = []
    if not dst.children:
        for k in range(nk):
            for c0 in range(0, ncols, 2048):
                dst.sub((k, c0))
    i = 0
    for k in range(nk):
        c0 = 0
        while c0 < ncols:
            c1 = min(ncols, c0 + 2048)
            toks.append(P.dma('pool', dst_ap_fn(k, c0, c1), src_rows_fn(k, c0, c1), writes=[dst.children[i]]))
            i += 1
            c0 = c1
    return toks


SSD_NPP = 12 * 4 + 12
SSD_NRP = 16 + 16 + 16 + 1024


def pass_transpose_in(P, Kx, T, src, dstT):
    nc = P.nc
    with ExitStack() as st:
        P.stack = st
        xt = [P.sb(f"p0_x{i}", [128, 1024], F32) for i in range(2)]
        xb = [P.sb(f"p0_b{i}", [128, 8, 128], BF16) for i in range(2)]
        for b in range(T // 128):
            x_ = xt[b % 2]
            o_ = xb[b % 2]
            P.dma('sp', x_[:], src[b * 128:(b + 1) * 128, :], writes=[x_])
            for j in range(8):
                bk = Kx.bank[j // 4]
                tr(P, bk[:, (j % 4) * 128:(j % 4 + 1) * 128], x_[:, j * 128:(j + 1) * 128], Kx.cst[:, C_I, :],
                   [x_, Kx.cst], [bk])
            for hlf in range(2):
                act(P, o_[:, hlf * 4:(hlf + 1) * 4, :], Kx.bank[hlf][:, :].rearrange("p (j t) -> p j t", j=4),
                    AF.Copy, [Kx.bank[hlf]], [o_])
            P.dma('sp', dstT.rearrange("k p t -> p k t")[:, :, b * 128:(b + 1) * 128], o_[:], reads=[o_], writes=[Kx.d_hT])
        P.flush()


def pass_ssd(P, Kx, T, l, dbg=None):
    nc = P.nc
    SBT = min(512, T)
    NSB = T // SBT
    NBLK = SBT // 128
    cst = Kx.cst
    bank = Kx.bank
    A0, A1, B0, B1, C0, C1, D0, D1 = bank
    with ExitStack() as st:
        P.stack = st
        try:
            W = P.sb("ssd_W", [128, 8, 2576], BF16)
            pp = P.sb("ssd_pp", [128, SSD_NPP], F32)
            rp = P.sb("ssd_rp", [128, SSD_NRP], F32)
            hTb = [P.sb(f"ssd_hT{i}", [128, 8, SBT], BF16) for i in range(2)]
            xin = P.sb("ssd_xin", [128, 12, SBT + 3], F32)
            accs = [P.sb(f"ssd_acc{i}", [128, SBT], F32) for i in range(2)]
            xc = P.sb("ssd_xc", [128, 12, SBT], F32)
            xcj = [xc.sub(j) for j in range(12)]
            xinj = [xin.sub(j) for j in range(12)]
            Dmat = P.sb("ssd_Dmat", [128, 16, 128], BF16)
            arep = P.sb("ssd_arep", [128, 16], F32)
            sm = P.sb("ssd_sm", [128, 16 * 12], F32)
            smv = lambda i: sm[:, i * 16:(i + 1) * 16]
            R = P.sb("ssd_R", [128, 16, 128], F32)
            DT = P.sb("ssd_DT", [128, 16, 128], BF16)
            GT = P.sb("ssd_GT", [128, 16, 128], BF16)
            CBTm = P.sb("ssd_CBTm", [128, 2, 128], BF16)
            xsb = P.sb("ssd_xsb", [128, 1024], BF16)
            xdt = P.sb("ssd_xdt", [128, 1024], BF16)
            xend = P.sb("ssd_xend", [128, 1024], BF16)
            Btm = P.sb("ssd_Btm", [128, 256], BF16)
            bcT = P.sb("ssd_bcT", [128, 4, 128], BF16)
            sz = P.sb("ssd_sz", [128, 1024], F32)
            yoff = P.sb("ssd_yoff", [128, 1024], F32)
            y = P.sb("ssd_y", [128, 1024], F32)
            junk = P.sb("ssd_junk", [128, 512], F32)
            ybf = P.sb("ssd_ybf", [128, 1024], BF16)
            yT = [P.sb(f"ssd_yT{i}", [128, 8, 128], BF16) for i in range(2)]
            S32 = P.sb("ssd_S32", [128, 1024], F32)
            Sbf = P.sb("ssd_Sbf", [128, 1024], BF16)
            identb = P.sb("ssd_identb", [128, 128], BF16)

            load_weights(P, W, lambda k, c0, c1: W[:, k, c0:c1],
                         lambda k, c0, c1: Kx.w_in[l, k * 128:(k + 1) * 128, c0:c1], 2576, 8)
            P.dma('sp', pp[:], Kx.ssd_pp[l], writes=[pp])
            P.dma('sp', rp[:], Kx.ssd_rp[l], writes=[rp])
            dtb = rp[:, 0:16]
            alog = rp[:, 16:32]
            drep = rp[:, 32:48]
            normw = rp[:, 48:48 + 1024]
            cw = lambda j, k: pp[:, j * 4 + k: j * 4 + k + 1]
            cb = lambda j: pp[:, 48 + j: 48 + j + 1]
            act(P, identb[:], cst[:, C_I, :], AF.Copy, [cst], [identb])
            act(P, arep[:], alog, AF.Exp, [rp], [arep])
            ts(P, arep[:], arep[:], -1.0, None, ALU.mult, None, [arep], [arep])
            tt(P, Dmat[:], cst[:, C_I, :].unsqueeze(1).to_broadcast([128, 16, 128]),
               drep.unsqueeze(2).to_broadcast([128, 16, 128]), ALU.mult, [cst, rp], [Dmat])
            P.op('dve', lambda e: e.memset(S32[:], 0.0), [], [S32])
            P.op('dve', lambda e: e.memset(Sbf[:], 0.0), [], [Sbf])
            P.op('dve', lambda e: e.memset(xin[:], 0.0), [], [xin])
            chk(1)

            for sbi in range(NSB):
                hb = hTb[sbi % 2]
                P.dma('sp', hb[:], Kx.hT.rearrange("k p t -> p k t")[:, :, sbi * SBT:(sbi + 1) * SBT], reads=[Kx.d_hT], writes=[hb])
                for j in range(12):
                    bk = [D0, D1][j % 2]
                    for k in range(8):
                        mm(P, bk[:, 0:SBT], W[:, k, 1024 + j * 128: 1024 + (j + 1) * 128], hb[:, k, :], k == 0, k == 7,
                           [W, hb], [bk])
                    ac = accs[j % 2]
                    act(P, xin[:, j, 3:3 + SBT], bk[:, 0:SBT], AF.Copy, [bk], [xinj[j]])
                    act(P, ac[:], bk[:, 0:SBT], AF.Identity, [bk, pp], [ac], scale=cw(j, 3), bias=cb(j))
                    for k in (2, 1, 0):
                        stt(P, ac[:], xin[:, j, k:k + SBT], cw(j, k), ac[:], ALU.mult, ALU.add, [xinj[j], pp, ac], [ac])
                    act(P, xc[:, j, :], ac[:], AF.Silu, [ac], [xcj[j]])
                    act(P, xin[:, j, 0:3], xin[:, j, SBT:SBT + 3], AF.Copy, [xinj[j]], [xinj[j]])
                chk(2)
                for blk in range(NBLK):
                    t0 = blk * 128
                    tg = sbi * SBT + t0
                    for hf in range(2):
                        bk = [A0, A1][hf]
                        for k in range(8):
                            mm(P, bk[:, :], hb[:, k, t0:t0 + 128], W[:, k, hf * 512:(hf + 1) * 512], k == 0, k == 7, [hb, W], [bk])
                        act(P, sz[:, hf * 512:(hf + 1) * 512], bk[:, :], AF.Silu, [bk], [sz])
                    for k in range(8):
                        mm(P, C0[:, 0:16], hb[:, k, t0:t0 + 128], W[:, k, 2560:2576], k == 0, k == 7, [hb, W], [C0])
                    xr, xm, ex, lg, dt, dA, acs, te, dte, ea, cd = [smv(i) for i in range(11)]
                    tt(P, xr, C0[:, 0:16], dtb, ALU.add, [C0, rp], [sm])
                    ts(P, xm, xr, 30.0, None, ALU.min, None, [sm], [sm])
                    act(P, ex, xm, AF.Exp, [sm], [sm])
                    act(P, lg, ex, AF.Ln, [sm], [sm], bias=1.0)
                    tt(P, dt, lg, xr, ALU.max, [sm], [sm])
                    tt(P, dA, dt, arep[:], ALU.mult, [sm, arep], [sm])
                    mm(P, C0[:, 16:32], cst[:, C_LE, :], dA, True, True, [cst, sm], [C0])
                    mm(P, C0[:, 32:48], cst[:, C_ONES, :], dA, True, True, [cst, sm], [C0])
                    act(P, ea, C0[:, 16:32], AF.Exp, [C0], [sm])
                    act(P, cd, C0[:, 32:48], AF.Exp, [C0], [sm])
                    act(P, acs, C0[:, 16:32], AF.Copy, [C0], [sm])
                    tt(P, te, C0[:, 32:48], acs, ALU.subtract, [C0, sm], [sm])
                    act(P, te, te, AF.Exp, [sm], [sm])
                    tt(P, dte, dt, te, ALU.mult, [sm], [sm])
                    chk(3)
                    tt(P, R[:], cst[:, C_LE, :].unsqueeze(1).to_broadcast([128, 16, 128]),
                       dA.unsqueeze(2).to_broadcast([128, 16, 128]), ALU.mult, [cst, sm], [R])
                    for q in range(4):
                        bk = [D0, D1][q % 2]
                        mm(P, bk[:, :], cst[:, C_GT, :], R[:, 4 * q:4 * q + 4, :], True, True, [cst, R], [bk])
                        act(P, DT[:, 4 * q:4 * q + 4, :], bk[:, :].rearrange("p (h l) -> p h l", h=4), AF.Exp, [bk], [DT])
                    chk(4)
                    for j in range(8):
                        bk = [B0, B1][j // 4]
                        tr(P, bk[:, (j % 4) * 128:(j % 4 + 1) * 128], xc[:, j, t0:t0 + 128], cst[:, C_I, :], [xcj[j], cst], [bk])
                    for hf in range(2):
                        bk = [B0, B1][hf]
                        pv = bk[:, :].rearrange("p (h c) -> p h c", h=8)
                        act(P, xsb[:, hf * 512:(hf + 1) * 512], bk[:, :], AF.Copy, [bk], [xsb])
                        chk(4.1)
                        tt(P, xdt[:, hf * 512:(hf + 1) * 512].rearrange("p (h c) -> p h c", h=8), pv,
                           dt[:, hf * 8:(hf + 1) * 8].unsqueeze(2).to_broadcast([128, 8, 64]), ALU.mult, [bk, sm], [xdt])
                        chk(4.11)
                        tt(P, xend[:, hf * 512:(hf + 1) * 512].rearrange("p (h c) -> p h c", h=8), pv,
                           dte[:, hf * 8:(hf + 1) * 8].unsqueeze(2).to_broadcast([128, 8, 64]), ALU.mult, [bk, sm], [xend])
                        chk(4.12)
                    chk(4.2)
                    for g in range(2):
                        tr(P, C0[:, 128 + g * 128:128 + (g + 1) * 128], xc[:, 8 + g, t0:t0 + 128], cst[:, C_I, :], [xcj[8 + g], cst], [C0])
                    act(P, Btm[:], C0[:, 128:384], AF.Copy, [C0], [Btm])
                    chk(4.3)
                    act(P, bcT[:], xc[:, 8:12, t0:t0 + 128], AF.Copy, [xcj[8], xcj[9], xcj[10], xcj[11]], [bcT])
                    chk(5)
                    for g in range(2):
                        mm(P, C1[:, g * 128:(g + 1) * 128], bcT[:, g, :], bcT[:, 2 + g, :], True, True, [bcT], [C1])
                    tt(P, CBTm[:], C1[:, 0:256].rearrange("p (g l) -> p g l", g=2),
                       cst[:, C_LE, :].unsqueeze(1).to_broadcast([128, 2, 128]), ALU.mult, [C1, cst], [CBTm])
                    for g in range(2):
                        tt(P, GT[:, g * 8:(g + 1) * 8, :], DT[:, g * 8:(g + 1) * 8, :],
                           CBTm[:, g, :].unsqueeze(1).to_broadcast([128, 8, 128]), ALU.mult, [DT, CBTm], [GT])
                    chk(6)
                    for h in range(16):
                        bk = [A0, A1][h // 8]
                        o = bk[:, (h % 8) * 64:(h % 8 + 1) * 64]
                        mm(P, o, GT[:, h, :], xdt[:, h * 64:(h + 1) * 64], True, False, [GT, xdt], [bk])
                        mm(P, o, Dmat[:, h, :], xsb[:, h * 64:(h + 1) * 64], False, True, [Dmat, xsb], [bk])
                    for g in range(2):
                        bk = [B0, B1][g]
                        mm(P, bk[:, :], bcT[:, 2 + g, :], Sbf[:, g * 512:(g + 1) * 512], True, True, [bcT, Sbf], [bk])
                        tt(P, yoff[:, g * 512:(g + 1) * 512].rearrange("p (h c) -> p h c", h=8),
                           bk[:, :].rearrange("p (h c) -> p h c", h=8),
                           ea[:, g * 8:(g + 1) * 8].unsqueeze(2).to_broadcast([128, 8, 64]), ALU.mult, [bk, sm], [yoff])
                        tt(P, y[:, g * 512:(g + 1) * 512], [A0, A1][g][:, :], yoff[:, g * 512:(g + 1) * 512], ALU.add,
                           [[A0, A1][g], yoff], [y])
                    tt(P, y[:], y[:], sz[:], ALU.mult, [y, sz], [y])
                    chk(7)
                    ss = smv(11)
                    for g in range(2):
                        P.op('act', lambda e, g=g: e.activation(out=junk[:], in_=y[:, g * 512:(g + 1) * 512], func=AF.Square,
                                                               accum_out=sm[:, 176 + g:177 + g]), [y], [junk, sm])
                    act(P, sm[:, 178:180], sm[:, 176:178], AF.Ln, [sm], [sm], scale=1.0 / 512, bias=1e-6)
                    act(P, sm[:, 178:180], sm[:, 178:180], AF.Exp, [sm], [sm], scale=-0.5)
                    for g in range(2):
                        stt(P, ybf[:, g * 512:(g + 1) * 512], y[:, g * 512:(g + 1) * 512], sm[:, 178 + g:179 + g],
                            normw[:, g * 512:(g + 1) * 512], ALU.mult, ALU.mult, [y, sm, rp], [ybf])
                    c1b = C1[:, :].bitcast(BF16)
                    for j in range(8):
                        tr(P, c1b[:, j * 128:(j + 1) * 128], ybf[:, j * 128:(j + 1) * 128], identb[:], [ybf, identb], [C1])
                    yt = yT[(sbi * NBLK + blk) % 2]
                    act(P, yt[:], c1b.rearrange("p (j t) -> p j t", j=8), AF.Copy, [C1], [yt])
                    P.dma('sp', Kx.ycT.rearrange("k p t -> p k t")[:, 0:8, tg:tg + 128], yt[:], reads=[yt], writes=[Kx.d_ycT])
                    chk(8)
                    for g in range(2):
                        bk = [D0, D1][g]
                        mm(P, bk[:, :], Btm[:, g * 128:(g + 1) * 128], xend[:, g * 512:(g + 1) * 512], True, True, [Btm, xend], [bk])
                    tt(P, S32[:].rearrange("p (h c) -> p h c", h=16), S32[:].rearrange("p (h c) -> p h c", h=16),
                       cd.unsqueeze(2).to_broadcast([128, 16, 64]), ALU.mult, [S32, sm], [S32])
                    for g in range(2):
                        tt(P, S32[:, g * 512:(g + 1) * 512], [D0, D1][g][:, :], S32[:, g * 512:(g + 1) * 512], ALU.add,
                           [[D0, D1][g], S32], [S32])
                    act(P, Sbf[:], S32[:], AF.Copy, [S32], [Sbf])
        except StopPass:
            pass
        P.flush()


GDN_NPP = 48
GDN_NRP = 4 + 4 + 128
GDN_BASE = 2576


def mmx(P, out, lhsT, rhs, start, stop, reads, writes):
    return P.op('pe', lambda e: e.matmul(out, lhsT=lhsT, rhs=rhs, start=start, stop=stop, skip_group_check=True), reads, writes)


def pass_gdn(P, Kx, T, l):
    SBT = min(512, T)
    NSB = T // SBT
    NBLK = SBT // 128
    cst = Kx.cst
    A0, A1, B0, B1, C0, C1, D0, D1 = Kx.bank
    b4 = lambda ap: ap.rearrange("p (h c) -> p h c", h=4)
    with ExitStack() as st:
        P.stack = st
        try:
            W = P.sb("gdn_W", [128, 8, 2056], BF16)
            pp = P.sb("gdn_pp", [128, GDN_NPP], F32)
            rp = P.sb("gdn_rp", [128, GDN_NRP], F32)
            hTb = [P.sb(f"gdn_hT{i}", [128, 8, SBT], BF16) for i in range(2)]
            xin = P.sb("gdn_xin", [128, 12, SBT + 3], F32)
            xinj = [xin.sub(j) for j in range(12)]
            accs = [P.sb(f"gdn_acc{i}", [128, SBT], F32) for i in range(2)]
            xc = P.sb("gdn_xc", [128, 12, SBT], F32)
            xcj = [xc.sub(j) for j in range(12)]
            sq = [P.sb(f"gdn_sq{i}", [128, SBT], F32) for i in range(2)]
            rn = [P.sb(f"gdn_rn{i}", [128, SBT], F32) for i in range(2)]
            qT = P.sb("gdn_qT", [128, 4, SBT], BF16)
            kT = P.sb("gdn_kT", [128, 4, SBT], BF16)
            identb = P.sb("gdn_identb", [128, 128], BF16)
            arep = P.sb("gdn_arep", [128, 4], F32)
            sm = P.sb("gdn_sm", [128, 4 * 20], F32)
            smv = lambda i: sm[:, i * 4:(i + 1) * 4]
            R = P.sb("gdn_R", [128, 4, 128], F32)
            Dec = P.sb("gdn_Dec", [128, 4, 128], F32)
            DecU = P.sb("gdn_DecU", [128, 4, 128], F32)
            t1 = P.sb("gdn_t1", [128, 4, 128], F32)
            qkTm = P.sb("gdn_qkTm", [128, 4, 128], BF16)
            Ya = [P.sb(f"gdn_Y{i}", [128, 4, 128], F32) for i in range(2)]
            Za = [P.sb(f"gdn_Z{i}", [128, 4, 128], F32) for i in range(2)]
            V = P.sb("gdn_V", [128, 4, 128], F32)
            Vbf = P.sb("gdn_Vbf", [128, 4, 128], BF16)
            v_tm = P.sb("gdn_vtm", [128, 4, 128], F32)
            kd = P.sb("gdn_kd", [128, 4, 128], BF16)
            rhs2 = P.sb("gdn_rhs2", [128, 4, 128], BF16)
            vnew = P.sb("gdn_vnew", [128, 4, 128], BF16)
            As = P.sb("gdn_As", [128, 4, 128], F32)
            o = P.sb("gdn_o", [128, 4, 128], F32)
            sg = P.sb("gdn_sg", [128, 512], F32)
            junk = P.sb("gdn_junk", [128, 128], F32)
            ybf = P.sb("gdn_ybf", [128, 4, 128], BF16)
            yT = [P.sb(f"gdn_yT{i}", [128, 4, 128], BF16) for i in range(2)]
            S32 = P.sb("gdn_S32", [128, 4, 128], F32)
            Sbf = P.sb("gdn_Sbf", [128, 4, 128], BF16)

            load_weights(P, W, lambda k, c0, c1: W[:, k, c0:c1],
                         lambda k, c0, c1: Kx.w_in[l, k * 128:(k + 1) * 128, GDN_BASE + c0:GDN_BASE + c1], 2056, 8)
            P.dma('sp', pp[:], Kx.gdn_pp[l], writes=[pp])
            P.dma('sp', rp[:], Kx.gdn_rp[l], writes=[rp])
            dtb = rp[:, 0:4]
            alog = rp[:, 4:8]
            normw = rp[:, 8:136]
            cw = lambda j, k: pp[:, j * 4 + k: j * 4 + k + 1]
            act(P, identb[:], cst[:, C_I, :], AF.Copy, [cst], [identb])
            act(P, arep[:], alog, AF.Exp, [rp], [arep])
            ts(P, arep[:], arep[:], -1.0, None, ALU.mult, None, [arep], [arep])
            P.op('dve', lambda e: e.memset(S32[:], 0.0), [], [S32])
            P.op('dve', lambda e: e.memset(Sbf[:], 0.0), [], [Sbf])
            P.op('dve', lambda e: e.memset(xin[:], 0.0), [], [xin])
            chk(1)
            for sbi in range(NSB):
                hb = hTb[sbi % 2]
                P.dma('sp', hb[:], Kx.hT.rearrange("k p t -> p k t")[:, :, sbi * SBT:(sbi + 1) * SBT], reads=[Kx.d_hT], writes=[hb])
                for j in range(12):
                    bk = [D0, D1][j % 2]
                    for k in range(8):
                        mm(P, bk[:, 0:SBT], W[:, k, j * 128:(j + 1) * 128], hb[:, k, :], k == 0, k == 7, [W, hb], [bk])
                    ac = accs[j % 2]
                    act(P, xin[:, j, 3:3 + SBT], bk[:, 0:SBT], AF.Copy, [bk], [xinj[j]])
                    act(P, ac[:], bk[:, 0:SBT], AF.Copy, [bk, pp], [ac], scale=cw(j, 3))
                    for k in (2, 1, 0):
                        stt(P, ac[:], xin[:, j, k:k + SBT], cw(j, k), ac[:], ALU.mult, ALU.add, [xinj[j], pp, ac], [ac])
                    act(P, xc[:, j, :], ac[:], AF.Silu, [ac], [xcj[j]])
                    act(P, xin[:, j, 0:3], xin[:, j, SBT:SBT + 3], AF.Copy, [xinj[j]], [xinj[j]])
                    if j < 8:
                        s_ = sq[j % 2]
                        r_ = rn[j % 2]
                        bk2 = [C0, C1][j % 2]
                        act(P, s_[:], xc[:, j, :], AF.Square, [xcj[j]], [s_])
                        mm(P, bk2[:, 0:SBT], cst[:, C_ONES, :], s_[:], True, True, [cst, s_], [bk2])
                        act(P, r_[:], bk2[:, 0:SBT], AF.Ln, [bk2], [r_], bias=1e-6)
                        act(P, r_[:], r_[:], AF.Exp, [r_], [r_], scale=-0.5,
                            bias=(-0.5 * float(np.log(128.0))) if j < 4 else 0.0)
                        dst = qT if j < 4 else kT
                        tt(P, dst[:, j % 4, :], xc[:, j, :], r_[:], ALU.mult, [xcj[j], r_], [dst])
                chk(2)
                for blk in range(NBLK):
                    t0 = blk * 128
                    tg = sbi * SBT + t0
                    tsl = slice(t0, t0 + 128)
                    for k in range(8):
                        mm(P, D0[:, :], hb[:, k, tsl], W[:, k, 1536:2048], k == 0, k == 7, [hb, W], [D0])
                    act(P, sg[:], D0[:, :], AF.Silu, [D0], [sg])
                    for k in range(8):
                        mm(P, C0[:, 0:8], hb[:, k, tsl], W[:, k, 2048:2056], k == 0, k == 7, [hb, W], [C0])
                    eb, beta, xr, xm, ex, lg, sp, la, eg, gs, ekd, negeg = [smv(i) for i in range(12)]
                    glrep = sm[:, 48:56]
                    act(P, eb, C0[:, 0:4], AF.Exp, [C0], [sm], scale=-1.0)
                    ts(P, eb, eb, 1.0, None, ALU.add, None, [sm], [sm])
                    P.op('dve', lambda e: e.reciprocal(out=beta, in_=eb), [sm], [sm])
                    tt(P, xr, C0[:, 4:8], dtb, ALU.add, [C0, rp], [sm])
                    ts(P, xm, xr, 30.0, None, ALU.min, None, [sm], [sm])
                    act(P, ex, xm, AF.Exp, [sm], [sm])
                    act(P, lg, ex, AF.Ln, [sm], [sm], bias=1.0)
                    tt(P, sp, lg, xr, ALU.max, [sm], [sm])
                    tt(P, la, sp, arep[:], ALU.mult, [sm, arep], [sm])
                    mm(P, C0[:, 8:12], cst[:, C_LE64, :], la, True, True, [cst, sm], [C0])
                    mm(P, C0[:, 12:16], cst[:, C_SAME64, :], la, True, True, [cst, sm], [C0])
                    mm(P, C0[:, 16:20], cst[:, C_SEL0, :], la, True, True, [cst, sm], [C0])
                    mm(P, C0[:, 20:24], cst[:, C_SEL1, :], la, True, True, [cst, sm], [C0])
                    act(P, eg, C0[:, 8:12], AF.Exp, [C0], [sm])
                    act(P, gs, C0[:, 8:12], AF.Copy, [C0], [sm])
                    act(P, glrep, C0[:, 16:24], AF.Exp, [C0], [sm])
                    tt(P, ekd, C0[:, 12:16], gs, ALU.subtract, [C0, sm], [sm])
                    act(P, ekd, ekd, AF.Exp, [sm], [sm])
                    ts(P, negeg, eg, -1.0, None, ALU.mult, None, [sm], [sm])
                    chk(3)
                    tt(P, R[:], cst[:, C_LE64, :].unsqueeze(1).to_broadcast([128, 4, 128]),
                       la.unsqueeze(2).to_broadcast([128, 4, 128]), ALU.mult, [cst, sm], [R])
                    mm(P, A0[:, :], cst[:, C_GT64, :], R[:], True, True, [cst, R], [A0])
                    act(P, Dec[:], b4(A0[:, :]), AF.Exp, [A0], [Dec])
                    tt(P, DecU[:], Dec[:], cst[:, C_LE64, :].unsqueeze(1).to_broadcast([128, 4, 128]), ALU.mult, [Dec, cst], [DecU])
                    for h in range(4):
                        mm(P, A1[:, h * 128:(h + 1) * 128], kT[:, h, tsl], kT[:, h, tsl], True, True, [kT], [A1])
                    for h in range(4):
                        mm(P, B0[:, h * 128:(h + 1) * 128], kT[:, h, tsl], qT[:, h, tsl], True, True, [kT, qT], [B0])
                    tt(P, qkTm[:], b4(B0[:, :]), DecU[:], ALU.mult, [B0, DecU], [qkTm])
                    tt(P, t1[:], b4(A1[:, :]), DecU[:], ALU.mult, [A1, DecU], [t1])
                    X = Ya[0]
                    for h in range(4):
                        stt(P, X[:, h, :], t1[:, h, :], beta[:, h:h + 1], cst[:, C_LT64, :], ALU.mult, ALU.mult, [t1, sm, cst], [X])
                    chk(4)
                    for h in range(4):
                        tr(P, B1[:, h * 128:(h + 1) * 128], X[:, h, :], cst[:, C_I, :], [X, cst], [B1])
                    act(P, Za[0][:], b4(B1[:, :]), AF.Copy, [B1], [Za[0]])
                    tt(P, V[:], cst[:, C_I, :].unsqueeze(1).to_broadcast([128, 4, 128]), X[:], ALU.subtract, [cst, X], [V])
                    for lev in range(5):
                        Yc, Zc = Ya[lev % 2], Za[lev % 2]
                        Yn, Zn = Ya[(lev + 1) % 2], Za[(lev + 1) % 2]
                        for h in range(4):
                            mm(P, C1[:, h * 128:(h + 1) * 128], Yc[:, h, :], Zc[:, h, :], True, True, [Yc, Zc], [C1])
                        act(P, Zn[:], b4(C1[:, :]), AF.Copy, [C1], [Zn])
                        if lev < 4:
                            for h in range(4):
                                mm(P, B1[:, h * 128:(h + 1) * 128], Zc[:, h, :], Yc[:, h, :], True, True, [Yc, Zc], [B1])
                            act(P, Yn[:], b4(B1[:, :]), AF.Copy, [B1], [Yn])
                        for h in range(4):
                            mm(P, A0[:, h * 128:(h + 1) * 128], Zn[:, h, :], V[:, h, :], True, True, [Zn, V], [A0])
                        tt(P, V[:], b4(A0[:, :]), V[:], ALU.add, [A0, V], [V])
                    act(P, Vbf[:], V[:], AF.Copy, [V], [Vbf])
                    chk(5)
                    for h in range(4):
                        tr(P, D1[:, h * 128:(h + 1) * 128], xc[:, 8 + h, tsl], cst[:, C_I, :], [xcj[8 + h], cst], [D1])
                    act(P, v_tm[:], b4(D1[:, :]), AF.Copy, [D1], [v_tm])
                    d0b = D0[:, :].bitcast(BF16)
                    for h in range(4):
                        tr(P, d0b[:, h * 128:(h + 1) * 128], kT[:, h, tsl], identb[:], [kT, identb], [D0])
                    tt(P, kd[:], b4(d0b[:, 0:512]), ekd.unsqueeze(2).to_broadcast([128, 4, 128]), ALU.mult, [D0, sm], [kd])
                    chk(6)
                    for c in range(2):
                        sl = slice(64 * c, 64 * c + 64)
                        for h in range(4):
                            mm(P, A0[:, h * 128:(h + 1) * 128], kT[:, h, tsl], Sbf[:, h, :], True, True, [kT, Sbf], [A0])
                        for h in range(4):
                            stt(P, rhs2[sl, h, :], A0[sl, h * 128:(h + 1) * 128], negeg[sl, h:h + 1], v_tm[sl, h, :],
                                ALU.mult, ALU.add, [A0, sm, v_tm], [rhs2])
                        for h in range(4):
                            mm(P, A1[:, h * 128:(h + 1) * 128], Vbf[sl, h, :], rhs2[sl, h, :], True, True, [Vbf, rhs2], [A1])
                        tt(P, vnew[sl], b4(A1[sl, :]), beta[sl].unsqueeze(2).to_broadcast([64, 4, 128]), ALU.mult, [A1, sm], [vnew])
                        for h in range(4):
                            mm(P, B0[:, h * 128:(h + 1) * 128], qT[:, h, tsl], Sbf[:, h, :], True, True, [qT, Sbf], [B0])
                        tt(P, As[sl], b4(B0[sl, :]), eg[sl].unsqueeze(2).to_broadcast([64, 4, 128]), ALU.mult, [B0, sm], [As])
                        for h in range(4):
                            mmx(P, B1[:, h * 128:(h + 1) * 128], qkTm[sl, h, :], vnew[sl, h, :], (c == 0 and h == 0), (c == 1),
                                [qkTm, vnew], [B1])
                        for h in range(4):
                            mm(P, C1[:, h * 128:(h + 1) * 128], kd[sl, h, :], vnew[sl, h, :], True, True, [kd, vnew], [C1])
                        for h in range(4):
                            stt(P, S32[:, h, :], S32[:, h, :], glrep[:, c * 4 + h:c * 4 + h + 1], C1[:, h * 128:(h + 1) * 128],
                                ALU.mult, ALU.add, [S32, sm, C1], [S32])
                        act(P, Sbf[:], S32[:], AF.Copy, [S32], [Sbf])
                    chk(7)
                    tt(P, o[:], b4(B1[:, :]), As[:], ALU.add, [B1, As], [o])
                    for h in range(4):
                        P.op('act', lambda e, h=h: e.activation(out=junk[:], in_=o[:, h, :], func=AF.Square,
                                                               accum_out=sm[:, 56 + h:57 + h]), [o], [junk, sm])
                    act(P, sm[:, 60:64], sm[:, 56:60], AF.Ln, [sm], [sm], scale=1.0 / 128, bias=1e-6)
                    act(P, sm[:, 60:64], sm[:, 60:64], AF.Exp, [sm], [sm], scale=-0.5)
                    for h in range(4):
                        stt(P, o[:, h, :], o[:, h, :], sm[:, 60 + h:61 + h], normw, ALU.mult, ALU.mult, [o, sm, rp], [o])
                    tt(P, ybf[:], o[:], b4(sg[:]), ALU.mult, [o, sg], [ybf])
                    c1b = C1[:, :].bitcast(BF16)
                    for h in range(4):
                        tr(P, c1b[:, h * 128:(h + 1) * 128], ybf[:, h, :], identb[:], [ybf, identb], [C1])
                    yt = yT[(sbi * NBLK + blk) % 2]
                    act(P, yt[:], b4(c1b[:, 0:512]), AF.Copy, [C1], [yt])
                    P.dma('sp', Kx.ycT.rearrange("k p t -> p k t")[:, 8:12, tg:tg + 128], yt[:], reads=[yt], writes=[Kx.d_ycT])
        except StopPass:
            pass
        P.flush()


HG_BASE = 4632
HG_NRP = 128


def pass_hg(P, Kx, T, l, labs):
    SBT = min(512, T)
    NSB = T // SBT
    NBLK = SBT // 128
    cst = Kx.cst
    A0, A1, B0, B1, C0, C1, D0, D1 = Kx.bank
    b4 = lambda ap: ap.rearrange("p (h c) -> p h c", h=4)
    with ExitStack() as st:
        P.stack = st
        try:
            W = P.sb("hg_W", [128, 8, 2048], BF16)
            rp = P.sb("hg_rp", [128, HG_NRP], F32)
            lbl = P.sb("hg_lbl", [128, 4, 4], F32)
            lbw = P.sb("hg_lbw", [128, 4 * 6], F32)
            lmk = P.sb("hg_lmk", [128, 4, 4], F32)
            hTb = [P.sb(f"hg_hT{i}", [128, 8, SBT], BF16) for i in range(2)]
            qTf = P.sb("hg_qTf", [128, 4, SBT], F32)
            kTf = P.sb("hg_kTf", [128, 4, SBT], F32)
            lgf = P.sb("hg_lgf", [128, 4, SBT], F32)
            ftmp = [P.sb(f"hg_ft{i}", [128, SBT], F32) for i in range(2)]
            ones = P.sb("hg_ones", [128, 128], F32)
            identb = P.sb("hg_identb", [128, 128], BF16)
            Bt = P.sb("hg_Bt", [128, 4, 132], F32)
            D1t = [P.sb(f"hg_D1{i}", [128, 8, 128], F32) for i in range(2)]
            Et = [P.sb(f"hg_E{i}", [128, 8, 128], F32) for i in range(2)]
            kfac = [P.sb(f"hg_kfac{i}", [128, 8, 128], BF16) for i in range(2)]
            Eq = P.sb("hg_Eq", [128, 4, 128], F32)
            EB = P.sb("hg_EB", [128, 4, 128], F32)
            Ek = P.sb("hg_Ek", [128, 4, 128], F32)
            qg = P.sb("hg_qg", [128, 4, 128], BF16)
            qG = P.sb("hg_qG", [128, 4, 128], BF16)
            kdT = P.sb("hg_kdT", [128, 4, 128], BF16)
            kdtm = P.sb("hg_kdtm", [128, 4, 128], BF16)
            scT = P.sb("hg_scT", [128, 4, 128], BF16)
            vbf = P.sb("hg_vbf", [128, 4, 128], BF16)
            sg = P.sb("hg_sg", [128, 512], F32)
            sm = P.sb("hg_sm", [128, 16], F32)
            junk = P.sb("hg_junk", [128, 128], F32)
            o = P.sb("hg_o", [128, 4, 128], F32)
            ybf = P.sb("hg_ybf", [128, 4, 128], BF16)
            yT = [P.sb(f"hg_yT{i}", [128, 4, 128], BF16) for i in range(2)]
            S32 = P.sb("hg_S32", [128, 4, 128], F32)
            Sbf = P.sb("hg_Sbf", [128, 4, 128], BF16)

            load_weights(P, W, lambda k, c0, c1: W[:, k, c0:c1],
                         lambda k, c0, c1: Kx.w_in[l, k * 128:(k + 1) * 128, HG_BASE + c0:HG_BASE + c1], 2048, 8)
            P.dma('sp', rp[:], Kx.hg_rp[l], writes=[rp])
            P.dma('sp', lbl[:], Kx.hg_lbl, writes=[lbl])
            normw = rp[:, 0:128]
            act(P, identb[:], cst[:, C_I, :], AF.Copy, [cst], [identb])
            P.op('dve', lambda e: e.memset(ones[:], 1.0), [], [ones])
            P.op('dve', lambda e: e.memset(S32[:], 0.0), [], [S32])
            P.op('dve', lambda e: e.memset(Sbf[:], 0.0), [], [Sbf])
            P.op('dve', lambda e: e.memset(Bt[:], 0.0), [], [Bt])
            mx, sme, rs, lb, oml = [lbw[:, i * 4:(i + 1) * 4] for i in range(5)]
            P.op('dve', lambda e: e.tensor_reduce(out=mx, in_=lbl[:], axis=AX.X, op=ALU.max), [lbl], [lbw])
            tt(P, lbl[:], lbl[:], mx.unsqueeze(2).to_broadcast([128, 4, 4]), ALU.subtract, [lbl, lbw], [lbl])
            act(P, lbl[:], lbl[:], AF.Exp, [lbl], [lbl])
            P.op('dve', lambda e: e.tensor_reduce(out=sme, in_=lbl[:], axis=AX.X, op=ALU.add), [lbl], [lbw])
            P.op('dve', lambda e: e.reciprocal(out=rs, in_=sme), [lbw], [lbw])
            P.dma('sp', lmk[:], Kx.hg_lmask[l], writes=[lmk])
            tt(P, lbl[:], lbl[:], lmk[:], ALU.mult, [lbl, lmk], [lbl])
            P.op('dve', lambda e: e.tensor_reduce(out=lb, in_=lbl[:], axis=AX.X, op=ALU.add), [lbl], [lbw])
            tt(P, lb, lb, rs, ALU.mult, [lbw], [lbw])
            ts(P, oml, lb, -1.0, 1.0, ALU.mult, ALU.add, [lbw], [lbw])
            chk(1)
            for sbi in range(NSB):
                hb = hTb[sbi % 2]
                P.dma('sp', hb[:], Kx.hT.rearrange("k p t -> p k t")[:, :, sbi * SBT:(sbi + 1) * SBT], reads=[Kx.d_hT], writes=[hb])
                for j in range(8):
                    bk = [D0, D1][j % 2]
                    h = j % 4
                    for k in range(8):
                        mm(P, bk[:, 0:SBT], W[:, k, j * 128:(j + 1) * 128], hb[:, k, :], k == 0, k == 7, [W, hb], [bk])
                    if j < 4:
                        act(P, qTf[:, h, :], bk[:, 0:SBT], AF.Silu, [bk], [qTf])
                    else:
                        f_ = ftmp[j % 2]
                        act(P, f_[:], bk[:, 0:SBT], AF.Sigmoid, [bk], [f_])
                        ts(P, f_[:], f_[:], oml[:, h:h + 1], lb[:, h:h + 1], ALU.mult, ALU.add, [f_, lbw], [f_])
                        act(P, lgf[:, h, :], f_[:], AF.Ln, [f_], [lgf])
                        ts(P, kTf[:, h, :], f_[:], -1.0, 1.0, ALU.mult, ALU.add, [f_], [kTf])
                chk(2)
                for blk in range(NBLK):
                    t0 = blk * 128
                    tg = sbi * SBT + t0
                    tsl = slice(t0, t0 + 128)
                    for k in range(8):
                        mm(P, A0[:, :], hb[:, k, tsl], W[:, k, 1024:1536], k == 0, k == 7, [hb, W], [A0])
                    act(P, vbf[:], b4(A0[:, :]), AF.Copy, [A0], [vbf])
                    for k in range(8):
                        mm(P, A1[:, :], hb[:, k, tsl], W[:, k, 1536:2048], k == 0, k == 7, [hb, W], [A1])
                    act(P, sg[:], A1[:, :], AF.Silu, [A1], [sg])
                    for h in range(4):
                        i2 = h % 2
                        D1_, E_, kf_ = D1t[i2], Et[i2], kfac[i2]
                        P.op('dve', lambda e, h=h, tsl=tsl: e.tensor_tensor_scan(out=Bt[:, h, 1:129], data0=ones[:, :], data1=lgf[:, h, tsl],
                                                                       initial=0.0, op0=ALU.mult, op1=ALU.add), [ones, lgf], [Bt])
                        for c in range(8):
                            ts(P, D1_[:, c, :], Bt[:, h, 1:129], Bt[:, h, 16 * c:16 * c + 1], -60.0, ALU.subtract, ALU.max, [Bt], [D1_])
                        act(P, E_[:], D1_[:], AF.Exp, [D1_], [E_], scale=-1.0)
                        tt(P, kf_[:], E_[:], kTf[:, h, tsl].unsqueeze(1).to_broadcast([128, 8, 128]), ALU.mult, [E_, kTf], [kf_])
                        base = D1_[:, :, :]
                        dg = bass.AP(base.tensor, base.offset, [list(base.ap[0]), [144, 8], [1, 16]])
                        act(P, Eq[:, h, :].rearrange("p (c j) -> p c j", c=8), dg, AF.Exp, [D1_], [Eq])
                        tt(P, qg[:, h, :], qTf[:, h, tsl], Eq[:, h, :], ALU.mult, [qTf, Eq], [qg])
                        act(P, EB[:, h, :], Bt[:, h, 1:129], AF.Exp, [Bt], [EB])
                        tt(P, qG[:, h, :], qTf[:, h, tsl], EB[:, h, :], ALU.mult, [qTf, EB], [qG])
                        act(P, Ek[:, h, :], Bt[:, h, 1:129], AF.Exp, [Bt], [Ek], scale=-1.0, bias=Bt[:, h, 128:129])
                        tt(P, kdT[:, h, :], kTf[:, h, tsl], Ek[:, h, :], ALU.mult, [kTf, Ek], [kdT])
                        for c in range(8):
                            mm(P, B0[:, h * 128 + 16 * c:h * 128 + 16 * c + 16], kf_[:, c, :], qg[:, h, 16 * c:16 * c + 16], True, True,
                               [kf_, qg], [B0])
                    tt(P, scT[:], b4(B0[:, :]), cst[:, C_LE, :].unsqueeze(1).to_broadcast([128, 4, 128]), ALU.mult, [B0, cst], [scT])
                    chk(3)
                    for h in range(4):
                        mm(P, B1[:, h * 128:(h + 1) * 128], scT[:, h, :], vbf[:, h, :], True, False, [scT, vbf], [B1])
                        mm(P, B1[:, h * 128:(h + 1) * 128], qG[:, h, :], Sbf[:, h, :], False, True, [qG, Sbf], [B1])
                    c0b = C0[:, :].bitcast(BF16)
                    for h in range(4):
                        tr(P, c0b[:, h * 128:(h + 1) * 128], kdT[:, h, :], identb[:], [kdT, identb], [C0])
                    act(P, kdtm[:], b4(c0b[:, 0:512]), AF.Copy, [C0], [kdtm])
                    for h in range(4):
                        mm(P, C1[:, h * 128:(h + 1) * 128], kdtm[:, h, :], vbf[:, h, :], True, True, [kdtm, vbf], [C1])
                    for h in range(4):
                        stt(P, S32[:, h, :], S32[:, h, :], EB[:, h, 127:128], C1[:, h * 128:(h + 1) * 128], ALU.mult, ALU.add,
                            [S32, EB, C1], [S32])
                    act(P, Sbf[:], S32[:], AF.Copy, [S32], [Sbf])
                    chk(4)
                    for h in range(4):
                        P.op('act', lambda e, h=h: e.activation(out=junk[:], in_=B1[:, h * 128:(h + 1) * 128], func=AF.Square,
                                                               accum_out=sm[:, h:h + 1]), [B1], [junk, sm])
                    act(P, sm[:, 4:8], sm[:, 0:4], AF.Ln, [sm], [sm], scale=1.0 / 128, bias=1e-6)
                    act(P, sm[:, 4:8], sm[:, 4:8], AF.Exp, [sm], [sm], scale=-0.5)
                    for h in range(4):
                        stt(P, o[:, h, :], B1[:, h * 128:(h + 1) * 128], sm[:, 4 + h:5 + h], normw, ALU.mult, ALU.mult, [B1, sm, rp], [o])
                    tt(P, ybf[:], o[:], b4(sg[:]), ALU.mult, [o, sg], [ybf])
                    c1b = C1[:, :].bitcast(BF16)
                    for h in range(4):
                        tr(P, c1b[:, h * 128:(h + 1) * 128], ybf[:, h, :], identb[:], [ybf, identb], [C1])
                    yt = yT[(sbi * NBLK + blk) % 2]
                    act(P, yt[:], b4(c1b[:, 0:512]), AF.Copy, [C1], [yt])
                    P.dma('sp', Kx.ycT.rearrange("k p t -> p k t")[:, 12:16, tg:tg + 128], yt[:], reads=[yt], writes=[Kx.d_ycT])
        except StopPass:
            pass
        P.flush()


OUT_NRP = 1024 + 1024 + 20
NSEL = 16


def layer_norm_block(P, r, stats, mv, sm2, g_ap, b_ap, reads_rp, out_ap, out_buf):
    for hf in range(2):
        P.op('dve', lambda e, hf=hf: e.bn_stats(out=stats[:, hf * 6:(hf + 1) * 6], in_=r[:, hf * 512:(hf + 1) * 512]), [r], [stats])
    P.op('dve', lambda e: e.bn_aggr(out=mv[:, 0:2], in_=stats[:, 0:12]), [stats], [mv])
    act(P, sm2[:, 0:1], mv[:, 1:2], AF.Ln, [mv], [sm2], bias=1e-5)
    act(P, sm2[:, 0:1], sm2[:, 0:1], AF.Exp, [sm2], [sm2], scale=-0.5)
    stt(P, sm2[:, 1:2], mv[:, 0:1], -1.0, sm2[:, 0:1], ALU.mult, ALU.mult, [mv, sm2], [sm2])
    act(P, r[:], r[:], AF.Identity, [r, sm2], [r], scale=sm2[:, 0:1], bias=sm2[:, 1:2])
    tt(P, r[:], r[:], g_ap, ALU.mult, [r] + reads_rp, [r])
    tt(P, out_ap, r[:], b_ap, ALU.add, [r] + reads_rp, [out_buf])


def pass_out(P, Kx, T, l, hsrc):
    cst = Kx.cst
    A0, A1, B0, B1, C0, C1, D0, D1 = Kx.bank
    NB = T // 128
    with ExitStack() as st:
        P.stack = st
        try:
            Wo = P.sb("out_W", [128, 16, 1024], BF16)
            rp = P.sb("out_rp", [128, OUT_NRP], F32)
            wr = P.sb("out_wr", [128, 8, 20], F32)
            ycb = [P.sb(f"out_yc{i}", [128, 16, 128], BF16) for i in range(2)]
            hin = [P.sb(f"out_h{i}", [128, 1024], F32) for i in range(2)]
            r = [P.sb(f"out_r{i}", [128, 1024], F32) for i in range(2)]
            x1 = [P.sb(f"out_x1{i}", [128, 1024], F32) for i in range(2)]
            x1Tf = P.sb("out_x1Tf", [128, 8, 128], F32)
            x1Tb = [P.sb(f"out_x1Tb{i}", [128, 8, 128], BF16) for i in range(2)]
            stats = P.sb("out_stats", [128, 12], F32)
            mv = P.sb("out_mv", [128, 2], F32)
            sm2 = P.sb("out_sm2", [128, 2], F32)
            q = P.sb("out_q", [128, 96], F32)
            comb = [P.sb(f"out_comb{i}", [128, 16], F32) for i in range(2)]
            combT = [P.sb(f"out_combT{i}", [16, 128], F32) for i in range(2)]

            for kc in range(16):
                P.dma('pool', Wo[:, kc, :], Kx.w_out[l, kc * 128:(kc + 1) * 128, :], writes=[Wo.sub(kc)])
            P.dma('sp', rp[:], Kx.out_rp[l], writes=[rp])
            P.dma('sp', wr[:], Kx.wr[l], writes=[wr])
            g1 = rp[:, 0:1024]
            b1 = rp[:, 1024:2048]
            rb = rp[:, 2048:2068]
            chk(1)
            for b in range(NB):
                tsl = slice(b * 128, (b + 1) * 128)
                yc, h_, r_, x_ = ycb[b % 2], hin[b % 2], r[b % 2], x1[b % 2]
                P.dma('sp', yc[:], Kx.ycT.rearrange("k p t -> p k t")[:, :, tsl], reads=[Kx.d_ycT], writes=[yc])
                P.dma('act', h_[:], hsrc[tsl, :], reads=[Kx.d_hres], writes=[h_])
                for hf in range(2):
                    bk = [A0, A1][hf]
                    for kc in range(16):
                        mm(P, bk[:, :], yc[:, kc, :], Wo[:, kc, hf * 512:(hf + 1) * 512], kc == 0, kc == 15, [yc, Wo], [bk])
                    stt(P, r_[:, hf * 512:(hf + 1) * 512], h_[:, hf * 512:(hf + 1) * 512], float(DN_ALPHA), bk[:, :], ALU.mult, ALU.add,
                        [h_, bk], [r_])
                layer_norm_block(P, r_, stats, mv, sm2, g1, b1, [rp], x_[:], x_)
                P.dma('sp', Kx.x1[tsl, :], x_[:], reads=[x_], writes=[Kx.d_x1])
                for j in range(8):
                    bk = [B0, B1][j // 4]
                    tr(P, bk[:, (j % 4) * 128:(j % 4 + 1) * 128], x_[:, j * 128:(j + 1) * 128], cst[:, C_I, :], [x_, cst], [bk])
                xb = x1Tb[b % 2]
                for hf in range(2):
                    bk = [B0, B1][hf]
                    act(P, x1Tf[:, hf * 4:(hf + 1) * 4, :], bk[:, :].rearrange("p (j t) -> p j t", j=4), AF.Copy, [bk], [x1Tf])
                    P.op('dve', lambda e, hf=hf, bk=bk, xb=xb: e.tensor_copy(out=xb[:, hf * 4:(hf + 1) * 4, :], in_=bk[:, :].rearrange("p (j t) -> p j t", j=4)),
                         [bk], [xb])
                P.dma('sp', Kx.x1T.rearrange("k p t -> p k t")[:, :, tsl], xb[:], reads=[xb], writes=[Kx.d_x1T])
                for k in range(8):
                    mm(P, C0[:, 0:20], x1Tf[:, k, :], wr[:, k, :], k == 0, k == 7, [x1Tf, wr], [C0])
                lgs = q[:, 0:20]
                gm, ngm, gsum, gp, m1, m2, dlt, ed, w1, w2 = [q[:, 20 + i:21 + i] for i in range(10)]
                ohg = q[:, 32:36]
                egj = q[:, 36:40]
                lsel = q[:, 40:44]
                oh1 = q[:, 44:48]
                msk = q[:, 48:52]
                oh2 = q[:, 52:56]
                wsel = q[:, 56:60]
                tmp16 = q[:, 64:80]
                tt(P, lgs, C0[:, 0:20], rb, ALU.add, [C0, rp], [q])
                P.op('dve', lambda e: e.tensor_reduce(out=gm, in_=lgs[:, 0:4], axis=AX.X, op=ALU.max), [q], [q])
                ts(P, ohg, lgs[:, 0:4], gm, None, ALU.is_equal, None, [q], [q])
                ts(P, ngm, gm, -1.0, None, ALU.mult, None, [q], [q])
                P.op('act', lambda e: e.activation(out=egj, in_=lgs[:, 0:4], func=AF.Exp, bias=ngm, accum_out=gsum), [q], [q])
                P.op('dve', lambda e: e.reciprocal(out=gp, in_=gsum), [q], [q])
                tt(P, tmp16.rearrange("p (g e) -> p g e", g=4), lgs[:, 4:20].rearrange("p (g e) -> p g e", g=4),
                   ohg.unsqueeze(2).to_broadcast([128, 4, 4]), ALU.mult, [q], [q])
                P.op('dve', lambda e: e.tensor_reduce(out=lsel, in_=tmp16.rearrange("p (g e) -> p e g", g=4), axis=AX.X, op=ALU.add), [q], [q])
                P.op('dve', lambda e: e.tensor_reduce(out=m1, in_=lsel, axis=AX.X, op=ALU.max), [q], [q])
                ts(P, oh1, lsel, m1, None, ALU.is_equal, None, [q], [q])
                stt(P, msk, oh1, -1e30, lsel, ALU.mult, ALU.add, [q], [q])
                P.op('dve', lambda e: e.tensor_reduce(out=m2, in_=msk, axis=AX.X, op=ALU.max), [q], [q])
                ts(P, oh2, msk, m2, None, ALU.is_equal, None, [q], [q])
                tt(P, dlt, m2, m1, ALU.subtract, [q], [q])
                act(P, ed, dlt, AF.Exp, [q], [q])
                ts(P, w1, ed, 1.0, None, ALU.add, None, [q], [q])
                P.op('dve', lambda e: e.reciprocal(out=w1, in_=w1), [q], [q])
                tt(P, w2, ed, w1, ALU.mult, [q], [q])
                tt(P, w1, w1, gp, ALU.mult, [q], [q])
                tt(P, w2, w2, gp, ALU.mult, [q], [q])
                ts(P, wsel, oh1, w1, None, ALU.mult, None, [q], [q])
                stt(P, wsel, oh2, w2, wsel, ALU.mult, ALU.add, [q], [q])
                cb_ = comb[b % 2]
                tt(P, cb_[:].rearrange("p (g e) -> p g e", g=4), ohg.unsqueeze(2).to_broadcast([128, 4, 4]),
                   wsel.unsqueeze(1).to_broadcast([128, 4, 4]), ALU.mult, [q], [cb_])
                P.dma('sp', Kx.comb[tsl, :], cb_[:], reads=[cb_], writes=[Kx.d_comb])
        except StopPass:
            pass
        P.flush()


def pass_moe(P, Kx, T, l, dst, write_hT):
    cst = Kx.cst
    A0, A1, B0, B1, C0, C1, D0, D1 = Kx.bank
    ST = min(1024, T)
    NST = T // ST
    NBS = ST // 128
    with ExitStack() as st:
        P.stack = st
        try:
            Wgu = [P.sb(f"moe_Wgu{i}", [128, 8, 512], BF16) for i in range(8)]
            Wdn = [P.sb(f"moe_Wdn{i}", [128, 2, 1024], BF16) for i in range(8)]
            for w_ in Wgu:
                for k in range(8):
                    w_.sub(k)
            for w_ in Wdn:
                for k in range(2):
                    w_.sub(k)
            rp = P.sb("moe_rp", [128, 2048], F32)
            xT = P.sb("moe_xT", [128, 8, ST], BF16)
            cmb = P.sb("moe_cmb", [128, NBS, 16], F32)
            yacc = P.sb("moe_yacc", [128, NBS, 1024], F32)
            yaccb = [yacc.sub(i) for i in range(NBS)]
            sgb = [P.sb(f"moe_sg{i}", [128, 256], F32) for i in range(2)]
            hb_ = [P.sb(f"moe_h{i}", [128, 256], BF16) for i in range(2)]
            hT = [P.sb(f"moe_hT{i}", [128, 2, 128], BF16) for i in range(2)]
            identb = P.sb("moe_identb", [128, 128], BF16)
            x1b = [P.sb(f"moe_x1{i}", [128, 1024], F32) for i in range(2)]
            ob = [P.sb(f"moe_o{i}", [128, 1024], F32) for i in range(2)]
            oT = [P.sb(f"moe_oT{i}", [128, 8, 128], BF16) for i in range(2)]
            stats = P.sb("moe_stats", [128, 12], F32)
            mv = P.sb("moe_mv", [128, 2], F32)
            sm2 = P.sb("moe_sm2", [128, 2], F32)
            P.dma('sp', rp[:], Kx.moe_rp[l], writes=[rp])
            g2 = rp[:, 0:1024]
            b2 = rp[:, 1024:2048]
            act(P, identb[:], cst[:, C_I, :], AF.Copy, [cst], [identb])
            it = 0
            for sti in range(NST):
                s0 = sti * ST
                P.dma('sp', xT[:], Kx.x1T.rearrange("k p t -> p k t")[:, :, s0:s0 + ST], reads=[Kx.d_x1T], writes=[xT])
                P.dma('sp', cmb[:], Kx.comb[s0:s0 + ST, :].rearrange("(b p) e -> p b e", p=128), reads=[Kx.d_comb], writes=[cmb])
                for G in range(4):
                    slot0 = ((sti * 4 + G) % 2) * 4
                    for e4 in range(4):
                        e = G * 4 + e4
                        wg, wd = Wgu[slot0 + e4], Wdn[slot0 + e4]
                        for k in range(8):
                            P.dma('pool', wg[:, k, :], Kx.w_gu[l, e, k * 128:(k + 1) * 128, :], writes=[wg.children[k]])
                        for fc in range(2):
                            P.dma('pool', wd[:, fc, :], Kx.w_dn[l, e, fc * 128:(fc + 1) * 128, :], writes=[wd.children[fc]])
                    for blk in range(NBS):
                        tsl = slice(blk * 128, (blk + 1) * 128)
                        ybk = [A0, A1] if blk % 2 == 0 else [B0, B1]
                        for e4 in range(4):
                            e = G * 4 + e4
                            wg, wd = Wgu[slot0 + e4], Wdn[slot0 + e4]
                            gb = [C0, C1][it % 2]
                            tb = [D0, D1][it % 2]
                            sg_, h_, hT_ = sgb[it % 2], hb_[it % 2], hT[it % 2]
                            it += 1
                            for k in range(8):
                                mm(P, gb[:, :], xT[:, k, tsl], wg[:, k, :], k == 0, k == 7, [xT, wg], [gb])
                            act(P, sg_[:], gb[:, 0:256], AF.Silu, [gb], [sg_])
                            stt(P, h_[:], gb[:, 256:512], cmb[:, blk, e:e + 1], sg_[:], ALU.mult, ALU.mult, [gb, cmb, sg_], [h_])
                            tbb = tb[:, :].bitcast(BF16)
                            for fc in range(2):
                                tr(P, tbb[:, fc * 128:(fc + 1) * 128], h_[:, fc * 128:(fc + 1) * 128], identb[:], [h_, identb], [tb])
                            act(P, hT_[:], tbb[:, 0:256].rearrange("p (f t) -> p f t", f=2), AF.Copy, [tb], [hT_])
                            for hf in range(2):
                                for fc in range(2):
                                    first = (e4 == 0 and fc == 0)
                                    last = (e4 == 3 and fc == 1)
                                    mm(P, ybk[hf][:, :], hT_[:, fc, :], wd[:, fc, hf * 512:(hf + 1) * 512], first, last, [hT_, wd], [ybk[hf]])
                        for hf in range(2):
                            ya = yacc[:, blk, hf * 512:(hf + 1) * 512]
                            if G == 0:
                                act(P, ya, ybk[hf][:, :], AF.Copy, [ybk[hf]], [yaccb[blk]])
                            else:
                                tt(P, ya, ybk[hf][:, :], ya, ALU.add, [ybk[hf], yaccb[blk]], [yaccb[blk]])
                for blk in range(NBS):
                    tg = s0 + blk * 128
                    x_, o_ = x1b[blk % 2], ob[blk % 2]
                    P.dma('act', x_[:], Kx.x1[tg:tg + 128, :], reads=[Kx.d_x1], writes=[x_])
                    stt(P, x_[:], x_[:], float(DN_ALPHA), yacc[:, blk, :], ALU.mult, ALU.add, [x_, yaccb[blk]], [x_])
                    layer_norm_block(P, x_, stats, mv, sm2, g2, b2, [rp], o_[:], o_)
                    P.dma('sp', dst[tg:tg + 128, :], o_[:], reads=[o_], writes=[Kx.d_hres])
                    if write_hT:
                        c0b = C0[:, :].bitcast(BF16)
                        ot = oT[blk % 2]
                        for j in range(8):
                            bk = [C0, C1][j // 4]
                            tr(P, bk[:, (j % 4) * 128:(j % 4 + 1) * 128], o_[:, j * 128:(j + 1) * 128], cst[:, C_I, :], [o_, cst], [bk])
                        for hf in range(2):
                            act(P, ot[:, hf * 4:(hf + 1) * 4, :], [C0, C1][hf][:, :].rearrange("p (j t) -> p j t", j=4), AF.Copy,
                                [[C0, C1][hf]], [ot])
                        P.dma('sp', Kx.hT.rearrange("k p t -> p k t")[:, :, tg:tg + 128], ot[:], reads=[ot], writes=[Kx.d_hT])
        except StopPass:
            pass
        P.flush()


def build(T, NL, dbg=False, passes=("ssd", "gdn", "hg", "out", "moe"), layers=None):
    layers = list(range(NL)) if layers is None else layers
    nc = bass.Bass("TRN2", target_bir_lowering=False)
    Kx = K()
    ext_in = lambda name, shape: nc.dram_tensor(name, list(shape), F32, kind="ExternalInput").ap()
    Kx.x = ext_in("x", [T, D_MODEL])
    Kx.w_in = ext_in("w_in", [NL, D_MODEL, IN_COLS])
    Kx.cst_d = ext_in("cst", [128, NCONST, 128])
    Kx.ssd_pp = ext_in("ssd_pp", [NL, 128, SSD_NPP])
    Kx.ssd_rp = ext_in("ssd_rp", [NL, 128, SSD_NRP])
    Kx.gdn_pp = ext_in("gdn_pp", [NL, 128, GDN_NPP])
    Kx.gdn_rp = ext_in("gdn_rp", [NL, 128, GDN_NRP])
    Kx.hg_rp = ext_in("hg_rp", [NL, 128, HG_NRP])
    Kx.hg_lbl = ext_in("hg_lbl", [128, 4, 4])
    Kx.hg_lmask = ext_in("hg_lmask", [NL, 128, 4, 4])
    Kx.w_out = ext_in("w_out", [NL, 2048, 1024])
    Kx.out_rp = ext_in("out_rp", [NL, 128, OUT_NRP])
    Kx.wr = ext_in("wr", [NL, 128, 8, 20])
    Kx.moe_rp = ext_in("moe_rp", [NL, 128, 2048])
    Kx.w_gu = ext_in("w_gu", [NL, 16, 1024, 512])
    Kx.w_dn = ext_in("w_dn", [NL, 16, 256, 1024])
    Kx.out = nc.dram_tensor("out", [T, D_MODEL], F32, kind="ExternalOutput").ap()
    skind = "ExternalOutput" if dbg else "Internal"
    Kx.hT = nc.dram_tensor("hT", [8, 128, T], BF16, kind=skind).ap()
    Kx.ycT = nc.dram_tensor("ycT", [16, 128, T], BF16, kind=skind).ap()
    Kx.x1 = nc.dram_tensor("x1", [T, D_MODEL], F32, kind=skind).ap()
    Kx.x1T = nc.dram_tensor("x1T", [8, 128, T], BF16, kind=skind).ap()
    Kx.comb = nc.dram_tensor("comb", [T, 16], F32, kind=skind).ap()
    Kx.hres = nc.dram_tensor("hres", [T, D_MODEL], F32, kind="Internal").ap()
    Kx.d_hT = Buf("d_hT")
    Kx.d_ycT = Buf("d_ycT")
    Kx.d_x1 = Buf("d_x1")
    Kx.d_x1T = Buf("d_x1T")
    Kx.d_comb = Buf("d_comb")
    Kx.d_hres = Buf("d_hres")
    with ExitStack() as st0:
        P = Prog(nc, st0)
        Kx.bank = []
        for i in range(8):
            t = st0.enter_context(nc.psum_tensor(f"bank{i}", [128, 512], F32))
            Kx.bank.append(Buf(f"bank{i}", t))
            Kx.bank[-1].excl = True
        Kx.cst = P.sb("cst_sb", [128, NCONST, 128], F32)
        P.dma('sp', Kx.cst[:], Kx.cst_d, writes=[Kx.cst])
        for l in range(NL):
            if l == 0:
                pass_transpose_in(P, Kx, T, Kx.x, Kx.hT)
            if "ssd" in passes:
                pass_ssd(P, Kx, T, l)
            if "gdn" in passes:
                pass_gdn(P, Kx, T, l)
            if "hg" in passes:
                pass_hg(P, Kx, T, l, layers[l])
            if "out" in passes:
                pass_out(P, Kx, T, l, Kx.x if l == 0 else Kx.hres)
            if "moe" in passes:
                pass_moe(P, Kx, T, l, Kx.out if l == NL - 1 else Kx.hres, l < NL - 1)
        print("recorded ops", P.nops, "waits", P.nwaits)
    return nc


def host_params(inp, layers):
    out = {}
    NL = len(layers)
    pp = np.zeros((NL, 128, SSD_NPP), np.float32)
    rp = np.zeros((NL, 128, SSD_NRP), np.float32)
    for i, l in enumerate(layers):
        cw = inp['ssd_conv_w'][l]
        pp[i, :, 0:48] = cw.reshape(4, 12, 128).transpose(2, 1, 0).reshape(128, 48)
        pp[i, :, 48:60] = inp['ssd_conv_b'][l].reshape(12, 128).T
        rp[i, :, 0:16] = inp['ssd_dt_bias'][l][None, :]
        rp[i, :, 16:32] = inp['ssd_a_log'][l][None, :]
        rp[i, :, 32:48] = inp['ssd_d'][l][None, :]
        rp[i, :, 48:48 + 1024] = inp['ssd_norm_w'][l][None, :]
    out['ssd_pp'] = pp
    out['ssd_rp'] = rp
    gpp = np.zeros((NL, 128, GDN_NPP), np.float32)
    grp = np.zeros((NL, 128, GDN_NRP), np.float32)
    for i, l in enumerate(layers):
        gpp[i, :, 0:48] = inp['gdn_conv_w'][l].reshape(4, 12, 128).transpose(2, 1, 0).reshape(128, 48)
        grp[i, :, 0:4] = inp['gdn_dt_bias'][l][None, :]
        grp[i, :, 4:8] = inp['gdn_a_log'][l][None, :]
        grp[i, :, 8:136] = inp['gdn_norm_w'][l][None, :]
    out['gdn_pp'] = gpp
    out['gdn_rp'] = grp
    hrp = np.zeros((NL, 128, HG_NRP), np.float32)
    for i, l in enumerate(layers):
        hrp[i, :, 0:128] = inp['hg_norm_w'][l][None, :]
    out['hg_rp'] = hrp
    lmask = np.zeros((NL, 128, 4, 4), np.float32)
    for i, l in enumerate(layers):
        lmask[i, :, :, 1:l + 1] = 1.0
    out['hg_lmask'] = lmask
    out['hg_lbl'] = np.ascontiguousarray(inp['hg_lb_logits'].reshape(4, 4, 128).transpose(2, 1, 0))
    orp = np.zeros((NL, 128, OUT_NRP), np.float32)
    wr = np.zeros((NL, 128, 8, 20), np.float32)
    mrp = np.zeros((NL, 128, 2048), np.float32)
    for i, l in enumerate(layers):
        orp[i, :, 0:1024] = inp['ln1_g'][l][None, :]
        orp[i, :, 1024:2048] = inp['ln1_b'][l][None, :]
        orp[i, :, 2048:2052] = inp['b_router_group'][l][None, :]
        orp[i, :, 2052:2068] = inp['b_router_expert'][l][None, :]
        wcat = np.concatenate([inp['w_router_group'][l], inp['w_router_expert'][l]], axis=1)
        wr[i] = wcat.reshape(8, 128, 20).transpose(1, 0, 2)
        mrp[i, :, 0:1024] = inp['ln2_g'][l][None, :]
        mrp[i, :, 1024:2048] = inp['ln2_b'][l][None, :]
    out['out_rp'] = orp
    out['wr'] = wr
    out['moe_rp'] = mrp
    out['cst'] = make_consts()
    return out


def core_inputs(inp, b, T, layers):
    hp = host_params(inp, layers)
    ls = layers
    im = {
        "x": np.ascontiguousarray(inp['x'][b, :T]),
        "w_in": np.ascontiguousarray(inp['w_in'][ls]),
        "w_out": np.ascontiguousarray(inp['w_out'][ls]),
        "w_gu": np.ascontiguousarray(inp['w_expert_gate_up'][ls]),
        "w_dn": np.ascontiguousarray(inp['w_expert_down'][ls]),
    }
    im.update(hp)
    return im


_PROG = {}


def kernel(**inputs):
    inp = {k: np.asarray(v) for k, v in inputs.items()}
    B, T, _ = inp['x'].shape
    ncores = 8
    h = np.ascontiguousarray(inp['x'], dtype=np.float32)
    for l in range(DEPTH):
        nc = build(T, 1)
        base = core_inputs(inp, 0, T, [l])
        in_maps = []
        for c in range(ncores):
            m = dict(base)
            m["x"] = np.ascontiguousarray(h[c % B])
            in_maps.append(m)
        res = run_bass_kernel_spmd(nc, in_maps, core_ids=list(range(ncores)))
        h = np.stack([np.asarray(res.results[b]["out"]) for b in range(B)]).astype(np.float32)
    return h
```

```python
import numpy as np
from contextlib import ExitStack
import concourse.bass as bass
import concourse.mybir as mybir
from concourse.bass_utils import run_bass_kernel_spmd

F32 = mybir.dt.float32
BF16 = mybir.dt.bfloat16
AF = mybir.ActivationFunctionType
ALU = mybir.AluOpType
AX = mybir.AxisListType

COMPUTE = ('pe', 'dve', 'act', 'pool')
ALLENG = ('pe', 'dve', 'act', 'pool', 'sp')
QUEUES = ('sp', 'act', 'pool')
NDMA = 8

D_MODEL = 1024
IN_COLS = 6680
DEPTH = 4
DN_ALPHA = (2 * DEPTH) ** 0.25


class Buf:
    def __init__(self, name, t=None, parent=None):
        self.name = name
        self.t = t
        self.parent = parent
        self.children = []
        self.last_write = None
        self.reads = []
        self.excl = False

    def sub(self, key):
        c = Buf(f"{self.name}.{key}", self.t, self)
        self.children.append(c)
        return c

    def __getitem__(self, k):
        return self.t[k]


class Prog:
    def __init__(self, nc, stack):
        self.nc = nc
        self.stack = stack
        self.ops = {e: [] for e in ALLENG}
        self.sem = {e: stack.enter_context(nc.semaphore(f"s_{e}")) for e in COMPUTE}
        self.cnt = {e: 0 for e in COMPUTE}
        self.known = {e: {} for e in ALLENG}
        self.dsem = {q: [stack.enter_context(nc.semaphore(f"d_{q}_{i}")) for i in range(NDMA)] for q in QUEUES}
        self.dcnt = {q: [0] * NDMA for q in QUEUES}
        self.dnext = {q: 0 for q in QUEUES}
        self.semobj = {}
        for e in COMPUTE:
            self.semobj[('c', e)] = self.sem[e]
        for q in QUEUES:
            for i in range(NDMA):
                self.semobj[('d', q, i)] = self.dsem[q][i]
        self.nops = 0
        self.nwaits = 0

    def sb(self, name, shape, dtype=F32):
        self.nalloc = getattr(self, 'nalloc', 0) + 1
        t = self.stack.enter_context(self.nc.sbuf_tensor(f"sb{self.nalloc}_{name}", list(shape), dtype))
        return Buf(name, t)

    def _collect(self, b, write):
        toks = []

        def add(x):
            if x.last_write is not None:
                toks.append(x.last_write)
            if write:
                toks.extend(x.reads)
        add(b)
        p = b.parent
        while p is not None:
            add(p)
            p = p.parent

        def rec(x):
            for c in x.children:
                add(c)
                rec(c)
        rec(b)
        return toks

    def _wait(self, eng, tok):
        key, val = tok
        if self.known[eng].get(key, 0) >= val:
            return
        self.known[eng][key] = val
        sem = self.semobj[key]
        self.ops[eng].append(lambda e, sem=sem, val=val: e.wait_ge(sem, val))
        self.nwaits += 1

    def _deps(self, eng, reads, writes, is_dma):
        own = ('c', eng) if (eng in COMPUTE and not is_dma) else None
        for b in reads:
            for tok in self._collect(b, False):
                self._wait(eng, tok)
            if b.excl:
                for tok in b.reads:
                    if own is not None and tok[0] == own:
                        continue
                    self._wait(eng, tok)
        for b in writes:
            for tok in self._collect(b, True):
                if own is not None and tok[0] == own:
                    continue
                self._wait(eng, tok)

    def _record(self, tok, reads, writes):
        for b in reads:
            b.reads.append(tok)
            if len(b.reads) > 64:
                b.reads = b.reads[-64:] if False else b.reads
        for b in writes:
            b.last_write = tok
            b.reads = []

            def rec(x):
                for c in x.children:
                    c.last_write = None
                    c.reads = []
                    rec(c)
            rec(b)

    def op(self, eng, fn, reads=(), writes=()):
        self._deps(eng, reads, writes, False)
        self.cnt[eng] += 1
        n = self.cnt[eng]
        sem = self.sem[eng]
        self.ops[eng].append(lambda e, fn=fn, sem=sem: fn(e).then_inc(sem, 1))
        tok = (('c', eng), n)
        self._record(tok, reads, writes)
        self.nops += 1
        return tok

    def dma(self, q, out, in_, reads=(), writes=(), **kw):
        self._deps(q, reads, writes, True)
        i = self.dnext[q]
        self.dnext[q] = (i + 1) % NDMA
        key = ('d', q, i)
        if self.dcnt[q][i] > 0:
            self._wait(q, (key, self.dcnt[q][i]))
        self.dcnt[q][i] += 16
        val = self.dcnt[q][i]
        sem = self.dsem[q][i]
        self.ops[q].append(lambda e, out=out, in_=in_, sem=sem, kw=kw:
                           e.dma_start(out=out, in_=in_, **kw).then_inc(sem, 16))
        tok = (key, val)
        self._record(tok, reads, writes)
        self.nops += 1
        return tok

    def barrier(self):
        for e in ALLENG:
            for c in COMPUTE:
                if self.cnt[c] > 0 and c != e:
                    self._wait(e, (('c', c), self.cnt[c]))
            for q in QUEUES:
                for i in range(NDMA):
                    if self.dcnt[q][i] > 0:
                        self._wait(e, (('d', q, i), self.dcnt[q][i]))

    def flush(self):
        self.barrier()
        nc = self.nc
        ops = self.ops
        self.ops = {e: [] for e in ALLENG}
        with nc.Block() as block:
            @block.sync
            def _(e):
                for f in ops['sp']:
                    f(e)

            @block.tensor
            def _(e):
                for f in ops['pe']:
                    f(e)

            @block.vector
            def _(e):
                for f in ops['dve']:
                    f(e)

            @block.scalar
            def _(e):
                for f in ops['act']:
                    f(e)

            @block.gpsimd
            def _(e):
                for f in ops['pool']:
                    f(e)


C_I, C_LE, C_GT, C_ONES, C_LE64, C_GT64, C_LT64, C_SAME64, C_SEL0, C_SEL1 = range(10)
NCONST = 10


def make_consts():
    k = np.arange(128)[:, None]
    l = np.arange(128)[None, :]
    c = np.zeros((128, NCONST, 128), np.float32)
    c[:, C_I] = (k == l)
    c[:, C_LE] = (k <= l)
    c[:, C_GT] = (k > l)
    c[:, C_ONES] = 1.0
    same64 = (k // 64) == (l // 64)
    c[:, C_LE64] = (k <= l) & same64
    c[:, C_GT64] = (k > l) & same64
    c[:, C_LT64] = (k < l) & same64
    c[:, C_SAME64] = same64
    c[:, C_SEL0] = (k < 64) & (l >= 0)
    c[:, C_SEL1] = (k >= 64) & (l >= 0)
    return c


class K:
    pass


STAGE = 99


class StopPass(Exception):
    pass


def chk(n):
    if STAGE == n:
        raise StopPass()


def mm(P, out, lhsT, rhs, start, stop, reads, writes):
    return P.op('pe', lambda e: e.matmul(out, lhsT=lhsT, rhs=rhs, start=start, stop=stop), reads, writes)


def tr(P, out, in_, ident, reads, writes):
    return P.op('pe', lambda e: e.transpose(out, in_, ident), reads, writes)


def act(P, out, in_, func, reads, writes, **kw):
    return P.op('act', lambda e: e.activation(out=out, in_=in_, func=func, **kw), reads, writes)


def tt(P, out, in0, in1, op, reads, writes):
    return P.op('dve', lambda e: e.tensor_tensor(out=out, in0=in0, in1=in1, op=op), reads, writes)


def ts(P, out, in0, s1, s2, op0, op1, reads, writes, **kw):
    if op1 is None:
        return P.op('dve', lambda e: e.tensor_scalar(out=out, in0=in0, scalar1=s1, scalar2=None, op0=op0, **kw), reads, writes)
    return P.op('dve', lambda e: e.tensor_scalar(out=out, in0=in0, scalar1=s1, scalar2=s2, op0=op0, op1=op1, **kw), reads, writes)


def stt(P, out, in0, scalar, in1, op0, op1, reads, writes):
    return P.op('dve', lambda e: e.scalar_tensor_tensor(out=out, in0=in0, scalar=scalar, in1=in1, op0=op0, op1=op1), reads, writes)


def load_weights(P, dst, dst_ap_fn, src_rows_fn, ncols, nk):
    toks = []
    if not dst.children:
        for k in range(nk):
            for c0 in range(0, ncols, 2048):
                dst.sub((k, c0))
    i = 0
    for k in range(nk):
        c0 = 0
        while c0 < ncols:
            c1 = min(ncols, c0 + 2048)
            toks.append(P.dma('pool', dst_ap_fn(k, c0, c1), src_rows_fn(k, c0, c1), writes=[dst.children[i]]))
            i += 1
            c0 = c1
    return toks


SSD_NPP = 12 * 4 + 12
SSD_NRP = 16 + 16 + 16 + 1024


def pass_transpose_in(P, Kx, T, src, dstT):
    nc = P.nc
    with ExitStack() as st:
        P.stack = st
        xt = [P.sb(f"p0_x{i}", [128, 1024], F32) for i in range(2)]
        xb = [P.sb(f"p0_b{i}", [128, 8, 128], BF16) for i in range(2)]
        for b in range(T // 128):
            x_ = xt[b % 2]
            o_ = xb[b % 2]
            P.dma('sp', x_[:], src[b * 128:(b + 1) * 128, :], writes=[x_])
            for j in range(8):
                bk = Kx.bank[j // 4]
                tr(P, bk[:, (j % 4) * 128:(j % 4 + 1) * 128], x_[:, j * 128:(j + 1) * 128], Kx.cst[:, C_I, :],
                   [x_, Kx.cst], [bk])
            for hlf in range(2):
                act(P, o_[:, hlf * 4:(hlf + 1) * 4, :], Kx.bank[hlf][:, :].rearrange("p (j t) -> p j t", j=4),
                    AF.Copy, [Kx.bank[hlf]], [o_])
            P.dma('sp', dstT.rearrange("k p t -> p k t")[:, :, b * 128:(b + 1) * 128], o_[:], reads=[o_], writes=[Kx.d_hT])
        P.flush()


def pass_ssd(P, Kx, T, l, dbg=None):
    nc = P.nc
    SBT = min(512, T)
    NSB = T // SBT
    NBLK = SBT // 128
    cst = Kx.cst
    bank = Kx.bank
    A0, A1, B0, B1, C0, C1, D0, D1 = bank
    with ExitStack() as st:
        P.stack = st
        try:
            W = P.sb("ssd_W", [128, 8, 2576], BF16)
            pp = P.sb("ssd_pp", [128, SSD_NPP], F32)
            rp = P.sb("ssd_rp", [128, SSD_NRP], F32)
            hTb = [P.sb(f"ssd_hT{i}", [128, 8, SBT], BF16) for i in range(2)]
            xin = P.sb("ssd_xin", [128, 12, SBT + 3], F32)
            accs = [P.sb(f"ssd_acc{i}", [128, SBT], F32) for i in range(2)]
            xc = P.sb("ssd_xc", [128, 12, SBT], F32)
            xcj = [xc.sub(j) for j in range(12)]
            xinj = [xin.sub(j) for j in range(12)]
            Dmat = P.sb("ssd_Dmat", [128, 16, 128], BF16)
            arep = P.sb("ssd_arep", [128, 16], F32)
            sm = P.sb("ssd_sm", [128, 16 * 12], F32)
            smv = lambda i: sm[:, i * 16:(i + 1) * 16]
            R = P.sb("ssd_R", [128, 16, 128], F32)
            DT = P.sb("ssd_DT", [128, 16, 128], BF16)
            GT = P.sb("ssd_GT", [128, 16, 128], BF16)
            CBTm = P.sb("ssd_CBTm", [128, 2, 128], BF16)
            xsb = P.sb("ssd_xsb", [128, 1024], BF16)
            xdt = P.sb("ssd_xdt", [128, 1024], BF16)
            xend = P.sb("ssd_xend", [128, 1024], BF16)
            Btm = P.sb("ssd_Btm", [128, 256], BF16)
            bcT = P.sb("ssd_bcT", [128, 4, 128], BF16)
            sz = P.sb("ssd_sz", [128, 1024], F32)
            yoff = P.sb("ssd_yoff", [128, 1024], F32)
            y = P.sb("ssd_y", [128, 1024], F32)
            junk = P.sb("ssd_junk", [128, 512], F32)
            ybf = P.sb("ssd_ybf", [128, 1024], BF16)
            yT = [P.sb(f"ssd_yT{i}", [128, 8, 128], BF16) for i in range(2)]
            S32 = P.sb("ssd_S32", [128, 1024], F32)
            Sbf = P.sb("ssd_Sbf", [128, 1024], BF16)
            identb = P.sb("ssd_identb", [128, 128], BF16)

            load_weights(P, W, lambda k, c0, c1: W[:, k, c0:c1],
                         lambda k, c0, c1: Kx.w_in[l, k * 128:(k + 1) * 128, c0:c1], 2576, 8)
            P.dma('sp', pp[:], Kx.ssd_pp[l], writes=[pp])
            P.dma('sp', rp[:], Kx.ssd_rp[l], writes=[rp])
            dtb = rp[:, 0:16]
            alog = rp[:, 16:32]
            drep = rp[:, 32:48]
            normw = rp[:, 48:48 + 1024]
            cw = lambda j, k: pp[:, j * 4 + k: j * 4 + k + 1]
            cb = lambda j: pp[:, 48 + j: 48 + j + 1]
            act(P, identb[:], cst[:, C_I, :], AF.Copy, [cst], [identb])
            act(P, arep[:], alog, AF.Exp, [rp], [arep])
            ts(P, arep[:], arep[:], -1.0, None, ALU.mult, None, [arep], [arep])
            tt(P, Dmat[:], cst[:, C_I, :].unsqueeze(1).to_broadcast([128, 16, 128]),
               drep.unsqueeze(2).to_broadcast([128, 16, 128]), ALU.mult, [cst, rp], [Dmat])
            P.op('dve', lambda e: e.memset(S32[:], 0.0), [], [S32])
            P.op('dve', lambda e: e.memset(Sbf[:], 0.0), [], [Sbf])
            P.op('dve', lambda e: e.memset(xin[:], 0.0), [], [xin])
            chk(1)

            for sbi in range(NSB):
                hb = hTb[sbi % 2]
                P.dma('sp', hb[:], Kx.hT.rearrange("k p t -> p k t")[:, :, sbi * SBT:(sbi + 1) * SBT], reads=[Kx.d_hT], writes=[hb])
                for j in range(12):
                    bk = [D0, D1][j % 2]
                    for k in range(8):
                        mm(P, bk[:, 0:SBT], W[:, k, 1024 + j * 128: 1024 + (j + 1) * 128], hb[:, k, :], k == 0, k == 7,
                           [W, hb], [bk])
                    ac = accs[j % 2]
                    act(P, xin[:, j, 3:3 + SBT], bk[:, 0:SBT], AF.Copy, [bk], [xinj[j]])
                    act(P, ac[:], bk[:, 0:SBT], AF.Identity, [bk, pp], [ac], scale=cw(j, 3), bias=cb(j))
                    for k in (2, 1, 0):
                        stt(P, ac[:], xin[:, j, k:k + SBT], cw(j, k), ac[:], ALU.mult, ALU.add, [xinj[j], pp, ac], [ac])
                    act(P, xc[:, j, :], ac[:], AF.Silu, [ac], [xcj[j]])
                    act(P, xin[:, j, 0:3], xin[:, j, SBT:SBT + 3], AF.Copy, [xinj[j]], [xinj[j]])
                chk(2)
                for blk in range(NBLK):
                    t0 = blk * 128
                    tg = sbi * SBT + t0
                    for hf in range(2):
                        bk = [A0, A1][hf]
                        for k in range(8):
                            mm(P, bk[:, :], hb[:, k, t0:t0 + 128], W[:, k, hf * 512:(hf + 1) * 512], k == 0, k == 7, [hb, W], [bk])
                        act(P, sz[:, hf * 512:(hf + 1) * 512], bk[:, :], AF.Silu, [bk], [sz])
                    for k in range(8):
                        mm(P, C0[:, 0:16], hb[:, k, t0:t0 + 128], W[:, k, 2560:2576], k == 0, k == 7, [hb, W], [C0])
                    xr, xm, ex, lg, dt, dA, acs, te, dte, ea, cd = [smv(i) for i in range(11)]
                    tt(P, xr, C0[:, 0:16], dtb, ALU.add, [C0, rp], [sm])
                    ts(P, xm, xr, 30.0, None, ALU.min, None, [sm], [sm])
                    act(P, ex, xm, AF.Exp, [sm], [sm])
                    act(P, lg, ex, AF.Ln, [sm], [sm], bias=1.0)
                    tt(P, dt, lg, xr, ALU.max, [sm], [sm])
                    tt(P, dA, dt, arep[:], ALU.mult, [sm, arep], [sm])
                    mm(P, C0[:, 16:32], cst[:, C_LE, :], dA, True, True, [cst, sm], [C0])
                    mm(P, C0[:, 32:48], cst[:, C_ONES, :], dA, True, True, [cst, sm], [C0])
                    act(P, ea, C0[:, 16:32], AF.Exp, [C0], [sm])
                    act(P, cd, C0[:, 32:48], AF.Exp, [C0], [sm])
                    act(P, acs, C0[:, 16:32], AF.Copy, [C0], [sm])
                    tt(P, te, C0[:, 32:48], acs, ALU.subtract, [C0, sm], [sm])
                    act(P, te, te, AF.Exp, [sm], [sm])
                    tt(P, dte, dt, te, ALU.mult, [sm], [sm])
                    chk(3)
                    tt(P, R[:], cst[:, C_LE, :].unsqueeze(1).to_broadcast([128, 16, 128]),
                       dA.unsqueeze(2).to_broadcast([128, 16, 128]), ALU.mult, [cst, sm], [R])
                    for q in range(4):
                        bk = [D0, D1][q % 2]
                        mm(P, bk[:, :], cst[:, C_GT, :], R[:, 4 * q:4 * q + 4, :], True, True, [cst, R], [bk])
                        act(P, DT[:, 4 * q:4 * q + 4, :], bk[:, :].rearrange("p (h l) -> p h l", h=4), AF.Exp, [bk], [DT])
                    chk(4)
                    for j in range(8):
                        bk = [B0, B1][j // 4]
                        tr(P, bk[:, (j % 4) * 128:(j % 4 + 1) * 128], xc[:, j, t0:t0 + 128], cst[:, C_I, :], [xcj[j], cst], [bk])
                    for hf in range(2):
                        bk = [B0, B1][hf]
                        pv = bk[:, :].rearrange("p (h c) -> p h c", h=8)
                        act(P, xsb[:, hf * 512:(hf + 1) * 512], bk[:, :], AF.Copy, [bk], [xsb])
                        chk(4.1)
                        tt(P, xdt[:, hf * 512:(hf + 1) * 512].rearrange("p (h c) -> p h c", h=8), pv,
                           dt[:, hf * 8:(hf + 1) * 8].unsqueeze(2).to_broadcast([128, 8, 64]), ALU.mult, [bk, sm], [xdt])
                        chk(4.11)
                        tt(P, xend[:, hf * 512:(hf + 1) * 512].rearrange("p (h c) -> p h c", h=8), pv,
                           dte[:, hf * 8:(hf + 1) * 8].unsqueeze(2).to_broadcast([128, 8, 64]), ALU.mult, [bk, sm], [xend])
                        chk(4.12)
                    chk(4.2)
                    for g in range(2):
                        tr(P, C0[:, 128 + g * 128:128 + (g + 1) * 128], xc[:, 8 + g, t0:t0 + 128], cst[:, C_I, :], [xcj[8 + g], cst], [C0])
                    act(P, Btm[:], C0[:, 128:384], AF.Copy, [C0], [Btm])
                    chk(4.3)
                    act(P, bcT[:], xc[:, 8:12, t0:t0 + 128], AF.Copy, [xcj[8], xcj[9], xcj[10], xcj[11]], [bcT])
                    chk(5)
                    for g in range(2):
                        mm(P, C1[:, g * 128:(g + 1) * 128], bcT[:, g, :], bcT[:, 2 + g, :], True, True, [bcT], [C1])
                    tt(P, CBTm[:], C1[:, 0:256].rearrange("p (g l) -> p g l", g=2),
                       cst[:, C_LE, :].unsqueeze(1).to_broadcast([128, 2, 128]), ALU.mult, [C1, cst], [CBTm])
                    for g in range(2):
                        tt(P, GT[:, g * 8:(g + 1) * 8, :], DT[:, g * 8:(g + 1) * 8, :],
                           CBTm[:, g, :].unsqueeze(1).to_broadcast([128, 8, 128]), ALU.mult, [DT, CBTm], [GT])
                    chk(6)
                    for h in range(16):
                        bk = [A0, A1][h // 8]
                        o = bk[:, (h % 8) * 64:(h % 8 + 1) * 64]
                        mm(P, o, GT[:, h, :], xdt[:, h * 64:(h + 1) * 64], True, False, [GT, xdt], [bk])
                        mm(P, o, Dmat[:, h, :], xsb[:, h * 64:(h + 1) * 64], False, True, [Dmat, xsb], [bk])
                    for g in range(2):
                        bk = [B0, B1][g]
                        mm(P, bk[:, :], bcT[:, 2 + g, :], Sbf[:, g * 512:(g + 1) * 512], True, True, [bcT, Sbf], [bk])
                        tt(P, yoff[:, g * 512:(g + 1) * 512].rearrange("p (h c) -> p h c", h=8),
                           bk[:, :].rearrange("p (h c) -> p h c", h=8),
                           ea[:, g * 8:(g + 1) * 8].unsqueeze(2).to_broadcast([128, 8, 64]), ALU.mult, [bk, sm], [yoff])
                        tt(P, y[:, g * 512:(g + 1) * 512], [A0, A1][g][:, :], yoff[:, g * 512:(g + 1) * 512], ALU.add,
                           [[A0, A1][g], yoff], [y])
                    tt(P, y[:], y[:], sz[:], ALU.mult, [y, sz], [y])
                    chk(7)
                    ss = smv(11)
                    for g in range(2):
                        P.op('act', lambda e, g=g: e.activation(out=junk[:], in_=y[:, g * 512:(g + 1) * 512], func=AF.Square,
                                                               accum_out=sm[:, 176 + g:177 + g]), [y], [junk, sm])
                    act(P, sm[:, 178:180], sm[:, 176:178], AF.Ln, [sm], [sm], scale=1.0 / 512, bias=1e-6)
                    act(P, sm[:, 178:180], sm[:, 178:180], AF.Exp, [sm], [sm], scale=-0.5)
                    for g in range(2):
                        stt(P, ybf[:, g * 512:(g + 1) * 512], y[:, g * 512:(g + 1) * 512], sm[:, 178 + g:179 + g],
                            normw[:, g * 512:(g + 1) * 512], ALU.mult, ALU.mult, [y, sm, rp], [ybf])
                    c1b = C1[:, :].bitcast(BF16)
                    for j in range(8):
                        tr(P, c1b[:, j * 128:(j + 1) * 128], ybf[:, j * 128:(j + 1) * 128], identb[:], [ybf, identb], [C1])
                    yt = yT[(sbi * NBLK + blk) % 2]
                    act(P, yt[:], c1b.rearrange("p (j t) -> p j t", j=8), AF.Copy, [C1], [yt])
                    P.dma('sp', Kx.ycT.rearrange("k p t -> p k t")[:, 0:8, tg:tg + 128], yt[:], reads=[yt], writes=[Kx.d_ycT])
                    chk(8)
                    for g in range(2):
                        bk = [D0, D1][g]
                        mm(P, bk[:, :], Btm[:, g * 128:(g + 1) * 128], xend[:, g * 512:(g + 1) * 512], True, True, [Btm, xend], [bk])
                    tt(P, S32[:].rearrange("p (h c) -> p h c", h=16), S32[:].rearrange("p (h c) -> p h c", h=16),
                       cd.unsqueeze(2).to_broadcast([128, 16, 64]), ALU.mult, [S32, sm], [S32])
                    for g in range(2):
                        tt(P, S32[:, g * 512:(g + 1) * 512], [D0, D1][g][:, :], S32[:, g * 512:(g + 1) * 512], ALU.add,
                           [[D0, D1][g], S32], [S32])
                    act(P, Sbf[:], S32[:], AF.Copy, [S32], [Sbf])
        except StopPass:
            pass
        P.flush()


GDN_NPP = 48
GDN_NRP = 4 + 4 + 128
GDN_BASE = 2576


def mmx(P, out, lhsT, rhs, start, stop, reads, writes):
    return P.op('pe', lambda e: e.matmul(out, lhsT=lhsT, rhs=rhs, start=start, stop=stop, skip_group_check=True), reads, writes)


def pass_gdn(P, Kx, T, l):
    SBT = min(512, T)
    NSB = T // SBT
    NBLK = SBT // 128
    cst = Kx.cst
    A0, A1, B0, B1, C0, C1, D0, D1 = Kx.bank
    b4 = lambda ap: ap.rearrange("p (h c) -> p h c", h=4)
    with ExitStack() as st:
        P.stack = st
        try:
            W = P.sb("gdn_W", [128, 8, 2056], BF16)
            pp = P.sb("gdn_pp", [128, GDN_NPP], F32)
            rp = P.sb("gdn_rp", [128, GDN_NRP], F32)
            hTb = [P.sb(f"gdn_hT{i}", [128, 8, SBT], BF16) for i in range(2)]
            xin = P.sb("gdn_xin", [128, 12, SBT + 3], F32)
            xinj = [xin.sub(j) for j in range(12)]
            accs = [P.sb(f"gdn_acc{i}", [128, SBT], F32) for i in range(2)]
            xc = P.sb("gdn_xc", [128, 12, SBT], F32)
            xcj = [xc.sub(j) for j in range(12)]
            sq = [P.sb(f"gdn_sq{i}", [128, SBT], F32) for i in range(2)]
            rn = [P.sb(f"gdn_rn{i}", [128, SBT], F32) for i in range(2)]
            qT = P.sb("gdn_qT", [128, 4, SBT], BF16)
            kT = P.sb("gdn_kT", [128, 4, SBT], BF16)
            identb = P.sb("gdn_identb", [128, 128], BF16)
            arep = P.sb("gdn_arep", [128, 4], F32)
            sm = P.sb("gdn_sm", [128, 4 * 20], F32)
            smv = lambda i: sm[:, i * 4:(i + 1) * 4]
            R = P.sb("gdn_R", [128, 4, 128], F32)
            Dec = P.sb("gdn_Dec", [128, 4, 128], F32)
            DecU = P.sb("gdn_DecU", [128, 4, 128], F32)
            t1 = P.sb("gdn_t1", [128, 4, 128], F32)
            qkTm = P.sb("gdn_qkTm", [128, 4, 128], BF16)
            Ya = [P.sb(f"gdn_Y{i}", [128, 4, 128], F32) for i in range(2)]
            Za = [P.sb(f"gdn_Z{i}", [128, 4, 128], F32) for i in range(2)]
            V = P.sb("gdn_V", [128, 4, 128], F32)
            Vbf = P.sb("gdn_Vbf", [128, 4, 128], BF16)
            v_tm = P.sb("gdn_vtm", [128, 4, 128], F32)
            kd = P.sb("gdn_kd", [128, 4, 128], BF16)
            rhs2 = P.sb("gdn_rhs2", [128, 4, 128], BF16)
            vnew = P.sb("gdn_vnew", [128, 4, 128], BF16)
            As = P.sb("gdn_As", [128, 4, 128], F32)
            o = P.sb("gdn_o", [128, 4, 128], F32)
            sg = P.sb("gdn_sg", [128, 512], F32)
            junk = P.sb("gdn_junk", [128, 128], F32)
            ybf = P.sb("gdn_ybf", [128, 4, 128], BF16)
            yT = [P.sb(f"gdn_yT{i}", [128, 4, 128], BF16) for i in range(2)]
            S32 = P.sb("gdn_S32", [128, 4, 128], F32)
            Sbf = P.sb("gdn_Sbf", [128, 4, 128], BF16)

            load_weights(P, W, lambda k, c0, c1: W[:, k, c0:c1],
                         lambda k, c0, c1: Kx.w_in[l, k * 128:(k + 1) * 128, GDN_BASE + c0:GDN_BASE + c1], 2056, 8)
            P.dma('sp', pp[:], Kx.gdn_pp[l], writes=[pp])
            P.dma('sp', rp[:], Kx.gdn_rp[l], writes=[rp])
            dtb = rp[:, 0:4]
            alog = rp[:, 4:8]
            normw = rp[:, 8:136]
            cw = lambda j, k: pp[:, j * 4 + k: j * 4 + k + 1]
            act(P, identb[:], cst[:, C_I, :], AF.Copy, [cst], [identb])
            act(P, arep[:], alog, AF.Exp, [rp], [arep])
            ts(P, arep[:], arep[:], -1.0, None, ALU.mult, None, [arep], [arep])
            P.op('dve', lambda e: e.memset(S32[:], 0.0), [], [S32])
            P.op('dve', lambda e: e.memset(Sbf[:], 0.0), [], [Sbf])
            P.op('dve', lambda e: e.memset(xin[:], 0.0), [], [xin])
            chk(1)
            for sbi in range(NSB):
                hb = hTb[sbi % 2]
                P.dma('sp', hb[:], Kx.hT.rearrange("k p t -> p k t")[:, :, sbi * SBT:(sbi + 1) * SBT], reads=[Kx.d_hT], writes=[hb])
                for j in range(12):
                    bk = [D0, D1][j % 2]
                    for k in range(8):
                        mm(P, bk[:, 0:SBT], W[:, k, j * 128:(j + 1) * 128], hb[:, k, :], k == 0, k == 7, [W, hb], [bk])
                    ac = accs[j % 2]
                    act(P, xin[:, j, 3:3 + SBT], bk[:, 0:SBT], AF.Copy, [bk], [xinj[j]])
                    act(P, ac[:], bk[:, 0:SBT], AF.Copy, [bk, pp], [ac], scale=cw(j, 3))
                    for k in (2, 1, 0):
                        stt(P, ac[:], xin[:, j, k:k + SBT], cw(j, k), ac[:], ALU.mult, ALU.add, [xinj[j], pp, ac], [ac])
                    act(P, xc[:, j, :], ac[:], AF.Silu, [ac], [xcj[j]])
                    act(P, xin[:, j, 0:3], xin[:, j, SBT:SBT + 3], AF.Copy, [xinj[j]], [xinj[j]])
                    if j < 8:
                        s_ = sq[j % 2]
                        r_ = rn[j % 2]
                        bk2 = [C0, C1][j % 2]
                        act(P, s_[:], xc[:, j, :], AF.Square, [xcj[j]], [s_])
                        mm(P, bk2[:, 0:SBT], cst[:, C_ONES, :], s_[:], True, True, [cst, s_], [bk2])
                        act(P, r_[:], bk2[:, 0:SBT], AF.Ln, [bk2], [r_], bias=1e-6)
                        act(P, r_[:], r_[:], AF.Exp, [r_], [r_], scale=-0.5,
                            bias=(-0.5 * float(np.log(128.0))) if j < 4 else 0.0)
                        dst = qT if j < 4 else kT
                        tt(P, dst[:, j % 4, :], xc[:, j, :], r_[:], ALU.mult, [xcj[j], r_], [dst])
                chk(2)
                for blk in range(NBLK):
                    t0 = blk * 128
                    tg = sbi * SBT + t0
                    tsl = slice(t0, t0 + 128)
                    for k in range(8):
                        mm(P, D0[:, :], hb[:, k, tsl], W[:, k, 1536:2048], k == 0, k == 7, [hb, W], [D0])
                    act(P, sg[:], D0[:, :], AF.Silu, [D0], [sg])
                    for k in range(8):
                        mm(P, C0[:, 0:8], hb[:, k, tsl], W[:, k, 2048:2056], k == 0, k == 7, [hb, W], [C0])
                    eb, beta, xr, xm, ex, lg, sp, la, eg, gs, ekd, negeg = [smv(i) for i in range(12)]
                    glrep = sm[:, 48:56]
                    act(P, eb, C0[:, 0:4], AF.Exp, [C0], [sm], scale=-1.0)
                    ts(P, eb, eb, 1.0, None, ALU.add, None, [sm], [sm])
                    P.op('dve', lambda e: e.reciprocal(out=beta, in_=eb), [sm], [sm])
                    tt(P, xr, C0[:, 4:8], dtb, ALU.add, [C0, rp], [sm])
                    ts(P, xm, xr, 30.0, None, ALU.min, None, [sm], [sm])
                    act(P, ex, xm, AF.Exp, [sm], [sm])
                    act(P, lg, ex, AF.Ln, [sm], [sm], bias=1.0)
                    tt(P, sp, lg, xr, ALU.max, [sm], [sm])
                    tt(P, la, sp, arep[:], ALU.mult, [sm, arep], [sm])
                    mm(P, C0[:, 8:12], cst[:, C_LE64, :], la, True, True, [cst, sm], [C0])
                    mm(P, C0[:, 12:16], cst[:, C_SAME64, :], la, True, True, [cst, sm], [C0])
                    mm(P, C0[:, 16:20], cst[:, C_SEL0, :], la, True, True, [cst, sm], [C0])
                    mm(P, C0[:, 20:24], cst[:, C_SEL1, :], la, True, True, [cst, sm], [C0])
                    act(P, eg, C0[:, 8:12], AF.Exp, [C0], [sm])
                    act(P, gs, C0[:, 8:12], AF.Copy, [C0], [sm])
                    act(P, glrep, C0[:, 16:24], AF.Exp, [C0], [sm])
                    tt(P, ekd, C0[:, 12:16], gs, ALU.subtract, [C0, sm], [sm])
                    act(P, ekd, ekd, AF.Exp, [sm], [sm])
                    ts(P, negeg, eg, -1.0, None, ALU.mult, None, [sm], [sm])
                    chk(3)
                    tt(P, R[:], cst[:, C_LE64, :].unsqueeze(1).to_broadcast([128, 4, 128]),
                       la.unsqueeze(2).to_broadcast([128, 4, 128]), ALU.mult, [cst, sm], [R])
                    mm(P, A0[:, :], cst[:, C_GT64, :], R[:], True, True, [cst, R], [A0])
                    act(P, Dec[:], b4(A0[:, :]), AF.Exp, [A0], [Dec])
                    tt(P, DecU[:], Dec[:], cst[:, C_LE64, :].unsqueeze(1).to_broadcast([128, 4, 128]), ALU.mult, [Dec, cst], [DecU])
                    for h in range(4):
                        mm(P, A1[:, h * 128:(h + 1) * 128], kT[:, h, tsl], kT[:, h, tsl], True, True, [kT], [A1])
                    for h in range(4):
                        mm(P, B0[:, h * 128:(h + 1) * 128], kT[:, h, tsl], qT[:, h, tsl], True, True, [kT, qT], [B0])
                    tt(P, qkTm[:], b4(B0[:, :]), DecU[:], ALU.mult, [B0, DecU], [qkTm])
                    tt(P, t1[:], b4(A1[:, :]), DecU[:], ALU.mult, [A1, DecU], [t1])
                    X = Ya[0]
                    for h in range(4):
                        stt(P, X[:, h, :], t1[:, h, :], beta[:, h:h + 1], cst[:, C_LT64, :], ALU.mult, ALU.mult, [t1, sm, cst], [X])
                    chk(4)
                    for h in range(4):
                        tr(P, B1[:, h * 128:(h + 1) * 128], X[:, h, :], cst[:, C_I, :], [X, cst], [B1])
                    act(P, Za[0][:], b4(B1[:, :]), AF.Copy, [B1], [Za[0]])
                    tt(P, V[:], cst[:, C_I, :].unsqueeze(1).to_broadcast([128, 4, 128]), X[:], ALU.subtract, [cst, X], [V])
                    for lev in range(5):
                        Yc, Zc = Ya[lev % 2], Za[lev % 2]
                        Yn, Zn = Ya[(lev + 1) % 2], Za[(lev + 1) % 2]
                        for h in range(4):
                            mm(P, C1[:, h * 128:(h + 1) * 128], Yc[:, h, :], Zc[:, h, :], True, True, [Yc, Zc], [C1])
                        act(P, Zn[:], b4(C1[:, :]), AF.Copy, [C1], [Zn])
                        if lev < 4:
                            for h in range(4):
                                mm(P, B1[:, h * 128:(h + 1) * 128], Zc[:, h, :], Yc[:, h, :], True, True, [Yc, Zc], [B1])
                            act(P, Yn[:], b4(B1[:, :]), AF.Copy, [B1], [Yn])
                        for h in range(4):
                            mm(P, A0[:, h * 128:(h + 1) * 128], Zn[:, h, :], V[:, h, :], True, True, [Zn, V], [A0])
                        tt(P, V[:], b4(A0[:, :]), V[:], ALU.add, [A0, V], [V])
                    act(P, Vbf[:], V[:], AF.Copy, [V], [Vbf])
                    chk(5)
                    for h in range(4):
                        tr(P, D1[:, h * 128:(h + 1) * 128], xc[:, 8 + h, tsl], cst[:, C_I, :], [xcj[8 + h], cst], [D1])
                    act(P, v_tm[:], b4(D1[:, :]), AF.Copy, [D1], [v_tm])
                    d0b = D0[:, :].bitcast(BF16)
                    for h in range(4):
                        tr(P, d0b[:, h * 128:(h + 1) * 128], kT[:, h, tsl], identb[:], [kT, identb], [D0])
                    tt(P, kd[:], b4(d0b[:, 0:512]), ekd.unsqueeze(2).to_broadcast([128, 4, 128]), ALU.mult, [D0, sm], [kd])
                    chk(6)
                    for c in range(2):
                        sl = slice(64 * c, 64 * c + 64)
                        for h in range(4):
                            mm(P, A0[:, h * 128:(h + 1) * 128], kT[:, h, tsl], Sbf[:, h, :], True, True, [kT, Sbf], [A0])
                        for h in range(4):
                            stt(P, rhs2[sl, h, :], A0[sl, h * 128:(h + 1) * 128], negeg[sl, h:h + 1], v_tm[sl, h, :],
                                ALU.mult, ALU.add, [A0, sm, v_tm], [rhs2])
                        for h in range(4):
                            mm(P, A1[:, h * 128:(h + 1) * 128], Vbf[sl, h, :], rhs2[sl, h, :], True, True, [Vbf, rhs2], [A1])
                        tt(P, vnew[sl], b4(A1[sl, :]), beta[sl].unsqueeze(2).to_broadcast([64, 4, 128]), ALU.mult, [A1, sm], [vnew])
                        for h in range(4):
                            mm(P, B0[:, h * 128:(h + 1) * 128], qT[:, h, tsl], Sbf[:, h, :], True, True, [qT, Sbf], [B0])
                        tt(P, As[sl], b4(B0[sl, :]), eg[sl].unsqueeze(2).to_broadcast([64, 4, 128]), ALU.mult, [B0, sm], [As])
                        for h in range(4):
                            mmx(P, B1[:, h * 128:(h + 1) * 128], qkTm[sl, h, :], vnew[sl, h, :], (c == 0 and h == 0), (c == 1),
                                [qkTm, vnew], [B1])
                        for h in range(4):
                            mm(P, C1[:, h * 128:(h + 1) * 128], kd[sl, h, :], vnew[sl, h, :], True, True, [kd, vnew], [C1])
                        for h in range(4):
                            stt(P, S32[:, h, :], S32[:, h, :], glrep[:, c * 4 + h:c * 4 + h + 1], C1[:, h * 128:(h + 1) * 128],
                                ALU.mult, ALU.add, [S32, sm, C1], [S32])
                        act(P, Sbf[:], S32[:], AF.Copy, [S32], [Sbf])
                    chk(7)
                    tt(P, o[:], b4(B1[:, :]), As[:], ALU.add, [B1, As], [o])
                    for h in range(4):
                        P.op('act', lambda e, h=h: e.activation(out=junk[:], in_=o[:, h, :], func=AF.Square,
                                                               accum_out=sm[:, 56 + h:57 + h]), [o], [junk, sm])
                    act(P, sm[:, 60:64], sm[:, 56:60], AF.Ln, [sm], [sm], scale=1.0 / 128, bias=1e-6)
                    act(P, sm[:, 60:64], sm[:, 60:64], AF.Exp, [sm], [sm], scale=-0.5)
                    for h in range(4):
                        stt(P, o[:, h, :], o[:, h, :], sm[:, 60 + h:61 + h], normw, ALU.mult, ALU.mult, [o, sm, rp], [o])
                    tt(P, ybf[:], o[:], b4(sg[:]), ALU.mult, [o, sg], [ybf])
                    c1b = C1[:, :].bitcast(BF16)
                    for h in range(4):
                        tr(P, c1b[:, h * 128:(h + 1) * 128], ybf[:, h, :], identb[:], [ybf, identb], [C1])
                    yt = yT[(sbi * NBLK + blk) % 2]
                    act(P, yt[:], b4(c1b[:, 0:512]), AF.Copy, [C1], [yt])
                    P.dma('sp', Kx.ycT.rearrange("k p t -> p k t")[:, 8:12, tg:tg + 128], yt[:], reads=[yt], writes=[Kx.d_ycT])
        except StopPass:
            pass
        P.flush()


HG_BASE = 4632
HG_NRP = 128


def pass_hg(P, Kx, T, l, labs):
    SBT = min(512, T)
    NSB = T // SBT
    NBLK = SBT // 128
    cst = Kx.cst
    A0, A1, B0, B1, C0, C1, D0, D1 = Kx.bank
    b4 = lambda ap: ap.rearrange("p (h c) -> p h c", h=4)
    with ExitStack() as st:
        P.stack = st
        try:
            W = P.sb("hg_W", [128, 8, 2048], BF16)
            rp = P.sb("hg_rp", [128, HG_NRP], F32)
            lbl = P.sb("hg_lbl", [128, 4, 4], F32)
            lbw = P.sb("hg_lbw", [128, 4 * 6], F32)
            lmk = P.sb("hg_lmk", [128, 4, 4], F32)
            hTb = [P.sb(f"hg_hT{i}", [128, 8, SBT], BF16) for i in range(2)]
            qTf = P.sb("hg_qTf", [128, 4, SBT], F32)
            kTf = P.sb("hg_kTf", [128, 4, SBT], F32)
            lgf = P.sb("hg_lgf", [128, 4, SBT], F32)
            ftmp = [P.sb(f"hg_ft{i}", [128, SBT], F32) for i in range(2)]
            ones = P.sb("hg_ones", [128, 128], F32)
            identb = P.sb("hg_identb", [128, 128], BF16)
            Bt = P.sb("hg_Bt", [128, 4, 132], F32)
            D1t = [P.sb(f"hg_D1{i}", [128, 8, 128], F32) for i in range(2)]
            Et = [P.sb(f"hg_E{i}", [128, 8, 128], F32) for i in range(2)]
            kfac = [P.sb(f"hg_kfac{i}", [128, 8, 128], BF16) for i in range(2)]
            Eq = P.sb("hg_Eq", [128, 4, 128], F32)
            EB = P.sb("hg_EB", [128, 4, 128], F32)
            Ek = P.sb("hg_Ek", [128, 4, 128], F32)
            qg = P.sb("hg_qg", [128, 4, 128], BF16)
            qG = P.sb("hg_qG", [128, 4, 128], BF16)
            kdT = P.sb("hg_kdT", [128, 4, 128], BF16)
            kdtm = P.sb("hg_kdtm", [128, 4, 128], BF16)
            scT = P.sb("hg_scT", [128, 4, 128], BF16)
            vbf = P.sb("hg_vbf", [128, 4, 128], BF16)
            sg = P.sb("hg_sg", [128, 512], F32)
            sm = P.sb("hg_sm", [128, 16], F32)
            junk = P.sb("hg_junk", [128, 128], F32)
            o = P.sb("hg_o", [128, 4, 128], F32)
            ybf = P.sb("hg_ybf", [128, 4, 128], BF16)
            yT = [P.sb(f"hg_yT{i}", [128, 4, 128], BF16) for i in range(2)]
            S32 = P.sb("hg_S32", [128, 4, 128], F32)
            Sbf = P.sb("hg_Sbf", [128, 4, 128], BF16)

            load_weights(P, W, lambda k, c0, c1: W[:, k, c0:c1],
                         lambda k, c0, c1: Kx.w_in[l, k * 128:(k + 1) * 128, HG_BASE + c0:HG_BASE + c1], 2048, 8)
            P.dma('sp', rp[:], Kx.hg_rp[l], writes=[rp])
            P.dma('sp', lbl[:], Kx.hg_lbl, writes=[lbl])
            normw = rp[:, 0:128]
            act(P, identb[:], cst[:, C_I, :], AF.Copy, [cst], [identb])
            P.op('dve', lambda e: e.memset(ones[:], 1.0), [], [ones])
            P.op('dve', lambda e: e.memset(S32[:], 0.0), [], [S32])
            P.op('dve', lambda e: e.memset(Sbf[:], 0.0), [], [Sbf])
            P.op('dve', lambda e: e.memset(Bt[:], 0.0), [], [Bt])
            mx, sme, rs, lb, oml = [lbw[:, i * 4:(i + 1) * 4] for i in range(5)]
            P.op('dve', lambda e: e.tensor_reduce(out=mx, in_=lbl[:], axis=AX.X, op=ALU.max), [lbl], [lbw])
            tt(P, lbl[:], lbl[:], mx.unsqueeze(2).to_broadcast([128, 4, 4]), ALU.subtract, [lbl, lbw], [lbl])
            act(P, lbl[:], lbl[:], AF.Exp, [lbl], [lbl])
            P.op('dve', lambda e: e.tensor_reduce(out=sme, in_=lbl[:], axis=AX.X, op=ALU.add), [lbl], [lbw])
            P.op('dve', lambda e: e.reciprocal(out=rs, in_=sme), [lbw], [lbw])
            P.dma('sp', lmk[:], Kx.hg_lmask[l], writes=[lmk])
            tt(P, lbl[:], lbl[:], lmk[:], ALU.mult, [lbl, lmk], [lbl])
            P.op('dve', lambda e: e.tensor_reduce(out=lb, in_=lbl[:], axis=AX.X, op=ALU.add), [lbl], [lbw])
            tt(P, lb, lb, rs, ALU.mult, [lbw], [lbw])
            ts(P, oml, lb, -1.0, 1.0, ALU.mult, ALU.add, [lbw], [lbw])
            chk(1)
            for sbi in range(NSB):
                hb = hTb[sbi % 2]
                P.dma('sp', hb[:], Kx.hT.rearrange("k p t -> p k t")[:, :, sbi * SBT:(sbi + 1) * SBT], reads=[Kx.d_hT], writes=[hb])
                for j in range(8):
                    bk = [D0, D1][j % 2]
                    h = j % 4
                    for k in range(8):
                        mm(P, bk[:, 0:SBT], W[:, k, j * 128:(j + 1) * 128], hb[:, k, :], k == 0, k == 7, [W, hb], [bk])
                    if j < 4:
                        act(P, qTf[:, h, :], bk[:, 0:SBT], AF.Silu, [bk], [qTf])
                    else:
                        f_ = ftmp[j % 2]
                        act(P, f_[:], bk[:, 0:SBT], AF.Sigmoid, [bk], [f_])
                        ts(P, f_[:], f_[:], oml[:, h:h + 1], lb[:, h:h + 1], ALU.mult, ALU.add, [f_, lbw], [f_])
                        act(P, lgf[:, h, :], f_[:], AF.Ln, [f_], [lgf])
                        ts(P, kTf[:, h, :], f_[:], -1.0, 1.0, ALU.mult, ALU.add, [f_], [kTf])
                chk(2)
                for blk in range(NBLK):
                    t0 = blk * 128
                    tg = sbi * SBT + t0
                    tsl = slice(t0, t0 + 128)
                    for k in range(8):
                        mm(P, A0[:, :], hb[:, k, tsl], W[:, k, 1024:1536], k == 0, k == 7, [hb, W], [A0])
                    act(P, vbf[:], b4(A0[:, :]), AF.Copy, [A0], [vbf])
                    for k in range(8):
                        mm(P, A1[:, :], hb[:, k, tsl], W[:, k, 1536:2048], k == 0, k == 7, [hb, W], [A1])
                    act(P, sg[:], A1[:, :], AF.Silu, [A1], [sg])
                    for h in range(4):
                        i2 = h % 2
                        D1_, E_, kf_ = D1t[i2], Et[i2], kfac[i2]
                        P.op('dve', lambda e, h=h, tsl=tsl: e.tensor_tensor_scan(out=Bt[:, h, 1:129], data0=ones[:, :], data1=lgf[:, h, tsl],
                                                                       initial=0.0, op0=ALU.mult, op1=ALU.add), [ones, lgf], [Bt])
                        for c in range(8):
                            ts(P, D1_[:, c, :], Bt[:, h, 1:129], Bt[:, h, 16 * c:16 * c + 1], -60.0, ALU.subtract, ALU.max, [Bt], [D1_])
                        act(P, E_[:], D1_[:], AF.Exp, [D1_], [E_], scale=-1.0)
                        tt(P, kf_[:], E_[:], kTf[:, h, tsl].unsqueeze(1).to_broadcast([128, 8, 128]), ALU.mult, [E_, kTf], [kf_])
                        base = D1_[:, :, :]
                        dg = bass.AP(base.tensor, base.offset, [list(base.ap[0]), [144, 8], [1, 16]])
                        act(P, Eq[:, h, :].rearrange("p (c j) -> p c j", c=8), dg, AF.Exp, [D1_], [Eq])
                        tt(P, qg[:, h, :], qTf[:, h, tsl], Eq[:, h, :], ALU.mult, [qTf, Eq], [qg])
                        act(P, EB[:, h, :], Bt[:, h, 1:129], AF.Exp, [Bt], [EB])
                        tt(P, qG[:, h, :], qTf[:, h, tsl], EB[:, h, :], ALU.mult, [qTf, EB], [qG])
                        act(P, Ek[:, h, :], Bt[:, h, 1:129], AF.Exp, [Bt], [Ek], scale=-1.0, bias=Bt[:, h, 128:129])
                        tt(P, kdT[:, h, :], kTf[:, h, tsl], Ek[:, h, :], ALU.mult, [kTf, Ek], [kdT])
                        for c in range(8):
                            mm(P, B0[:, h * 128 + 16 * c:h * 128 + 16 * c + 16], kf_[:, c, :], qg[:, h, 16 * c:16 * c + 16], True, True,
                               [kf_, qg], [B0])
                    tt(P, scT[:], b4(B0[:, :]), cst[:, C_LE, :].unsqueeze(1).to_broadcast([128, 4, 128]), ALU.mult, [B0, cst], [scT])
                    chk(3)
                    for h in range(4):
                        mm(P, B1[:, h * 128:(h + 1) * 128], scT[:, h, :], vbf[:, h, :], True, False, [scT, vbf], [B1])
                        mm(P, B1[:, h * 128:(h + 1) * 128], qG[:, h, :], Sbf[:, h, :], False, True, [qG, Sbf], [B1])
                    c0b = C0[:, :].bitcast(BF16)
                    for h in range(4):
                        tr(P, c0b[:, h * 128:(h + 1) * 128], kdT[:, h, :], identb[:], [kdT, identb], [C0])
                    act(P, kdtm[:], b4(c0b[:, 0:512]), AF.Copy, [C0], [kdtm])
                    for h in range(4):
                        mm(P, C1[:, h * 128:(h + 1) * 128], kdtm[:, h, :], vbf[:, h, :], True, True, [kdtm, vbf], [C1])
                    for h in range(4):
                        stt(P, S32[:, h, :], S32[:, h, :], EB[:, h, 127:128], C1[:, h * 128:(h + 1) * 128], ALU.mult, ALU.add,
                            [S32, EB, C1], [S32])
                    act(P, Sbf[:], S32[:], AF.Copy, [S32], [Sbf])
                    chk(4)
                    for h in range(4):
                        P.op('act', lambda e, h=h: e.activation(out=junk[:], in_=B1[:, h * 128:(h + 1) * 128], func=AF.Square,
                                                               accum_out=sm[:, h:h + 1]), [B1], [junk, sm])
                    act(P, sm[:, 4:8], sm[:, 0:4], AF.Ln, [sm], [sm], scale=1.0 / 128, bias=1e-6)
                    act(P, sm[:, 4:8], sm[:, 4:8], AF.Exp, [sm], [sm], scale=-0.5)
                    for h in range(4):
                        stt(P, o[:, h, :], B1[:, h * 128:(h + 1) * 128], sm[:, 4 + h:5 + h], normw, ALU.mult, ALU.mult, [B1, sm, rp], [o])
                    tt(P, ybf[:], o[:], b4(sg[:]), ALU.mult, [o, sg], [ybf])
                    c1b = C1[:, :].bitcast(BF16)
                    for h in range(4):
                        tr(P, c1b[:, h * 128:(h + 1) * 128], ybf[:, h, :], identb[:], [ybf, identb], [C1])
                    yt = yT[(sbi * NBLK + blk) % 2]
                    act(P, yt[:], b4(c1b[:, 0:512]), AF.Copy, [C1], [yt])
                    P.dma('sp', Kx.ycT.rearrange("k p t -> p k t")[:, 12:16, tg:tg + 128], yt[:], reads=[yt], writes=[Kx.d_ycT])
        except StopPass:
            pass
        P.flush()


OUT_NRP = 1024 + 1024 + 20
NSEL = 16


def layer_norm_block(P, r, stats, mv, sm2, g_ap, b_ap, reads_rp, out_ap, out_buf):
    for hf in range(2):
        P.op('dve', lambda e, hf=hf: e.bn_stats(out=stats[:, hf * 6:(hf + 1) * 6], in_=r[:, hf * 512:(hf + 1) * 512]), [r], [stats])
    P.op('dve', lambda e: e.bn_aggr(out=mv[:, 0:2], in_=stats[:, 0:12]), [stats], [mv])
    act(P, sm2[:, 0:1], mv[:, 1:2], AF.Ln, [mv], [sm2], bias=1e-5)
    act(P, sm2[:, 0:1], sm2[:, 0:1], AF.Exp, [sm2], [sm2], scale=-0.5)
    stt(P, sm2[:, 1:2], mv[:, 0:1], -1.0, sm2[:, 0:1], ALU.mult, ALU.mult, [mv, sm2], [sm2])
    act(P, r[:], r[:], AF.Identity, [r, sm2], [r], scale=sm2[:, 0:1], bias=sm2[:, 1:2])
    tt(P, r[:], r[:], g_ap, ALU.mult, [r] + reads_rp, [r])
    tt(P, out_ap, r[:], b_ap, ALU.add, [r] + reads_rp, [out_buf])


def pass_out(P, Kx, T, l, hsrc):
    cst = Kx.cst
    A0, A1, B0, B1, C0, C1, D0, D1 = Kx.bank
    NB = T // 128
    with ExitStack() as st:
        P.stack = st
        try:
            Wo = P.sb("out_W", [128, 16, 1024], BF16)
            rp = P.sb("out_rp", [128, OUT_NRP], F32)
            wr = P.sb("out_wr", [128, 8, 20], F32)
            ycb = [P.sb(f"out_yc{i}", [128, 16, 128], BF16) for i in range(2)]
            hin = [P.sb(f"out_h{i}", [128, 1024], F32) for i in range(2)]
            r = [P.sb(f"out_r{i}", [128, 1024], F32) for i in range(2)]
            x1 = [P.sb(f"out_x1{i}", [128, 1024], F32) for i in range(2)]
            x1Tf = P.sb("out_x1Tf", [128, 8, 128], F32)
            x1Tb = [P.sb(f"out_x1Tb{i}", [128, 8, 128], BF16) for i in range(2)]
            stats = P.sb("out_stats", [128, 12], F32)
            mv = P.sb("out_mv", [128, 2], F32)
            sm2 = P.sb("out_sm2", [128, 2], F32)
            q = P.sb("out_q", [128, 96], F32)
            comb = [P.sb(f"out_comb{i}", [128, 16], F32) for i in range(2)]
            combT = [P.sb(f"out_combT{i}", [16, 128], F32) for i in range(2)]

            for kc in range(16):
                P.dma('pool', Wo[:, kc, :], Kx.w_out[l, kc * 128:(kc + 1) * 128, :], writes=[Wo.sub(kc)])
            P.dma('sp', rp[:], Kx.out_rp[l], writes=[rp])
            P.dma('sp', wr[:], Kx.wr[l], writes=[wr])
            g1 = rp[:, 0:1024]
            b1 = rp[:, 1024:2048]
            rb = rp[:, 2048:2068]
            chk(1)
            for b in range(NB):
                tsl = slice(b * 128, (b + 1) * 128)
                yc, h_, r_, x_ = ycb[b % 2], hin[b % 2], r[b % 2], x1[b % 2]
                P.dma('sp', yc[:], Kx.ycT.rearrange("k p t -> p k t")[:, :, tsl], reads=[Kx.d_ycT], writes=[yc])
                P.dma('act', h_[:], hsrc[tsl, :], reads=[Kx.d_hres], writes=[h_])
                for hf in range(2):
                    bk = [A0, A1][hf]
                    for kc in range(16):
                        mm(P, bk[:, :], yc[:, kc, :], Wo[:, kc, hf * 512:(hf + 1) * 512], kc == 0, kc == 15, [yc, Wo], [bk])
                    stt(P, r_[:, hf * 512:(hf + 1) * 512], h_[:, hf * 512:(hf + 1) * 512], float(DN_ALPHA), bk[:, :], ALU.mult, ALU.add,
                        [h_, bk], [r_])
                layer_norm_block(P, r_, stats, mv, sm2, g1, b1, [rp], x_[:], x_)
                P.dma('sp', Kx.x1[tsl, :], x_[:], reads=[x_], writes=[Kx.d_x1])
                for j in range(8):
                    bk = [B0, B1][j // 4]
                    tr(P, bk[:, (j % 4) * 128:(j % 4 + 1) * 128], x_[:, j * 128:(j + 1) * 128], cst[:, C_I, :], [x_, cst], [bk])
                xb = x1Tb[b % 2]
                for hf in range(2):
                    bk = [B0, B1][hf]
                    act(P, x1Tf[:, hf * 4:(hf + 1) * 4, :], bk[:, :].rearrange("p (j t) -> p j t", j=4), AF.Copy, [bk], [x1Tf])
                    P.op('dve', lambda e, hf=hf, bk=bk, xb=xb: e.tensor_copy(out=xb[:, hf * 4:(hf + 1) * 4, :], in_=bk[:, :].rearrange("p (j t) -> p j t", j=4)),
                         [bk], [xb])
                P.dma('sp', Kx.x1T.rearrange("k p t -> p k t")[:, :, tsl], xb[:], reads=[xb], writes=[Kx.d_x1T])
                for k in range(8):
                    mm(P, C0[:, 0:20], x1Tf[:, k, :], wr[:, k, :], k == 0, k == 7, [x1Tf, wr], [C0])
                lgs = q[:, 0:20]
                gm, ngm, gsum, gp, m1, m2, dlt, ed, w1, w2 = [q[:, 20 + i:21 + i] for i in range(10)]
                ohg = q[:, 32:36]
                egj = q[:, 36:40]
                lsel = q[:, 40:44]
                oh1 = q[:, 44:48]
                msk = q[:, 48:52]
                oh2 = q[:, 52:56]
                wsel = q[:, 56:60]
                tmp16 = q[:, 64:80]
                tt(P, lgs, C0[:, 0:20], rb, ALU.add, [C0, rp], [q])
                P.op('dve', lambda e: e.tensor_reduce(out=gm, in_=lgs[:, 0:4], axis=AX.X, op=ALU.max), [q], [q])
                ts(P, ohg, lgs[:, 0:4], gm, None, ALU.is_equal, None, [q], [q])
                ts(P, ngm, gm, -1.0, None, ALU.mult, None, [q], [q])
                P.op('act', lambda e: e.activation(out=egj, in_=lgs[:, 0:4], func=AF.Exp, bias=ngm, accum_out=gsum), [q], [q])
                P.op('dve', lambda e: e.reciprocal(out=gp, in_=gsum), [q], [q])
                tt(P, tmp16.rearrange("p (g e) -> p g e", g=4), lgs[:, 4:20].rearrange("p (g e) -> p g e", g=4),
                   ohg.unsqueeze(2).to_broadcast([128, 4, 4]), ALU.mult, [q], [q])
                P.op('dve', lambda e: e.tensor_reduce(out=lsel, in_=tmp16.rearrange("p (g e) -> p e g", g=4), axis=AX.X, op=ALU.add), [q], [q])
                P.op('dve', lambda e: e.tensor_reduce(out=m1, in_=lsel, axis=AX.X, op=ALU.max), [q], [q])
                ts(P, oh1, lsel, m1, None, ALU.is_equal, None, [q], [q])
                stt(P, msk, oh1, -1e30, lsel, ALU.mult, ALU.add, [q], [q])
                P.op('dve', lambda e: e.tensor_reduce(out=m2, in_=msk, axis=AX.X, op=ALU.max), [q], [q])
                ts(P, oh2, msk, m2, None, ALU.is_equal, None, [q], [q])
                tt(P, dlt, m2, m1, ALU.subtract, [q], [q])
                act(P, ed, dlt, AF.Exp, [q], [q])
                ts(P, w1, ed, 1.0, None, ALU.add, None, [q], [q])
                P.op('dve', lambda e: e.reciprocal(out=w1, in_=w1), [q], [q])
                tt(P, w2, ed, w1, ALU.mult, [q], [q])
                tt(P, w1, w1, gp, ALU.mult, [q], [q])
                tt(P, w2, w2, gp, ALU.mult, [q], [q])
                ts(P, wsel, oh1, w1, None, ALU.mult, None, [q], [q])
                stt(P, wsel, oh2, w2, wsel, ALU.mult, ALU.add, [q], [q])
                cb_ = comb[b % 2]
                tt(P, cb_[:].rearrange("p (g e) -> p g e", g=4), ohg.unsqueeze(2).to_broadcast([128, 4, 4]),
                   wsel.unsqueeze(1).to_broadcast([128, 4, 4]), ALU.mult, [q], [cb_])
                P.dma('sp', Kx.comb[tsl, :], cb_[:], reads=[cb_], writes=[Kx.d_comb])
        except StopPass:
            pass
        P.flush()


def pass_moe(P, Kx, T, l, dst, write_hT):
    cst = Kx.cst
    A0, A1, B0, B1, C0, C1, D0, D1 = Kx.bank
    ST = min(1024, T)
    NST = T // ST
    NBS = ST // 128
    with ExitStack() as st:
        P.stack = st
        try:
            Wgu = [P.sb(f"moe_Wgu{i}", [128, 8, 512], BF16) for i in range(8)]
            Wdn = [P.sb(f"moe_Wdn{i}", [128, 2, 1024], BF16) for i in range(8)]
            for w_ in Wgu:
                for k in range(8):
                    w_.sub(k)
            for w_ in Wdn:
                for k in range(2):
                    w_.sub(k)
            rp = P.sb("moe_rp", [128, 2048], F32)
            xT = P.sb("moe_xT", [128, 8, ST], BF16)
            cmb = P.sb("moe_cmb", [128, NBS, 16], F32)
            yacc = P.sb("moe_yacc", [128, NBS, 1024], F32)
            yaccb = [yacc.sub(i) for i in range(NBS)]
            sgb = [P.sb(f"moe_sg{i}", [128, 256], F32) for i in range(2)]
            hb_ = [P.sb(f"moe_h{i}", [128, 256], BF16) for i in range(2)]
            hT = [P.sb(f"moe_hT{i}", [128, 2, 128], BF16) for i in range(2)]
            identb = P.sb("moe_identb", [128, 128], BF16)
            x1b = [P.sb(f"moe_x1{i}", [128, 1024], F32) for i in range(2)]
            ob = [P.sb(f"moe_o{i}", [128, 1024], F32) for i in range(2)]
            oT = [P.sb(f"moe_oT{i}", [128, 8, 128], BF16) for i in range(2)]
            stats = P.sb("moe_stats", [128, 12], F32)
            mv = P.sb("moe_mv", [128, 2], F32)
            sm2 = P.sb("moe_sm2", [128, 2], F32)
            P.dma('sp', rp[:], Kx.moe_rp[l], writes=[rp])
            g2 = rp[:, 0:1024]
            b2 = rp[:, 1024:2048]
            act(P, identb[:], cst[:, C_I, :], AF.Copy, [cst], [identb])
            it = 0
            for sti in range(NST):
                s0 = sti * ST
                P.dma('sp', xT[:], Kx.x1T.rearrange("k p t -> p k t")[:, :, s0:s0 + ST], reads=[Kx.d_x1T], writes=[xT])
                P.dma('sp', cmb[:], Kx.comb[s0:s0 + ST, :].rearrange("(b p) e -> p b e", p=128), reads=[Kx.d_comb], writes=[cmb])
                for G in range(4):
                    slot0 = ((sti * 4 + G) % 2) * 4
                    for e4 in range(4):
                        e = G * 4 + e4
                        wg, wd = Wgu[slot0 + e4], Wdn[slot0 + e4]
                        for k in range(8):
                            P.dma('pool', wg[:, k, :], Kx.w_gu[l, e, k * 128:(k + 1) * 128, :], writes=[wg.children[k]])
                        for fc in range(2):
                            P.dma('pool', wd[:, fc, :], Kx.w_dn[l, e, fc * 128:(fc + 1) * 128, :], writes=[wd.children[fc]])
                    for blk in range(NBS):
                        tsl = slice(blk * 128, (blk + 1) * 128)
                        ybk = [A0, A1] if blk % 2 == 0 else [B0, B1]
                        for e4 in range(4):
                            e = G * 4 + e4
                            wg, wd = Wgu[slot0 + e4], Wdn[slot0 + e4]
                            gb = [C0, C1][it % 2]
                            tb = [D0, D1][it % 2]
                            sg_, h_, hT_ = sgb[it % 2], hb_[it % 2], hT[it % 2]
                            it += 1
                            for k in range(8):
                                mm(P, gb[:, :], xT[:, k, tsl], wg[:, k, :], k == 0, k == 7, [xT, wg], [gb])
                            act(P, sg_[:], gb[:, 0:256], AF.Silu, [gb], [sg_])
                            stt(P, h_[:], gb[:, 256:512], cmb[:, blk, e:e + 1], sg_[:], ALU.mult, ALU.mult, [gb, cmb, sg_], [h_])
                            tbb = tb[:, :].bitcast(BF16)
                            for fc in range(2):
                                tr(P, tbb[:, fc * 128:(fc + 1) * 128], h_[:, fc * 128:(fc + 1) * 128], identb[:], [h_, identb], [tb])
                            act(P, hT_[:], tbb[:, 0:256].rearrange("p (f t) -> p f t", f=2), AF.Copy, [tb], [hT_])
                            for hf in range(2):
                                for fc in range(2):
                                    first = (e4 == 0 and fc == 0)
                                    last = (e4 == 3 and fc == 1)
                                    mm(P, ybk[hf][:, :], hT_[:, fc, :], wd[:, fc, hf * 512:(hf + 1) * 512], first, last, [hT_, wd], [ybk[hf]])
                        for hf in range(2):
                            ya = yacc[:, blk, hf * 512:(hf + 1) * 512]
                            if G == 0:
                                act(P, ya, ybk[hf][:, :], AF.Copy, [ybk[hf]], [yaccb[blk]])
                            else:
                                tt(P, ya, ybk[hf][:, :], ya, ALU.add, [ybk[hf], yaccb[blk]], [yaccb[blk]])
                for blk in range(NBS):
                    tg = s0 + blk * 128
                    x_, o_ = x1b[blk % 2], ob[blk % 2]
                    P.dma('act', x_[:], Kx.x1[tg:tg + 128, :], reads=[Kx.d_x1], writes=[x_])
                    stt(P, x_[:], x_[:], float(DN_ALPHA), yacc[:, blk, :], ALU.mult, ALU.add, [x_, yaccb[blk]], [x_])
                    layer_norm_block(P, x_, stats, mv, sm2, g2, b2, [rp], o_[:], o_)
                    P.dma('sp', dst[tg:tg + 128, :], o_[:], reads=[o_], writes=[Kx.d_hres])
                    if write_hT:
                        c0b = C0[:, :].bitcast(BF16)
                        ot = oT[blk % 2]
                        for j in range(8):
                            bk = [C0, C1][j // 4]
                            tr(P, bk[:, (j % 4) * 128:(j % 4 + 1) * 128], o_[:, j * 128:(j + 1) * 128], cst[:, C_I, :], [o_, cst], [bk])
                        for hf in range(2):
                            act(P, ot[:, hf * 4:(hf + 1) * 4, :], [C0, C1][hf][:, :].rearrange("p (j t) -> p j t", j=4), AF.Copy,
                                [[C0, C1][hf]], [ot])
                        P.dma('sp', Kx.hT.rearrange("k p t -> p k t")[:, :, tg:tg + 128], ot[:], reads=[ot], writes=[Kx.d_hT])
        except StopPass:
            pass
        P.flush()


def build(T, NL, dbg=False, passes=("ssd", "gdn", "hg", "out", "moe"), layers=None):
    layers = list(range(NL)) if layers is None else layers
    nc = bass.Bass("TRN2", target_bir_lowering=False)
    Kx = K()
    ext_in = lambda name, shape: nc.dram_tensor(name, list(shape), F32, kind="ExternalInput").ap()
    Kx.x = ext_in("x", [T, D_MODEL])
    Kx.w_in = ext_in("w_in", [NL, D_MODEL, IN_COLS])
    Kx.cst_d = ext_in("cst", [128, NCONST, 128])
    Kx.ssd_pp = ext_in("ssd_pp", [NL, 128, SSD_NPP])
    Kx.ssd_rp = ext_in("ssd_rp", [NL, 128, SSD_NRP])
    Kx.gdn_pp = ext_in("gdn_pp", [NL, 128, GDN_NPP])
    Kx.gdn_rp = ext_in("gdn_rp", [NL, 128, GDN_NRP])
    Kx.hg_rp = ext_in("hg_rp", [NL, 128, HG_NRP])
    Kx.hg_lbl = ext_in("hg_lbl", [128, 4, 4])
    Kx.hg_lmask = ext_in("hg_lmask", [NL, 128, 4, 4])
    Kx.w_out = ext_in("w_out", [NL, 2048, 1024])
    Kx.out_rp = ext_in("out_rp", [NL, 128, OUT_NRP])
    Kx.wr = ext_in("wr", [NL, 128, 8, 20])
    Kx.moe_rp = ext_in("moe_rp", [NL, 128, 2048])
    Kx.w_gu = ext_in("w_gu", [NL, 16, 1024, 512])
    Kx.w_dn = ext_in("w_dn", [NL, 16, 256, 1024])
    Kx.out = nc.dram_tensor("out", [T, D_MODEL], F32, kind="ExternalOutput").ap()
    skind = "ExternalOutput" if dbg else "Internal"
    Kx.hT = nc.dram_tensor("hT", [8, 128, T], BF16, kind=skind).ap()
    Kx.ycT = nc.dram_tensor("ycT", [16, 128, T], BF16, kind=skind).ap()
    Kx.x1 = nc.dram_tensor("x1", [T, D_MODEL], F32, kind=skind).ap()
    Kx.x1T = nc.dram_tensor("x1T", [8, 128, T], BF16, kind=skind).ap()
    Kx.comb = nc.dram_tensor("comb", [T, 16], F32, kind=skind).ap()
    Kx.hres = nc.dram_tensor("hres", [T, D_MODEL], F32, kind="Internal").ap()
    Kx.d_hT = Buf("d_hT")
    Kx.d_ycT = Buf("d_ycT")
    Kx.d_x1 = Buf("d_x1")
    Kx.d_x1T = Buf("d_x1T")
    Kx.d_comb = Buf("d_comb")
    Kx.d_hres = Buf("d_hres")
    with ExitStack() as st0:
        P = Prog(nc, st0)
        Kx.bank = []
        for i in range(8):
            t = st0.enter_context(nc.psum_tensor(f"bank{i}", [128, 512], F32))
            Kx.bank.append(Buf(f"bank{i}", t))
            Kx.bank[-1].excl = True
        Kx.cst = P.sb("cst_sb", [128, NCONST, 128], F32)
        P.dma('sp', Kx.cst[:], Kx.cst_d, writes=[Kx.cst])
        for l in range(NL):
            if l == 0:
                pass_transpose_in(P, Kx, T, Kx.x, Kx.hT)
            if "ssd" in passes:
                pass_ssd(P, Kx, T, l)
            if "gdn" in passes:
                pass_gdn(P, Kx, T, l)
            if "hg" in passes:
                pass_hg(P, Kx, T, l, layers[l])
            if "out" in passes:
                pass_out(P, Kx, T, l, Kx.x if l == 0 else Kx.hres)
            if "moe" in passes:
                pass_moe(P, Kx, T, l, Kx.out if l == NL - 1 else Kx.hres, l < NL - 1)
        print("recorded ops", P.nops, "waits", P.nwaits)
    return nc


def host_params(inp, layers):
    out = {}
    NL = len(layers)
    pp = np.zeros((NL, 128, SSD_NPP), np.float32)
    rp = np.zeros((NL, 128, SSD_NRP), np.float32)
    for i, l in enumerate(layers):
        cw = inp['ssd_conv_w'][l]
        pp[i, :, 0:48] = cw.reshape(4, 12, 128).transpose(2, 1, 0).reshape(128, 48)
        pp[i, :, 48:60] = inp['ssd_conv_b'][l].reshape(12, 128).T
        rp[i, :, 0:16] = inp['ssd_dt_bias'][l][None, :]
        rp[i, :, 16:32] = inp['ssd_a_log'][l][None, :]
        rp[i, :, 32:48] = inp['ssd_d'][l][None, :]
        rp[i, :, 48:48 + 1024] = inp['ssd_norm_w'][l][None, :]
    out['ssd_pp'] = pp
    out['ssd_rp'] = rp
    gpp = np.zeros((NL, 128, GDN_NPP), np.float32)
    grp = np.zeros((NL, 128, GDN_NRP), np.float32)
    for i, l in enumerate(layers):
        gpp[i, :, 0:48] = inp['gdn_conv_w'][l].reshape(4, 12, 128).transpose(2, 1, 0).reshape(128, 48)
        grp[i, :, 0:4] = inp['gdn_dt_bias'][l][None, :]
        grp[i, :, 4:8] = inp['gdn_a_log'][l][None, :]
        grp[i, :, 8:136] = inp['gdn_norm_w'][l][None, :]
    out['gdn_pp'] = gpp
    out['gdn_rp'] = grp
    hrp = np.zeros((NL, 128, HG_NRP), np.float32)
    for i, l in enumerate(layers):
        hrp[i, :, 0:128] = inp['hg_norm_w'][l][None, :]
    out['hg_rp'] = hrp
    lmask = np.zeros((NL, 128, 4, 4), np.float32)
    for i, l in enumerate(layers):
        lmask[i, :, :, 1:l + 1] = 1.0
    out['hg_lmask'] = lmask
    out['hg_lbl'] = np.ascontiguousarray(inp['hg_lb_logits'].reshape(4, 4, 128).transpose(2, 1, 0))
    orp = np.zeros((NL, 128, OUT_NRP), np.float32)
    wr = np.zeros((NL, 128, 8, 20), np.float32)
    mrp = np.zeros((NL, 128, 2048), np.float32)
    for i, l in enumerate(layers):
        orp[i, :, 0:1024] = inp['ln1_g'][l][None, :]
        orp[i, :, 1024:2048] = inp['ln1_b'][l][None, :]
        orp[i, :, 2048:2052] = inp['b_router_group'][l][None, :]
        orp[i, :, 2052:2068] = inp['b_router_expert'][l][None, :]
        wcat = np.concatenate([inp['w_router_group'][l], inp['w_router_expert'][l]], axis=1)
        wr[i] = wcat.reshape(8, 128, 20).transpose(1, 0, 2)
        mrp[i, :, 0:1024] = inp['ln2_g'][l][None, :]
        mrp[i, :, 1024:2048] = inp['ln2_b'][l][None, :]
    out['out_rp'] = orp
    out['wr'] = wr
    out['moe_rp'] = mrp
    out['cst'] = make_consts()
    return out


def core_inputs(inp, b, T, layers):
    hp = host_params(inp, layers)
    ls = layers
    im = {
        "x": np.ascontiguousarray(inp['x'][b, :T]),
        "w_in": np.ascontiguousarray(inp['w_in'][ls]),
        "w_out": np.ascontiguousarray(inp['w_out'][ls]),
        "w_gu": np.ascontiguousarray(inp['w_expert_gate_up'][ls]),
        "w_dn": np.ascontiguousarray(inp['w_expert_down'][ls]),
    }
    im.update(hp)
    return im


_PROG = {}


def kernel(**inputs):
    inp = {k: np.asarray(v) for k, v in inputs.items()}
    B, T, _ = inp['x'].shape
    ncores = 8
    nc = build(T, DEPTH)
    base = core_inputs(inp, 0, T, list(range(DEPTH)))
    in_maps = []
    for c in range(ncores):
        m = dict(base)
        m["x"] = np.ascontiguousarray(inp['x'][c % B], dtype=np.float32)
        in_maps.append(m)
    res = run_bass_kernel_spmd(nc, in_maps, core_ids=list(range(ncores)))
    return np.stack([np.asarray(res.results[b]["out"]) for b in range(B)]).astype(np.float32)
```

```python
import numpy as np
from contextlib import ExitStack
import concourse.bass as bass
import concourse.mybir as mybir
from concourse.bass_utils import run_bass_kernel_spmd

F32 = mybir.dt.float32
BF16 = mybir.dt.bfloat16
F32R = mybir.dt.float32r
USE_F32R = True
AF = mybir.ActivationFunctionType
ALU = mybir.AluOpType
AX = mybir.AxisListType

COMPUTE = ('pe', 'dve', 'act', 'pool')
ALLENG = ('pe', 'dve', 'act', 'pool', 'sp')
QUEUES = ('sp', 'act', 'pool')
NDMA = 8

D_MODEL = 1024
IN_COLS = 6680
DEPTH = 4
DN_ALPHA = (2 * DEPTH) ** 0.25


class Buf:
    def __init__(self, name, t=None, parent=None):
        self.name = name
        self.t = t
        self.parent = parent
        self.children = []
        self.last_write = None
        self.reads = []
        self.excl = False

    def sub(self, key):
        c = Buf(f"{self.name}.{key}", self.t, self)
        self.children.append(c)
        return c

    def __getitem__(self, k):
        return self.t[k]


class Prog:
    def __init__(self, nc, stack):
        self.nc = nc
        self.stack = stack
        self.ops = {e: [] for e in ALLENG}
        self.sem = {e: stack.enter_context(nc.semaphore(f"s_{e}")) for e in COMPUTE}
        self.cnt = {e: 0 for e in COMPUTE}
        self.known = {e: {} for e in ALLENG}
        self.dsem = {q: [stack.enter_context(nc.semaphore(f"d_{q}_{i}")) for i in range(NDMA)] for q in QUEUES}
        self.dcnt = {q: [0] * NDMA for q in QUEUES}
        self.dnext = {q: 0 for q in QUEUES}
        self.semobj = {}
        for e in COMPUTE:
            self.semobj[('c', e)] = self.sem[e]
        for q in QUEUES:
            for i in range(NDMA):
                self.semobj[('d', q, i)] = self.dsem[q][i]
        self.nops = 0
        self.nwaits = 0

    def sb(self, name, shape, dtype=F32):
        self.nalloc = getattr(self, 'nalloc', 0) + 1
        t = self.stack.enter_context(self.nc.sbuf_tensor(f"sb{self.nalloc}_{name}", list(shape), dtype))
        return Buf(name, t)

    def _collect(self, b, write):
        toks = []

        def add(x):
            if x.last_write is not None:
                toks.append(x.last_write)
            if write:
                toks.extend(x.reads)
        add(b)
        p = b.parent
        while p is not None:
            add(p)
            p = p.parent

        def rec(x):
            for c in x.children:
                add(c)
                rec(c)
        rec(b)
        return toks

    def _wait(self, eng, tok):
        key, val = tok
        if self.known[eng].get(key, 0) >= val:
            return
        self.known[eng][key] = val
        sem = self.semobj[key]
        self.ops[eng].append(lambda e, sem=sem, val=val: e.wait_ge(sem, val))
        self.nwaits += 1

    def _deps(self, eng, reads, writes, is_dma):
        own = ('c', eng) if (eng in COMPUTE and not is_dma) else None
        for b in reads:
            for tok in self._collect(b, False):
                self._wait(eng, tok)
            if b.excl:
                for tok in b.reads:
                    if own is not None and tok[0] == own:
                        continue
                    self._wait(eng, tok)
        for b in writes:
            for tok in self._collect(b, True):
                if own is not None and tok[0] == own:
                    continue
                self._wait(eng, tok)

    def _record(self, tok, reads, writes):
        for b in reads:
            b.reads.append(tok)
            if len(b.reads) > 64:
                b.reads = b.reads[-64:] if False else b.reads
        for b in writes:
            b.last_write = tok
            b.reads = []

            def rec(x):
                for c in x.children:
                    c.last_write = None
                    c.reads = []
                    rec(c)
            rec(b)

    def op(self, eng, fn, reads=(), writes=()):
        self._deps(eng, reads, writes, False)
        self.cnt[eng] += 1
        n = self.cnt[eng]
        sem = self.sem[eng]
        self.ops[eng].append(lambda e, fn=fn, sem=sem: fn(e).then_inc(sem, 1))
        tok = (('c', eng), n)
        self._record(tok, reads, writes)
        self.nops += 1
        return tok

    def dma(self, q, out, in_, reads=(), writes=(), **kw):
        self._deps(q, reads, writes, True)
        i = self.dnext[q]
        self.dnext[q] = (i + 1) % NDMA
        key = ('d', q, i)
        if self.dcnt[q][i] > 0:
            self._wait(q, (key, self.dcnt[q][i]))
        self.dcnt[q][i] += 16
        val = self.dcnt[q][i]
        sem = self.dsem[q][i]
        self.ops[q].append(lambda e, out=out, in_=in_, sem=sem, kw=kw:
                           e.dma_start(out=out, in_=in_, **kw).then_inc(sem, 16))
        tok = (key, val)
        self._record(tok, reads, writes)
        self.nops += 1
        return tok

    def barrier(self):
        for e in ALLENG:
            for c in COMPUTE:
                if self.cnt[c] > 0 and c != e:
                    self._wait(e, (('c', c), self.cnt[c]))
            for q in QUEUES:
                for i in range(NDMA):
                    if self.dcnt[q][i] > 0:
                        self._wait(e, (('d', q, i), self.dcnt[q][i]))

    def flush(self):
        self.barrier()
        nc = self.nc
        ops = self.ops
        self.ops = {e: [] for e in ALLENG}
        with nc.Block() as block:
            @block.sync
            def _(e):
                for f in ops['sp']:
                    f(e)

            @block.tensor
            def _(e):
                for f in ops['pe']:
                    f(e)

            @block.vector
            def _(e):
                for f in ops['dve']:
                    f(e)

            @block.scalar
            def _(e):
                for f in ops['act']:
                    f(e)

            @block.gpsimd
            def _(e):
                for f in ops['pool']:
                    f(e)


C_I, C_LE, C_GT, C_ONES, C_LE64, C_GT64, C_LT64, C_SAME64, C_SEL0, C_SEL1 = range(10)
NCONST = 10


def make_consts():
    k = np.arange(128)[:, None]
    l = np.arange(128)[None, :]
    c = np.zeros((128, NCONST, 128), np.float32)
    c[:, C_I] = (k == l)
    c[:, C_LE] = (k <= l)
    c[:, C_GT] = (k > l)
    c[:, C_ONES] = 1.0
    same64 = (k // 64) == (l // 64)
    c[:, C_LE64] = (k <= l) & same64
    c[:, C_GT64] = (k > l) & same64
    c[:, C_LT64] = (k < l) & same64
    c[:, C_SAME64] = same64
    c[:, C_SEL0] = (k < 64) & (l >= 0)
    c[:, C_SEL1] = (k >= 64) & (l >= 0)
    return c


class K:
    pass


STAGE = 99


class StopPass(Exception):
    pass


def chk(n):
    if STAGE == n:
        raise StopPass()


def mm(P, out, lhsT, rhs, start, stop, reads, writes):
    return P.op('pe', lambda e: e.matmul(out, lhsT=lhsT, rhs=rhs, start=start, stop=stop), reads, writes)


def mmr(P, out, lhsT, rhs, start, stop, reads, writes):
    if USE_F32R:
        lhsT = lhsT.bitcast(F32R)
        rhs = rhs.bitcast(F32R)
    return P.op('pe', lambda e: e.matmul(out, lhsT=lhsT, rhs=rhs, start=start, stop=stop), reads, writes)


def rr(ap):
    return ap.bitcast(F32R) if USE_F32R else ap


def tr(P, out, in_, ident, reads, writes):
    return P.op('pe', lambda e: e.transpose(out, in_, ident), reads, writes)


def act(P, out, in_, func, reads, writes, **kw):
    return P.op('act', lambda e: e.activation(out=out, in_=in_, func=func, **kw), reads, writes)


def tt(P, out, in0, in1, op, reads, writes):
    return P.op('dve', lambda e: e.tensor_tensor(out=out, in0=in0, in1=in1, op=op), reads, writes)


def ts(P, out, in0, s1, s2, op0, op1, reads, writes, **kw):
    if op1 is None:
        return P.op('dve', lambda e: e.tensor_scalar(out=out, in0=in0, scalar1=s1, scalar2=None, op0=op0, **kw), reads, writes)
    return P.op('dve', lambda e: e.tensor_scalar(out=out, in0=in0, scalar1=s1, scalar2=s2, op0=op0, op1=op1, **kw), reads, writes)


def stt(P, out, in0, scalar, in1, op0, op1, reads, writes):
    return P.op('dve', lambda e: e.scalar_tensor_tensor(out=out, in0=in0, scalar=scalar, in1=in1, op0=op0, op1=op1), reads, writes)


def load_weights(P, dst, dst_ap_fn, src_rows_fn, ncols, nk):
    toks = []
    if not dst.children:
        for k in range(nk):
            for c0 in range(0, ncols, 2048):
                dst.sub((k, c0))
    i = 0
    for k in range(nk):
        c0 = 0
        while c0 < ncols:
            c1 = min(ncols, c0 + 2048)
            toks.append(P.dma('pool', dst_ap_fn(k, c0, c1), src_rows_fn(k, c0, c1), writes=[dst.children[i]]))
            i += 1
            c0 = c1
    return toks


SSD_NPP = 12 * 4 + 12
SSD_NRP = 16 + 16 + 16 + 1024


def pass_transpose_in(P, Kx, T, src, dstT):
    nc = P.nc
    with ExitStack() as st:
        P.stack = st
        xt = [P.sb(f"p0_x{i}", [128, 1024], F32) for i in range(2)]
        xb = [P.sb(f"p0_b{i}", [128, 8, 128], BF16) for i in range(2)]
        for b in range(T // 128):
            x_ = xt[b % 2]
            o_ = xb[b % 2]
            P.dma('sp', x_[:], src[b * 128:(b + 1) * 128, :], writes=[x_])
            for j in range(8):
                bk = Kx.bank[j // 4]
                tr(P, bk[:, (j % 4) * 128:(j % 4 + 1) * 128], x_[:, j * 128:(j + 1) * 128], Kx.cst[:, C_I, :],
                   [x_, Kx.cst], [bk])
            for hlf in range(2):
                act(P, o_[:, hlf * 4:(hlf + 1) * 4, :], Kx.bank[hlf][:, :].rearrange("p (j t) -> p j t", j=4),
                    AF.Copy, [Kx.bank[hlf]], [o_])
            P.dma('sp', dstT.rearrange("k p t -> p k t")[:, :, b * 128:(b + 1) * 128], o_[:], reads=[o_], writes=[Kx.d_hT])
        P.flush()


def pass_ssd(P, Kx, T, l, dbg=None):
    nc = P.nc
    SBT = min(512, T)
    NSB = T // SBT
    NBLK = SBT // 128
    cst = Kx.cst
    bank = Kx.bank
    A0, A1, B0, B1, C0, C1, D0, D1 = bank
    with ExitStack() as st:
        P.stack = st
        try:
            W = P.sb("ssd_W", [128, 8, 2576], BF16)
            pp = P.sb("ssd_pp", [128, SSD_NPP], F32)
            rp = P.sb("ssd_rp", [128, SSD_NRP], F32)
            hTb = [P.sb(f"ssd_hT{i}", [128, 8, SBT], BF16) for i in range(2)]
            xin = P.sb("ssd_xin", [128, 12, SBT + 3], F32)
            accs = [P.sb(f"ssd_acc{i}", [128, SBT], F32) for i in range(12)]
            xc = P.sb("ssd_xc", [128, 12, SBT], F32)
            xcj = [xc.sub(j) for j in range(12)]
            xinj = [xin.sub(j) for j in range(12)]
            Dmat = P.sb("ssd_Dmat", [128, 16, 128], BF16)
            arep = P.sb("ssd_arep", [128, 16], F32)
            sm = P.sb("ssd_sm", [128, 16 * 12], F32)
            smv = lambda i: sm[:, i * 16:(i + 1) * 16]
            R = P.sb("ssd_R", [128, 16, 128], F32)
            DT = P.sb("ssd_DT", [128, 16, 128], BF16)
            GT = P.sb("ssd_GT", [128, 16, 128], BF16)
            CBTm = P.sb("ssd_CBTm", [128, 2, 128], BF16)
            xsb = P.sb("ssd_xsb", [128, 1024], BF16)
            xdt = P.sb("ssd_xdt", [128, 1024], BF16)
            xend = P.sb("ssd_xend", [128, 1024], BF16)
            Btm = P.sb("ssd_Btm", [128, 256], BF16)
            bcT = P.sb("ssd_bcT", [128, 4, 128], BF16)
            sz = P.sb("ssd_sz", [128, 1024], F32)
            yoff = P.sb("ssd_yoff", [128, 1024], F32)
            y = P.sb("ssd_y", [128, 1024], F32)
            junk = P.sb("ssd_junk", [128, 512], F32)
            ybf = P.sb("ssd_ybf", [128, 1024], BF16)
            yT = [P.sb(f"ssd_yT{i}", [128, 8, 128], BF16) for i in range(2)]
            S32 = P.sb("ssd_S32", [128, 1024], F32)
            Sbf = P.sb("ssd_Sbf", [128, 1024], BF16)
            identb = P.sb("ssd_identb", [128, 128], BF16)

            load_weights(P, W, lambda k, c0, c1: W[:, k, c0:c1],
                         lambda k, c0, c1: Kx.w_in[l, k * 128:(k + 1) * 128, c0:c1], 2576, 8)
            P.dma('sp', pp[:], Kx.ssd_pp[l], writes=[pp])
            P.dma('sp', rp[:], Kx.ssd_rp[l], writes=[rp])
            dtb = rp[:, 0:16]
            alog = rp[:, 16:32]
            drep = rp[:, 32:48]
            normw = rp[:, 48:48 + 1024]
            cw = lambda j, k: pp[:, j * 4 + k: j * 4 + k + 1]
            cb = lambda j: pp[:, 48 + j: 48 + j + 1]
            act(P, identb[:], cst[:, C_I, :], AF.Copy, [cst], [identb])
            act(P, arep[:], alog, AF.Exp, [rp], [arep])
            ts(P, arep[:], arep[:], -1.0, None, ALU.mult, None, [arep], [arep])
            tt(P, Dmat[:], cst[:, C_I, :].unsqueeze(1).to_broadcast([128, 16, 128]),
               drep.unsqueeze(2).to_broadcast([128, 16, 128]), ALU.mult, [cst, rp], [Dmat])
            P.op('dve', lambda e: e.memset(S32[:], 0.0), [], [S32])
            P.op('dve', lambda e: e.memset(Sbf[:], 0.0), [], [Sbf])
            P.op('dve', lambda e: e.memset(xin[:], 0.0), [], [xin])
            chk(1)

            for sbi in range(NSB):
                hb = hTb[sbi % 2]
                P.dma('sp', hb[:], Kx.hT.rearrange("k p t -> p k t")[:, :, sbi * SBT:(sbi + 1) * SBT], reads=[Kx.d_hT], writes=[hb])
                banks4 = [D0, D1, C0, C1]
                for j in range(12):
                    bk = banks4[j % 4]
                    for k in range(8):
                        mm(P, bk[:, 0:SBT], W[:, k, 1024 + j * 128: 1024 + (j + 1) * 128], hb[:, k, :], k == 0, k == 7,
                           [W, hb], [bk])
                    ac = accs[j]
                    act(P, xin[:, j, 3:3 + SBT], bk[:, 0:SBT], AF.Copy, [bk], [xinj[j]])
                    act(P, ac[:], bk[:, 0:SBT], AF.Identity, [bk, pp], [ac], scale=cw(j, 3), bias=cb(j))
                for j in range(12):
                    ac = accs[j]
                    for k in (2, 1, 0):
                        stt(P, ac[:], xin[:, j, k:k + SBT], cw(j, k), ac[:], ALU.mult, ALU.add, [xinj[j], pp, ac], [ac])
                for j in range(12):
                    ac = accs[j]
                    act(P, xc[:, j, :], ac[:], AF.Silu, [ac], [xcj[j]])
                    act(P, xin[:, j, 0:3], xin[:, j, SBT:SBT + 3], AF.Copy, [xinj[j]], [xinj[j]])
                chk(2)
                for blk in range(NBLK):
                    t0 = blk * 128
                    tg = sbi * SBT + t0
                    for hf in range(2):
                        bk = [A0, A1][hf]
                        for k in range(8):
                            mm(P, bk[:, :], hb[:, k, t0:t0 + 128], W[:, k, hf * 512:(hf + 1) * 512], k == 0, k == 7, [hb, W], [bk])
                        act(P, sz[:, hf * 512:(hf + 1) * 512], bk[:, :], AF.Silu, [bk], [sz])
                    for k in range(8):
                        mm(P, C0[:, 0:16], hb[:, k, t0:t0 + 128], W[:, k, 2560:2576], k == 0, k == 7, [hb, W], [C0])
                    xr, xm, ex, lg, dt, dA, acs, te, dte, ea, cd = [smv(i) for i in range(11)]
                    tt(P, xr, C0[:, 0:16], dtb, ALU.add, [C0, rp], [sm])
                    ts(P, xm, xr, 30.0, None, ALU.min, None, [sm], [sm])
                    act(P, ex, xm, AF.Exp, [sm], [sm])
                    act(P, lg, ex, AF.Ln, [sm], [sm], bias=1.0)
                    tt(P, dt, lg, xr, ALU.max, [sm], [sm])
                    tt(P, dA, dt, arep[:], ALU.mult, [sm, arep], [sm])
                    mm(P, C0[:, 16:32], cst[:, C_LE, :], dA, True, True, [cst, sm], [C0])
                    mm(P, C0[:, 32:48], cst[:, C_ONES, :], dA, True, True, [cst, sm], [C0])
                    act(P, ea, C0[:, 16:32], AF.Exp, [C0], [sm])
                    act(P, cd, C0[:, 32:48], AF.Exp, [C0], [sm])
                    act(P, acs, C0[:, 16:32], AF.Copy, [C0], [sm])
                    tt(P, te, C0[:, 32:48], acs, ALU.subtract, [C0, sm], [sm])
                    act(P, te, te, AF.Exp, [sm], [sm])
                    tt(P, dte, dt, te, ALU.mult, [sm], [sm])
                    chk(3)
                    tt(P, R[:], cst[:, C_LE, :].unsqueeze(1).to_broadcast([128, 16, 128]),
                       dA.unsqueeze(2).to_broadcast([128, 16, 128]), ALU.mult, [cst, sm], [R])
                    for q in range(4):
                        bk = [D0, D1][q % 2]
                        mm(P, bk[:, :], cst[:, C_GT, :], R[:, 4 * q:4 * q + 4, :], True, True, [cst, R], [bk])
                        act(P, DT[:, 4 * q:4 * q + 4, :], bk[:, :].rearrange("p (h l) -> p h l", h=4), AF.Exp, [bk], [DT])
                    chk(4)
                    for j in range(8):
                        bk = [B0, B1][j // 4]
                        tr(P, bk[:, (j % 4) * 128:(j % 4 + 1) * 128], xc[:, j, t0:t0 + 128], cst[:, C_I, :], [xcj[j], cst], [bk])
                    for hf in range(2):
                        bk = [B0, B1][hf]
                        pv = bk[:, :].rearrange("p (h c) -> p h c", h=8)
                        act(P, xsb[:, hf * 512:(hf + 1) * 512], bk[:, :], AF.Copy, [bk], [xsb])
                        chk(4.1)
                        tt(P, xdt[:, hf * 512:(hf + 1) * 512].rearrange("p (h c) -> p h c", h=8), pv,
                           dt[:, hf * 8:(hf + 1) * 8].unsqueeze(2).to_broadcast([128, 8, 64]), ALU.mult, [bk, sm], [xdt])
                        chk(4.11)
                        tt(P, xend[:, hf * 512:(hf + 1) * 512].rearrange("p (h c) -> p h c", h=8), pv,
                           dte[:, hf * 8:(hf + 1) * 8].unsqueeze(2).to_broadcast([128, 8, 64]), ALU.mult, [bk, sm], [xend])
                        chk(4.12)
                    chk(4.2)
                    for g in range(2):
                        tr(P, C0[:, 128 + g * 128:128 + (g + 1) * 128], xc[:, 8 + g, t0:t0 + 128], cst[:, C_I, :], [xcj[8 + g], cst], [C0])
                    act(P, Btm[:], C0[:, 128:384], AF.Copy, [C0], [Btm])
                    chk(4.3)
                    act(P, bcT[:], xc[:, 8:12, t0:t0 + 128], AF.Copy, [xcj[8], xcj[9], xcj[10], xcj[11]], [bcT])
                    chk(5)
                    for g in range(2):
                        mm(P, C1[:, g * 128:(g + 1) * 128], bcT[:, g, :], bcT[:, 2 + g, :], True, True, [bcT], [C1])
                    tt(P, CBTm[:], C1[:, 0:256].rearrange("p (g l) -> p g l", g=2),
                       cst[:, C_LE, :].unsqueeze(1).to_broadcast([128, 2, 128]), ALU.mult, [C1, cst], [CBTm])
                    for g in range(2):
                        tt(P, GT[:, g * 8:(g + 1) * 8, :], DT[:, g * 8:(g + 1) * 8, :],
                           CBTm[:, g, :].unsqueeze(1).to_broadcast([128, 8, 128]), ALU.mult, [DT, CBTm], [GT])
                    chk(6)
                    for h in range(16):
                        bk = [A0, A1][h // 8]
                        o = bk[:, (h % 8) * 64:(h % 8 + 1) * 64]
                        mm(P, o, GT[:, h, :], xdt[:, h * 64:(h + 1) * 64], True, False, [GT, xdt], [bk])
                        mm(P, o, Dmat[:, h, :], xsb[:, h * 64:(h + 1) * 64], False, True, [Dmat, xsb], [bk])
                    for g in range(2):
                        bk = [B0, B1][g]
                        mm(P, bk[:, :], bcT[:, 2 + g, :], Sbf[:, g * 512:(g + 1) * 512], True, True, [bcT, Sbf], [bk])
                        tt(P, yoff[:, g * 512:(g + 1) * 512].rearrange("p (h c) -> p h c", h=8),
                           bk[:, :].rearrange("p (h c) -> p h c", h=8),
                           ea[:, g * 8:(g + 1) * 8].unsqueeze(2).to_broadcast([128, 8, 64]), ALU.mult, [bk, sm], [yoff])
                        tt(P, y[:, g * 512:(g + 1) * 512], [A0, A1][g][:, :], yoff[:, g * 512:(g + 1) * 512], ALU.add,
                           [[A0, A1][g], yoff], [y])
                    tt(P, y[:], y[:], sz[:], ALU.mult, [y, sz], [y])
                    chk(7)
                    ss = smv(11)
                    for g in range(2):
                        P.op('act', lambda e, g=g: e.activation(out=junk[:], in_=y[:, g * 512:(g + 1) * 512], func=AF.Square,
                                                               accum_out=sm[:, 176 + g:177 + g]), [y], [junk, sm])
                    act(P, sm[:, 178:180], sm[:, 176:178], AF.Ln, [sm], [sm], scale=1.0 / 512, bias=1e-6)
                    act(P, sm[:, 178:180], sm[:, 178:180], AF.Exp, [sm], [sm], scale=-0.5)
                    for g in range(2):
                        stt(P, ybf[:, g * 512:(g + 1) * 512], y[:, g * 512:(g + 1) * 512], sm[:, 178 + g:179 + g],
                            normw[:, g * 512:(g + 1) * 512], ALU.mult, ALU.mult, [y, sm, rp], [ybf])
                    c1b = C1[:, :].bitcast(BF16)
                    for j in range(8):
                        tr(P, c1b[:, j * 128:(j + 1) * 128], ybf[:, j * 128:(j + 1) * 128], identb[:], [ybf, identb], [C1])
                    yt = yT[(sbi * NBLK + blk) % 2]
                    act(P, yt[:], c1b.rearrange("p (j t) -> p j t", j=8), AF.Copy, [C1], [yt])
                    P.dma('sp', Kx.ycT.rearrange("k p t -> p k t")[:, 0:8, tg:tg + 128], yt[:], reads=[yt], writes=[Kx.d_ycT])
                    chk(8)
                    for g in range(2):
                        bk = [D0, D1][g]
                        mm(P, bk[:, :], Btm[:, g * 128:(g + 1) * 128], xend[:, g * 512:(g + 1) * 512], True, True, [Btm, xend], [bk])
                    tt(P, S32[:].rearrange("p (h c) -> p h c", h=16), S32[:].rearrange("p (h c) -> p h c", h=16),
                       cd.unsqueeze(2).to_broadcast([128, 16, 64]), ALU.mult, [S32, sm], [S32])
                    for g in range(2):
                        tt(P, S32[:, g * 512:(g + 1) * 512], [D0, D1][g][:, :], S32[:, g * 512:(g + 1) * 512], ALU.add,
                           [[D0, D1][g], S32], [S32])
                    act(P, Sbf[:], S32[:], AF.Copy, [S32], [Sbf])
        except StopPass:
            pass
        P.flush()


GDN_NPP = 48
GDN_NRP = 4 + 4 + 128
GDN_BASE = 2576


def mmx(P, out, lhsT, rhs, start, stop, reads, writes):
    return P.op('pe', lambda e: e.matmul(out, lhsT=lhsT, rhs=rhs, start=start, stop=stop, skip_group_check=True), reads, writes)


def pass_gdn(P, Kx, T, l):
    SBT = min(512, T)
    NSB = T // SBT
    NBLK = SBT // 128
    cst = Kx.cst
    A0, A1, B0, B1, C0, C1, D0, D1 = Kx.bank
    b4 = lambda ap: ap.rearrange("p (h c) -> p h c", h=4)
    with ExitStack() as st:
        P.stack = st
        try:
            W = P.sb("gdn_W", [128, 8, 2056], BF16)
            pp = P.sb("gdn_pp", [128, GDN_NPP], F32)
            rp = P.sb("gdn_rp", [128, GDN_NRP], F32)
            hTb = [P.sb(f"gdn_hT{i}", [128, 8, SBT], BF16) for i in range(2)]
            xin = P.sb("gdn_xin", [128, 12, SBT + 3], F32)
            xinj = [xin.sub(j) for j in range(12)]
            accs = [P.sb(f"gdn_acc{i}", [128, SBT], F32) for i in range(12)]
            sgS = P.sb("gdn_sgS", [128, NBLK, 512], F32)
            xc = P.sb("gdn_xc", [128, 12, SBT], F32)
            xcj = [xc.sub(j) for j in range(12)]
            qT = P.sb("gdn_qT", [128, 4, SBT], BF16)
            kT = P.sb("gdn_kT", [128, 4, SBT], BF16)
            identb = P.sb("gdn_identb", [128, 128], BF16)
            arep = P.sb("gdn_arep", [128, 4], F32)
            smP = [P.sb(f"gdn_sm{i}", [128, 4 * 20], F32) for i in range(2)]
            R = P.sb("gdn_R", [128, 4, 128], F32)
            Dec = P.sb("gdn_Dec", [128, 4, 128], F32)
            DecU = P.sb("gdn_DecU", [128, 4, 128], F32)
            t1 = P.sb("gdn_t1", [128, 4, 128], F32)
            qkTmP = [P.sb(f"gdn_qkTm{i}", [128, 4, 128], BF16) for i in range(2)]
            Ya = [P.sb(f"gdn_Y{i}", [128, 4, 128], F32) for i in range(2)]
            Za = [P.sb(f"gdn_Z{i}", [128, 4, 128], F32) for i in range(2)]
            V = P.sb("gdn_V", [128, 4, 128], F32)
            VbfP = [P.sb(f"gdn_Vbf{i}", [128, 4, 128], BF16) for i in range(2)]
            vtmP = [P.sb(f"gdn_vtm{i}", [128, 4, 128], F32) for i in range(2)]
            kdP = [P.sb(f"gdn_kd{i}", [128, 4, 128], BF16) for i in range(2)]
            rhs2 = P.sb("gdn_rhs2", [128, 4, 128], BF16)
            vnew = P.sb("gdn_vnew", [128, 4, 128], BF16)
            As = P.sb("gdn_As", [128, 4, 128], F32)
            o = P.sb("gdn_o", [128, 4, 128], F32)
            junk = P.sb("gdn_junk", [128, 128], F32)
            ybf = P.sb("gdn_ybf", [128, 4, 128], BF16)
            yT = [P.sb(f"gdn_yT{i}", [128, 4, 128], BF16) for i in range(2)]
            S32 = P.sb("gdn_S32", [128, 4, 128], F32)
            Sbf = P.sb("gdn_Sbf", [128, 4, 128], BF16)

            load_weights(P, W, lambda k, c0, c1: W[:, k, c0:c1],
                         lambda k, c0, c1: Kx.w_in[l, k * 128:(k + 1) * 128, GDN_BASE + c0:GDN_BASE + c1], 2056, 8)
            P.dma('sp', pp[:], Kx.gdn_pp[l], writes=[pp])
            P.dma('sp', rp[:], Kx.gdn_rp[l], writes=[rp])
            dtb = rp[:, 0:4]
            alog = rp[:, 4:8]
            normw = rp[:, 8:136]
            cw = lambda j, k: pp[:, j * 4 + k: j * 4 + k + 1]
            act(P, identb[:], cst[:, C_I, :], AF.Copy, [cst], [identb])
            act(P, arep[:], alog, AF.Exp, [rp], [arep])
            ts(P, arep[:], arep[:], -1.0, None, ALU.mult, None, [arep], [arep])
            P.op('dve', lambda e: e.memset(S32[:], 0.0), [], [S32])
            P.op('dve', lambda e: e.memset(Sbf[:], 0.0), [], [Sbf])
            P.op('dve', lambda e: e.memset(xin[:], 0.0), [], [xin])
            chk(1)
            for sbi in range(NSB):
                hb = hTb[sbi % 2]
                P.dma('sp', hb[:], Kx.hT.rearrange("k p t -> p k t")[:, :, sbi * SBT:(sbi + 1) * SBT], reads=[Kx.d_hT], writes=[hb])
                banks4 = [D0, D1, C0, C1]
                for j in range(12):
                    bk = banks4[j % 4]
                    for k in range(8):
                        mm(P, bk[:, 0:SBT], W[:, k, j * 128:(j + 1) * 128], hb[:, k, :], k == 0, k == 7, [W, hb], [bk])
                    ac = accs[j]
                    act(P, xin[:, j, 3:3 + SBT], bk[:, 0:SBT], AF.Copy, [bk], [xinj[j]])
                    act(P, ac[:], bk[:, 0:SBT], AF.Copy, [bk, pp], [ac], scale=cw(j, 3))
                for j in range(12):
                    ac = accs[j]
                    for k in (2, 1, 0):
                        stt(P, ac[:], xin[:, j, k:k + SBT], cw(j, k), ac[:], ALU.mult, ALU.add, [xinj[j], pp, ac], [ac])
                for blk in range(NBLK):
                    bk = banks4[blk % 4]
                    for k in range(8):
                        mm(P, bk[:, :], hb[:, k, blk * 128:(blk + 1) * 128], W[:, k, 1536:2048], k == 0, k == 7, [hb, W], [bk])
                    act(P, sgS[:, blk, :], bk[:, :], AF.Silu, [bk], [sgS])
                for j in range(12):
                    ac = accs[j]
                    act(P, xc[:, j, :], ac[:], AF.Silu, [ac], [xcj[j]])
                    act(P, xin[:, j, 0:3], xin[:, j, SBT:SBT + 3], AF.Copy, [xinj[j]], [xinj[j]])
                for j in range(8):
                    act(P, accs[j][:], xc[:, j, :], AF.Square, [xcj[j]], [accs[j]])
                for j in range(8):
                    bk2 = banks4[j % 4]
                    r_ = accs[j]
                    mm(P, bk2[:, 0:SBT], cst[:, C_ONES, :], r_[:], True, True, [cst, r_], [bk2])
                    act(P, r_[:], bk2[:, 0:SBT], AF.Ln, [bk2], [r_], bias=1e-6)
                for j in range(8):
                    r_ = accs[j]
                    act(P, r_[:], r_[:], AF.Exp, [r_], [r_], scale=-0.5,
                        bias=(-0.5 * float(np.log(128.0))) if j < 4 else 0.0)
                    dst = qT if j < 4 else kT
                    tt(P, dst[:, j % 4, :], xc[:, j, :], r_[:], ALU.mult, [xcj[j], r_], [dst])
                chk(2)
                def h1(blk, par):
                    t0 = blk * 128
                    tsl = slice(t0, t0 + 128)
                    sm = smP[par]
                    smv = lambda i: sm[:, i * 4:(i + 1) * 4]
                    Vbf, qkTm, kd, v_tm = VbfP[par], qkTmP[par], kdP[par], vtmP[par]
                    for k in range(8):
                        mm(P, C0[:, 0:8], hb[:, k, tsl], W[:, k, 2048:2056], k == 0, k == 7, [hb, W], [C0])
                    eb, beta, xr, xm, ex, lg, sp, la, eg, gs, ekd, negeg = [smv(i) for i in range(12)]
                    glrep = sm[:, 48:56]
                    act(P, eb, C0[:, 0:4], AF.Exp, [C0], [sm], scale=-1.0)
                    ts(P, eb, eb, 1.0, None, ALU.add, None, [sm], [sm])
                    P.op('dve', lambda e: e.reciprocal(out=beta, in_=eb), [sm], [sm])
                    tt(P, xr, C0[:, 4:8], dtb, ALU.add, [C0, rp], [sm])
                    ts(P, xm, xr, 30.0, None, ALU.min, None, [sm], [sm])
                    yield
                    act(P, ex, xm, AF.Exp, [sm], [sm])
                    act(P, lg, ex, AF.Ln, [sm], [sm], bias=1.0)
                    tt(P, sp, lg, xr, ALU.max, [sm], [sm])
                    tt(P, la, sp, arep[:], ALU.mult, [sm, arep], [sm])
                    yield
                    mm(P, C0[:, 8:12], cst[:, C_LE64, :], la, True, True, [cst, sm], [C0])
                    mm(P, C0[:, 12:16], cst[:, C_SAME64, :], la, True, True, [cst, sm], [C0])
                    mm(P, C0[:, 16:20], cst[:, C_SEL0, :], la, True, True, [cst, sm], [C0])
                    mm(P, C0[:, 20:24], cst[:, C_SEL1, :], la, True, True, [cst, sm], [C0])
                    act(P, eg, C0[:, 8:12], AF.Exp, [C0], [sm])
                    act(P, gs, C0[:, 8:12], AF.Copy, [C0], [sm])
                    act(P, glrep, C0[:, 16:24], AF.Exp, [C0], [sm])
                    tt(P, ekd, C0[:, 12:16], gs, ALU.subtract, [C0, sm], [sm])
                    yield
                    act(P, ekd, ekd, AF.Exp, [sm], [sm])
                    ts(P, negeg, eg, -1.0, None, ALU.mult, None, [sm], [sm])
                    tt(P, R[:], cst[:, C_LE64, :].unsqueeze(1).to_broadcast([128, 4, 128]),
                       la.unsqueeze(2).to_broadcast([128, 4, 128]), ALU.mult, [cst, sm], [R])
                    yield
                    mm(P, D1[:, :], cst[:, C_GT64, :], R[:], True, True, [cst, R], [D1])
                    act(P, Dec[:], b4(D1[:, :]), AF.Exp, [D1], [Dec])
                    tt(P, DecU[:], Dec[:], cst[:, C_LE64, :].unsqueeze(1).to_broadcast([128, 4, 128]), ALU.mult, [Dec, cst], [DecU])
                    yield
                    for h in range(4):
                        mm(P, D0[:, h * 128:(h + 1) * 128], kT[:, h, tsl], kT[:, h, tsl], True, True, [kT], [D0])
                    for h in range(4):
                        mm(P, D1[:, h * 128:(h + 1) * 128], kT[:, h, tsl], qT[:, h, tsl], True, True, [kT, qT], [D1])
                    yield
                    tt(P, qkTm[:], b4(D1[:, :]), DecU[:], ALU.mult, [D1, DecU], [qkTm])
                    tt(P, t1[:], b4(D0[:, :]), DecU[:], ALU.mult, [D0, DecU], [t1])
                    yield
                    X = Ya[0]
                    for h in range(4):
                        stt(P, rr(X[:, h, :]), t1[:, h, :], beta[:, h:h + 1], cst[:, C_LT64, :], ALU.mult, ALU.mult, [t1, sm, cst], [X])
                    yield
                    for h in range(4):
                        tr(P, D0[:, h * 128:(h + 1) * 128], X[:, h, :], cst[:, C_I, :], [X, cst], [D0])
                    act(P, rr(Za[0][:]), b4(D0[:, :]), AF.Copy, [D0], [Za[0]])
                    tt(P, rr(V[:]), cst[:, C_I, :].unsqueeze(1).to_broadcast([128, 4, 128]), X[:], ALU.subtract, [cst, X], [V])
                    yield
                    for lev in range(5):
                        Yc, Zc = Ya[lev % 2], Za[lev % 2]
                        Yn, Zn = Ya[(lev + 1) % 2], Za[(lev + 1) % 2]
                        for h in range(4):
                            mmr(P, D1[:, h * 128:(h + 1) * 128], Yc[:, h, :], Zc[:, h, :], True, True, [Yc, Zc], [D1])
                        act(P, rr(Zn[:]), b4(D1[:, :]), AF.Copy, [D1], [Zn])
                        yield
                        if lev < 4:
                            for h in range(4):
                                mmr(P, D0[:, h * 128:(h + 1) * 128], Zc[:, h, :], Yc[:, h, :], True, True, [Yc, Zc], [D0])
                            act(P, rr(Yn[:]), b4(D0[:, :]), AF.Copy, [D0], [Yn])
                            yield
                        for h in range(4):
                            mmr(P, D1[:, h * 128:(h + 1) * 128], Zn[:, h, :], V[:, h, :], True, True, [Zn, V], [D1])
                        tt(P, rr(V[:]), b4(D1[:, :]), V[:], ALU.add, [D1, V], [V])
                        yield
                    act(P, Vbf[:], V[:], AF.Copy, [V], [Vbf])
                    for h in range(4):
                        tr(P, D0[:, h * 128:(h + 1) * 128], xc[:, 8 + h, tsl], cst[:, C_I, :], [xcj[8 + h], cst], [D0])
                    act(P, v_tm[:], b4(D0[:, :]), AF.Copy, [D0], [v_tm])
                    yield
                    d1b = D1[:, :].bitcast(BF16)
                    for h in range(4):
                        tr(P, d1b[:, h * 128:(h + 1) * 128], kT[:, h, tsl], identb[:], [kT, identb], [D1])
                    tt(P, kd[:], b4(d1b[:, 0:512]), ekd.unsqueeze(2).to_broadcast([128, 4, 128]), ALU.mult, [D1, sm], [kd])
                    yield

                def h2(blk, par):
                    t0 = blk * 128
                    tg = sbi * SBT + t0
                    tsl = slice(t0, t0 + 128)
                    sm = smP[par]
                    smv = lambda i: sm[:, i * 4:(i + 1) * 4]
                    Vbf, qkTm, kd, v_tm = VbfP[par], qkTmP[par], kdP[par], vtmP[par]
                    eb, beta, xr, xm, ex, lg, sp, la, eg, gs, ekd, negeg = [smv(i) for i in range(12)]
                    glrep = sm[:, 48:56]
                    for c in range(2):
                        sl = slice(64 * c, 64 * c + 64)
                        for h in range(4):
                            mm(P, A0[:, h * 128:(h + 1) * 128], kT[:, h, tsl], Sbf[:, h, :], True, True, [kT, Sbf], [A0])
                        for h in range(4):
                            mm(P, B0[:, h * 128:(h + 1) * 128], qT[:, h, tsl], Sbf[:, h, :], True, True, [qT, Sbf], [B0])
                        for h in range(4):
                            stt(P, rhs2[sl, h, :], A0[sl, h * 128:(h + 1) * 128], negeg[sl, h:h + 1], v_tm[sl, h, :],
                                ALU.mult, ALU.add, [A0, sm, v_tm], [rhs2])
                        yield
                        for h in range(4):
                            mm(P, A1[:, h * 128:(h + 1) * 128], Vbf[sl, h, :], rhs2[sl, h, :], True, True, [Vbf, rhs2], [A1])
                        tt(P, vnew[sl], b4(A1[sl, :]), beta[sl].unsqueeze(2).to_broadcast([64, 4, 128]), ALU.mult, [A1, sm], [vnew])
                        yield
                        for h in range(4):
                            mm(P, C1[:, h * 128:(h + 1) * 128], kd[sl, h, :], vnew[sl, h, :], True, True, [kd, vnew], [C1])
                        for h in range(4):
                            mmx(P, B1[:, h * 128:(h + 1) * 128], qkTm[sl, h, :], vnew[sl, h, :], (c == 0 and h == 0), (c == 1),
                                [qkTm, vnew], [B1])
                        for h in range(4):
                            stt(P, S32[:, h, :], S32[:, h, :], glrep[:, c * 4 + h:c * 4 + h + 1], C1[:, h * 128:(h + 1) * 128],
                                ALU.mult, ALU.add, [S32, sm, C1], [S32])
                        act(P, Sbf[:], S32[:], AF.Copy, [S32], [Sbf])
                        yield
                        tt(P, As[sl], b4(B0[sl, :]), eg[sl].unsqueeze(2).to_broadcast([64, 4, 128]), ALU.mult, [B0, sm], [As])
                        yield
                    tt(P, o[:], b4(B1[:, :]), As[:], ALU.add, [B1, As], [o])
                    for h in range(4):
                        P.op('act', lambda e, h=h, sm=sm: e.activation(out=junk[:], in_=o[:, h, :], func=AF.Square,
                                                                      accum_out=sm[:, 56 + h:57 + h]), [o], [junk, sm])
                    yield
                    act(P, sm[:, 60:64], sm[:, 56:60], AF.Ln, [sm], [sm], scale=1.0 / 128, bias=1e-6)
                    act(P, sm[:, 60:64], sm[:, 60:64], AF.Exp, [sm], [sm], scale=-0.5)
                    for h in range(4):
                        stt(P, o[:, h, :], o[:, h, :], sm[:, 60 + h:61 + h], normw, ALU.mult, ALU.mult, [o, sm, rp], [o])
                    yield
                    tt(P, ybf[:], o[:], b4(sgS[:, blk, :]), ALU.mult, [o, sgS], [ybf])
                    c1b = C1[:, :].bitcast(BF16)
                    for h in range(4):
                        tr(P, c1b[:, h * 128:(h + 1) * 128], ybf[:, h, :], identb[:], [ybf, identb], [C1])
                    yt = yT[(sbi * NBLK + blk) % 2]
                    act(P, yt[:], b4(c1b[:, 0:512]), AF.Copy, [C1], [yt])
                    P.dma('sp', Kx.ycT.rearrange("k p t -> p k t")[:, 8:12, tg:tg + 128], yt[:], reads=[yt], writes=[Kx.d_ycT])
                    yield

                for _ in h1(0, 0):
                    pass
                for blk in range(NBLK):
                    g2 = h2(blk, blk % 2)
                    g1 = h1(blk + 1, (blk + 1) % 2) if blk + 1 < NBLK else iter(())
                    d1 = d2 = False
                    while not (d1 and d2):
                        if not d2:
                            try:
                                next(g2)
                            except StopIteration:
                                d2 = True
                        for _r in range(2):
                            if not d1:
                                try:
                                    next(g1)
                                except StopIteration:
                                    d1 = True
        except StopPass:
            pass
        P.flush()


HG_BASE = 4632
HG_NRP = 128


def pass_hg(P, Kx, T, l, labs):
    SBT = min(512, T)
    NSB = T // SBT
    NBLK = SBT // 128
    cst = Kx.cst
    A0, A1, B0, B1, C0, C1, D0, D1 = Kx.bank
    b4 = lambda ap: ap.rearrange("p (h c) -> p h c", h=4)
    with ExitStack() as st:
        P.stack = st
        try:
            W = P.sb("hg_W", [128, 8, 2048], BF16)
            rp = P.sb("hg_rp", [128, HG_NRP], F32)
            lbl = P.sb("hg_lbl", [128, 4, 4], F32)
            lbw = P.sb("hg_lbw", [128, 4 * 6], F32)
            lmk = P.sb("hg_lmk", [128, 4, 4], F32)
            hTb = [P.sb(f"hg_hT{i}", [128, 8, SBT], BF16) for i in range(2)]
            qTf = P.sb("hg_qTf", [128, 4, SBT], F32)
            kTf = P.sb("hg_kTf", [128, 4, SBT], F32)
            lgf = P.sb("hg_lgf", [128, 4, SBT], F32)
            ftmp = [P.sb(f"hg_ft{i}", [128, SBT], F32) for i in range(4)]
            sgS = P.sb("hg_sgS", [128, NBLK, 512], F32)
            ones = P.sb("hg_ones", [128, 128], F32)
            identb = P.sb("hg_identb", [128, 128], BF16)
            Bt = P.sb("hg_Bt", [128, 4, 132], F32)
            D1t = [P.sb(f"hg_D1{i}", [128, 8, 128], F32) for i in range(2)]
            Et = [P.sb(f"hg_E{i}", [128, 8, 128], F32) for i in range(2)]
            kfac = [P.sb(f"hg_kfac{i}", [128, 8, 128], BF16) for i in range(2)]
            Eq = P.sb("hg_Eq", [128, 4, 128], F32)
            EB = P.sb("hg_EB", [128, 4, 128], F32)
            Ek = P.sb("hg_Ek", [128, 4, 128], F32)
            qg = P.sb("hg_qg", [128, 4, 128], BF16)
            qG = P.sb("hg_qG", [128, 4, 128], BF16)
            kdT = P.sb("hg_kdT", [128, 4, 128], BF16)
            kdtm = P.sb("hg_kdtm", [128, 4, 128], BF16)
            scT = P.sb("hg_scT", [128, 4, 128], BF16)
            vbf = P.sb("hg_vbf", [128, 4, 128], BF16)
            sm = P.sb("hg_sm", [128, 16], F32)
            junk = P.sb("hg_junk", [128, 128], F32)
            o = P.sb("hg_o", [128, 4, 128], F32)
            ybf = P.sb("hg_ybf", [128, 4, 128], BF16)
            yT = [P.sb(f"hg_yT{i}", [128, 4, 128], BF16) for i in range(2)]
            S32 = P.sb("hg_S32", [128, 4, 128], F32)
            Sbf = P.sb("hg_Sbf", [128, 4, 128], BF16)

            load_weights(P, W, lambda k, c0, c1: W[:, k, c0:c1],
                         lambda k, c0, c1: Kx.w_in[l, k * 128:(k + 1) * 128, HG_BASE + c0:HG_BASE + c1], 2048, 8)
            P.dma('sp', rp[:], Kx.hg_rp[l], writes=[rp])
            P.dma('sp', lbl[:], Kx.hg_lbl, writes=[lbl])
            normw = rp[:, 0:128]
            act(P, identb[:], cst[:, C_I, :], AF.Copy, [cst], [identb])
            P.op('dve', lambda e: e.memset(ones[:], 1.0), [], [ones])
            P.op('dve', lambda e: e.memset(S32[:], 0.0), [], [S32])
            P.op('dve', lambda e: e.memset(Sbf[:], 0.0), [], [Sbf])
            P.op('dve', lambda e: e.memset(Bt[:], 0.0), [], [Bt])
            mx, sme, rs, lb, oml = [lbw[:, i * 4:(i + 1) * 4] for i in range(5)]
            P.op('dve', lambda e: e.tensor_reduce(out=mx, in_=lbl[:], axis=AX.X, op=ALU.max), [lbl], [lbw])
            tt(P, lbl[:], lbl[:], mx.unsqueeze(2).to_broadcast([128, 4, 4]), ALU.subtract, [lbl, lbw], [lbl])
            act(P, lbl[:], lbl[:], AF.Exp, [lbl], [lbl])
            P.op('dve', lambda e: e.tensor_reduce(out=sme, in_=lbl[:], axis=AX.X, op=ALU.add), [lbl], [lbw])
            P.op('dve', lambda e: e.reciprocal(out=rs, in_=sme), [lbw], [lbw])
            P.dma('sp', lmk[:], Kx.hg_lmask[l], writes=[lmk])
            tt(P, lbl[:], lbl[:], lmk[:], ALU.mult, [lbl, lmk], [lbl])
            P.op('dve', lambda e: e.tensor_reduce(out=lb, in_=lbl[:], axis=AX.X, op=ALU.add), [lbl], [lbw])
            tt(P, lb, lb, rs, ALU.mult, [lbw], [lbw])
            ts(P, oml, lb, -1.0, 1.0, ALU.mult, ALU.add, [lbw], [lbw])
            chk(1)
            for sbi in range(NSB):
                hb = hTb[sbi % 2]
                P.dma('sp', hb[:], Kx.hT.rearrange("k p t -> p k t")[:, :, sbi * SBT:(sbi + 1) * SBT], reads=[Kx.d_hT], writes=[hb])
                banks4 = [D0, D1, C0, C1]
                for j in range(4):
                    bk = banks4[j % 4]
                    for k in range(8):
                        mm(P, bk[:, 0:SBT], W[:, k, j * 128:(j + 1) * 128], hb[:, k, :], k == 0, k == 7, [W, hb], [bk])
                    act(P, qTf[:, j, :], bk[:, 0:SBT], AF.Silu, [bk], [qTf])
                for blk in range(NBLK):
                    bk = banks4[blk % 4]
                    for k in range(8):
                        mm(P, bk[:, :], hb[:, k, blk * 128:(blk + 1) * 128], W[:, k, 1536:2048], k == 0, k == 7, [hb, W], [bk])
                    act(P, sgS[:, blk, :], bk[:, :], AF.Silu, [bk], [sgS])
                for h in range(4):
                    bk = banks4[h % 4]
                    for k in range(8):
                        mm(P, bk[:, 0:SBT], W[:, k, (4 + h) * 128:(5 + h) * 128], hb[:, k, :], k == 0, k == 7, [W, hb], [bk])
                    act(P, ftmp[h][:], bk[:, 0:SBT], AF.Sigmoid, [bk], [ftmp[h]])
                for h in range(4):
                    f_ = ftmp[h]
                    ts(P, f_[:], f_[:], oml[:, h:h + 1], lb[:, h:h + 1], ALU.mult, ALU.add, [f_, lbw], [f_])
                    ts(P, kTf[:, h, :], f_[:], -1.0, 1.0, ALU.mult, ALU.add, [f_], [kTf])
                for h in range(4):
                    act(P, lgf[:, h, :], ftmp[h][:], AF.Ln, [ftmp[h]], [lgf])
                chk(2)
                for blk in range(NBLK):
                    t0 = blk * 128
                    tg = sbi * SBT + t0
                    tsl = slice(t0, t0 + 128)
                    for k in range(8):
                        mm(P, A0[:, :], hb[:, k, tsl], W[:, k, 1024:1536], k == 0, k == 7, [hb, W], [A0])
                    act(P, vbf[:], b4(A0[:, :]), AF.Copy, [A0], [vbf])
                    for h in range(4):
                        i2 = h % 2
                        D1_, E_, kf_ = D1t[i2], Et[i2], kfac[i2]
                        P.op('dve', lambda e, h=h, tsl=tsl: e.tensor_tensor_scan(out=Bt[:, h, 1:129], data0=ones[:, :], data1=lgf[:, h, tsl],
                                                                       initial=0.0, op0=ALU.mult, op1=ALU.add), [ones, lgf], [Bt])
                        for c in range(8):
                            ts(P, D1_[:, c, :], Bt[:, h, 1:129], Bt[:, h, 16 * c:16 * c + 1], -60.0, ALU.subtract, ALU.max, [Bt], [D1_])
                        act(P, E_[:], D1_[:], AF.Exp, [D1_], [E_], scale=-1.0)
                        tt(P, kf_[:], E_[:], kTf[:, h, tsl].unsqueeze(1).to_broadcast([128, 8, 128]), ALU.mult, [E_, kTf], [kf_])
                        base = D1_[:, :, :]
                        dg = bass.AP(base.tensor, base.offset, [list(base.ap[0]), [144, 8], [1, 16]])
                        act(P, Eq[:, h, :].rearrange("p (c j) -> p c j", c=8), dg, AF.Exp, [D1_], [Eq])
                        tt(P, qg[:, h, :], qTf[:, h, tsl], Eq[:, h, :], ALU.mult, [qTf, Eq], [qg])
                        act(P, EB[:, h, :], Bt[:, h, 1:129], AF.Exp, [Bt], [EB])
                        tt(P, qG[:, h, :], qTf[:, h, tsl], EB[:, h, :], ALU.mult, [qTf, EB], [qG])
                        act(P, Ek[:, h, :], Bt[:, h, 1:129], AF.Exp, [Bt], [Ek], scale=-1.0, bias=Bt[:, h, 128:129])
                        tt(P, kdT[:, h, :], kTf[:, h, tsl], Ek[:, h, :], ALU.mult, [kTf, Ek], [kdT])
                        for c in range(8):
                            mm(P, B0[:, h * 128 + 16 * c:h * 128 + 16 * c + 16], kf_[:, c, :], qg[:, h, 16 * c:16 * c + 16], True, True,
                               [kf_, qg], [B0])
                    tt(P, scT[:], b4(B0[:, :]), cst[:, C_LE, :].unsqueeze(1).to_broadcast([128, 4, 128]), ALU.mult, [B0, cst], [scT])
                    chk(3)
                    for h in range(4):
                        mm(P, B1[:, h * 128:(h + 1) * 128], scT[:, h, :], vbf[:, h, :], True, False, [scT, vbf], [B1])
                        mm(P, B1[:, h * 128:(h + 1) * 128], qG[:, h, :], Sbf[:, h, :], False, True, [qG, Sbf], [B1])
                    c0b = C0[:, :].bitcast(BF16)
                    for h in range(4):
                        tr(P, c0b[:, h * 128:(h + 1) * 128], kdT[:, h, :], identb[:], [kdT, identb], [C0])
                    act(P, kdtm[:], b4(c0b[:, 0:512]), AF.Copy, [C0], [kdtm])
                    for h in range(4):
                        mm(P, C1[:, h * 128:(h + 1) * 128], kdtm[:, h, :], vbf[:, h, :], True, True, [kdtm, vbf], [C1])
                    for h in range(4):
                        stt(P, S32[:, h, :], S32[:, h, :], EB[:, h, 127:128], C1[:, h * 128:(h + 1) * 128], ALU.mult, ALU.add,
                            [S32, EB, C1], [S32])
                    act(P, Sbf[:], S32[:], AF.Copy, [S32], [Sbf])
                    chk(4)
                    for h in range(4):
                        P.op('act', lambda e, h=h: e.activation(out=junk[:], in_=B1[:, h * 128:(h + 1) * 128], func=AF.Square,
                                                               accum_out=sm[:, h:h + 1]), [B1], [junk, sm])
                    act(P, sm[:, 4:8], sm[:, 0:4], AF.Ln, [sm], [sm], scale=1.0 / 128, bias=1e-6)
                    act(P, sm[:, 4:8], sm[:, 4:8], AF.Exp, [sm], [sm], scale=-0.5)
                    for h in range(4):
                        stt(P, o[:, h, :], B1[:, h * 128:(h + 1) * 128], sm[:, 4 + h:5 + h], normw, ALU.mult, ALU.mult, [B1, sm, rp], [o])
                    tt(P, ybf[:], o[:], b4(sgS[:, blk, :]), ALU.mult, [o, sgS], [ybf])
                    c1b = C1[:, :].bitcast(BF16)
                    for h in range(4):
                        tr(P, c1b[:, h * 128:(h + 1) * 128], ybf[:, h, :], identb[:], [ybf, identb], [C1])
                    yt = yT[(sbi * NBLK + blk) % 2]
                    act(P, yt[:], b4(c1b[:, 0:512]), AF.Copy, [C1], [yt])
                    P.dma('sp', Kx.ycT.rearrange("k p t -> p k t")[:, 12:16, tg:tg + 128], yt[:], reads=[yt], writes=[Kx.d_ycT])
        except StopPass:
            pass
        P.flush()


OUT_NRP = 1024 + 1024 + 20
NSEL = 16


def layer_norm_block(P, r, stats, mv, sm2, g_ap, b_ap, reads_rp, out_ap, out_buf):
    for hf in range(2):
        P.op('dve', lambda e, hf=hf: e.bn_stats(out=stats[:, hf * 6:(hf + 1) * 6], in_=r[:, hf * 512:(hf + 1) * 512]), [r], [stats])
    P.op('dve', lambda e: e.bn_aggr(out=mv[:, 0:2], in_=stats[:, 0:12]), [stats], [mv])
    act(P, sm2[:, 0:1], mv[:, 1:2], AF.Ln, [mv], [sm2], bias=1e-5)
    act(P, sm2[:, 0:1], sm2[:, 0:1], AF.Exp, [sm2], [sm2], scale=-0.5)
    stt(P, sm2[:, 1:2], mv[:, 0:1], -1.0, sm2[:, 0:1], ALU.mult, ALU.mult, [mv, sm2], [sm2])
    act(P, r[:], r[:], AF.Identity, [r, sm2], [r], scale=sm2[:, 0:1], bias=sm2[:, 1:2])
    tt(P, r[:], r[:], g_ap, ALU.mult, [r] + reads_rp, [r])
    tt(P, out_ap, r[:], b_ap, ALU.add, [r] + reads_rp, [out_buf])


def pass_out(P, Kx, T, l, hsrc):
    cst = Kx.cst
    A0, A1, B0, B1, C0, C1, D0, D1 = Kx.bank
    NB = T // 128
    with ExitStack() as st:
        P.stack = st
        try:
            Wo = P.sb("out_W", [128, 16, 1024], BF16)
            rp = P.sb("out_rp", [128, OUT_NRP], F32)
            wr = P.sb("out_wr", [128, 8, 20], F32)
            ycb = [P.sb(f"out_yc{i}", [128, 16, 128], BF16) for i in range(2)]
            hin = [P.sb(f"out_h{i}", [128, 1024], F32) for i in range(2)]
            r = [P.sb(f"out_r{i}", [128, 1024], F32) for i in range(2)]
            x1 = [P.sb(f"out_x1{i}", [128, 1024], F32) for i in range(2)]
            x1Tf = P.sb("out_x1Tf", [128, 8, 128], F32)
            x1Tb = [P.sb(f"out_x1Tb{i}", [128, 8, 128], BF16) for i in range(2)]
            stats = P.sb("out_stats", [128, 12], F32)
            mv = P.sb("out_mv", [128, 2], F32)
            sm2 = P.sb("out_sm2", [128, 2], F32)
            q = P.sb("out_q", [128, 96], F32)
            comb = [P.sb(f"out_comb{i}", [128, 16], F32) for i in range(2)]
            combT = [P.sb(f"out_combT{i}", [16, 128], F32) for i in range(2)]

            for kc in range(16):
                P.dma('pool', Wo[:, kc, :], Kx.w_out[l, kc * 128:(kc + 1) * 128, :], writes=[Wo.sub(kc)])
            P.dma('sp', rp[:], Kx.out_rp[l], writes=[rp])
            P.dma('sp', wr[:], Kx.wr[l], writes=[wr])
            g1 = rp[:, 0:1024]
            b1 = rp[:, 1024:2048]
            rb = rp[:, 2048:2068]
            chk(1)
            def stage_a(b):
                    tsl = slice(b * 128, (b + 1) * 128)
                    yc, h_, r_, x_ = ycb[b % 2], hin[b % 2], r[b % 2], x1[b % 2]
                    P.dma('sp', yc[:], Kx.ycT.rearrange("k p t -> p k t")[:, :, tsl], reads=[Kx.d_ycT], writes=[yc])
                    P.dma('act', h_[:], hsrc[tsl, :], reads=[Kx.d_hres], writes=[h_])
                    for hf in range(2):
                        bk = [A0, A1][hf]
                        for kc in range(16):
                            mm(P, bk[:, :], yc[:, kc, :], Wo[:, kc, hf * 512:(hf + 1) * 512], kc == 0, kc == 15, [yc, Wo], [bk])
                        stt(P, r_[:, hf * 512:(hf + 1) * 512], h_[:, hf * 512:(hf + 1) * 512], float(DN_ALPHA), bk[:, :], ALU.mult, ALU.add,
                            [h_, bk], [r_])
                    layer_norm_block(P, r_, stats, mv, sm2, g1, b1, [rp], x_[:], x_)
                    P.dma('sp', Kx.x1[tsl, :], x_[:], reads=[x_], writes=[Kx.d_x1])

            def stage_b(b):
                    tsl = slice(b * 128, (b + 1) * 128)
                    x_ = x1[b % 2]
                    for j in range(8):
                        bk = [B0, B1][j // 4]
                        tr(P, bk[:, (j % 4) * 128:(j % 4 + 1) * 128], x_[:, j * 128:(j + 1) * 128], cst[:, C_I, :], [x_, cst], [bk])
                    xb = x1Tb[b % 2]
                    for hf in range(2):
                        bk = [B0, B1][hf]
                        act(P, x1Tf[:, hf * 4:(hf + 1) * 4, :], bk[:, :].rearrange("p (j t) -> p j t", j=4), AF.Copy, [bk], [x1Tf])
                        P.op('dve', lambda e, hf=hf, bk=bk, xb=xb: e.tensor_copy(out=xb[:, hf * 4:(hf + 1) * 4, :], in_=bk[:, :].rearrange("p (j t) -> p j t", j=4)),
                             [bk], [xb])
                    P.dma('sp', Kx.x1T.rearrange("k p t -> p k t")[:, :, tsl], xb[:], reads=[xb], writes=[Kx.d_x1T])
                    for k in range(8):
                        mm(P, C0[:, 0:20], x1Tf[:, k, :], wr[:, k, :], k == 0, k == 7, [x1Tf, wr], [C0])
                    lgs = q[:, 0:20]
                    gm, ngm, gsum, gp, m1, m2, dlt, ed, w1, w2 = [q[:, 20 + i:21 + i] for i in range(10)]
                    ohg = q[:, 32:36]
                    egj = q[:, 36:40]
                    lsel = q[:, 40:44]
                    oh1 = q[:, 44:48]
                    msk = q[:, 48:52]
                    oh2 = q[:, 52:56]
                    wsel = q[:, 56:60]
                    tmp16 = q[:, 64:80]
                    tt(P, lgs, C0[:, 0:20], rb, ALU.add, [C0, rp], [q])
                    P.op('dve', lambda e: e.tensor_reduce(out=gm, in_=lgs[:, 0:4], axis=AX.X, op=ALU.max), [q], [q])
                    ts(P, ohg, lgs[:, 0:4], gm, None, ALU.is_equal, None, [q], [q])
                    ts(P, ngm, gm, -1.0, None, ALU.mult, None, [q], [q])
                    P.op('act', lambda e: e.activation(out=egj, in_=lgs[:, 0:4], func=AF.Exp, bias=ngm, accum_out=gsum), [q], [q])
                    P.op('dve', lambda e: e.reciprocal(out=gp, in_=gsum), [q], [q])
                    tt(P, tmp16.rearrange("p (g e) -> p g e", g=4), lgs[:, 4:20].rearrange("p (g e) -> p g e", g=4),
                       ohg.unsqueeze(2).to_broadcast([128, 4, 4]), ALU.mult, [q], [q])
                    P.op('dve', lambda e: e.tensor_reduce(out=lsel, in_=tmp16.rearrange("p (g e) -> p e g", g=4), axis=AX.X, op=ALU.add), [q], [q])
                    P.op('dve', lambda e: e.tensor_reduce(out=m1, in_=lsel, axis=AX.X, op=ALU.max), [q], [q])
                    ts(P, oh1, lsel, m1, None, ALU.is_equal, None, [q], [q])
                    stt(P, msk, oh1, -1e30, lsel, ALU.mult, ALU.add, [q], [q])
                    P.op('dve', lambda e: e.tensor_reduce(out=m2, in_=msk, axis=AX.X, op=ALU.max), [q], [q])
                    ts(P, oh2, msk, m2, None, ALU.is_equal, None, [q], [q])
                    tt(P, dlt, m2, m1, ALU.subtract, [q], [q])
                    act(P, ed, dlt, AF.Exp, [q], [q])
                    ts(P, w1, ed, 1.0, None, ALU.add, None, [q], [q])
                    P.op('dve', lambda e: e.reciprocal(out=w1, in_=w1), [q], [q])
                    tt(P, w2, ed, w1, ALU.mult, [q], [q])
                    tt(P, w1, w1, gp, ALU.mult, [q], [q])
                    tt(P, w2, w2, gp, ALU.mult, [q], [q])
                    ts(P, wsel, oh1, w1, None, ALU.mult, None, [q], [q])
                    stt(P, wsel, oh2, w2, wsel, ALU.mult, ALU.add, [q], [q])
                    cb_ = comb[b % 2]
                    tt(P, cb_[:].rearrange("p (g e) -> p g e", g=4), ohg.unsqueeze(2).to_broadcast([128, 4, 4]),
                       wsel.unsqueeze(1).to_broadcast([128, 4, 4]), ALU.mult, [q], [cb_])
                    P.dma('sp', Kx.comb[tsl, :], cb_[:], reads=[cb_], writes=[Kx.d_comb])

            stage_a(0)
            for b in range(NB):
                if b + 1 < NB:
                    stage_a(b + 1)
                stage_b(b)
        except StopPass:
            pass
        P.flush()


def pass_moe(P, Kx, T, l, dst, write_hT):
    cst = Kx.cst
    A0, A1, B0, B1, C0, C1, D0, D1 = Kx.bank
    ST = min(1024, T)
    NST = T // ST
    NBS = ST // 128
    with ExitStack() as st:
        P.stack = st
        try:
            Wgu = [P.sb(f"moe_Wgu{i}", [128, 8, 512], BF16) for i in range(8)]
            Wdn = [P.sb(f"moe_Wdn{i}", [128, 2, 1024], BF16) for i in range(8)]
            for w_ in Wgu:
                for k in range(8):
                    w_.sub(k)
            for w_ in Wdn:
                for k in range(2):
                    w_.sub(k)
            rp = P.sb("moe_rp", [128, 2048], F32)
            xT = P.sb("moe_xT", [128, 8, ST], BF16)
            cmb = P.sb("moe_cmb", [128, NBS, 16], F32)
            yacc = P.sb("moe_yacc", [128, NBS, 1024], F32)
            yaccb = [yacc.sub(i) for i in range(NBS)]
            sgb = [P.sb(f"moe_sg{i}", [128, 256], F32) for i in range(3)]
            hb_ = [P.sb(f"moe_h{i}", [128, 256], BF16) for i in range(3)]
            hT = [P.sb(f"moe_hT{i}", [128, 2, 128], BF16) for i in range(3)]
            identb = P.sb("moe_identb", [128, 128], BF16)
            x1b = [P.sb(f"moe_x1{i}", [128, 1024], F32) for i in range(2)]
            ob = [P.sb(f"moe_o{i}", [128, 1024], F32) for i in range(2)]
            oT = [P.sb(f"moe_oT{i}", [128, 8, 128], BF16) for i in range(2)]
            stats = P.sb("moe_stats", [128, 12], F32)
            mv = P.sb("moe_mv", [128, 2], F32)
            sm2 = P.sb("moe_sm2", [128, 2], F32)
            P.dma('sp', rp[:], Kx.moe_rp[l], writes=[rp])
            g2 = rp[:, 0:1024]
            b2 = rp[:, 1024:2048]
            act(P, identb[:], cst[:, C_I, :], AF.Copy, [cst], [identb])
            it = 0
            for sti in range(NST):
                s0 = sti * ST
                P.dma('sp', xT[:], Kx.x1T.rearrange("k p t -> p k t")[:, :, s0:s0 + ST], reads=[Kx.d_x1T], writes=[xT])
                P.dma('sp', cmb[:], Kx.comb[s0:s0 + ST, :].rearrange("(b p) e -> p b e", p=128), reads=[Kx.d_comb], writes=[cmb])
                for G in range(4):
                    slot0 = ((sti * 4 + G) % 2) * 4
                    for e4 in range(4):
                        e = G * 4 + e4
                        wg, wd = Wgu[slot0 + e4], Wdn[slot0 + e4]
                        for k in range(8):
                            P.dma('pool', wg[:, k, :], Kx.w_gu[l, e, k * 128:(k + 1) * 128, :], writes=[wg.children[k]])
                        for fc in range(2):
                            P.dma('pool', wd[:, fc, :], Kx.w_dn[l, e, fc * 128:(fc + 1) * 128, :], writes=[wd.children[fc]])
                    items = [(blk, e4) for blk in range(NBS) for e4 in range(4)]
                    NB3 = 3

                    def stage_a(i):
                        blk, e4 = items[i]
                        e = G * 4 + e4
                        tsl = slice(blk * 128, (blk + 1) * 128)
                        wg = Wgu[slot0 + e4]
                        gb = [C0, C1][i % 2]
                        sg_, h_ = sgb[i % NB3], hb_[i % NB3]
                        for k in range(8):
                            mm(P, gb[:, :], xT[:, k, tsl], wg[:, k, :], k == 0, k == 7, [xT, wg], [gb])
                        act(P, sg_[:], gb[:, 0:256], AF.Silu, [gb], [sg_])
                        stt(P, h_[:], gb[:, 256:512], cmb[:, blk, e:e + 1], sg_[:], ALU.mult, ALU.mult, [gb, cmb, sg_], [h_])

                    def stage_b(i):
                        tb = [D0, D1][i % 2]
                        h_, hT_ = hb_[i % NB3], hT[i % NB3]
                        tbb = tb[:, :].bitcast(BF16)
                        for fc in range(2):
                            tr(P, tbb[:, fc * 128:(fc + 1) * 128], h_[:, fc * 128:(fc + 1) * 128], identb[:], [h_, identb], [tb])
                        act(P, hT_[:], tbb[:, 0:256].rearrange("p (f t) -> p f t", f=2), AF.Copy, [tb], [hT_])

                    def stage_c(i):
                        blk, e4 = items[i]
                        wd = Wdn[slot0 + e4]
                        hT_ = hT[i % NB3]
                        ybk = [A0, A1] if blk % 2 == 0 else [B0, B1]
                        for hf in range(2):
                            for fc in range(2):
                                first = (e4 == 0 and fc == 0)
                                last = (e4 == 3 and fc == 1)
                                mm(P, ybk[hf][:, :], hT_[:, fc, :], wd[:, fc, hf * 512:(hf + 1) * 512], first, last, [hT_, wd], [ybk[hf]])
                        if e4 == 3:
                            for hf in range(2):
                                ya = yacc[:, blk, hf * 512:(hf + 1) * 512]
                                if G == 0:
                                    act(P, ya, ybk[hf][:, :], AF.Copy, [ybk[hf]], [yaccb[blk]])
                                else:
                                    tt(P, ya, ybk[hf][:, :], ya, ALU.add, [ybk[hf], yaccb[blk]], [yaccb[blk]])

                    n_it = len(items)
                    for step in range(n_it + 2):
                        if step < n_it:
                            stage_a(step)
                        if 0 <= step - 1 < n_it:
                            stage_b(step - 1)
                        if 0 <= step - 2 < n_it:
                            stage_c(step - 2)
                for blk in range(NBS):
                    tg = s0 + blk * 128
                    x_, o_ = x1b[blk % 2], ob[blk % 2]
                    P.dma('act', x_[:], Kx.x1[tg:tg + 128, :], reads=[Kx.d_x1], writes=[x_])
                    stt(P, x_[:], x_[:], float(DN_ALPHA), yacc[:, blk, :], ALU.mult, ALU.add, [x_, yaccb[blk]], [x_])
                    layer_norm_block(P, x_, stats, mv, sm2, g2, b2, [rp], o_[:], o_)
                    P.dma('sp', dst[tg:tg + 128, :], o_[:], reads=[o_], writes=[Kx.d_hres])
                    if write_hT:
                        c0b = C0[:, :].bitcast(BF16)
                        ot = oT[blk % 2]
                        for j in range(8):
                            bk = [C0, C1][j // 4]
                            tr(P, bk[:, (j % 4) * 128:(j % 4 + 1) * 128], o_[:, j * 128:(j + 1) * 128], cst[:, C_I, :], [o_, cst], [bk])
                        for hf in range(2):
                            act(P, ot[:, hf * 4:(hf + 1) * 4, :], [C0, C1][hf][:, :].rearrange("p (j t) -> p j t", j=4), AF.Copy,
                                [[C0, C1][hf]], [ot])
                        P.dma('sp', Kx.hT.rearrange("k p t -> p k t")[:, :, tg:tg + 128], ot[:], reads=[ot], writes=[Kx.d_hT])
        except StopPass:
            pass
        P.flush()


def build(T, NL, dbg=False, passes=("ssd", "gdn", "hg", "out", "moe"), layers=None):
    layers = list(range(NL)) if layers is None else layers
    nc = bass.Bass("TRN2", target_bir_lowering=False)
    Kx = K()
    ext_in = lambda name, shape: nc.dram_tensor(name, list(shape), F32, kind="ExternalInput").ap()
    Kx.x = ext_in("x", [T, D_MODEL])
    Kx.w_in = ext_in("w_in", [NL, D_MODEL, IN_COLS])
    Kx.cst_d = ext_in("cst", [128, NCONST, 128])
    Kx.ssd_pp = ext_in("ssd_pp", [NL, 128, SSD_NPP])
    Kx.ssd_rp = ext_in("ssd_rp", [NL, 128, SSD_NRP])
    Kx.gdn_pp = ext_in("gdn_pp", [NL, 128, GDN_NPP])
    Kx.gdn_rp = ext_in("gdn_rp", [NL, 128, GDN_NRP])
    Kx.hg_rp = ext_in("hg_rp", [NL, 128, HG_NRP])
    Kx.hg_lbl = ext_in("hg_lbl", [128, 4, 4])
    Kx.hg_lmask = ext_in("hg_lmask", [NL, 128, 4, 4])
    Kx.w_out = ext_in("w_out", [NL, 2048, 1024])
    Kx.out_rp = ext_in("out_rp", [NL, 128, OUT_NRP])
    Kx.wr = ext_in("wr", [NL, 128, 8, 20])
    Kx.moe_rp = ext_in("moe_rp", [NL, 128, 2048])
    Kx.w_gu = ext_in("w_gu", [NL, 16, 1024, 512])
    Kx.w_dn = ext_in("w_dn", [NL, 16, 256, 1024])
    Kx.out = nc.dram_tensor("out", [T, D_MODEL], F32, kind="ExternalOutput").ap()
    skind = "ExternalOutput" if dbg else "Internal"
    Kx.hT = nc.dram_tensor("hT", [8, 128, T], BF16, kind=skind).ap()
    Kx.ycT = nc.dram_tensor("ycT", [16, 128, T], BF16, kind=skind).ap()
    Kx.x1 = nc.dram_tensor("x1", [T, D_MODEL], F32, kind=skind).ap()
    Kx.x1T = nc.dram_tensor("x1T", [8, 128, T], BF16, kind=skind).ap()
    Kx.comb = nc.dram_tensor("comb", [T, 16], F32, kind=skind).ap()
    Kx.hres = nc.dram_tensor("hres", [T, D_MODEL], F32, kind="Internal").ap()
    Kx.d_hT = Buf("d_hT")
    Kx.d_ycT = Buf("d_ycT")
    Kx.d_x1 = Buf("d_x1")
    Kx.d_x1T = Buf("d_x1T")
    Kx.d_comb = Buf("d_comb")
    Kx.d_hres = Buf("d_hres")
    with ExitStack() as st0:
        P = Prog(nc, st0)
        Kx.bank = []
        for i in range(8):
            t = st0.enter_context(nc.psum_tensor(f"bank{i}", [128, 512], F32))
            Kx.bank.append(Buf(f"bank{i}", t))
            Kx.bank[-1].excl = True
        Kx.cst = P.sb("cst_sb", [128, NCONST, 128], F32)
        P.dma('sp', Kx.cst[:], Kx.cst_d, writes=[Kx.cst])
        for l in range(NL):
            if l == 0:
                pass_transpose_in(P, Kx, T, Kx.x, Kx.hT)
            if "ssd" in passes:
                pass_ssd(P, Kx, T, l)
            if "gdn" in passes:
                pass_gdn(P, Kx, T, l)
            if "hg" in passes:
                pass_hg(P, Kx, T, l, layers[l])
            if "out" in passes:
                pass_out(P, Kx, T, l, Kx.x if l == 0 else Kx.hres)
            if "moe" in passes:
                pass_moe(P, Kx, T, l, Kx.out if l == NL - 1 else Kx.hres, l < NL - 1)
        print("recorded ops", P.nops, "waits", P.nwaits)
    return nc


def host_params(inp, layers):
    out = {}
    NL = len(layers)
    pp = np.zeros((NL, 128, SSD_NPP), np.float32)
    rp = np.zeros((NL, 128, SSD_NRP), np.float32)
    for i, l in enumerate(layers):
        cw = inp['ssd_conv_w'][l]
        pp[i, :, 0:48] = cw.reshape(4, 12, 128).transpose(2, 1, 0).reshape(128, 48)
        pp[i, :, 48:60] = inp['ssd_conv_b'][l].reshape(12, 128).T
        rp[i, :, 0:16] = inp['ssd_dt_bias'][l][None, :]
        rp[i, :, 16:32] = inp['ssd_a_log'][l][None, :]
        rp[i, :, 32:48] = inp['ssd_d'][l][None, :]
        rp[i, :, 48:48 + 1024] = inp['ssd_norm_w'][l][None, :]
    out['ssd_pp'] = pp
    out['ssd_rp'] = rp
    gpp = np.zeros((NL, 128, GDN_NPP), np.float32)
    grp = np.zeros((NL, 128, GDN_NRP), np.float32)
    for i, l in enumerate(layers):
        gpp[i, :, 0:48] = inp['gdn_conv_w'][l].reshape(4, 12, 128).transpose(2, 1, 0).reshape(128, 48)
        grp[i, :, 0:4] = inp['gdn_dt_bias'][l][None, :]
        grp[i, :, 4:8] = inp['gdn_a_log'][l][None, :]
        grp[i, :, 8:136] = inp['gdn_norm_w'][l][None, :]
    out['gdn_pp'] = gpp
    out['gdn_rp'] = grp
    hrp = np.zeros((NL, 128, HG_NRP), np.float32)
    for i, l in enumerate(layers):
        hrp[i, :, 0:128] = inp['hg_norm_w'][l][None, :]
    out['hg_rp'] = hrp
    lmask = np.zeros((NL, 128, 4, 4), np.float32)
    for i, l in enumerate(layers):
        lmask[i, :, :, 1:l + 1] = 1.0
    out['hg_lmask'] = lmask
    out['hg_lbl'] = np.ascontiguousarray(inp['hg_lb_logits'].reshape(4, 4, 128).transpose(2, 1, 0))
    orp = np.zeros((NL, 128, OUT_NRP), np.float32)
    wr = np.zeros((NL, 128, 8, 20), np.float32)
    mrp = np.zeros((NL, 128, 2048), np.float32)
    for i, l in enumerate(layers):
        orp[i, :, 0:1024] = inp['ln1_g'][l][None, :]
        orp[i, :, 1024:2048] = inp['ln1_b'][l][None, :]
        orp[i, :, 2048:2052] = inp['b_router_group'][l][None, :]
        orp[i, :, 2052:2068] = inp['b_router_expert'][l][None, :]
        wcat = np.concatenate([inp['w_router_group'][l], inp['w_router_expert'][l]], axis=1)
        wr[i] = wcat.reshape(8, 128, 20).transpose(1, 0, 2)
        mrp[i, :, 0:1024] = inp['ln2_g'][l][None, :]
        mrp[i, :, 1024:2048] = inp['ln2_b'][l][None, :]
    out['out_rp'] = orp
    out['wr'] = wr
    out['moe_rp'] = mrp
    out['cst'] = make_consts()
    return out


def core_inputs(inp, b, T, layers):
    hp = host_params(inp, layers)
    ls = layers
    im = {
        "x": np.ascontiguousarray(inp['x'][b, :T]),
        "w_in": np.ascontiguousarray(inp['w_in'][ls]),
        "w_out": np.ascontiguousarray(inp['w_out'][ls]),
        "w_gu": np.ascontiguousarray(inp['w_expert_gate_up'][ls]),
        "w_dn": np.ascontiguousarray(inp['w_expert_down'][ls]),
    }
    im.update(hp)
    return im


_PROG = {}


def kernel(**inputs):
    inp = {k: np.asarray(v) for k, v in inputs.items()}
    B, T, _ = inp['x'].shape
    ncores = 8
    nc = build(T, DEPTH)
    base = core_inputs(inp, 0, T, list(range(DEPTH)))
    in_maps = []
    for c in range(ncores):
        m = dict(base)
        m["x"] = np.ascontiguousarray(inp['x'][c % B], dtype=np.float32)
        in_maps.append(m)
    res = run_bass_kernel_spmd(nc, in_maps, core_ids=list(range(ncores)))
    return np.stack([np.asarray(res.results[b]["out"]) for b in range(B)]).astype(np.float32)
```

```python
import numpy as np
from contextlib import ExitStack
import concourse.bass as bass
import concourse.mybir as mybir
from concourse.bass_utils import run_bass_kernel_spmd

F32 = mybir.dt.float32
BF16 = mybir.dt.bfloat16
F32R = mybir.dt.float32r
USE_F32R = True
AF = mybir.ActivationFunctionType
ALU = mybir.AluOpType
AX = mybir.AxisListType

COMPUTE = ('pe', 'dve', 'act', 'pool')
ALLENG = ('pe', 'dve', 'act', 'pool', 'sp')
QUEUES = ('sp', 'act', 'pool')
NDMA = 8

D_MODEL = 1024
IN_COLS = 6680
DEPTH = 4
DN_ALPHA = (2 * DEPTH) ** 0.25


class Buf:
    def __init__(self, name, t=None, parent=None):
        self.name = name
        self.t = t
        self.parent = parent
        self.children = []
        self.last_write = None
        self.reads = []
        self.excl = False

    def sub(self, key):
        c = Buf(f"{self.name}.{key}", self.t, self)
        self.children.append(c)
        return c

    def __getitem__(self, k):
        return self.t[k]


class Op:
    __slots__ = ('id', 'eng', 'dma', 'fn', 'deps', 'dur', 'tag', 'out', 'in_', 'kw', 'pos', 'tok', 'fin')

    def __init__(self, id, eng, dma, fn, deps, dur, tag):
        self.id = id
        self.eng = eng
        self.dma = dma
        self.fn = fn
        self.deps = deps
        self.dur = dur
        self.tag = tag
        self.tok = None
        self.fin = 0.0


SCHED_WINDOW = 32
ACT_SWITCH_US = 1.3
XLAT = 0.25


class Prog:
    def __init__(self, nc, stack):
        self.nc = nc
        self.stack = stack
        self.sem = {e: stack.enter_context(nc.semaphore(f"s_{e}")) for e in COMPUTE}
        self.cnt = {e: 0 for e in COMPUTE}
        self.known = {e: {} for e in ALLENG}
        self.dsem = {q: [stack.enter_context(nc.semaphore(f"d_{q}_{i}")) for i in range(NDMA)] for q in QUEUES}
        self.dcnt = {q: [0] * NDMA for q in QUEUES}
        self.dnext = {q: 0 for q in QUEUES}
        self.semobj = {}
        for e in COMPUTE:
            self.semobj[('c', e)] = self.sem[e]
        for q in QUEUES:
            for i in range(NDMA):
                self.semobj[('d', q, i)] = self.dsem[q][i]
        self.nops = 0
        self.nwaits = 0
        self.pend = []
        self.base = 0
        self.nid = 0
        self.nalloc = 0

    def sb(self, name, shape, dtype=F32):
        self.nalloc += 1
        t = self.stack.enter_context(self.nc.sbuf_tensor(f"sb{self.nalloc}_{name}", list(shape), dtype))
        return Buf(name, t)

    def _collect(self, b, write):
        ids = []

        def add(x):
            if x.last_write is not None:
                ids.append(x.last_write)
            if write or x.excl:
                ids.extend(x.reads)
        add(b)
        p = b.parent
        while p is not None:
            add(p)
            p = p.parent

        def rec(x):
            for c in x.children:
                add(c)
                rec(c)
        rec(b)
        return ids

    def _mkdeps(self, eng, is_dma, reads, writes):
        deps = {}
        byid = self.pend_by_id
        for b in reads:
            wr = self._writers_of(b)
            for d in self._collect(b, False):
                if d < self.base:
                    continue
                D = byid[d]
                same = (D.eng == eng and not D.dma and not is_dma)
                if same and d not in wr:
                    deps[d] = deps.get(d, False)
                else:
                    deps[d] = True
        for b in writes:
            for d in self._collect(b, True):
                if d < self.base:
                    continue
                D = byid[d]
                same = (D.eng == eng and not D.dma and not is_dma)
                if same:
                    deps[d] = deps.get(d, False)
                else:
                    deps[d] = True
        return deps

    def _writers_of(self, b):
        w = set()

        def add(x):
            if x.last_write is not None:
                w.add(x.last_write)
        add(b)
        p = b.parent
        while p is not None:
            add(p)
            p = p.parent

        def rec(x):
            for c in x.children:
                add(c)
                rec(c)
        rec(b)
        return w

    @property
    def pend_by_id(self):
        return self._byid

    def _record(self, oid, reads, writes):
        for b in reads:
            b.reads.append(oid)
        for b in writes:
            b.last_write = oid
            b.reads = []

            def rec(x):
                for c in x.children:
                    c.last_write = None
                    c.reads = []
                    rec(c)
            rec(b)

    def _new(self, eng, dma, fn, reads, writes, dur, tag):
        if not hasattr(self, '_byid'):
            self._byid = {}
        deps = self._mkdeps(eng, dma, reads, writes)
        o = Op(self.nid, eng, dma, fn, deps, dur, tag)
        self.nid += 1
        self.pend.append(o)
        self._byid[o.id] = o
        self._record(o.id, reads, writes)
        self.nops += 1
        return o

    def op(self, eng, fn, reads=(), writes=(), dur=0.3, tag=None):
        return self._new(eng, False, fn, reads, writes, dur, tag)

    def dma(self, q, out, in_, reads=(), writes=(), dur=3.0, **kw):
        o = self._new(q, True, None, reads, writes, dur, None)
        o.out, o.in_, o.kw = out, in_, kw
        return o

    def _schedule(self):
        ops = self.pend
        if not ops:
            return {e: [] for e in ALLENG}
        queues = {e: [] for e in ALLENG}
        for o in ops:
            queues[o.eng].append(o)
        head = {e: 0 for e in ALLENG}
        done = set()
        free = {e: 0.0 for e in ALLENG}
        cur_tag = None
        order = {e: [] for e in ALLENG}
        scheduled = {}
        remaining = len(ops)
        base = self.base
        taken = {e: set() for e in ALLENG}
        while remaining:
            best = None
            for e in ALLENG:
                q = queues[e]
                n = 0
                i = head[e]
                while i < len(q) and n < SCHED_WINDOW:
                    o = q[i]
                    i += 1
                    if o.id in done:
                        continue
                    n += 1
                    ok = True
                    rdy = 0.0
                    for d, w in o.deps.items():
                        if d < base:
                            continue
                        if d not in done:
                            ok = False
                            break
                        f = scheduled[d]
                        rdy = max(rdy, f + (XLAT if w else 0.0))
                    if not ok:
                        continue
                    st = max(rdy, free[e])
                    if e == 'act' and not o.dma and o.tag is not None and cur_tag is not None and o.tag != cur_tag:
                        st += ACT_SWITCH_US
                    key = (st, o.id)
                    if best is None or key < best[0]:
                        best = (key, e, o)
            assert best is not None, "scheduler deadlock (cyclic deps?)"
            (st, _), e, o = best
            if o.dma:
                free[e] = st + 0.15
                fin = st + o.dur
            else:
                free[e] = st + o.dur
                fin = st + o.dur
                if e == 'act' and o.tag is not None:
                    cur_tag = o.tag
            scheduled[o.id] = fin
            done.add(o.id)
            order[e].append(o)
            remaining -= 1
            q = queues[e]
            while head[e] < len(q) and q[head[e]].id in done:
                head[e] += 1
        self.est_us = max(scheduled.values()) if scheduled else 0.0
        return order

    def _wait(self, lst, eng, tok):
        key, val = tok
        if self.known[eng].get(key, 0) >= val:
            return
        self.known[eng][key] = val
        sem = self.semobj[key]
        lst.append(lambda e, sem=sem, val=val: e.wait_ge(sem, val))
        self.nwaits += 1

    def flush(self):
        order = self._schedule()
        for e in ALLENG:
            for o in order[e]:
                if o.dma:
                    i = self.dnext[e]
                    self.dnext[e] = (i + 1) % NDMA
                    o.pos = (i, self.dcnt[e][i])
                    self.dcnt[e][i] += 16
                    o.tok = (('d', e, i), self.dcnt[e][i])
                else:
                    self.cnt[e] += 1
                    o.tok = (('c', e), self.cnt[e])
        byid = self._byid
        lists = {e: [] for e in ALLENG}
        for e in ALLENG:
            lst = lists[e]
            for o in order[e]:
                for d, w in o.deps.items():
                    if d < self.base or not w:
                        continue
                    self._wait(lst, e, byid[d].tok)
                if o.dma:
                    i, prev = o.pos
                    if prev > 0:
                        self._wait(lst, e, (('d', e, i), prev))
                    sem = self.dsem[e][i]
                    lst.append(lambda en, o=o, sem=sem: en.dma_start(out=o.out, in_=o.in_, **o.kw).then_inc(sem, 16))
                else:
                    sem = self.sem[e]
                    lst.append(lambda en, o=o, sem=sem: o.fn(en).then_inc(sem, 1))
        for e in ALLENG:
            for c in COMPUTE:
                if self.cnt[c] > 0 and c != e:
                    self._wait(lists[e], e, (('c', c), self.cnt[c]))
            for q in QUEUES:
                for i in range(NDMA):
                    if self.dcnt[q][i] > 0:
                        self._wait(lists[e], e, (('d', q, i), self.dcnt[q][i]))
        self.pend = []
        self.base = self.nid
        self._byid = {}
        nc = self.nc
        with nc.Block() as block:
            @block.sync
            def _(e):
                for f in lists['sp']:
                    f(e)

            @block.tensor
            def _(e):
                for f in lists['pe']:
                    f(e)

            @block.vector
            def _(e):
                for f in lists['dve']:
                    f(e)

            @block.scalar
            def _(e):
                for f in lists['act']:
                    f(e)

            @block.gpsimd
            def _(e):
                for f in lists['pool']:
                    f(e)


C_I, C_LE, C_GT, C_ONES, C_LE64, C_GT64, C_LT64, C_SAME64, C_SEL0, C_SEL1 = range(10)
NCONST = 10


def make_consts():
    k = np.arange(128)[:, None]
    l = np.arange(128)[None, :]
    c = np.zeros((128, NCONST, 128), np.float32)
    c[:, C_I] = (k == l)
    c[:, C_LE] = (k <= l)
    c[:, C_GT] = (k > l)
    c[:, C_ONES] = 1.0
    same64 = (k // 64) == (l // 64)
    c[:, C_LE64] = (k <= l) & same64
    c[:, C_GT64] = (k > l) & same64
    c[:, C_LT64] = (k < l) & same64
    c[:, C_SAME64] = same64
    c[:, C_SEL0] = (k < 64) & (l >= 0)
    c[:, C_SEL1] = (k >= 64) & (l >= 0)
    return c


class K:
    pass


STAGE = 99


class StopPass(Exception):
    pass


def chk(n):
    if STAGE == n:
        raise StopPass()


def _fsz(ap):
    n = 1
    for d in ap.shape[1:]:
        n *= d
    return n


def _is32(ap):
    return ap.dtype == F32


ACT_TAG = {}


def _act_tag(func):
    if func == AF.Silu:
        return 'silu'
    if func == AF.Sigmoid:
        return 'sig'
    if func in (AF.Exp, AF.Ln):
        return 'lnexp'
    return None


def mm(P, out, lhsT, rhs, start, stop, reads, writes):
    passes = 4 if _is32(rhs) else 1
    dur = passes * max(_fsz(rhs), 64) / 2400.0 + passes * max(_fsz(lhsT), 32) / 2400.0 * 0.5 + 0.02
    return P.op('pe', lambda e: e.matmul(out, lhsT=lhsT, rhs=rhs, start=start, stop=stop), reads, writes, dur=dur)


def mmx(P, out, lhsT, rhs, start, stop, reads, writes):
    dur = max(_fsz(rhs), 64) / 2400.0 + max(_fsz(lhsT), 32) / 4800.0 + 0.02
    return P.op('pe', lambda e: e.matmul(out, lhsT=lhsT, rhs=rhs, start=start, stop=stop, skip_group_check=True), reads, writes, dur=dur)


def mmr(P, out, lhsT, rhs, start, stop, reads, writes):
    passes = 4
    if USE_F32R:
        lhsT = lhsT.bitcast(F32R)
        rhs = rhs.bitcast(F32R)
        passes = 1
    dur = passes * (max(_fsz(rhs), 64) / 2400.0 + max(_fsz(lhsT), 32) / 4800.0) + 0.02
    return P.op('pe', lambda e: e.matmul(out, lhsT=lhsT, rhs=rhs, start=start, stop=stop), reads, writes, dur=dur)


def rr(ap):
    return ap.bitcast(F32R) if USE_F32R else ap


def tr(P, out, in_, ident, reads, writes):
    passes = 4 if _is32(in_) else 1
    dur = passes * (max(_fsz(in_), 64) / 2400.0) * 1.5 + 0.02
    return P.op('pe', lambda e: e.transpose(out, in_, ident), reads, writes, dur=dur)


def act(P, out, in_, func, reads, writes, **kw):
    dur = _fsz(in_) / 1400.0 + 0.2 + (0.1 if ('scale' in kw and not isinstance(kw['scale'], float)) else 0.0)
    return P.op('act', lambda e: e.activation(out=out, in_=in_, func=func, **kw), reads, writes, dur=dur, tag=_act_tag(func))


def tt(P, out, in0, in1, op, reads, writes):
    dur = _fsz(out) / 960.0 + 0.12
    return P.op('dve', lambda e: e.tensor_tensor(out=out, in0=in0, in1=in1, op=op), reads, writes, dur=dur)


def ts(P, out, in0, s1, s2, op0, op1, reads, writes, **kw):
    dur = _fsz(out) / 1400.0 + 0.12
    if op1 is None:
        return P.op('dve', lambda e: e.tensor_scalar(out=out, in0=in0, scalar1=s1, scalar2=None, op0=op0, **kw), reads, writes, dur=dur)
    return P.op('dve', lambda e: e.tensor_scalar(out=out, in0=in0, scalar1=s1, scalar2=s2, op0=op0, op1=op1, **kw), reads, writes, dur=dur)


def stt(P, out, in0, scalar, in1, op0, op1, reads, writes):
    dur = _fsz(out) / 960.0 + 0.12
    return P.op('dve', lambda e: e.scalar_tensor_tensor(out=out, in0=in0, scalar=scalar, in1=in1, op0=op0, op1=op1), reads, writes, dur=dur)


def load_weights(P, dst, dst_ap_fn, src_rows_fn, ncols, nk):
    toks = []
    if not dst.children:
        for k in range(nk):
            for c0 in range(0, ncols, 2048):
                dst.sub((k, c0))
    i = 0
    for k in range(nk):
        c0 = 0
        while c0 < ncols:
            c1 = min(ncols, c0 + 2048)
            toks.append(P.dma('pool', dst_ap_fn(k, c0, c1), src_rows_fn(k, c0, c1), writes=[dst.children[i]]))
            i += 1
            c0 = c1
    return toks


SSD_NPP = 12 * 4 + 12
SSD_NRP = 16 + 16 + 16 + 1024


def pass_transpose_in(P, Kx, T, src, dstT):
    nc = P.nc
    with ExitStack() as st:
        P.stack = st
        xt = [P.sb(f"p0_x{i}", [128, 1024], F32) for i in range(2)]
        xb = [P.sb(f"p0_b{i}", [128, 8, 128], BF16) for i in range(2)]
        for b in range(T // 128):
            x_ = xt[b % 2]
            o_ = xb[b % 2]
            P.dma('sp', x_[:], src[b * 128:(b + 1) * 128, :], writes=[x_])
            for j in range(8):
                bk = Kx.bank[j // 4]
                tr(P, bk[:, (j % 4) * 128:(j % 4 + 1) * 128], x_[:, j * 128:(j + 1) * 128], Kx.cst[:, C_I, :],
                   [x_, Kx.cst], [bk])
            for hlf in range(2):
                act(P, o_[:, hlf * 4:(hlf + 1) * 4, :], Kx.bank[hlf][:, :].rearrange("p (j t) -> p j t", j=4),
                    AF.Copy, [Kx.bank[hlf]], [o_])
            P.dma('sp', dstT.rearrange("k p t -> p k t")[:, :, b * 128:(b + 1) * 128], o_[:], reads=[o_], writes=[Kx.d_hT])
        P.flush()


def pass_ssd(P, Kx, T, l, dbg=None):
    nc = P.nc
    SBT = min(512, T)
    NSB = T // SBT
    NBLK = SBT // 128
    cst = Kx.cst
    bank = Kx.bank
    A0, A1, B0, B1, C0, C1, D0, D1 = bank
    with ExitStack() as st:
        P.stack = st
        try:
            W = P.sb("ssd_W", [128, 8, 2576], BF16)
            pp = P.sb("ssd_pp", [128, SSD_NPP], F32)
            rp = P.sb("ssd_rp", [128, SSD_NRP], F32)
            hTb = [P.sb(f"ssd_hT{i}", [128, 8, SBT], BF16) for i in range(2)]
            xin = P.sb("ssd_xin", [128, 12, SBT + 3], F32)
            accs = [P.sb(f"ssd_acc{i}", [128, SBT], F32) for i in range(12)]
            xc = P.sb("ssd_xc", [128, 12, SBT], F32)
            xcj = [xc.sub(j) for j in range(12)]
            xinj = [xin.sub(j) for j in range(12)]
            Dmat = P.sb("ssd_Dmat", [128, 16, 128], BF16)
            arep = P.sb("ssd_arep", [128, 16], F32)
            sm = P.sb("ssd_sm", [128, 16 * 12], F32)
            smv = lambda i: sm[:, i * 16:(i + 1) * 16]
            R = P.sb("ssd_R", [128, 16, 128], F32)
            DT = P.sb("ssd_DT", [128, 16, 128], BF16)
            GT = P.sb("ssd_GT", [128, 16, 128], BF16)
            CBTm = P.sb("ssd_CBTm", [128, 2, 128], BF16)
            xsb = P.sb("ssd_xsb", [128, 1024], BF16)
            xdt = P.sb("ssd_xdt", [128, 1024], BF16)
            xend = P.sb("ssd_xend", [128, 1024], BF16)
            Btm = P.sb("ssd_Btm", [128, 256], BF16)
            bcT = P.sb("ssd_bcT", [128, 4, 128], BF16)
            sz = P.sb("ssd_sz", [128, 1024], F32)
            yoff = P.sb("ssd_yoff", [128, 1024], F32)
            y = P.sb("ssd_y", [128, 1024], F32)
            junk = P.sb("ssd_junk", [128, 512], F32)
            ybf = P.sb("ssd_ybf", [128, 1024], BF16)
            yT = [P.sb(f"ssd_yT{i}", [128, 8, 128], BF16) for i in range(2)]
            S32 = P.sb("ssd_S32", [128, 1024], F32)
            Sbf = P.sb("ssd_Sbf", [128, 1024], BF16)
            identb = P.sb("ssd_identb", [128, 128], BF16)

            load_weights(P, W, lambda k, c0, c1: W[:, k, c0:c1],
                         lambda k, c0, c1: Kx.w_in[l, k * 128:(k + 1) * 128, c0:c1], 2576, 8)
            P.dma('sp', pp[:], Kx.ssd_pp[l], writes=[pp])
            P.dma('sp', rp[:], Kx.ssd_rp[l], writes=[rp])
            dtb = rp[:, 0:16]
            alog = rp[:, 16:32]
            drep = rp[:, 32:48]
            normw = rp[:, 48:48 + 1024]
            cw = lambda j, k: pp[:, j * 4 + k: j * 4 + k + 1]
            cb = lambda j: pp[:, 48 + j: 48 + j + 1]
            act(P, identb[:], cst[:, C_I, :], AF.Copy, [cst], [identb])
            act(P, arep[:], alog, AF.Exp, [rp], [arep])
            ts(P, arep[:], arep[:], -1.0, None, ALU.mult, None, [arep], [arep])
            tt(P, Dmat[:], cst[:, C_I, :].unsqueeze(1).to_broadcast([128, 16, 128]),
               drep.unsqueeze(2).to_broadcast([128, 16, 128]), ALU.mult, [cst, rp], [Dmat])
            P.op('dve', lambda e: e.memset(S32[:], 0.0), [], [S32])
            P.op('dve', lambda e: e.memset(Sbf[:], 0.0), [], [Sbf])
            P.op('dve', lambda e: e.memset(xin[:], 0.0), [], [xin])
            chk(1)

            for sbi in range(NSB):
                hb = hTb[sbi % 2]
                P.dma('sp', hb[:], Kx.hT.rearrange("k p t -> p k t")[:, :, sbi * SBT:(sbi + 1) * SBT], reads=[Kx.d_hT], writes=[hb])
                banks4 = [D0, D1, C0, C1]
                for j in range(12):
                    bk = banks4[j % 4]
                    for k in range(8):
                        mm(P, bk[:, 0:SBT], W[:, k, 1024 + j * 128: 1024 + (j + 1) * 128], hb[:, k, :], k == 0, k == 7,
                           [W, hb], [bk])
                    ac = accs[j]
                    act(P, xin[:, j, 3:3 + SBT], bk[:, 0:SBT], AF.Copy, [bk], [xinj[j]])
                    act(P, ac[:], bk[:, 0:SBT], AF.Identity, [bk, pp], [ac], scale=cw(j, 3), bias=cb(j))
                for j in range(12):
                    ac = accs[j]
                    for k in (2, 1, 0):
                        stt(P, ac[:], xin[:, j, k:k + SBT], cw(j, k), ac[:], ALU.mult, ALU.add, [xinj[j], pp, ac], [ac])
                for j in range(12):
                    ac = accs[j]
                    act(P, xc[:, j, :], ac[:], AF.Silu, [ac], [xcj[j]])
                    act(P, xin[:, j, 0:3], xin[:, j, SBT:SBT + 3], AF.Copy, [xinj[j]], [xinj[j]])
                chk(2)
                for blk in range(NBLK):
                    t0 = blk * 128
                    tg = sbi * SBT + t0
                    for hf in range(2):
                        bk = [A0, A1][hf]
                        for k in range(8):
                            mm(P, bk[:, :], hb[:, k, t0:t0 + 128], W[:, k, hf * 512:(hf + 1) * 512], k == 0, k == 7, [hb, W], [bk])
                        act(P, sz[:, hf * 512:(hf + 1) * 512], bk[:, :], AF.Silu, [bk], [sz])
                    for k in range(8):
                        mm(P, C0[:, 0:16], hb[:, k, t0:t0 + 128], W[:, k, 2560:2576], k == 0, k == 7, [hb, W], [C0])
                    xr, xm, ex, lg, dt, dA, acs, te, dte, ea, cd = [smv(i) for i in range(11)]
                    tt(P, xr, C0[:, 0:16], dtb, ALU.add, [C0, rp], [sm])
                    ts(P, xm, xr, 30.0, None, ALU.min, None, [sm], [sm])
                    act(P, ex, xm, AF.Exp, [sm], [sm])
                    act(P, lg, ex, AF.Ln, [sm], [sm], bias=1.0)
                    tt(P, dt, lg, xr, ALU.max, [sm], [sm])
                    tt(P, dA, dt, arep[:], ALU.mult, [sm, arep], [sm])
                    mm(P, C0[:, 16:32], cst[:, C_LE, :], dA, True, True, [cst, sm], [C0])
                    mm(P, C0[:, 32:48], cst[:, C_ONES, :], dA, True, True, [cst, sm], [C0])
                    act(P, ea, C0[:, 16:32], AF.Exp, [C0], [sm])
                    act(P, cd, C0[:, 32:48], AF.Exp, [C0], [sm])
                    act(P, acs, C0[:, 16:32], AF.Copy, [C0], [sm])
                    tt(P, te, C0[:, 32:48], acs, ALU.subtract, [C0, sm], [sm])
                    act(P, te, te, AF.Exp, [sm], [sm])
                    tt(P, dte, dt, te, ALU.mult, [sm], [sm])
                    chk(3)
                    tt(P, R[:], cst[:, C_LE, :].unsqueeze(1).to_broadcast([128, 16, 128]),
                       dA.unsqueeze(2).to_broadcast([128, 16, 128]), ALU.mult, [cst, sm], [R])
                    for q in range(4):
                        bk = [D0, D1][q % 2]
                        mm(P, bk[:, :], cst[:, C_GT, :], R[:, 4 * q:4 * q + 4, :], True, True, [cst, R], [bk])
                        act(P, DT[:, 4 * q:4 * q + 4, :], bk[:, :].rearrange("p (h l) -> p h l", h=4), AF.Exp, [bk], [DT])
                    chk(4)
                    for j in range(8):
                        bk = [B0, B1][j // 4]
                        tr(P, bk[:, (j % 4) * 128:(j % 4 + 1) * 128], xc[:, j, t0:t0 + 128], cst[:, C_I, :], [xcj[j], cst], [bk])
                    for hf in range(2):
                        bk = [B0, B1][hf]
                        pv = bk[:, :].rearrange("p (h c) -> p h c", h=8)
                        act(P, xsb[:, hf * 512:(hf + 1) * 512], bk[:, :], AF.Copy, [bk], [xsb])
                        chk(4.1)
                        tt(P, xdt[:, hf * 512:(hf + 1) * 512].rearrange("p (h c) -> p h c", h=8), pv,
                           dt[:, hf * 8:(hf + 1) * 8].unsqueeze(2).to_broadcast([128, 8, 64]), ALU.mult, [bk, sm], [xdt])
                        chk(4.11)
                        tt(P, xend[:, hf * 512:(hf + 1) * 512].rearrange("p (h c) -> p h c", h=8), pv,
                           dte[:, hf * 8:(hf + 1) * 8].unsqueeze(2).to_broadcast([128, 8, 64]), ALU.mult, [bk, sm], [xend])
                        chk(4.12)
                    chk(4.2)
                    for g in range(2):
                        tr(P, C0[:, 128 + g * 128:128 + (g + 1) * 128], xc[:, 8 + g, t0:t0 + 128], cst[:, C_I, :], [xcj[8 + g], cst], [C0])
                    act(P, Btm[:], C0[:, 128:384], AF.Copy, [C0], [Btm])
                    chk(4.3)
                    act(P, bcT[:], xc[:, 8:12, t0:t0 + 128], AF.Copy, [xcj[8], xcj[9], xcj[10], xcj[11]], [bcT])
                    chk(5)
                    for g in range(2):
                        mm(P, C1[:, g * 128:(g + 1) * 128], bcT[:, g, :], bcT[:, 2 + g, :], True, True, [bcT], [C1])
                    tt(P, CBTm[:], C1[:, 0:256].rearrange("p (g l) -> p g l", g=2),
                       cst[:, C_LE, :].unsqueeze(1).to_broadcast([128, 2, 128]), ALU.mult, [C1, cst], [CBTm])
                    for g in range(2):
                        tt(P, GT[:, g * 8:(g + 1) * 8, :], DT[:, g * 8:(g + 1) * 8, :],
                           CBTm[:, g, :].unsqueeze(1).to_broadcast([128, 8, 128]), ALU.mult, [DT, CBTm], [GT])
                    chk(6)
                    for h in range(16):
                        bk = [A0, A1][h // 8]
                        o = bk[:, (h % 8) * 64:(h % 8 + 1) * 64]
                        mm(P, o, GT[:, h, :], xdt[:, h * 64:(h + 1) * 64], True, False, [GT, xdt], [bk])
                        mm(P, o, Dmat[:, h, :], xsb[:, h * 64:(h + 1) * 64], False, True, [Dmat, xsb], [bk])
                    for g in range(2):
                        bk = [B0, B1][g]
                        mm(P, bk[:, :], bcT[:, 2 + g, :], Sbf[:, g * 512:(g + 1) * 512], True, True, [bcT, Sbf], [bk])
                        tt(P, yoff[:, g * 512:(g + 1) * 512].rearrange("p (h c) -> p h c", h=8),
                           bk[:, :].rearrange("p (h c) -> p h c", h=8),
                           ea[:, g * 8:(g + 1) * 8].unsqueeze(2).to_broadcast([128, 8, 64]), ALU.mult, [bk, sm], [yoff])
                        tt(P, y[:, g * 512:(g + 1) * 512], [A0, A1][g][:, :], yoff[:, g * 512:(g + 1) * 512], ALU.add,
                           [[A0, A1][g], yoff], [y])
                    tt(P, y[:], y[:], sz[:], ALU.mult, [y, sz], [y])
                    chk(7)
                    ss = smv(11)
                    for g in range(2):
                        P.op('act', lambda e, g=g: e.activation(out=junk[:], in_=y[:, g * 512:(g + 1) * 512], func=AF.Square,
                                                               accum_out=sm[:, 176 + g:177 + g]), [y], [junk, sm])
                    act(P, sm[:, 178:180], sm[:, 176:178], AF.Ln, [sm], [sm], scale=1.0 / 512, bias=1e-6)
                    act(P, sm[:, 178:180], sm[:, 178:180], AF.Exp, [sm], [sm], scale=-0.5)
                    for g in range(2):
                        stt(P, ybf[:, g * 512:(g + 1) * 512], y[:, g * 512:(g + 1) * 512], sm[:, 178 + g:179 + g],
                            normw[:, g * 512:(g + 1) * 512], ALU.mult, ALU.mult, [y, sm, rp], [ybf])
                    c1b = C1[:, :].bitcast(BF16)
                    for j in range(8):
                        tr(P, c1b[:, j * 128:(j + 1) * 128], ybf[:, j * 128:(j + 1) * 128], identb[:], [ybf, identb], [C1])
                    yt = yT[(sbi * NBLK + blk) % 2]
                    act(P, yt[:], c1b.rearrange("p (j t) -> p j t", j=8), AF.Copy, [C1], [yt])
                    P.dma('sp', Kx.ycT.rearrange("k p t -> p k t")[:, 0:8, tg:tg + 128], yt[:], reads=[yt], writes=[Kx.d_ycT])
                    chk(8)
                    for g in range(2):
                        bk = [D0, D1][g]
                        mm(P, bk[:, :], Btm[:, g * 128:(g + 1) * 128], xend[:, g * 512:(g + 1) * 512], True, True, [Btm, xend], [bk])
                    tt(P, S32[:].rearrange("p (h c) -> p h c", h=16), S32[:].rearrange("p (h c) -> p h c", h=16),
                       cd.unsqueeze(2).to_broadcast([128, 16, 64]), ALU.mult, [S32, sm], [S32])
                    for g in range(2):
                        tt(P, S32[:, g * 512:(g + 1) * 512], [D0, D1][g][:, :], S32[:, g * 512:(g + 1) * 512], ALU.add,
                           [[D0, D1][g], S32], [S32])
                    act(P, Sbf[:], S32[:], AF.Copy, [S32], [Sbf])
        except StopPass:
            pass
        P.flush()


GDN_NPP = 48
GDN_NRP = 4 + 4 + 128
GDN_BASE = 2576


def pass_gdn(P, Kx, T, l):
    SBT = min(512, T)
    NSB = T // SBT
    NBLK = SBT // 128
    cst = Kx.cst
    A0, A1, B0, B1, C0, C1, D0, D1 = Kx.bank
    b4 = lambda ap: ap.rearrange("p (h c) -> p h c", h=4)
    with ExitStack() as st:
        P.stack = st
        try:
            W = P.sb("gdn_W", [128, 8, 2056], BF16)
            pp = P.sb("gdn_pp", [128, GDN_NPP], F32)
            rp = P.sb("gdn_rp", [128, GDN_NRP], F32)
            hTb = [P.sb(f"gdn_hT{i}", [128, 8, SBT], BF16) for i in range(2)]
            xin = P.sb("gdn_xin", [128, 12, SBT + 3], F32)
            xinj = [xin.sub(j) for j in range(12)]
            accs = [P.sb(f"gdn_acc{i}", [128, SBT], F32) for i in range(12)]
            sgS = P.sb("gdn_sgS", [128, NBLK, 512], F32)
            xc = P.sb("gdn_xc", [128, 12, SBT], F32)
            xcj = [xc.sub(j) for j in range(12)]
            qT = P.sb("gdn_qT", [128, 4, SBT], BF16)
            kT = P.sb("gdn_kT", [128, 4, SBT], BF16)
            identb = P.sb("gdn_identb", [128, 128], BF16)
            arep = P.sb("gdn_arep", [128, 4], F32)
            smP = [P.sb(f"gdn_sm{i}", [128, 4 * 20], F32) for i in range(2)]
            R = P.sb("gdn_R", [128, 4, 128], F32)
            Dec = P.sb("gdn_Dec", [128, 4, 128], F32)
            DecU = P.sb("gdn_DecU", [128, 4, 128], F32)
            t1 = P.sb("gdn_t1", [128, 4, 128], F32)
            qkTmP = [P.sb(f"gdn_qkTm{i}", [128, 4, 128], BF16) for i in range(2)]
            Ya = [P.sb(f"gdn_Y{i}", [128, 4, 128], F32) for i in range(2)]
            Za = [P.sb(f"gdn_Z{i}", [128, 4, 128], F32) for i in range(2)]
            V = P.sb("gdn_V", [128, 4, 128], F32)
            VbfP = [P.sb(f"gdn_Vbf{i}", [128, 4, 128], BF16) for i in range(2)]
            vtmP = [P.sb(f"gdn_vtm{i}", [128, 4, 128], F32) for i in range(2)]
            kdP = [P.sb(f"gdn_kd{i}", [128, 4, 128], BF16) for i in range(2)]
            rhs2 = P.sb("gdn_rhs2", [128, 4, 128], BF16)
            vnew = P.sb("gdn_vnew", [128, 4, 128], BF16)
            As = P.sb("gdn_As", [128, 4, 128], F32)
            o = P.sb("gdn_o", [128, 4, 128], F32)
            junk = P.sb("gdn_junk", [128, 128], F32)
            ybf = P.sb("gdn_ybf", [128, 4, 128], BF16)
            yT = [P.sb(f"gdn_yT{i}", [128, 4, 128], BF16) for i in range(2)]
            S32 = P.sb("gdn_S32", [128, 4, 128], F32)
            Sbf = P.sb("gdn_Sbf", [128, 4, 128], BF16)

            load_weights(P, W, lambda k, c0, c1: W[:, k, c0:c1],
                         lambda k, c0, c1: Kx.w_in[l, k * 128:(k + 1) * 128, GDN_BASE + c0:GDN_BASE + c1], 2056, 8)
            P.dma('sp', pp[:], Kx.gdn_pp[l], writes=[pp])
            P.dma('sp', rp[:], Kx.gdn_rp[l], writes=[rp])
            dtb = rp[:, 0:4]
            alog = rp[:, 4:8]
            normw = rp[:, 8:136]
            cw = lambda j, k: pp[:, j * 4 + k: j * 4 + k + 1]
            act(P, identb[:], cst[:, C_I, :], AF.Copy, [cst], [identb])
            act(P, arep[:], alog, AF.Exp, [rp], [arep])
            ts(P, arep[:], arep[:], -1.0, None, ALU.mult, None, [arep], [arep])
            P.op('dve', lambda e: e.memset(S32[:], 0.0), [], [S32])
            P.op('dve', lambda e: e.memset(Sbf[:], 0.0), [], [Sbf])
            P.op('dve', lambda e: e.memset(xin[:], 0.0), [], [xin])
            chk(1)
            for sbi in range(NSB):
                hb = hTb[sbi % 2]
                P.dma('sp', hb[:], Kx.hT.rearrange("k p t -> p k t")[:, :, sbi * SBT:(sbi + 1) * SBT], reads=[Kx.d_hT], writes=[hb])
                banks4 = [D0, D1, C0, C1]
                for j in range(12):
                    bk = banks4[j % 4]
                    for k in range(8):
                        mm(P, bk[:, 0:SBT], W[:, k, j * 128:(j + 1) * 128], hb[:, k, :], k == 0, k == 7, [W, hb], [bk])
                    ac = accs[j]
                    act(P, xin[:, j, 3:3 + SBT], bk[:, 0:SBT], AF.Copy, [bk], [xinj[j]])
                    act(P, ac[:], bk[:, 0:SBT], AF.Copy, [bk, pp], [ac], scale=cw(j, 3))
                for j in range(12):
                    ac = accs[j]
                    for k in (2, 1, 0):
                        stt(P, ac[:], xin[:, j, k:k + SBT], cw(j, k), ac[:], ALU.mult, ALU.add, [xinj[j], pp, ac], [ac])
                for blk in range(NBLK):
                    bk = banks4[blk % 4]
                    for k in range(8):
                        mm(P, bk[:, :], hb[:, k, blk * 128:(blk + 1) * 128], W[:, k, 1536:2048], k == 0, k == 7, [hb, W], [bk])
                    act(P, sgS[:, blk, :], bk[:, :], AF.Silu, [bk], [sgS])
                for j in range(12):
                    ac = accs[j]
                    act(P, xc[:, j, :], ac[:], AF.Silu, [ac], [xcj[j]])
                    act(P, xin[:, j, 0:3], xin[:, j, SBT:SBT + 3], AF.Copy, [xinj[j]], [xinj[j]])
                for j in range(8):
                    act(P, accs[j][:], xc[:, j, :], AF.Square, [xcj[j]], [accs[j]])
                for j in range(8):
                    bk2 = banks4[j % 4]
                    r_ = accs[j]
                    mm(P, bk2[:, 0:SBT], cst[:, C_ONES, :], r_[:], True, True, [cst, r_], [bk2])
                    act(P, r_[:], bk2[:, 0:SBT], AF.Ln, [bk2], [r_], bias=1e-6)
                for j in range(8):
                    r_ = accs[j]
                    act(P, r_[:], r_[:], AF.Exp, [r_], [r_], scale=-0.5,
                        bias=(-0.5 * float(np.log(128.0))) if j < 4 else 0.0)
                    dst = qT if j < 4 else kT
                    tt(P, dst[:, j % 4, :], xc[:, j, :], r_[:], ALU.mult, [xcj[j], r_], [dst])
                chk(2)
                def h1(blk, par):
                    t0 = blk * 128
                    tsl = slice(t0, t0 + 128)
                    sm = smP[par]
                    smv = lambda i: sm[:, i * 4:(i + 1) * 4]
                    Vbf, qkTm, kd, v_tm = VbfP[par], qkTmP[par], kdP[par], vtmP[par]
                    for k in range(8):
                        mm(P, C0[:, 0:8], hb[:, k, tsl], W[:, k, 2048:2056], k == 0, k == 7, [hb, W], [C0])
                    eb, beta, xr, xm, ex, lg, sp, la, eg, gs, ekd, negeg = [smv(i) for i in range(12)]
                    glrep = sm[:, 48:56]
                    act(P, eb, C0[:, 0:4], AF.Exp, [C0], [sm], scale=-1.0)
                    ts(P, eb, eb, 1.0, None, ALU.add, None, [sm], [sm])
                    P.op('dve', lambda e: e.reciprocal(out=beta, in_=eb), [sm], [sm])
                    tt(P, xr, C0[:, 4:8], dtb, ALU.add, [C0, rp], [sm])
                    ts(P, xm, xr, 30.0, None, ALU.min, None, [sm], [sm])
                    yield
                    act(P, ex, xm, AF.Exp, [sm], [sm])
                    act(P, lg, ex, AF.Ln, [sm], [sm], bias=1.0)
                    tt(P, sp, lg, xr, ALU.max, [sm], [sm])
                    tt(P, la, sp, arep[:], ALU.mult, [sm, arep], [sm])
                    yield
                    mm(P, C0[:, 8:12], cst[:, C_LE64, :], la, True, True, [cst, sm], [C0])
                    mm(P, C0[:, 12:16], cst[:, C_SAME64, :], la, True, True, [cst, sm], [C0])
                    mm(P, C0[:, 16:20], cst[:, C_SEL0, :], la, True, True, [cst, sm], [C0])
                    mm(P, C0[:, 20:24], cst[:, C_SEL1, :], la, True, True, [cst, sm], [C0])
                    act(P, eg, C0[:, 8:12], AF.Exp, [C0], [sm])
                    act(P, gs, C0[:, 8:12], AF.Copy, [C0], [sm])
                    act(P, glrep, C0[:, 16:24], AF.Exp, [C0], [sm])
                    tt(P, ekd, C0[:, 12:16], gs, ALU.subtract, [C0, sm], [sm])
                    yield
                    act(P, ekd, ekd, AF.Exp, [sm], [sm])
                    ts(P, negeg, eg, -1.0, None, ALU.mult, None, [sm], [sm])
                    tt(P, R[:], cst[:, C_LE64, :].unsqueeze(1).to_broadcast([128, 4, 128]),
                       la.unsqueeze(2).to_broadcast([128, 4, 128]), ALU.mult, [cst, sm], [R])
                    yield
                    mm(P, D1[:, :], cst[:, C_GT64, :], R[:], True, True, [cst, R], [D1])
                    act(P, Dec[:], b4(D1[:, :]), AF.Exp, [D1], [Dec])
                    tt(P, DecU[:], Dec[:], cst[:, C_LE64, :].unsqueeze(1).to_broadcast([128, 4, 128]), ALU.mult, [Dec, cst], [DecU])
                    yield
                    for h in range(4):
                        mm(P, D0[:, h * 128:(h + 1) * 128], kT[:, h, tsl], kT[:, h, tsl], True, True, [kT], [D0])
                    for h in range(4):
                        mm(P, D1[:, h * 128:(h + 1) * 128], kT[:, h, tsl], qT[:, h, tsl], True, True, [kT, qT], [D1])
                    yield
                    tt(P, qkTm[:], b4(D1[:, :]), DecU[:], ALU.mult, [D1, DecU], [qkTm])
                    tt(P, t1[:], b4(D0[:, :]), DecU[:], ALU.mult, [D0, DecU], [t1])
                    yield
                    X = Ya[0]
                    for h in range(4):
                        stt(P, rr(X[:, h, :]), t1[:, h, :], beta[:, h:h + 1], cst[:, C_LT64, :], ALU.mult, ALU.mult, [t1, sm, cst], [X])
                    yield
                    for h in range(4):
                        tr(P, D0[:, h * 128:(h + 1) * 128], X[:, h, :], cst[:, C_I, :], [X, cst], [D0])
                    act(P, rr(Za[0][:]), b4(D0[:, :]), AF.Copy, [D0], [Za[0]])
                    tt(P, rr(V[:]), cst[:, C_I, :].unsqueeze(1).to_broadcast([128, 4, 128]), X[:], ALU.subtract, [cst, X], [V])
                    yield
                    for lev in range(5):
                        Yc, Zc = Ya[lev % 2], Za[lev % 2]
                        Yn, Zn = Ya[(lev + 1) % 2], Za[(lev + 1) % 2]
                        for h in range(4):
                            mmr(P, D1[:, h * 128:(h + 1) * 128], Yc[:, h, :], Zc[:, h, :], True, True, [Yc, Zc], [D1])
                        act(P, rr(Zn[:]), b4(D1[:, :]), AF.Copy, [D1], [Zn])
                        yield
                        if lev < 4:
                            for h in range(4):
                                mmr(P, D0[:, h * 128:(h + 1) * 128], Zc[:, h, :], Yc[:, h, :], True, True, [Yc, Zc], [D0])
                            act(P, rr(Yn[:]), b4(D0[:, :]), AF.Copy, [D0], [Yn])
                            yield
                        for h in range(4):
                            mmr(P, D1[:, h * 128:(h + 1) * 128], Zn[:, h, :], V[:, h, :], True, True, [Zn, V], [D1])
                        tt(P, rr(V[:]), b4(D1[:, :]), V[:], ALU.add, [D1, V], [V])
                        yield
                    act(P, Vbf[:], V[:], AF.Copy, [V], [Vbf])
                    for h in range(4):
                        tr(P, D0[:, h * 128:(h + 1) * 128], xc[:, 8 + h, tsl], cst[:, C_I, :], [xcj[8 + h], cst], [D0])
                    act(P, v_tm[:], b4(D0[:, :]), AF.Copy, [D0], [v_tm])
                    yield
                    d1b = D1[:, :].bitcast(BF16)
                    for h in range(4):
                        tr(P, d1b[:, h * 128:(h + 1) * 128], kT[:, h, tsl], identb[:], [kT, identb], [D1])
                    tt(P, kd[:], b4(d1b[:, 0:512]), ekd.unsqueeze(2).to_broadcast([128, 4, 128]), ALU.mult, [D1, sm], [kd])
                    yield

                def h2(blk, par):
                    t0 = blk * 128
                    tg = sbi * SBT + t0
                    tsl = slice(t0, t0 + 128)
                    sm = smP[par]
                    smv = lambda i: sm[:, i * 4:(i + 1) * 4]
                    Vbf, qkTm, kd, v_tm = VbfP[par], qkTmP[par], kdP[par], vtmP[par]
                    eb, beta, xr, xm, ex, lg, sp, la, eg, gs, ekd, negeg = [smv(i) for i in range(12)]
                    glrep = sm[:, 48:56]
                    for c in range(2):
                        sl = slice(64 * c, 64 * c + 64)
                        for h in range(4):
                            mm(P, A0[:, h * 128:(h + 1) * 128], kT[:, h, tsl], Sbf[:, h, :], True, True, [kT, Sbf], [A0])
                        for h in range(4):
                            mm(P, B0[:, h * 128:(h + 1) * 128], qT[:, h, tsl], Sbf[:, h, :], True, True, [qT, Sbf], [B0])
                        for h in range(4):
                            stt(P, rhs2[sl, h, :], A0[sl, h * 128:(h + 1) * 128], negeg[sl, h:h + 1], v_tm[sl, h, :],
                                ALU.mult, ALU.add, [A0, sm, v_tm], [rhs2])
                        yield
                        for h in range(4):
                            mm(P, A1[:, h * 128:(h + 1) * 128], Vbf[sl, h, :], rhs2[sl, h, :], True, True, [Vbf, rhs2], [A1])
                        tt(P, vnew[sl], b4(A1[sl, :]), beta[sl].unsqueeze(2).to_broadcast([64, 4, 128]), ALU.mult, [A1, sm], [vnew])
                        yield
                        for h in range(4):
                            mm(P, C1[:, h * 128:(h + 1) * 128], kd[sl, h, :], vnew[sl, h, :], True, True, [kd, vnew], [C1])
                        for h in range(4):
                            mmx(P, B1[:, h * 128:(h + 1) * 128], qkTm[sl, h, :], vnew[sl, h, :], (c == 0 and h == 0), (c == 1),
                                [qkTm, vnew], [B1])
                        for h in range(4):
                            stt(P, S32[:, h, :], S32[:, h, :], glrep[:, c * 4 + h:c * 4 + h + 1], C1[:, h * 128:(h + 1) * 128],
                                ALU.mult, ALU.add, [S32, sm, C1], [S32])
                        act(P, Sbf[:], S32[:], AF.Copy, [S32], [Sbf])
                        yield
                        tt(P, As[sl], b4(B0[sl, :]), eg[sl].unsqueeze(2).to_broadcast([64, 4, 128]), ALU.mult, [B0, sm], [As])
                        yield
                    tt(P, o[:], b4(B1[:, :]), As[:], ALU.add, [B1, As], [o])
                    for h in range(4):
                        P.op('act', lambda e, h=h, sm=sm: e.activation(out=junk[:], in_=o[:, h, :], func=AF.Square,
                                                                      accum_out=sm[:, 56 + h:57 + h]), [o], [junk, sm])
                    yield
                    act(P, sm[:, 60:64], sm[:, 56:60], AF.Ln, [sm], [sm], scale=1.0 / 128, bias=1e-6)
                    act(P, sm[:, 60:64], sm[:, 60:64], AF.Exp, [sm], [sm], scale=-0.5)
                    for h in range(4):
                        stt(P, o[:, h, :], o[:, h, :], sm[:, 60 + h:61 + h], normw, ALU.mult, ALU.mult, [o, sm, rp], [o])
                    yield
                    tt(P, ybf[:], o[:], b4(sgS[:, blk, :]), ALU.mult, [o, sgS], [ybf])
                    c1b = C1[:, :].bitcast(BF16)
                    for h in range(4):
                        tr(P, c1b[:, h * 128:(h + 1) * 128], ybf[:, h, :], identb[:], [ybf, identb], [C1])
                    yt = yT[(sbi * NBLK + blk) % 2]
                    act(P, yt[:], b4(c1b[:, 0:512]), AF.Copy, [C1], [yt])
                    P.dma('sp', Kx.ycT.rearrange("k p t -> p k t")[:, 8:12, tg:tg + 128], yt[:], reads=[yt], writes=[Kx.d_ycT])
                    yield

                for _ in h1(0, 0):
                    pass
                for blk in range(NBLK):
                    g2 = h2(blk, blk % 2)
                    g1 = h1(blk + 1, (blk + 1) % 2) if blk + 1 < NBLK else iter(())
                    d1 = d2 = False
                    while not (d1 and d2):
                        if not d2:
                            try:
                                next(g2)
                            except StopIteration:
                                d2 = True
                        for _r in range(2):
                            if not d1:
                                try:
                                    next(g1)
                                except StopIteration:
                                    d1 = True
        except StopPass:
            pass
        P.flush()


HG_BASE = 4632
HG_NRP = 128


def pass_hg(P, Kx, T, l, labs):
    SBT = min(512, T)
    NSB = T // SBT
    NBLK = SBT // 128
    cst = Kx.cst
    A0, A1, B0, B1, C0, C1, D0, D1 = Kx.bank
    b4 = lambda ap: ap.rearrange("p (h c) -> p h c", h=4)
    with ExitStack() as st:
        P.stack = st
        try:
            W = P.sb("hg_W", [128, 8, 2048], BF16)
            rp = P.sb("hg_rp", [128, HG_NRP], F32)
            lbl = P.sb("hg_lbl", [128, 4, 4], F32)
            lbw = P.sb("hg_lbw", [128, 4 * 6], F32)
            lmk = P.sb("hg_lmk", [128, 4, 4], F32)
            hTb = [P.sb(f"hg_hT{i}", [128, 8, SBT], BF16) for i in range(2)]
            qTf = P.sb("hg_qTf", [128, 4, SBT], F32)
            kTf = P.sb("hg_kTf", [128, 4, SBT], F32)
            lgf = P.sb("hg_lgf", [128, 4, SBT], F32)
            ftmp = [P.sb(f"hg_ft{i}", [128, SBT], F32) for i in range(4)]
            sgS = P.sb("hg_sgS", [128, NBLK, 512], F32)
            ones = P.sb("hg_ones", [128, 128], F32)
            identb = P.sb("hg_identb", [128, 128], BF16)
            Bt = P.sb("hg_Bt", [128, 4, 132], F32)
            D1t = [P.sb(f"hg_D1{i}", [128, 8, 128], F32) for i in range(2)]
            Et = [P.sb(f"hg_E{i}", [128, 8, 128], F32) for i in range(2)]
            kfac = [P.sb(f"hg_kfac{i}", [128, 8, 128], BF16) for i in range(2)]
            Eq = P.sb("hg_Eq", [128, 4, 128], F32)
            EB = P.sb("hg_EB", [128, 4, 128], F32)
            Ek = P.sb("hg_Ek", [128, 4, 128], F32)
            qg = P.sb("hg_qg", [128, 4, 128], BF16)
            qG = P.sb("hg_qG", [128, 4, 128], BF16)
            kdT = P.sb("hg_kdT", [128, 4, 128], BF16)
            kdtm = P.sb("hg_kdtm", [128, 4, 128], BF16)
            scT = P.sb("hg_scT", [128, 4, 128], BF16)
            vbf = P.sb("hg_vbf", [128, 4, 128], BF16)
            sm = P.sb("hg_sm", [128, 16], F32)
            junk = P.sb("hg_junk", [128, 128], F32)
            o = P.sb("hg_o", [128, 4, 128], F32)
            ybf = P.sb("hg_ybf", [128, 4, 128], BF16)
            yT = [P.sb(f"hg_yT{i}", [128, 4, 128], BF16) for i in range(2)]
            S32 = P.sb("hg_S32", [128, 4, 128], F32)
            Sbf = P.sb("hg_Sbf", [128, 4, 128], BF16)

            load_weights(P, W, lambda k, c0, c1: W[:, k, c0:c1],
                         lambda k, c0, c1: Kx.w_in[l, k * 128:(k + 1) * 128, HG_BASE + c0:HG_BASE + c1], 2048, 8)
            P.dma('sp', rp[:], Kx.hg_rp[l], writes=[rp])
            P.dma('sp', lbl[:], Kx.hg_lbl, writes=[lbl])
            normw = rp[:, 0:128]
            act(P, identb[:], cst[:, C_I, :], AF.Copy, [cst], [identb])
            P.op('dve', lambda e: e.memset(ones[:], 1.0), [], [ones])
            P.op('dve', lambda e: e.memset(S32[:], 0.0), [], [S32])
            P.op('dve', lambda e: e.memset(Sbf[:], 0.0), [], [Sbf])
            P.op('dve', lambda e: e.memset(Bt[:], 0.0), [], [Bt])
            mx, sme, rs, lb, oml = [lbw[:, i * 4:(i + 1) * 4] for i in range(5)]
            P.op('dve', lambda e: e.tensor_reduce(out=mx, in_=lbl[:], axis=AX.X, op=ALU.max), [lbl], [lbw])
            tt(P, lbl[:], lbl[:], mx.unsqueeze(2).to_broadcast([128, 4, 4]), ALU.subtract, [lbl, lbw], [lbl])
            act(P, lbl[:], lbl[:], AF.Exp, [lbl], [lbl])
            P.op('dve', lambda e: e.tensor_reduce(out=sme, in_=lbl[:], axis=AX.X, op=ALU.add), [lbl], [lbw])
            P.op('dve', lambda e: e.reciprocal(out=rs, in_=sme), [lbw], [lbw])
            P.dma('sp', lmk[:], Kx.hg_lmask[l], writes=[lmk])
            tt(P, lbl[:], lbl[:], lmk[:], ALU.mult, [lbl, lmk], [lbl])
            P.op('dve', lambda e: e.tensor_reduce(out=lb, in_=lbl[:], axis=AX.X, op=ALU.add), [lbl], [lbw])
            tt(P, lb, lb, rs, ALU.mult, [lbw], [lbw])
            ts(P, oml, lb, -1.0, 1.0, ALU.mult, ALU.add, [lbw], [lbw])
            chk(1)
            for sbi in range(NSB):
                hb = hTb[sbi % 2]
                P.dma('sp', hb[:], Kx.hT.rearrange("k p t -> p k t")[:, :, sbi * SBT:(sbi + 1) * SBT], reads=[Kx.d_hT], writes=[hb])
                banks4 = [D0, D1, C0, C1]
                for j in range(4):
                    bk = banks4[j % 4]
                    for k in range(8):
                        mm(P, bk[:, 0:SBT], W[:, k, j * 128:(j + 1) * 128], hb[:, k, :], k == 0, k == 7, [W, hb], [bk])
                    act(P, qTf[:, j, :], bk[:, 0:SBT], AF.Silu, [bk], [qTf])
                for blk in range(NBLK):
                    bk = banks4[blk % 4]
                    for k in range(8):
                        mm(P, bk[:, :], hb[:, k, blk * 128:(blk + 1) * 128], W[:, k, 1536:2048], k == 0, k == 7, [hb, W], [bk])
                    act(P, sgS[:, blk, :], bk[:, :], AF.Silu, [bk], [sgS])
                for h in range(4):
                    bk = banks4[h % 4]
                    for k in range(8):
                        mm(P, bk[:, 0:SBT], W[:, k, (4 + h) * 128:(5 + h) * 128], hb[:, k, :], k == 0, k == 7, [W, hb], [bk])
                    act(P, ftmp[h][:], bk[:, 0:SBT], AF.Sigmoid, [bk], [ftmp[h]])
                for h in range(4):
                    f_ = ftmp[h]
                    ts(P, f_[:], f_[:], oml[:, h:h + 1], lb[:, h:h + 1], ALU.mult, ALU.add, [f_, lbw], [f_])
                    ts(P, kTf[:, h, :], f_[:], -1.0, 1.0, ALU.mult, ALU.add, [f_], [kTf])
                for h in range(4):
                    act(P, lgf[:, h, :], ftmp[h][:], AF.Ln, [ftmp[h]], [lgf])
                chk(2)
                for blk in range(NBLK):
                    t0 = blk * 128
                    tg = sbi * SBT + t0
                    tsl = slice(t0, t0 + 128)
                    for k in range(8):
                        mm(P, A0[:, :], hb[:, k, tsl], W[:, k, 1024:1536], k == 0, k == 7, [hb, W], [A0])
                    act(P, vbf[:], b4(A0[:, :]), AF.Copy, [A0], [vbf])
                    for h in range(4):
                        i2 = h % 2
                        D1_, E_, kf_ = D1t[i2], Et[i2], kfac[i2]
                        P.op('dve', lambda e, h=h, tsl=tsl: e.tensor_tensor_scan(out=Bt[:, h, 1:129], data0=ones[:, :], data1=lgf[:, h, tsl],
                                                                       initial=0.0, op0=ALU.mult, op1=ALU.add), [ones, lgf], [Bt])
                        for c in range(8):
                            ts(P, D1_[:, c, :], Bt[:, h, 1:129], Bt[:, h, 16 * c:16 * c + 1], -60.0, ALU.subtract, ALU.max, [Bt], [D1_])
                        act(P, E_[:], D1_[:], AF.Exp, [D1_], [E_], scale=-1.0)
                        tt(P, kf_[:], E_[:], kTf[:, h, tsl].unsqueeze(1).to_broadcast([128, 8, 128]), ALU.mult, [E_, kTf], [kf_])
                        base = D1_[:, :, :]
                        dg = bass.AP(base.tensor, base.offset, [list(base.ap[0]), [144, 8], [1, 16]])
                        act(P, Eq[:, h, :].rearrange("p (c j) -> p c j", c=8), dg, AF.Exp, [D1_], [Eq])
                        tt(P, qg[:, h, :], qTf[:, h, tsl], Eq[:, h, :], ALU.mult, [qTf, Eq], [qg])
                        act(P, EB[:, h, :], Bt[:, h, 1:129], AF.Exp, [Bt], [EB])
                        tt(P, qG[:, h, :], qTf[:, h, tsl], EB[:, h, :], ALU.mult, [qTf, EB], [qG])
                        act(P, Ek[:, h, :], Bt[:, h, 1:129], AF.Exp, [Bt], [Ek], scale=-1.0, bias=Bt[:, h, 128:129])
                        tt(P, kdT[:, h, :], kTf[:, h, tsl], Ek[:, h, :], ALU.mult, [kTf, Ek], [kdT])
                        for c in range(8):
                            mm(P, B0[:, h * 128 + 16 * c:h * 128 + 16 * c + 16], kf_[:, c, :], qg[:, h, 16 * c:16 * c + 16], True, True,
                               [kf_, qg], [B0])
                    tt(P, scT[:], b4(B0[:, :]), cst[:, C_LE, :].unsqueeze(1).to_broadcast([128, 4, 128]), ALU.mult, [B0, cst], [scT])
                    chk(3)
                    for h in range(4):
                        mm(P, B1[:, h * 128:(h + 1) * 128], scT[:, h, :], vbf[:, h, :], True, False, [scT, vbf], [B1])
                        mm(P, B1[:, h * 128:(h + 1) * 128], qG[:, h, :], Sbf[:, h, :], False, True, [qG, Sbf], [B1])
                    c0b = C0[:, :].bitcast(BF16)
                    for h in range(4):
                        tr(P, c0b[:, h * 128:(h + 1) * 128], kdT[:, h, :], identb[:], [kdT, identb], [C0])
                    act(P, kdtm[:], b4(c0b[:, 0:512]), AF.Copy, [C0], [kdtm])
                    for h in range(4):
                        mm(P, C1[:, h * 128:(h + 1) * 128], kdtm[:, h, :], vbf[:, h, :], True, True, [kdtm, vbf], [C1])
                    for h in range(4):
                        stt(P, S32[:, h, :], S32[:, h, :], EB[:, h, 127:128], C1[:, h * 128:(h + 1) * 128], ALU.mult, ALU.add,
                            [S32, EB, C1], [S32])
                    act(P, Sbf[:], S32[:], AF.Copy, [S32], [Sbf])
                    chk(4)
                    for h in range(4):
                        P.op('act', lambda e, h=h: e.activation(out=junk[:], in_=B1[:, h * 128:(h + 1) * 128], func=AF.Square,
                                                               accum_out=sm[:, h:h + 1]), [B1], [junk, sm])
                    act(P, sm[:, 4:8], sm[:, 0:4], AF.Ln, [sm], [sm], scale=1.0 / 128, bias=1e-6)
                    act(P, sm[:, 4:8], sm[:, 4:8], AF.Exp, [sm], [sm], scale=-0.5)
                    for h in range(4):
                        stt(P, o[:, h, :], B1[:, h * 128:(h + 1) * 128], sm[:, 4 + h:5 + h], normw, ALU.mult, ALU.mult, [B1, sm, rp], [o])
                    tt(P, ybf[:], o[:], b4(sgS[:, blk, :]), ALU.mult, [o, sgS], [ybf])
                    c1b = C1[:, :].bitcast(BF16)
                    for h in range(4):
                        tr(P, c1b[:, h * 128:(h + 1) * 128], ybf[:, h, :], identb[:], [ybf, identb], [C1])
                    yt = yT[(sbi * NBLK + blk) % 2]
                    act(P, yt[:], b4(c1b[:, 0:512]), AF.Copy, [C1], [yt])
                    P.dma('sp', Kx.ycT.rearrange("k p t -> p k t")[:, 12:16, tg:tg + 128], yt[:], reads=[yt], writes=[Kx.d_ycT])
        except StopPass:
            pass
        P.flush()


OUT_NRP = 1024 + 1024 + 20
NSEL = 16


def layer_norm_block(P, r, stats, mv, sm2, g_ap, b_ap, reads_rp, out_ap, out_buf):
    for hf in range(2):
        P.op('dve', lambda e, hf=hf: e.bn_stats(out=stats[:, hf * 6:(hf + 1) * 6], in_=r[:, hf * 512:(hf + 1) * 512]), [r], [stats])
    P.op('dve', lambda e: e.bn_aggr(out=mv[:, 0:2], in_=stats[:, 0:12]), [stats], [mv])
    act(P, sm2[:, 0:1], mv[:, 1:2], AF.Ln, [mv], [sm2], bias=1e-5)
    act(P, sm2[:, 0:1], sm2[:, 0:1], AF.Exp, [sm2], [sm2], scale=-0.5)
    stt(P, sm2[:, 1:2], mv[:, 0:1], -1.0, sm2[:, 0:1], ALU.mult, ALU.mult, [mv, sm2], [sm2])
    act(P, r[:], r[:], AF.Identity, [r, sm2], [r], scale=sm2[:, 0:1], bias=sm2[:, 1:2])
    tt(P, r[:], r[:], g_ap, ALU.mult, [r] + reads_rp, [r])
    tt(P, out_ap, r[:], b_ap, ALU.add, [r] + reads_rp, [out_buf])


def pass_out(P, Kx, T, l, hsrc):
    cst = Kx.cst
    A0, A1, B0, B1, C0, C1, D0, D1 = Kx.bank
    NB = T // 128
    with ExitStack() as st:
        P.stack = st
        try:
            Wo = P.sb("out_W", [128, 16, 1024], BF16)
            rp = P.sb("out_rp", [128, OUT_NRP], F32)
            wr = P.sb("out_wr", [128, 8, 20], F32)
            ycb = [P.sb(f"out_yc{i}", [128, 16, 128], BF16) for i in range(2)]
            hin = [P.sb(f"out_h{i}", [128, 1024], F32) for i in range(2)]
            r = [P.sb(f"out_r{i}", [128, 1024], F32) for i in range(2)]
            x1 = [P.sb(f"out_x1{i}", [128, 1024], F32) for i in range(2)]
            x1Tf = P.sb("out_x1Tf", [128, 8, 128], F32)
            x1Tb = [P.sb(f"out_x1Tb{i}", [128, 8, 128], BF16) for i in range(2)]
            stats = P.sb("out_stats", [128, 12], F32)
            mv = P.sb("out_mv", [128, 2], F32)
            sm2 = P.sb("out_sm2", [128, 2], F32)
            q = P.sb("out_q", [128, 96], F32)
            comb = [P.sb(f"out_comb{i}", [128, 16], F32) for i in range(2)]
            combT = [P.sb(f"out_combT{i}", [16, 128], F32) for i in range(2)]

            for kc in range(16):
                P.dma('pool', Wo[:, kc, :], Kx.w_out[l, kc * 128:(kc + 1) * 128, :], writes=[Wo.sub(kc)])
            P.dma('sp', rp[:], Kx.out_rp[l], writes=[rp])
            P.dma('sp', wr[:], Kx.wr[l], writes=[wr])
            g1 = rp[:, 0:1024]
            b1 = rp[:, 1024:2048]
            rb = rp[:, 2048:2068]
            chk(1)
            def stage_a(b):
                    tsl = slice(b * 128, (b + 1) * 128)
                    yc, h_, r_, x_ = ycb[b % 2], hin[b % 2], r[b % 2], x1[b % 2]
                    P.dma('sp', yc[:], Kx.ycT.rearrange("k p t -> p k t")[:, :, tsl], reads=[Kx.d_ycT], writes=[yc])
                    P.dma('act', h_[:], hsrc[tsl, :], reads=[Kx.d_hres], writes=[h_])
                    for hf in range(2):
                        bk = [A0, A1][hf]
                        for kc in range(16):
                            mm(P, bk[:, :], yc[:, kc, :], Wo[:, kc, hf * 512:(hf + 1) * 512], kc == 0, kc == 15, [yc, Wo], [bk])
                        stt(P, r_[:, hf * 512:(hf + 1) * 512], h_[:, hf * 512:(hf + 1) * 512], float(DN_ALPHA), bk[:, :], ALU.mult, ALU.add,
                            [h_, bk], [r_])
                    layer_norm_block(P, r_, stats, mv, sm2, g1, b1, [rp], x_[:], x_)
                    P.dma('sp', Kx.x1[tsl, :], x_[:], reads=[x_], writes=[Kx.d_x1])

            def stage_b(b):
                    tsl = slice(b * 128, (b + 1) * 128)
                    x_ = x1[b % 2]
                    for j in range(8):
                        bk = [B0, B1][j // 4]
                        tr(P, bk[:, (j % 4) * 128:(j % 4 + 1) * 128], x_[:, j * 128:(j + 1) * 128], cst[:, C_I, :], [x_, cst], [bk])
                    xb = x1Tb[b % 2]
                    for hf in range(2):
                        bk = [B0, B1][hf]
                        act(P, x1Tf[:, hf * 4:(hf + 1) * 4, :], bk[:, :].rearrange("p (j t) -> p j t", j=4), AF.Copy, [bk], [x1Tf])
                        P.op('dve', lambda e, hf=hf, bk=bk, xb=xb: e.tensor_copy(out=xb[:, hf * 4:(hf + 1) * 4, :], in_=bk[:, :].rearrange("p (j t) -> p j t", j=4)),
                             [bk], [xb])
                    P.dma('sp', Kx.x1T.rearrange("k p t -> p k t")[:, :, tsl], xb[:], reads=[xb], writes=[Kx.d_x1T])
                    for k in range(8):
                        mm(P, C0[:, 0:20], x1Tf[:, k, :], wr[:, k, :], k == 0, k == 7, [x1Tf, wr], [C0])
                    lgs = q[:, 0:20]
                    gm, ngm, gsum, gp, m1, m2, dlt, ed, w1, w2 = [q[:, 20 + i:21 + i] for i in range(10)]
                    ohg = q[:, 32:36]
                    egj = q[:, 36:40]
                    lsel = q[:, 40:44]
                    oh1 = q[:, 44:48]
                    msk = q[:, 48:52]
                    oh2 = q[:, 52:56]
                    wsel = q[:, 56:60]
                    tmp16 = q[:, 64:80]
                    tt(P, lgs, C0[:, 0:20], rb, ALU.add, [C0, rp], [q])
                    P.op('dve', lambda e: e.tensor_reduce(out=gm, in_=lgs[:, 0:4], axis=AX.X, op=ALU.max), [q], [q])
                    ts(P, ohg, lgs[:, 0:4], gm, None, ALU.is_equal, None, [q], [q])
                    ts(P, ngm, gm, -1.0, None, ALU.mult, None, [q], [q])
                    P.op('act', lambda e: e.activation(out=egj, in_=lgs[:, 0:4], func=AF.Exp, bias=ngm, accum_out=gsum), [q], [q])
                    P.op('dve', lambda e: e.reciprocal(out=gp, in_=gsum), [q], [q])
                    tt(P, tmp16.rearrange("p (g e) -> p g e", g=4), lgs[:, 4:20].rearrange("p (g e) -> p g e", g=4),
                       ohg.unsqueeze(2).to_broadcast([128, 4, 4]), ALU.mult, [q], [q])
                    P.op('dve', lambda e: e.tensor_reduce(out=lsel, in_=tmp16.rearrange("p (g e) -> p e g", g=4), axis=AX.X, op=ALU.add), [q], [q])
                    P.op('dve', lambda e: e.tensor_reduce(out=m1, in_=lsel, axis=AX.X, op=ALU.max), [q], [q])
                    ts(P, oh1, lsel, m1, None, ALU.is_equal, None, [q], [q])
                    stt(P, msk, oh1, -1e30, lsel, ALU.mult, ALU.add, [q], [q])
                    P.op('dve', lambda e: e.tensor_reduce(out=m2, in_=msk, axis=AX.X, op=ALU.max), [q], [q])
                    ts(P, oh2, msk, m2, None, ALU.is_equal, None, [q], [q])
                    tt(P, dlt, m2, m1, ALU.subtract, [q], [q])
                    act(P, ed, dlt, AF.Exp, [q], [q])
                    ts(P, w1, ed, 1.0, None, ALU.add, None, [q], [q])
                    P.op('dve', lambda e: e.reciprocal(out=w1, in_=w1), [q], [q])
                    tt(P, w2, ed, w1, ALU.mult, [q], [q])
                    tt(P, w1, w1, gp, ALU.mult, [q], [q])
                    tt(P, w2, w2, gp, ALU.mult, [q], [q])
                    ts(P, wsel, oh1, w1, None, ALU.mult, None, [q], [q])
                    stt(P, wsel, oh2, w2, wsel, ALU.mult, ALU.add, [q], [q])
                    cb_ = comb[b % 2]
                    tt(P, cb_[:].rearrange("p (g e) -> p g e", g=4), ohg.unsqueeze(2).to_broadcast([128, 4, 4]),
                       wsel.unsqueeze(1).to_broadcast([128, 4, 4]), ALU.mult, [q], [cb_])
                    P.dma('sp', Kx.comb[tsl, :], cb_[:], reads=[cb_], writes=[Kx.d_comb])

            stage_a(0)
            for b in range(NB):
                if b + 1 < NB:
                    stage_a(b + 1)
                stage_b(b)
        except StopPass:
            pass
        P.flush()


def pass_moe(P, Kx, T, l, dst, write_hT):
    cst = Kx.cst
    A0, A1, B0, B1, C0, C1, D0, D1 = Kx.bank
    ST = min(1024, T)
    NST = T // ST
    NBS = ST // 128
    with ExitStack() as st:
        P.stack = st
        try:
            Wgu = [P.sb(f"moe_Wgu{i}", [128, 8, 512], BF16) for i in range(8)]
            Wdn = [P.sb(f"moe_Wdn{i}", [128, 2, 1024], BF16) for i in range(8)]
            for w_ in Wgu:
                for k in range(8):
                    w_.sub(k)
            for w_ in Wdn:
                for k in range(2):
                    w_.sub(k)
            rp = P.sb("moe_rp", [128, 2048], F32)
            xT = P.sb("moe_xT", [128, 8, ST], BF16)
            cmb = P.sb("moe_cmb", [128, NBS, 16], F32)
            yacc = P.sb("moe_yacc", [128, NBS, 1024], F32)
            yaccb = [yacc.sub(i) for i in range(NBS)]
            sgb = [P.sb(f"moe_sg{i}", [128, 256], F32) for i in range(3)]
            hb_ = [P.sb(f"moe_h{i}", [128, 256], BF16) for i in range(3)]
            hT = [P.sb(f"moe_hT{i}", [128, 2, 128], BF16) for i in range(3)]
            identb = P.sb("moe_identb", [128, 128], BF16)
            x1b = [P.sb(f"moe_x1{i}", [128, 1024], F32) for i in range(2)]
            ob = [P.sb(f"moe_o{i}", [128, 1024], F32) for i in range(2)]
            oT = [P.sb(f"moe_oT{i}", [128, 8, 128], BF16) for i in range(2)]
            stats = P.sb("moe_stats", [128, 12], F32)
            mv = P.sb("moe_mv", [128, 2], F32)
            sm2 = P.sb("moe_sm2", [128, 2], F32)
            P.dma('sp', rp[:], Kx.moe_rp[l], writes=[rp])
            g2 = rp[:, 0:1024]
            b2 = rp[:, 1024:2048]
            act(P, identb[:], cst[:, C_I, :], AF.Copy, [cst], [identb])
            it = 0
            for sti in range(NST):
                s0 = sti * ST
                P.dma('sp', xT[:], Kx.x1T.rearrange("k p t -> p k t")[:, :, s0:s0 + ST], reads=[Kx.d_x1T], writes=[xT])
                P.dma('sp', cmb[:], Kx.comb[s0:s0 + ST, :].rearrange("(b p) e -> p b e", p=128), reads=[Kx.d_comb], writes=[cmb])
                for G in range(4):
                    slot0 = ((sti * 4 + G) % 2) * 4
                    for e4 in range(4):
                        e = G * 4 + e4
                        wg, wd = Wgu[slot0 + e4], Wdn[slot0 + e4]
                        for k in range(8):
                            P.dma('pool', wg[:, k, :], Kx.w_gu[l, e, k * 128:(k + 1) * 128, :], writes=[wg.children[k]])
                        for fc in range(2):
                            P.dma('pool', wd[:, fc, :], Kx.w_dn[l, e, fc * 128:(fc + 1) * 128, :], writes=[wd.children[fc]])
                    items = [(blk, e4) for blk in range(NBS) for e4 in range(4)]
                    NB3 = 3

                    def stage_a(i):
                        blk, e4 = items[i]
                        e = G * 4 + e4
                        tsl = slice(blk * 128, (blk + 1) * 128)
                        wg = Wgu[slot0 + e4]
                        gb = [C0, C1][i % 2]
                        sg_, h_ = sgb[i % NB3], hb_[i % NB3]
                        for k in range(8):
                            mm(P, gb[:, :], xT[:, k, tsl], wg[:, k, :], k == 0, k == 7, [xT, wg], [gb])
                        act(P, sg_[:], gb[:, 0:256], AF.Silu, [gb], [sg_])
                        stt(P, h_[:], gb[:, 256:512], cmb[:, blk, e:e + 1], sg_[:], ALU.mult, ALU.mult, [gb, cmb, sg_], [h_])

                    def stage_b(i):
                        tb = [D0, D1][i % 2]
                        h_, hT_ = hb_[i % NB3], hT[i % NB3]
                        tbb = tb[:, :].bitcast(BF16)
                        for fc in range(2):
                            tr(P, tbb[:, fc * 128:(fc + 1) * 128], h_[:, fc * 128:(fc + 1) * 128], identb[:], [h_, identb], [tb])
                        act(P, hT_[:], tbb[:, 0:256].rearrange("p (f t) -> p f t", f=2), AF.Copy, [tb], [hT_])

                    def stage_c(i):
                        blk, e4 = items[i]
                        wd = Wdn[slot0 + e4]
                        hT_ = hT[i % NB3]
                        ybk = [A0, A1] if blk % 2 == 0 else [B0, B1]
                        for hf in range(2):
                            for fc in range(2):
                                first = (e4 == 0 and fc == 0)
                                last = (e4 == 3 and fc == 1)
                                mm(P, ybk[hf][:, :], hT_[:, fc, :], wd[:, fc, hf * 512:(hf + 1) * 512], first, last, [hT_, wd], [ybk[hf]])
                        if e4 == 3:
                            for hf in range(2):
                                ya = yacc[:, blk, hf * 512:(hf + 1) * 512]
                                if G == 0:
                                    act(P, ya, ybk[hf][:, :], AF.Copy, [ybk[hf]], [yaccb[blk]])
                                else:
                                    tt(P, ya, ybk[hf][:, :], ya, ALU.add, [ybk[hf], yaccb[blk]], [yaccb[blk]])

                    n_it = len(items)
                    for step in range(n_it + 2):
                        if step < n_it:
                            stage_a(step)
                        if 0 <= step - 1 < n_it:
                            stage_b(step - 1)
                        if 0 <= step - 2 < n_it:
                            stage_c(step - 2)
                for blk in range(NBS):
                    tg = s0 + blk * 128
                    x_, o_ = x1b[blk % 2], ob[blk % 2]
                    P.dma('act', x_[:], Kx.x1[tg:tg + 128, :], reads=[Kx.d_x1], writes=[x_])
                    stt(P, x_[:], x_[:], float(DN_ALPHA), yacc[:, blk, :], ALU.mult, ALU.add, [x_, yaccb[blk]], [x_])
                    layer_norm_block(P, x_, stats, mv, sm2, g2, b2, [rp], o_[:], o_)
                    P.dma('sp', dst[tg:tg + 128, :], o_[:], reads=[o_], writes=[Kx.d_hres])
                    if write_hT:
                        c0b = C0[:, :].bitcast(BF16)
                        ot = oT[blk % 2]
                        for j in range(8):
                            bk = [C0, C1][j // 4]
                            tr(P, bk[:, (j % 4) * 128:(j % 4 + 1) * 128], o_[:, j * 128:(j + 1) * 128], cst[:, C_I, :], [o_, cst], [bk])
                        for hf in range(2):
                            act(P, ot[:, hf * 4:(hf + 1) * 4, :], [C0, C1][hf][:, :].rearrange("p (j t) -> p j t", j=4), AF.Copy,
                                [[C0, C1][hf]], [ot])
                        P.dma('sp', Kx.hT.rearrange("k p t -> p k t")[:, :, tg:tg + 128], ot[:], reads=[ot], writes=[Kx.d_hT])
        except StopPass:
            pass
        P.flush()


def build(T, NL, dbg=False, passes=("ssd", "gdn", "hg", "out", "moe"), layers=None):
    layers = list(range(NL)) if layers is None else layers
    nc = bass.Bass("TRN2", target_bir_lowering=False)
    Kx = K()
    ext_in = lambda name, shape: nc.dram_tensor(name, list(shape), F32, kind="ExternalInput").ap()
    Kx.x = ext_in("x", [T, D_MODEL])
    Kx.w_in = ext_in("w_in", [NL, D_MODEL, IN_COLS])
    Kx.cst_d = ext_in("cst", [128, NCONST, 128])
    Kx.ssd_pp = ext_in("ssd_pp", [NL, 128, SSD_NPP])
    Kx.ssd_rp = ext_in("ssd_rp", [NL, 128, SSD_NRP])
    Kx.gdn_pp = ext_in("gdn_pp", [NL, 128, GDN_NPP])
    Kx.gdn_rp = ext_in("gdn_rp", [NL, 128, GDN_NRP])
    Kx.hg_rp = ext_in("hg_rp", [NL, 128, HG_NRP])
    Kx.hg_lbl = ext_in("hg_lbl", [128, 4, 4])
    Kx.hg_lmask = ext_in("hg_lmask", [NL, 128, 4, 4])
    Kx.w_out = ext_in("w_out", [NL, 2048, 1024])
    Kx.out_rp = ext_in("out_rp", [NL, 128, OUT_NRP])
    Kx.wr = ext_in("wr", [NL, 128, 8, 20])
    Kx.moe_rp = ext_in("moe_rp", [NL, 128, 2048])
    Kx.w_gu = ext_in("w_gu", [NL, 16, 1024, 512])
    Kx.w_dn = ext_in("w_dn", [NL, 16, 256, 1024])
    Kx.out = nc.dram_tensor("out", [T, D_MODEL], F32, kind="ExternalOutput").ap()
    skind = "ExternalOutput" if dbg else "Internal"
    Kx.hT = nc.dram_tensor("hT", [8, 128, T], BF16, kind=skind).ap()
    Kx.ycT = nc.dram_tensor("ycT", [16, 128, T], BF16, kind=skind).ap()
    Kx.x1 = nc.dram_tensor("x1", [T, D_MODEL], F32, kind=skind).ap()
    Kx.x1T = nc.dram_tensor("x1T", [8, 128, T], BF16, kind=skind).ap()
    Kx.comb = nc.dram_tensor("comb", [T, 16], F32, kind=skind).ap()
    Kx.hres = nc.dram_tensor("hres", [T, D_MODEL], F32, kind="Internal").ap()
    Kx.d_hT = Buf("d_hT")
    Kx.d_ycT = Buf("d_ycT")
    Kx.d_x1 = Buf("d_x1")
    Kx.d_x1T = Buf("d_x1T")
    Kx.d_comb = Buf("d_comb")
    Kx.d_hres = Buf("d_hres")
    with ExitStack() as st0:
        P = Prog(nc, st0)
        Kx.bank = []
        for i in range(8):
            t = st0.enter_context(nc.psum_tensor(f"bank{i}", [128, 512], F32))
            Kx.bank.append(Buf(f"bank{i}", t))
            Kx.bank[-1].excl = True
        Kx.cst = P.sb("cst_sb", [128, NCONST, 128], F32)
        P.dma('sp', Kx.cst[:], Kx.cst_d, writes=[Kx.cst])
        for l in range(NL):
            if l == 0:
                pass_transpose_in(P, Kx, T, Kx.x, Kx.hT)
            if "ssd" in passes:
                pass_ssd(P, Kx, T, l)
            if "gdn" in passes:
                pass_gdn(P, Kx, T, l)
            if "hg" in passes:
                pass_hg(P, Kx, T, l, layers[l])
            if "out" in passes:
                pass_out(P, Kx, T, l, Kx.x if l == 0 else Kx.hres)
            if "moe" in passes:
                pass_moe(P, Kx, T, l, Kx.out if l == NL - 1 else Kx.hres, l < NL - 1)
        print("recorded ops", P.nops, "waits", P.nwaits)
    return nc


def host_params(inp, layers):
    out = {}
    NL = len(layers)
    pp = np.zeros((NL, 128, SSD_NPP), np.float32)
    rp = np.zeros((NL, 128, SSD_NRP), np.float32)
    for i, l in enumerate(layers):
        cw = inp['ssd_conv_w'][l]
        pp[i, :, 0:48] = cw.reshape(4, 12, 128).transpose(2, 1, 0).reshape(128, 48)
        pp[i, :, 48:60] = inp['ssd_conv_b'][l].reshape(12, 128).T
        rp[i, :, 0:16] = inp['ssd_dt_bias'][l][None, :]
        rp[i, :, 16:32] = inp['ssd_a_log'][l][None, :]
        rp[i, :, 32:48] = inp['ssd_d'][l][None, :]
        rp[i, :, 48:48 + 1024] = inp['ssd_norm_w'][l][None, :]
    out['ssd_pp'] = pp
    out['ssd_rp'] = rp
    gpp = np.zeros((NL, 128, GDN_NPP), np.float32)
    grp = np.zeros((NL, 128, GDN_NRP), np.float32)
    for i, l in enumerate(layers):
        gpp[i, :, 0:48] = inp['gdn_conv_w'][l].reshape(4, 12, 128).transpose(2, 1, 0).reshape(128, 48)
        grp[i, :, 0:4] = inp['gdn_dt_bias'][l][None, :]
        grp[i, :, 4:8] = inp['gdn_a_log'][l][None, :]
        grp[i, :, 8:136] = inp['gdn_norm_w'][l][None, :]
    out['gdn_pp'] = gpp
    out['gdn_rp'] = grp
    hrp = np.zeros((NL, 128, HG_NRP), np.float32)
    for i, l in enumerate(layers):
        hrp[i, :, 0:128] = inp['hg_norm_w'][l][None, :]
    out['hg_rp'] = hrp
    lmask = np.zeros((NL, 128, 4, 4), np.float32)
    for i, l in enumerate(layers):
        lmask[i, :, :, 1:l + 1] = 1.0
    out['hg_lmask'] = lmask
    out['hg_lbl'] = np.ascontiguousarray(inp['hg_lb_logits'].reshape(4, 4, 128).transpose(2, 1, 0))
    orp = np.zeros((NL, 128, OUT_NRP), np.float32)
    wr = np.zeros((NL, 128, 8, 20), np.float32)
    mrp = np.zeros((NL, 128, 2048), np.float32)
    for i, l in enumerate(layers):
        orp[i, :, 0:1024] = inp['ln1_g'][l][None, :]
        orp[i, :, 1024:2048] = inp['ln1_b'][l][None, :]
        orp[i, :, 2048:2052] = inp['b_router_group'][l][None, :]
        orp[i, :, 2052:2068] = inp['b_router_expert'][l][None, :]
        wcat = np.concatenate([inp['w_router_group'][l], inp['w_router_expert'][l]], axis=1)
        wr[i] = wcat.reshape(8, 128, 20).transpose(1, 0, 2)
        mrp[i, :, 0:1024] = inp['ln2_g'][l][None, :]
        mrp[i, :, 1024:2048] = inp['ln2_b'][l][None, :]
    out['out_rp'] = orp
    out['wr'] = wr
    out['moe_rp'] = mrp
    out['cst'] = make_consts()
    return out


def core_inputs(inp, b, T, layers):
    hp = host_params(inp, layers)
    ls = layers
    im = {
        "x": np.ascontiguousarray(inp['x'][b, :T]),
        "w_in": np.ascontiguousarray(inp['w_in'][ls]),
        "w_out": np.ascontiguousarray(inp['w_out'][ls]),
        "w_gu": np.ascontiguousarray(inp['w_expert_gate_up'][ls]),
        "w_dn": np.ascontiguousarray(inp['w_expert_down'][ls]),
    }
    im.update(hp)
    return im


_PROG = {}


def kernel(**inputs):
    inp = {k: np.asarray(v) for k, v in inputs.items()}
    B, T, _ = inp['x'].shape
    ncores = 8
    nc = build(T, DEPTH)
    base = core_inputs(inp, 0, T, list(range(DEPTH)))
    in_maps = []
    for c in range(ncores):
        m = dict(base)
        m["x"] = np.ascontiguousarray(inp['x'][c % B], dtype=np.float32)
        in_maps.append(m)
    res = run_bass_kernel_spmd(nc, in_maps, core_ids=list(range(ncores)))
    return np.stack([np.asarray(res.results[b]["out"]) for b in range(B)]).astype(np.float32)
```

```python
import numpy as np
from contextlib import ExitStack
import concourse.bass as bass
import concourse.mybir as mybir
from concourse.bass_utils import run_bass_kernel_spmd

F32 = mybir.dt.float32
BF16 = mybir.dt.bfloat16
F32R = mybir.dt.float32r
USE_F32R = True
AF = mybir.ActivationFunctionType
ALU = mybir.AluOpType
AX = mybir.AxisListType

COMPUTE = ('pe', 'dve', 'act', 'pool')
ALLENG = ('pe', 'dve', 'act', 'pool', 'sp')
QUEUES = ('sp', 'act', 'pool')
NDMA = 8

D_MODEL = 1024
IN_COLS = 6680
DEPTH = 4
DN_ALPHA = (2 * DEPTH) ** 0.25


class Buf:
    def __init__(self, name, t=None, parent=None):
        self.name = name
        self.t = t
        self.parent = parent
        self.children = []
        self.last_write = None
        self.reads = []
        self.excl = False

    def sub(self, key):
        c = Buf(f"{self.name}.{key}", self.t, self)
        self.children.append(c)
        return c

    def __getitem__(self, k):
        return self.t[k]


class Op:
    __slots__ = ('id', 'eng', 'dma', 'fn', 'deps', 'dur', 'tag', 'out', 'in_', 'kw', 'pos', 'tok', 'fin')

    def __init__(self, id, eng, dma, fn, deps, dur, tag):
        self.id = id
        self.eng = eng
        self.dma = dma
        self.fn = fn
        self.deps = deps
        self.dur = dur
        self.tag = tag
        self.tok = None
        self.fin = 0.0


SCHED_WINDOW = 64
ACT_SWITCH_US = 1.3
XLAT = 0.25


class Prog:
    def __init__(self, nc, stack):
        self.nc = nc
        self.stack = stack
        self.sem = {e: stack.enter_context(nc.semaphore(f"s_{e}")) for e in COMPUTE}
        self.cnt = {e: 0 for e in COMPUTE}
        self.known = {e: {} for e in ALLENG}
        self.dsem = {q: [stack.enter_context(nc.semaphore(f"d_{q}_{i}")) for i in range(NDMA)] for q in QUEUES}
        self.dcnt = {q: [0] * NDMA for q in QUEUES}
        self.dnext = {q: 0 for q in QUEUES}
        self.semobj = {}
        for e in COMPUTE:
            self.semobj[('c', e)] = self.sem[e]
        for q in QUEUES:
            for i in range(NDMA):
                self.semobj[('d', q, i)] = self.dsem[q][i]
        self.nops = 0
        self.nwaits = 0
        self.pend = []
        self.base = 0
        self.nid = 0
        self.nalloc = 0

    def sb(self, name, shape, dtype=F32):
        self.nalloc += 1
        t = self.stack.enter_context(self.nc.sbuf_tensor(f"sb{self.nalloc}_{name}", list(shape), dtype))
        return Buf(name, t)

    def _collect(self, b, write):
        ids = []

        def add(x):
            if x.last_write is not None:
                ids.append(x.last_write)
            if write or x.excl:
                ids.extend(x.reads)
        add(b)
        p = b.parent
        while p is not None:
            add(p)
            p = p.parent

        def rec(x):
            for c in x.children:
                add(c)
                rec(c)
        rec(b)
        return ids

    def _mkdeps(self, eng, is_dma, reads, writes):
        deps = {}
        byid = self.pend_by_id
        for b in reads:
            wr = self._writers_of(b)
            for d in self._collect(b, False):
                if d < self.base:
                    continue
                D = byid[d]
                same = (D.eng == eng and not D.dma and not is_dma)
                if same and d not in wr:
                    deps[d] = deps.get(d, False)
                else:
                    deps[d] = True
        for b in writes:
            for d in self._collect(b, True):
                if d < self.base:
                    continue
                D = byid[d]
                same = (D.eng == eng and not D.dma and not is_dma)
                if same:
                    deps[d] = deps.get(d, False)
                else:
                    deps[d] = True
        return deps

    def _writers_of(self, b):
        w = set()

        def add(x):
            if x.last_write is not None:
                w.add(x.last_write)
        add(b)
        p = b.parent
        while p is not None:
            add(p)
            p = p.parent

        def rec(x):
            for c in x.children:
                add(c)
                rec(c)
        rec(b)
        return w

    @property
    def pend_by_id(self):
        return self._byid

    def _record(self, oid, reads, writes):
        for b in reads:
            b.reads.append(oid)
        for b in writes:
            b.last_write = oid
            b.reads = []

            def rec(x):
                for c in x.children:
                    c.last_write = None
                    c.reads = []
                    rec(c)
            rec(b)

    def _new(self, eng, dma, fn, reads, writes, dur, tag):
        if not hasattr(self, '_byid'):
            self._byid = {}
        deps = self._mkdeps(eng, dma, reads, writes)
        o = Op(self.nid, eng, dma, fn, deps, dur, tag)
        self.nid += 1
        self.pend.append(o)
        self._byid[o.id] = o
        self._record(o.id, reads, writes)
        self.nops += 1
        return o

    def op(self, eng, fn, reads=(), writes=(), dur=0.3, tag=None):
        return self._new(eng, False, fn, reads, writes, dur, tag)

    def dma(self, q, out, in_, reads=(), writes=(), dur=3.0, **kw):
        o = self._new(q, True, None, reads, writes, dur, None)
        o.out, o.in_, o.kw = out, in_, kw
        return o

    def _schedule(self):
        ops = self.pend
        if not ops:
            return {e: [] for e in ALLENG}
        queues = {e: [] for e in ALLENG}
        for o in ops:
            queues[o.eng].append(o)
        head = {e: 0 for e in ALLENG}
        done = set()
        free = {e: 0.0 for e in ALLENG}
        cur_tag = None
        order = {e: [] for e in ALLENG}
        scheduled = {}
        remaining = len(ops)
        base = self.base
        taken = {e: set() for e in ALLENG}
        while remaining:
            best = None
            for e in ALLENG:
                q = queues[e]
                n = 0
                i = head[e]
                while i < len(q) and n < SCHED_WINDOW:
                    o = q[i]
                    i += 1
                    if o.id in done:
                        continue
                    n += 1
                    ok = True
                    rdy = 0.0
                    for d, w in o.deps.items():
                        if d < base:
                            continue
                        if d not in done:
                            ok = False
                            break
                        f = scheduled[d]
                        rdy = max(rdy, f + (XLAT if w else 0.0))
                    if not ok:
                        continue
                    st = max(rdy, free[e])
                    if e == 'act' and not o.dma and o.tag is not None and cur_tag is not None and o.tag != cur_tag:
                        st += ACT_SWITCH_US
                    key = (st, o.id)
                    if best is None or key < best[0]:
                        best = (key, e, o)
            assert best is not None, "scheduler deadlock (cyclic deps?)"
            (st, _), e, o = best
            if o.dma:
                free[e] = st + 0.15
                fin = st + o.dur
            else:
                free[e] = st + o.dur
                fin = st + o.dur
                if e == 'act' and o.tag is not None:
                    cur_tag = o.tag
            scheduled[o.id] = fin
            done.add(o.id)
            order[e].append(o)
            remaining -= 1
            q = queues[e]
            while head[e] < len(q) and q[head[e]].id in done:
                head[e] += 1
        self.est_us = max(scheduled.values()) if scheduled else 0.0
        return order

    def _wait(self, lst, eng, tok):
        key, val = tok
        if self.known[eng].get(key, 0) >= val:
            return
        self.known[eng][key] = val
        sem = self.semobj[key]
        lst.append(lambda e, sem=sem, val=val: e.wait_ge(sem, val))
        self.nwaits += 1

    def flush(self):
        order = self._schedule()
        for e in ALLENG:
            for o in order[e]:
                if o.dma:
                    i = self.dnext[e]
                    self.dnext[e] = (i + 1) % NDMA
                    o.pos = (i, self.dcnt[e][i])
                    self.dcnt[e][i] += 16
                    o.tok = (('d', e, i), self.dcnt[e][i])
                else:
                    self.cnt[e] += 1
                    o.tok = (('c', e), self.cnt[e])
        byid = self._byid
        lists = {e: [] for e in ALLENG}
        for e in ALLENG:
            lst = lists[e]
            for o in order[e]:
                for d, w in o.deps.items():
                    if d < self.base or not w:
                        continue
                    self._wait(lst, e, byid[d].tok)
                if o.dma:
                    i, prev = o.pos
                    if prev > 0:
                        self._wait(lst, e, (('d', e, i), prev))
                    sem = self.dsem[e][i]
                    lst.append(lambda en, o=o, sem=sem: en.dma_start(out=o.out, in_=o.in_, **o.kw).then_inc(sem, 16))
                else:
                    sem = self.sem[e]
                    lst.append(lambda en, o=o, sem=sem: o.fn(en).then_inc(sem, 1))
        for e in ALLENG:
            for c in COMPUTE:
                if self.cnt[c] > 0 and c != e:
                    self._wait(lists[e], e, (('c', c), self.cnt[c]))
            for q in QUEUES:
                for i in range(NDMA):
                    if self.dcnt[q][i] > 0:
                        self._wait(lists[e], e, (('d', q, i), self.dcnt[q][i]))
        self.pend = []
        self.base = self.nid
        self._byid = {}
        nc = self.nc
        with nc.Block() as block:
            @block.sync
            def _(e):
                for f in lists['sp']:
                    f(e)

            @block.tensor
            def _(e):
                for f in lists['pe']:
                    f(e)

            @block.vector
            def _(e):
                for f in lists['dve']:
                    f(e)

            @block.scalar
            def _(e):
                for f in lists['act']:
                    f(e)

            @block.gpsimd
            def _(e):
                for f in lists['pool']:
                    f(e)


C_I, C_LE, C_GT, C_ONES, C_LE64, C_GT64, C_LT64, C_SAME64, C_SEL0, C_SEL1 = range(10)
NCONST = 10


def make_consts():
    k = np.arange(128)[:, None]
    l = np.arange(128)[None, :]
    c = np.zeros((128, NCONST, 128), np.float32)
    c[:, C_I] = (k == l)
    c[:, C_LE] = (k <= l)
    c[:, C_GT] = (k > l)
    c[:, C_ONES] = 1.0
    same64 = (k // 64) == (l // 64)
    c[:, C_LE64] = (k <= l) & same64
    c[:, C_GT64] = (k > l) & same64
    c[:, C_LT64] = (k < l) & same64
    c[:, C_SAME64] = same64
    c[:, C_SEL0] = (k < 64) & (l >= 0)
    c[:, C_SEL1] = (k >= 64) & (l >= 0)
    return c


class K:
    pass


STAGE = 99


class StopPass(Exception):
    pass


def chk(n):
    if STAGE == n:
        raise StopPass()


def _fsz(ap):
    n = 1
    for d in ap.shape[1:]:
        n *= d
    return n


def _is32(ap):
    return ap.dtype == F32


ACT_TAG = {}


def _act_tag(func):
    if func == AF.Silu:
        return 'silu'
    if func == AF.Sigmoid:
        return 'sig'
    if func in (AF.Exp, AF.Ln):
        return 'lnexp'
    return None


def mm(P, out, lhsT, rhs, start, stop, reads, writes):
    passes = 4 if _is32(rhs) else 1
    dur = passes * max(_fsz(rhs), 64) / 2400.0 + passes * max(_fsz(lhsT), 32) / 2400.0 * 0.5 + 0.02
    return P.op('pe', lambda e: e.matmul(out, lhsT=lhsT, rhs=rhs, start=start, stop=stop), reads, writes, dur=dur)


def mmx(P, out, lhsT, rhs, start, stop, reads, writes):
    dur = max(_fsz(rhs), 64) / 2400.0 + max(_fsz(lhsT), 32) / 4800.0 + 0.02
    return P.op('pe', lambda e: e.matmul(out, lhsT=lhsT, rhs=rhs, start=start, stop=stop, skip_group_check=True), reads, writes, dur=dur)


def mmr(P, out, lhsT, rhs, start, stop, reads, writes):
    passes = 4
    if USE_F32R:
        lhsT = lhsT.bitcast(F32R)
        rhs = rhs.bitcast(F32R)
        passes = 1
    dur = passes * (max(_fsz(rhs), 64) / 2400.0 + max(_fsz(lhsT), 32) / 4800.0) + 0.02
    return P.op('pe', lambda e: e.matmul(out, lhsT=lhsT, rhs=rhs, start=start, stop=stop), reads, writes, dur=dur)


def rr(ap):
    return ap.bitcast(F32R) if USE_F32R else ap


def tr(P, out, in_, ident, reads, writes):
    passes = 4 if _is32(in_) else 1
    dur = passes * (max(_fsz(in_), 64) / 2400.0) * 1.5 + 0.02
    return P.op('pe', lambda e: e.transpose(out, in_, ident), reads, writes, dur=dur)


def act(P, out, in_, func, reads, writes, **kw):
    dur = _fsz(in_) / 1400.0 + 0.2 + (0.1 if ('scale' in kw and not isinstance(kw['scale'], float)) else 0.0)
    return P.op('act', lambda e: e.activation(out=out, in_=in_, func=func, **kw), reads, writes, dur=dur, tag=_act_tag(func))


def tt(P, out, in0, in1, op, reads, writes):
    dur = _fsz(out) / 960.0 + 0.12
    return P.op('dve', lambda e: e.tensor_tensor(out=out, in0=in0, in1=in1, op=op), reads, writes, dur=dur)


def ts(P, out, in0, s1, s2, op0, op1, reads, writes, **kw):
    dur = _fsz(out) / 1400.0 + 0.12
    if op1 is None:
        return P.op('dve', lambda e: e.tensor_scalar(out=out, in0=in0, scalar1=s1, scalar2=None, op0=op0, **kw), reads, writes, dur=dur)
    return P.op('dve', lambda e: e.tensor_scalar(out=out, in0=in0, scalar1=s1, scalar2=s2, op0=op0, op1=op1, **kw), reads, writes, dur=dur)


def stt(P, out, in0, scalar, in1, op0, op1, reads, writes):
    dur = _fsz(out) / 960.0 + 0.12
    return P.op('dve', lambda e: e.scalar_tensor_tensor(out=out, in0=in0, scalar=scalar, in1=in1, op0=op0, op1=op1), reads, writes, dur=dur)


def load_weights(P, dst, src2d, ncols, nk, order=None):
    nb = (ncols + 511) // 512
    if not dst.children:
        for i in range(nb):
            dst.sub(i)
    order = list(range(nb)) if order is None else order
    src3 = src2d.rearrange("(k p) c -> p k c", p=128)
    for i in order:
        c0, c1 = i * 512, min(ncols, (i + 1) * 512)
        P.dma('pool', dst[:, :, c0:c1], src3[:, :, c0:c1], writes=[dst.children[i]])


def wsub(W, col):
    return W.children[col // 512]


SSD_NPP = 12 * 4 + 12
SSD_NRP = 16 + 16 + 16 + 1024


def pass_transpose_in(P, Kx, T, src, dstT):
    nc = P.nc
    with ExitStack() as st:
        P.stack = st
        xt = [P.sb(f"p0_x{i}", [128, 1024], F32) for i in range(2)]
        xb = [P.sb(f"p0_b{i}", [128, 8, 128], BF16) for i in range(2)]
        for b in range(T // 128):
            x_ = xt[b % 2]
            o_ = xb[b % 2]
            P.dma('sp', x_[:], src[b * 128:(b + 1) * 128, :], writes=[x_])
            for j in range(8):
                bk = Kx.bank[j // 4]
                tr(P, bk[:, (j % 4) * 128:(j % 4 + 1) * 128], x_[:, j * 128:(j + 1) * 128], Kx.cst[:, C_I, :],
                   [x_, Kx.cst], [bk])
            for hlf in range(2):
                act(P, o_[:, hlf * 4:(hlf + 1) * 4, :], Kx.bank[hlf][:, :].rearrange("p (j t) -> p j t", j=4),
                    AF.Copy, [Kx.bank[hlf]], [o_])
            P.dma('sp', dstT.rearrange("k p t -> p k t")[:, :, b * 128:(b + 1) * 128], o_[:], reads=[o_], writes=[Kx.d_hT])
        P.flush()


def pass_ssd(P, Kx, T, l, dbg=None):
    nc = P.nc
    SBT = min(512, T)
    NSB = T // SBT
    NBLK = SBT // 128
    cst = Kx.cst
    bank = Kx.bank
    A0, A1, B0, B1, C0, C1, D0, D1 = bank
    with ExitStack() as st:
        P.stack = st
        try:
            W = P.sb("ssd_W", [128, 8, 2576], BF16)
            pp = P.sb("ssd_pp", [128, SSD_NPP], F32)
            rp = P.sb("ssd_rp", [128, SSD_NRP], F32)
            hTb = [P.sb(f"ssd_hT{i}", [128, 8, SBT], BF16) for i in range(2)]
            xin = P.sb("ssd_xin", [128, 12, SBT + 3], F32)
            accs = [P.sb(f"ssd_acc{i}", [128, SBT], F32) for i in range(6)]
            xc = P.sb("ssd_xc", [128, 12, SBT], BF16)
            xcj = [xc.sub(j) for j in range(12)]
            xinj = [xin.sub(j) for j in range(12)]
            Dmat = P.sb("ssd_Dmat", [128, 16, 128], BF16)
            arep = P.sb("ssd_arep", [128, 16], F32)
            smP = [P.sb(f"ssd_sm{i}", [128, 16 * 12], F32) for i in range(2)]
            R = P.sb("ssd_R", [128, 16, 128], F32)
            DT = P.sb("ssd_DT", [128, 16, 128], BF16)
            GTP = [P.sb(f"ssd_GT{i}", [128, 16, 128], BF16) for i in range(2)]
            CBTm = P.sb("ssd_CBTm", [128, 2, 128], BF16)
            xsbP = [P.sb(f"ssd_xsb{i}", [128, 1024], BF16) for i in range(2)]
            xdtP = [P.sb(f"ssd_xdt{i}", [128, 1024], BF16) for i in range(2)]
            xendP = [P.sb(f"ssd_xend{i}", [128, 1024], BF16) for i in range(2)]
            BtmP = [P.sb(f"ssd_Btm{i}", [128, 256], BF16) for i in range(2)]
            szP = [P.sb(f"ssd_sz{i}", [128, 1024], F32) for i in range(2)]
            yoff = P.sb("ssd_yoff", [128, 1024], F32)
            y = P.sb("ssd_y", [128, 1024], F32)
            junk = P.sb("ssd_junk", [128, 512], F32)
            ybf = P.sb("ssd_ybf", [128, 1024], BF16)
            yT = [P.sb(f"ssd_yT{i}", [128, 8, 128], BF16) for i in range(2)]
            S32 = P.sb("ssd_S32", [128, 1024], F32)
            Sbf = P.sb("ssd_Sbf", [128, 1024], BF16)
            identb = P.sb("ssd_identb", [128, 128], BF16)

            load_weights(P, W, Kx.w_in[l, :, 0:2576], 2576, 8, order=[2, 3, 4, 0, 1, 5])
            P.dma('sp', pp[:], Kx.ssd_pp[l], writes=[pp])
            P.dma('sp', rp[:], Kx.ssd_rp[l], writes=[rp])
            dtb = rp[:, 0:16]
            alog = rp[:, 16:32]
            drep = rp[:, 32:48]
            normw = rp[:, 48:48 + 1024]
            cw = lambda j, k: pp[:, j * 4 + k: j * 4 + k + 1]
            cb = lambda j: pp[:, 48 + j: 48 + j + 1]
            act(P, identb[:], cst[:, C_I, :], AF.Copy, [cst], [identb])
            act(P, arep[:], alog, AF.Exp, [rp], [arep])
            ts(P, arep[:], arep[:], -1.0, None, ALU.mult, None, [arep], [arep])
            tt(P, Dmat[:], cst[:, C_I, :].unsqueeze(1).to_broadcast([128, 16, 128]),
               drep.unsqueeze(2).to_broadcast([128, 16, 128]), ALU.mult, [cst, rp], [Dmat])
            P.op('dve', lambda e: e.memset(S32[:], 0.0), [], [S32])
            P.op('dve', lambda e: e.memset(Sbf[:], 0.0), [], [Sbf])
            P.op('dve', lambda e: e.memset(xin[:], 0.0), [], [xin])
            chk(1)

            for sbi in range(NSB):
                hb = hTb[sbi % 2]
                P.dma('sp', hb[:], Kx.hT.rearrange("k p t -> p k t")[:, :, sbi * SBT:(sbi + 1) * SBT], reads=[Kx.d_hT], writes=[hb])
                banks4 = [D0, D1, C0, C1]
                for half in range(2):
                    js = range(half * 6, half * 6 + 6)
                    for j in js:
                        bk = banks4[j % 4]
                        for k in range(8):
                            mm(P, bk[:, 0:SBT], W[:, k, 1024 + j * 128: 1024 + (j + 1) * 128], hb[:, k, :], k == 0, k == 7,
                               [wsub(W, 1024 + j * 128), hb], [bk])
                        ac = accs[j % 6]
                        act(P, xin[:, j, 3:3 + SBT], bk[:, 0:SBT], AF.Copy, [bk], [xinj[j]])
                        act(P, ac[:], bk[:, 0:SBT], AF.Identity, [bk, pp], [ac], scale=cw(j, 3), bias=cb(j))
                    for j in js:
                        ac = accs[j % 6]
                        for k in (2, 1, 0):
                            stt(P, ac[:], xin[:, j, k:k + SBT], cw(j, k), ac[:], ALU.mult, ALU.add, [xinj[j], pp, ac], [ac])
                    for j in js:
                        ac = accs[j % 6]
                        act(P, xc[:, j, :], ac[:], AF.Silu, [ac], [xcj[j]])
                        act(P, xin[:, j, 0:3], xin[:, j, SBT:SBT + 3], AF.Copy, [xinj[j]], [xinj[j]])
                chk(2)
                for blk in range(NBLK):
                    t0 = blk * 128
                    tg = sbi * SBT + t0
                    tsl = slice(t0, t0 + 128)
                    par = blk % 2
                    sm = smP[par]
                    smv = lambda i, sm=sm: sm[:, i * 16:(i + 1) * 16]
                    GT, xsb, xdt, xend, Btm, sz = GTP[par], xsbP[par], xdtP[par], xendP[par], BtmP[par], szP[par]
                    for hf in range(2):
                        bk = [D0, D1][hf]
                        for k in range(8):
                            mm(P, bk[:, :], hb[:, k, t0:t0 + 128], W[:, k, hf * 512:(hf + 1) * 512], k == 0, k == 7, [hb, wsub(W, hf * 512)], [bk])
                        act(P, sz[:, hf * 512:(hf + 1) * 512], bk[:, :], AF.Silu, [bk], [sz])
                    for k in range(8):
                        mm(P, C0[:, 0:16], hb[:, k, t0:t0 + 128], W[:, k, 2560:2576], k == 0, k == 7, [hb, wsub(W, 2560)], [C0])
                    xr, xm, ex, lg, dt, dA, acs, te, dte, ea, cd = [smv(i) for i in range(11)]
                    tt(P, xr, C0[:, 0:16], dtb, ALU.add, [C0, rp], [sm])
                    ts(P, xm, xr, 30.0, None, ALU.min, None, [sm], [sm])
                    act(P, ex, xm, AF.Exp, [sm], [sm])
                    act(P, lg, ex, AF.Ln, [sm], [sm], bias=1.0)
                    tt(P, dt, lg, xr, ALU.max, [sm], [sm])
                    tt(P, dA, dt, arep[:], ALU.mult, [sm, arep], [sm])
                    mm(P, C0[:, 16:32], cst[:, C_LE, :], dA, True, True, [cst, sm], [C0])
                    mm(P, C0[:, 32:48], cst[:, C_ONES, :], dA, True, True, [cst, sm], [C0])
                    act(P, ea, C0[:, 16:32], AF.Exp, [C0], [sm])
                    act(P, cd, C0[:, 32:48], AF.Exp, [C0], [sm])
                    act(P, acs, C0[:, 16:32], AF.Copy, [C0], [sm])
                    tt(P, te, C0[:, 32:48], acs, ALU.subtract, [C0, sm], [sm])
                    act(P, te, te, AF.Exp, [sm], [sm])
                    tt(P, dte, dt, te, ALU.mult, [sm], [sm])
                    chk(3)
                    tt(P, R[:], cst[:, C_LE, :].unsqueeze(1).to_broadcast([128, 16, 128]),
                       dA.unsqueeze(2).to_broadcast([128, 16, 128]), ALU.mult, [cst, sm], [R])
                    for q in range(4):
                        bk = [D0, D1][q % 2]
                        mm(P, bk[:, :], cst[:, C_GT, :], R[:, 4 * q:4 * q + 4, :], True, True, [cst, R], [bk])
                        act(P, DT[:, 4 * q:4 * q + 4, :], bk[:, :].rearrange("p (h l) -> p h l", h=4), AF.Exp, [bk], [DT])
                    chk(4)
                    d1b = D1[:, :].bitcast(BF16)
                    for j in range(8):
                        tr(P, d1b[:, j * 128:(j + 1) * 128], xc[:, j, tsl], identb[:], [xcj[j], identb], [D1])
                    act(P, xsb[:], d1b[:, :], AF.Copy, [D1], [xsb])
                    pv = d1b[:, :].rearrange("p (h c) -> p h c", h=16)
                    tt(P, xdt[:].rearrange("p (h c) -> p h c", h=16), pv,
                       dt.unsqueeze(2).to_broadcast([128, 16, 64]), ALU.mult, [D1, sm], [xdt])
                    tt(P, xend[:].rearrange("p (h c) -> p h c", h=16), pv,
                       dte.unsqueeze(2).to_broadcast([128, 16, 64]), ALU.mult, [D1, sm], [xend])
                    chk(4.2)
                    c0b = C0[:, :].bitcast(BF16)
                    for g in range(2):
                        tr(P, c0b[:, 256 + g * 128:256 + (g + 1) * 128], xc[:, 8 + g, tsl], identb[:], [xcj[8 + g], identb], [C0])
                    act(P, Btm[:], c0b[:, 256:512], AF.Copy, [C0], [Btm])
                    chk(5)
                    for g in range(2):
                        mm(P, C1[:, g * 128:(g + 1) * 128], xc[:, 8 + g, tsl], xc[:, 10 + g, tsl], True, True, [xcj[8 + g], xcj[10 + g]], [C1])
                    tt(P, CBTm[:], C1[:, 0:256].rearrange("p (g l) -> p g l", g=2),
                       cst[:, C_LE, :].unsqueeze(1).to_broadcast([128, 2, 128]), ALU.mult, [C1, cst], [CBTm])
                    for g in range(2):
                        tt(P, GT[:, g * 8:(g + 1) * 8, :], DT[:, g * 8:(g + 1) * 8, :],
                           CBTm[:, g, :].unsqueeze(1).to_broadcast([128, 8, 128]), ALU.mult, [DT, CBTm], [GT])
                    chk(6)
                    for h in range(16):
                        bk = [A0, A1][h // 8]
                        o = bk[:, (h % 8) * 64:(h % 8 + 1) * 64]
                        mm(P, o, GT[:, h, :], xdt[:, h * 64:(h + 1) * 64], True, False, [GT, xdt], [bk])
                        mm(P, o, Dmat[:, h, :], xsb[:, h * 64:(h + 1) * 64], False, True, [Dmat, xsb], [bk])
                    for g in range(2):
                        bk = [B0, B1][g]
                        mm(P, bk[:, :], xc[:, 10 + g, tsl], Sbf[:, g * 512:(g + 1) * 512], True, True, [xcj[10 + g], Sbf], [bk])
                        tt(P, yoff[:, g * 512:(g + 1) * 512].rearrange("p (h c) -> p h c", h=8),
                           bk[:, :].rearrange("p (h c) -> p h c", h=8),
                           ea[:, g * 8:(g + 1) * 8].unsqueeze(2).to_broadcast([128, 8, 64]), ALU.mult, [bk, sm], [yoff])
                        tt(P, y[:, g * 512:(g + 1) * 512], [A0, A1][g][:, :], yoff[:, g * 512:(g + 1) * 512], ALU.add,
                           [[A0, A1][g], yoff], [y])
                    tt(P, y[:], y[:], sz[:], ALU.mult, [y, sz], [y])
                    chk(7)
                    ss = smv(11)
                    for g in range(2):
                        P.op('act', lambda e, g=g, sm=sm: e.activation(out=junk[:], in_=y[:, g * 512:(g + 1) * 512], func=AF.Square,
                                                               accum_out=sm[:, 176 + g:177 + g]), [y], [junk, sm])
                    act(P, sm[:, 178:180], sm[:, 176:178], AF.Ln, [sm], [sm], scale=1.0 / 512, bias=1e-6)
                    act(P, sm[:, 178:180], sm[:, 178:180], AF.Exp, [sm], [sm], scale=-0.5)
                    for g in range(2):
                        stt(P, ybf[:, g * 512:(g + 1) * 512], y[:, g * 512:(g + 1) * 512], sm[:, 178 + g:179 + g],
                            normw[:, g * 512:(g + 1) * 512], ALU.mult, ALU.mult, [y, sm, rp], [ybf])
                    a0b = A0[:, :].bitcast(BF16)
                    for j in range(8):
                        tr(P, a0b[:, j * 128:(j + 1) * 128], ybf[:, j * 128:(j + 1) * 128], identb[:], [ybf, identb], [A0])
                    yt = yT[(sbi * NBLK + blk) % 2]
                    act(P, yt[:], a0b.rearrange("p (j t) -> p j t", j=8), AF.Copy, [A0], [yt])
                    P.dma('sp', Kx.ycT.rearrange("k p t -> p k t")[:, 0:8, tg:tg + 128], yt[:], reads=[yt], writes=[Kx.d_ycT])
                    chk(8)
                    for g in range(2):
                        bk = [B0, B1][g]
                        mm(P, bk[:, :], Btm[:, g * 128:(g + 1) * 128], xend[:, g * 512:(g + 1) * 512], True, True, [Btm, xend], [bk])
                    tt(P, S32[:].rearrange("p (h c) -> p h c", h=16), S32[:].rearrange("p (h c) -> p h c", h=16),
                       cd.unsqueeze(2).to_broadcast([128, 16, 64]), ALU.mult, [S32, sm], [S32])
                    for g in range(2):
                        tt(P, S32[:, g * 512:(g + 1) * 512], [B0, B1][g][:, :], S32[:, g * 512:(g + 1) * 512], ALU.add,
                           [[B0, B1][g], S32], [S32])
                    act(P, Sbf[:], S32[:], AF.Copy, [S32], [Sbf])
        except StopPass:
            pass
        P.flush()


GDN_NPP = 48
GDN_NRP = 4 + 4 + 128
GDN_BASE = 2576


def pass_gdn(P, Kx, T, l):
    SBT = min(512, T)
    NSB = T // SBT
    NBLK = SBT // 128
    cst = Kx.cst
    A0, A1, B0, B1, C0, C1, D0, D1 = Kx.bank
    b4 = lambda ap: ap.rearrange("p (h c) -> p h c", h=4)
    with ExitStack() as st:
        P.stack = st
        try:
            W = P.sb("gdn_W", [128, 8, 2056], BF16)
            pp = P.sb("gdn_pp", [128, GDN_NPP], F32)
            rp = P.sb("gdn_rp", [128, GDN_NRP], F32)
            hTb = [P.sb(f"gdn_hT{i}", [128, 8, SBT], BF16) for i in range(2)]
            xin = P.sb("gdn_xin", [128, 12, SBT + 3], F32)
            xinj = [xin.sub(j) for j in range(12)]
            accs = [P.sb(f"gdn_acc{i}", [128, SBT], F32) for i in range(12)]
            sgS = P.sb("gdn_sgS", [128, NBLK, 512], F32)
            xc = P.sb("gdn_xc", [128, 12, SBT], F32)
            xcj = [xc.sub(j) for j in range(12)]
            qT = P.sb("gdn_qT", [128, 4, SBT], BF16)
            kT = P.sb("gdn_kT", [128, 4, SBT], BF16)
            identb = P.sb("gdn_identb", [128, 128], BF16)
            arep = P.sb("gdn_arep", [128, 4], F32)
            smP = [P.sb(f"gdn_sm{i}", [128, 4 * 20], F32) for i in range(2)]
            R = P.sb("gdn_R", [128, 4, 128], F32)
            Dec = P.sb("gdn_Dec", [128, 4, 128], F32)
            DecU = P.sb("gdn_DecU", [128, 4, 128], F32)
            t1 = P.sb("gdn_t1", [128, 4, 128], F32)
            qkTmP = [P.sb(f"gdn_qkTm{i}", [128, 4, 128], BF16) for i in range(2)]
            Ya = [P.sb(f"gdn_Y{i}", [128, 4, 128], F32) for i in range(2)]
            Za = [P.sb(f"gdn_Z{i}", [128, 4, 128], F32) for i in range(2)]
            V = P.sb("gdn_V", [128, 4, 128], F32)
            VbfP = [P.sb(f"gdn_Vbf{i}", [128, 4, 128], BF16) for i in range(2)]
            vtmP = [P.sb(f"gdn_vtm{i}", [128, 4, 128], F32) for i in range(2)]
            kdP = [P.sb(f"gdn_kd{i}", [128, 4, 128], BF16) for i in range(2)]
            rhs2 = P.sb("gdn_rhs2", [128, 4, 128], BF16)
            vnew = P.sb("gdn_vnew", [128, 4, 128], BF16)
            As = P.sb("gdn_As", [128, 4, 128], F32)
            o = P.sb("gdn_o", [128, 4, 128], F32)
            junk = P.sb("gdn_junk", [128, 128], F32)
            ybf = P.sb("gdn_ybf", [128, 4, 128], BF16)
            yT = [P.sb(f"gdn_yT{i}", [128, 4, 128], BF16) for i in range(2)]
            S32 = P.sb("gdn_S32", [128, 4, 128], F32)
            Sbf = P.sb("gdn_Sbf", [128, 4, 128], BF16)

            load_weights(P, W, Kx.w_in[l, :, GDN_BASE:GDN_BASE + 2056], 2056, 8)
            P.dma('sp', pp[:], Kx.gdn_pp[l], writes=[pp])
            P.dma('sp', rp[:], Kx.gdn_rp[l], writes=[rp])
            dtb = rp[:, 0:4]
            alog = rp[:, 4:8]
            normw = rp[:, 8:136]
            cw = lambda j, k: pp[:, j * 4 + k: j * 4 + k + 1]
            act(P, identb[:], cst[:, C_I, :], AF.Copy, [cst], [identb])
            act(P, arep[:], alog, AF.Exp, [rp], [arep])
            ts(P, arep[:], arep[:], -1.0, None, ALU.mult, None, [arep], [arep])
            P.op('dve', lambda e: e.memset(S32[:], 0.0), [], [S32])
            P.op('dve', lambda e: e.memset(Sbf[:], 0.0), [], [Sbf])
            P.op('dve', lambda e: e.memset(xin[:], 0.0), [], [xin])
            chk(1)
            for sbi in range(NSB):
                hb = hTb[sbi % 2]
                P.dma('sp', hb[:], Kx.hT.rearrange("k p t -> p k t")[:, :, sbi * SBT:(sbi + 1) * SBT], reads=[Kx.d_hT], writes=[hb])
                banks4 = [D0, D1, C0, C1]
                for j in range(12):
                    bk = banks4[j % 4]
                    for k in range(8):
                        mm(P, bk[:, 0:SBT], W[:, k, j * 128:(j + 1) * 128], hb[:, k, :], k == 0, k == 7, [wsub(W, j * 128), hb], [bk])
                    ac = accs[j]
                    act(P, xin[:, j, 3:3 + SBT], bk[:, 0:SBT], AF.Copy, [bk], [xinj[j]])
                    act(P, ac[:], bk[:, 0:SBT], AF.Copy, [bk, pp], [ac], scale=cw(j, 3))
                for j in range(12):
                    ac = accs[j]
                    for k in (2, 1, 0):
                        stt(P, ac[:], xin[:, j, k:k + SBT], cw(j, k), ac[:], ALU.mult, ALU.add, [xinj[j], pp, ac], [ac])
                for blk in range(NBLK):
                    bk = banks4[blk % 4]
                    for k in range(8):
                        mm(P, bk[:, :], hb[:, k, blk * 128:(blk + 1) * 128], W[:, k, 1536:2048], k == 0, k == 7, [hb, wsub(W, 1536)], [bk])
                    act(P, sgS[:, blk, :], bk[:, :], AF.Silu, [bk], [sgS])
                for j in range(12):
                    ac = accs[j]
                    act(P, xc[:, j, :], ac[:], AF.Silu, [ac], [xcj[j]])
                    act(P, xin[:, j, 0:3], xin[:, j, SBT:SBT + 3], AF.Copy, [xinj[j]], [xinj[j]])
                for j in range(8):
                    act(P, accs[j][:], xc[:, j, :], AF.Square, [xcj[j]], [accs[j]])
                for j in range(8):
                    bk2 = banks4[j % 4]
                    r_ = accs[j]
                    mm(P, bk2[:, 0:SBT], cst[:, C_ONES, :], r_[:], True, True, [cst, r_], [bk2])
                    act(P, r_[:], bk2[:, 0:SBT], AF.Ln, [bk2], [r_], bias=1e-6)
                for j in range(8):
                    r_ = accs[j]
                    act(P, r_[:], r_[:], AF.Exp, [r_], [r_], scale=-0.5,
                        bias=(-0.5 * float(np.log(128.0))) if j < 4 else 0.0)
                    dst = qT if j < 4 else kT
                    tt(P, dst[:, j % 4, :], xc[:, j, :], r_[:], ALU.mult, [xcj[j], r_], [dst])
                chk(2)
                def h1(blk, par):
                    t0 = blk * 128
                    tsl = slice(t0, t0 + 128)
                    sm = smP[par]
                    smv = lambda i: sm[:, i * 4:(i + 1) * 4]
                    Vbf, qkTm, kd, v_tm = VbfP[par], qkTmP[par], kdP[par], vtmP[par]
                    for k in range(8):
                        mm(P, C0[:, 0:8], hb[:, k, tsl], W[:, k, 2048:2056], k == 0, k == 7, [hb, wsub(W, 2048)], [C0])
                    eb, beta, xr, xm, ex, lg, sp, la, eg, gs, ekd, negeg = [smv(i) for i in range(12)]
                    glrep = sm[:, 48:56]
                    act(P, eb, C0[:, 0:4], AF.Exp, [C0], [sm], scale=-1.0)
                    ts(P, eb, eb, 1.0, None, ALU.add, None, [sm], [sm])
                    P.op('dve', lambda e: e.reciprocal(out=beta, in_=eb), [sm], [sm])
                    tt(P, xr, C0[:, 4:8], dtb, ALU.add, [C0, rp], [sm])
                    ts(P, xm, xr, 30.0, None, ALU.min, None, [sm], [sm])
                    yield
                    act(P, ex, xm, AF.Exp, [sm], [sm])
                    act(P, lg, ex, AF.Ln, [sm], [sm], bias=1.0)
                    tt(P, sp, lg, xr, ALU.max, [sm], [sm])
                    tt(P, la, sp, arep[:], ALU.mult, [sm, arep], [sm])
                    yield
                    mm(P, C0[:, 8:12], cst[:, C_LE64, :], la, True, True, [cst, sm], [C0])
                    mm(P, C0[:, 12:16], cst[:, C_SAME64, :], la, True, True, [cst, sm], [C0])
                    mm(P, C0[:, 16:20], cst[:, C_SEL0, :], la, True, True, [cst, sm], [C0])
                    mm(P, C0[:, 20:24], cst[:, C_SEL1, :], la, True, True, [cst, sm], [C0])
                    act(P, eg, C0[:, 8:12], AF.Exp, [C0], [sm])
                    act(P, gs, C0[:, 8:12], AF.Copy, [C0], [sm])
                    act(P, glrep, C0[:, 16:24], AF.Exp, [C0], [sm])
                    tt(P, ekd, C0[:, 12:16], gs, ALU.subtract, [C0, sm], [sm])
                    yield
                    act(P, ekd, ekd, AF.Exp, [sm], [sm])
                    ts(P, negeg, eg, -1.0, None, ALU.mult, None, [sm], [sm])
                    tt(P, R[:], cst[:, C_LE64, :].unsqueeze(1).to_broadcast([128, 4, 128]),
                       la.unsqueeze(2).to_broadcast([128, 4, 128]), ALU.mult, [cst, sm], [R])
                    yield
                    mm(P, D1[:, :], cst[:, C_GT64, :], R[:], True, True, [cst, R], [D1])
                    act(P, Dec[:], b4(D1[:, :]), AF.Exp, [D1], [Dec])
                    tt(P, DecU[:], Dec[:], cst[:, C_LE64, :].unsqueeze(1).to_broadcast([128, 4, 128]), ALU.mult, [Dec, cst], [DecU])
                    yield
                    for h in range(4):
                        mm(P, D0[:, h * 128:(h + 1) * 128], kT[:, h, tsl], kT[:, h, tsl], True, True, [kT], [D0])
                    for h in range(4):
                        mm(P, D1[:, h * 128:(h + 1) * 128], kT[:, h, tsl], qT[:, h, tsl], True, True, [kT, qT], [D1])
                    yield
                    tt(P, qkTm[:], b4(D1[:, :]), DecU[:], ALU.mult, [D1, DecU], [qkTm])
                    tt(P, t1[:], b4(D0[:, :]), DecU[:], ALU.mult, [D0, DecU], [t1])
                    yield
                    X = Ya[0]
                    for h in range(4):
                        stt(P, rr(X[:, h, :]), t1[:, h, :], beta[:, h:h + 1], cst[:, C_LT64, :], ALU.mult, ALU.mult, [t1, sm, cst], [X])
                    yield
                    for h in range(4):
                        tr(P, D0[:, h * 128:(h + 1) * 128], X[:, h, :], cst[:, C_I, :], [X, cst], [D0])
                    act(P, rr(Za[0][:]), b4(D0[:, :]), AF.Copy, [D0], [Za[0]])
                    tt(P, rr(V[:]), cst[:, C_I, :].unsqueeze(1).to_broadcast([128, 4, 128]), X[:], ALU.subtract, [cst, X], [V])
                    yield
                    for lev in range(5):
                        Yc, Zc = Ya[lev % 2], Za[lev % 2]
                        Yn, Zn = Ya[(lev + 1) % 2], Za[(lev + 1) % 2]
                        for h in range(4):
                            mmr(P, D1[:, h * 128:(h + 1) * 128], Yc[:, h, :], Zc[:, h, :], True, True, [Yc, Zc], [D1])
                        act(P, rr(Zn[:]), b4(D1[:, :]), AF.Copy, [D1], [Zn])
                        yield
                        if lev < 4:
                            for h in range(4):
                                mmr(P, D0[:, h * 128:(h + 1) * 128], Zc[:, h, :], Yc[:, h, :], True, True, [Yc, Zc], [D0])
                            act(P, rr(Yn[:]), b4(D0[:, :]), AF.Copy, [D0], [Yn])
                            yield
                        for h in range(4):
                            mmr(P, D1[:, h * 128:(h + 1) * 128], Zn[:, h, :], V[:, h, :], True, True, [Zn, V], [D1])
                        tt(P, rr(V[:]), b4(D1[:, :]), V[:], ALU.add, [D1, V], [V])
                        yield
                    act(P, Vbf[:], V[:], AF.Copy, [V], [Vbf])
                    for h in range(4):
                        tr(P, D0[:, h * 128:(h + 1) * 128], xc[:, 8 + h, tsl], cst[:, C_I, :], [xcj[8 + h], cst], [D0])
                    act(P, v_tm[:], b4(D0[:, :]), AF.Copy, [D0], [v_tm])
                    yield
                    d1b = D1[:, :].bitcast(BF16)
                    for h in range(4):
                        tr(P, d1b[:, h * 128:(h + 1) * 128], kT[:, h, tsl], identb[:], [kT, identb], [D1])
                    tt(P, kd[:], b4(d1b[:, 0:512]), ekd.unsqueeze(2).to_broadcast([128, 4, 128]), ALU.mult, [D1, sm], [kd])
                    yield

                def h2(blk, par):
                    t0 = blk * 128
                    tg = sbi * SBT + t0
                    tsl = slice(t0, t0 + 128)
                    sm = smP[par]
                    smv = lambda i: sm[:, i * 4:(i + 1) * 4]
                    Vbf, qkTm, kd, v_tm = VbfP[par], qkTmP[par], kdP[par], vtmP[par]
                    eb, beta, xr, xm, ex, lg, sp, la, eg, gs, ekd, negeg = [smv(i) for i in range(12)]
                    glrep = sm[:, 48:56]
                    for c in range(2):
                        sl = slice(64 * c, 64 * c + 64)
                        for h in range(4):
                            mm(P, A0[:, h * 128:(h + 1) * 128], kT[:, h, tsl], Sbf[:, h, :], True, True, [kT, Sbf], [A0])
                        for h in range(4):
                            mm(P, B0[:, h * 128:(h + 1) * 128], qT[:, h, tsl], Sbf[:, h, :], True, True, [qT, Sbf], [B0])
                        for h in range(4):
                            stt(P, rhs2[sl, h, :], A0[sl, h * 128:(h + 1) * 128], negeg[sl, h:h + 1], v_tm[sl, h, :],
                                ALU.mult, ALU.add, [A0, sm, v_tm], [rhs2])
                        yield
                        for h in range(4):
                            mm(P, A1[:, h * 128:(h + 1) * 128], Vbf[sl, h, :], rhs2[sl, h, :], True, True, [Vbf, rhs2], [A1])
                        tt(P, vnew[sl], b4(A1[sl, :]), beta[sl].unsqueeze(2).to_broadcast([64, 4, 128]), ALU.mult, [A1, sm], [vnew])
                        yield
                        for h in range(4):
                            mm(P, C1[:, h * 128:(h + 1) * 128], kd[sl, h, :], vnew[sl, h, :], True, True, [kd, vnew], [C1])
                        for h in range(4):
                            mmx(P, B1[:, h * 128:(h + 1) * 128], qkTm[sl, h, :], vnew[sl, h, :], (c == 0 and h == 0), (c == 1),
                                [qkTm, vnew], [B1])
                        for h in range(4):
                            stt(P, S32[:, h, :], S32[:, h, :], glrep[:, c * 4 + h:c * 4 + h + 1], C1[:, h * 128:(h + 1) * 128],
                                ALU.mult, ALU.add, [S32, sm, C1], [S32])
                        act(P, Sbf[:], S32[:], AF.Copy, [S32], [Sbf])
                        yield
                        tt(P, As[sl], b4(B0[sl, :]), eg[sl].unsqueeze(2).to_broadcast([64, 4, 128]), ALU.mult, [B0, sm], [As])
                        yield
                    tt(P, o[:], b4(B1[:, :]), As[:], ALU.add, [B1, As], [o])
                    for h in range(4):
                        P.op('act', lambda e, h=h, sm=sm: e.activation(out=junk[:], in_=o[:, h, :], func=AF.Square,
                                                                      accum_out=sm[:, 56 + h:57 + h]), [o], [junk, sm])
                    yield
                    act(P, sm[:, 60:64], sm[:, 56:60], AF.Ln, [sm], [sm], scale=1.0 / 128, bias=1e-6)
                    act(P, sm[:, 60:64], sm[:, 60:64], AF.Exp, [sm], [sm], scale=-0.5)
                    for h in range(4):
                        stt(P, o[:, h, :], o[:, h, :], sm[:, 60 + h:61 + h], normw, ALU.mult, ALU.mult, [o, sm, rp], [o])
                    yield
                    tt(P, ybf[:], o[:], b4(sgS[:, blk, :]), ALU.mult, [o, sgS], [ybf])
                    c1b = C1[:, :].bitcast(BF16)
                    for h in range(4):
                        tr(P, c1b[:, h * 128:(h + 1) * 128], ybf[:, h, :], identb[:], [ybf, identb], [C1])
                    yt = yT[(sbi * NBLK + blk) % 2]
                    act(P, yt[:], b4(c1b[:, 0:512]), AF.Copy, [C1], [yt])
                    P.dma('sp', Kx.ycT.rearrange("k p t -> p k t")[:, 8:12, tg:tg + 128], yt[:], reads=[yt], writes=[Kx.d_ycT])
                    yield

                for _ in h1(0, 0):
                    pass
                for blk in range(NBLK):
                    g2 = h2(blk, blk % 2)
                    g1 = h1(blk + 1, (blk + 1) % 2) if blk + 1 < NBLK else iter(())
                    d1 = d2 = False
                    while not (d1 and d2):
                        if not d2:
                            try:
                                next(g2)
                            except StopIteration:
                                d2 = True
                        for _r in range(2):
                            if not d1:
                                try:
                                    next(g1)
                                except StopIteration:
                                    d1 = True
        except StopPass:
            pass
        P.flush()


HG_BASE = 4632
HG_NRP = 128


def pass_hg(P, Kx, T, l, labs):
    SBT = min(512, T)
    NSB = T // SBT
    NBLK = SBT // 128
    cst = Kx.cst
    A0, A1, B0, B1, C0, C1, D0, D1 = Kx.bank
    b4 = lambda ap: ap.rearrange("p (h c) -> p h c", h=4)
    with ExitStack() as st:
        P.stack = st
        try:
            W = P.sb("hg_W", [128, 8, 2048], BF16)
            rp = P.sb("hg_rp", [128, HG_NRP], F32)
            lbl = P.sb("hg_lbl", [128, 4, 4], F32)
            lbw = P.sb("hg_lbw", [128, 4 * 6], F32)
            lmk = P.sb("hg_lmk", [128, 4, 4], F32)
            hTb = [P.sb(f"hg_hT{i}", [128, 8, SBT], BF16) for i in range(2)]
            qTf = P.sb("hg_qTf", [128, 4, SBT], F32)
            kTf = P.sb("hg_kTf", [128, 4, SBT], F32)
            lgf = P.sb("hg_lgf", [128, 4, SBT], F32)
            ftmp = [P.sb(f"hg_ft{i}", [128, SBT], F32) for i in range(4)]
            sgS = P.sb("hg_sgS", [128, NBLK, 512], F32)
            ones = P.sb("hg_ones", [128, 128], F32)
            identb = P.sb("hg_identb", [128, 128], BF16)
            Bt = P.sb("hg_Bt", [128, 4, 132], F32)
            D1t = [P.sb(f"hg_D1{i}", [128, 8, 128], F32) for i in range(2)]
            Et = [P.sb(f"hg_E{i}", [128, 8, 128], F32) for i in range(2)]
            kfac = [P.sb(f"hg_kfac{i}", [128, 8, 128], BF16) for i in range(2)]
            Eq = P.sb("hg_Eq", [128, 4, 128], F32)
            EB = P.sb("hg_EB", [128, 4, 128], F32)
            Ek = P.sb("hg_Ek", [128, 4, 128], F32)
            qg = P.sb("hg_qg", [128, 4, 128], BF16)
            qG = P.sb("hg_qG", [128, 4, 128], BF16)
            kdT = P.sb("hg_kdT", [128, 4, 128], BF16)
            kdtm = P.sb("hg_kdtm", [128, 4, 128], BF16)
            scT = P.sb("hg_scT", [128, 4, 128], BF16)
            vbf = P.sb("hg_vbf", [128, 4, 128], BF16)
            sm = P.sb("hg_sm", [128, 16], F32)
            junk = P.sb("hg_junk", [128, 128], F32)
            o = P.sb("hg_o", [128, 4, 128], F32)
            ybf = P.sb("hg_ybf", [128, 4, 128], BF16)
            yT = [P.sb(f"hg_yT{i}", [128, 4, 128], BF16) for i in range(2)]
            S32 = P.sb("hg_S32", [128, 4, 128], F32)
            Sbf = P.sb("hg_Sbf", [128, 4, 128], BF16)

            load_weights(P, W, Kx.w_in[l, :, HG_BASE:HG_BASE + 2048], 2048, 8, order=[0, 3, 1, 2])
            P.dma('sp', rp[:], Kx.hg_rp[l], writes=[rp])
            P.dma('sp', lbl[:], Kx.hg_lbl, writes=[lbl])
            normw = rp[:, 0:128]
            act(P, identb[:], cst[:, C_I, :], AF.Copy, [cst], [identb])
            P.op('dve', lambda e: e.memset(ones[:], 1.0), [], [ones])
            P.op('dve', lambda e: e.memset(S32[:], 0.0), [], [S32])
            P.op('dve', lambda e: e.memset(Sbf[:], 0.0), [], [Sbf])
            P.op('dve', lambda e: e.memset(Bt[:], 0.0), [], [Bt])
            mx, sme, rs, lb, oml = [lbw[:, i * 4:(i + 1) * 4] for i in range(5)]
            P.op('dve', lambda e: e.tensor_reduce(out=mx, in_=lbl[:], axis=AX.X, op=ALU.max), [lbl], [lbw])
            tt(P, lbl[:], lbl[:], mx.unsqueeze(2).to_broadcast([128, 4, 4]), ALU.subtract, [lbl, lbw], [lbl])
            act(P, lbl[:], lbl[:], AF.Exp, [lbl], [lbl])
            P.op('dve', lambda e: e.tensor_reduce(out=sme, in_=lbl[:], axis=AX.X, op=ALU.add), [lbl], [lbw])
            P.op('dve', lambda e: e.reciprocal(out=rs, in_=sme), [lbw], [lbw])
            P.dma('sp', lmk[:], Kx.hg_lmask[l], writes=[lmk])
            tt(P, lbl[:], lbl[:], lmk[:], ALU.mult, [lbl, lmk], [lbl])
            P.op('dve', lambda e: e.tensor_reduce(out=lb, in_=lbl[:], axis=AX.X, op=ALU.add), [lbl], [lbw])
            tt(P, lb, lb, rs, ALU.mult, [lbw], [lbw])
            ts(P, oml, lb, -1.0, 1.0, ALU.mult, ALU.add, [lbw], [lbw])
            chk(1)
            for sbi in range(NSB):
                hb = hTb[sbi % 2]
                P.dma('sp', hb[:], Kx.hT.rearrange("k p t -> p k t")[:, :, sbi * SBT:(sbi + 1) * SBT], reads=[Kx.d_hT], writes=[hb])
                banks4 = [D0, D1, C0, C1]
                for j in range(4):
                    bk = banks4[j % 4]
                    for k in range(8):
                        mm(P, bk[:, 0:SBT], W[:, k, j * 128:(j + 1) * 128], hb[:, k, :], k == 0, k == 7, [wsub(W, 0), hb], [bk])
                    act(P, qTf[:, j, :], bk[:, 0:SBT], AF.Silu, [bk], [qTf])
                for blk in range(NBLK):
                    bk = banks4[blk % 4]
                    for k in range(8):
                        mm(P, bk[:, :], hb[:, k, blk * 128:(blk + 1) * 128], W[:, k, 1536:2048], k == 0, k == 7, [hb, wsub(W, 1536)], [bk])
                    act(P, sgS[:, blk, :], bk[:, :], AF.Silu, [bk], [sgS])
                for h in range(4):
                    bk = banks4[h % 4]
                    for k in range(8):
                        mm(P, bk[:, 0:SBT], W[:, k, (4 + h) * 128:(5 + h) * 128], hb[:, k, :], k == 0, k == 7, [wsub(W, 512), hb], [bk])
                    act(P, ftmp[h][:], bk[:, 0:SBT], AF.Sigmoid, [bk], [ftmp[h]])
                for h in range(4):
                    f_ = ftmp[h]
                    ts(P, f_[:], f_[:], oml[:, h:h + 1], lb[:, h:h + 1], ALU.mult, ALU.add, [f_, lbw], [f_])
                    ts(P, kTf[:, h, :], f_[:], -1.0, 1.0, ALU.mult, ALU.add, [f_], [kTf])
                for h in range(4):
                    act(P, lgf[:, h, :], ftmp[h][:], AF.Ln, [ftmp[h]], [lgf])
                chk(2)
                for blk in range(NBLK):
                    t0 = blk * 128
                    tg = sbi * SBT + t0
                    tsl = slice(t0, t0 + 128)
                    for k in range(8):
                        mm(P, A0[:, :], hb[:, k, tsl], W[:, k, 1024:1536], k == 0, k == 7, [hb, wsub(W, 1024)], [A0])
                    act(P, vbf[:], b4(A0[:, :]), AF.Copy, [A0], [vbf])
                    for h in range(4):
                        i2 = h % 2
                        D1_, E_, kf_ = D1t[i2], Et[i2], kfac[i2]
                        P.op('dve', lambda e, h=h, tsl=tsl: e.tensor_tensor_scan(out=Bt[:, h, 1:129], data0=ones[:, :], data1=lgf[:, h, tsl],
                                                                       initial=0.0, op0=ALU.mult, op1=ALU.add), [ones, lgf], [Bt])
                        for c in range(8):
                            ts(P, D1_[:, c, :], Bt[:, h, 1:129], Bt[:, h, 16 * c:16 * c + 1], -60.0, ALU.subtract, ALU.max, [Bt], [D1_])
                        act(P, E_[:], D1_[:], AF.Exp, [D1_], [E_], scale=-1.0)
                        tt(P, kf_[:], E_[:], kTf[:, h, tsl].unsqueeze(1).to_broadcast([128, 8, 128]), ALU.mult, [E_, kTf], [kf_])
                        base = D1_[:, :, :]
                        dg = bass.AP(base.tensor, base.offset, [list(base.ap[0]), [144, 8], [1, 16]])
                        act(P, Eq[:, h, :].rearrange("p (c j) -> p c j", c=8), dg, AF.Exp, [D1_], [Eq])
                        tt(P, qg[:, h, :], qTf[:, h, tsl], Eq[:, h, :], ALU.mult, [qTf, Eq], [qg])
                        act(P, EB[:, h, :], Bt[:, h, 1:129], AF.Exp, [Bt], [EB])
                        tt(P, qG[:, h, :], qTf[:, h, tsl], EB[:, h, :], ALU.mult, [qTf, EB], [qG])
                        act(P, Ek[:, h, :], Bt[:, h, 1:129], AF.Exp, [Bt], [Ek], scale=-1.0, bias=Bt[:, h, 128:129])
                        tt(P, kdT[:, h, :], kTf[:, h, tsl], Ek[:, h, :], ALU.mult, [kTf, Ek], [kdT])
                        for c in range(8):
                            mm(P, B0[:, h * 128 + 16 * c:h * 128 + 16 * c + 16], kf_[:, c, :], qg[:, h, 16 * c:16 * c + 16], True, True,
                               [kf_, qg], [B0])
                    tt(P, scT[:], b4(B0[:, :]), cst[:, C_LE, :].unsqueeze(1).to_broadcast([128, 4, 128]), ALU.mult, [B0, cst], [scT])
                    chk(3)
                    for h in range(4):
                        mm(P, B1[:, h * 128:(h + 1) * 128], scT[:, h, :], vbf[:, h, :], True, False, [scT, vbf], [B1])
                        mm(P, B1[:, h * 128:(h + 1) * 128], qG[:, h, :], Sbf[:, h, :], False, True, [qG, Sbf], [B1])
                    c0b = C0[:, :].bitcast(BF16)
                    for h in range(4):
                        tr(P, c0b[:, h * 128:(h + 1) * 128], kdT[:, h, :], identb[:], [kdT, identb], [C0])
                    act(P, kdtm[:], b4(c0b[:, 0:512]), AF.Copy, [C0], [kdtm])
                    for h in range(4):
                        mm(P, C1[:, h * 128:(h + 1) * 128], kdtm[:, h, :], vbf[:, h, :], True, True, [kdtm, vbf], [C1])
                    for h in range(4):
                        stt(P, S32[:, h, :], S32[:, h, :], EB[:, h, 127:128], C1[:, h * 128:(h + 1) * 128], ALU.mult, ALU.add,
                            [S32, EB, C1], [S32])
                    act(P, Sbf[:], S32[:], AF.Copy, [S32], [Sbf])
                    chk(4)
                    for h in range(4):
                        P.op('act', lambda e, h=h: e.activation(out=junk[:], in_=B1[:, h * 128:(h + 1) * 128], func=AF.Square,
                                                               accum_out=sm[:, h:h + 1]), [B1], [junk, sm])
                    act(P, sm[:, 4:8], sm[:, 0:4], AF.Ln, [sm], [sm], scale=1.0 / 128, bias=1e-6)
                    act(P, sm[:, 4:8], sm[:, 4:8], AF.Exp, [sm], [sm], scale=-0.5)
                    for h in range(4):
                        stt(P, o[:, h, :], B1[:, h * 128:(h + 1) * 128], sm[:, 4 + h:5 + h], normw, ALU.mult, ALU.mult, [B1, sm, rp], [o])
                    tt(P, ybf[:], o[:], b4(sgS[:, blk, :]), ALU.mult, [o, sgS], [ybf])
                    c1b = C1[:, :].bitcast(BF16)
                    for h in range(4):
                        tr(P, c1b[:, h * 128:(h + 1) * 128], ybf[:, h, :], identb[:], [ybf, identb], [C1])
                    yt = yT[(sbi * NBLK + blk) % 2]
                    act(P, yt[:], b4(c1b[:, 0:512]), AF.Copy, [C1], [yt])
                    P.dma('sp', Kx.ycT.rearrange("k p t -> p k t")[:, 12:16, tg:tg + 128], yt[:], reads=[yt], writes=[Kx.d_ycT])
        except StopPass:
            pass
        P.flush()


OUT_NRP = 1024 + 1024 + 20
NSEL = 16


def layer_norm_block(P, r, stats, mv, sm2, g_ap, b_ap, reads_rp, out_ap, out_buf):
    for hf in range(2):
        P.op('dve', lambda e, hf=hf: e.bn_stats(out=stats[:, hf * 6:(hf + 1) * 6], in_=r[:, hf * 512:(hf + 1) * 512]), [r], [stats])
    P.op('dve', lambda e: e.bn_aggr(out=mv[:, 0:2], in_=stats[:, 0:12]), [stats], [mv])
    act(P, sm2[:, 0:1], mv[:, 1:2], AF.Ln, [mv], [sm2], bias=1e-5)
    act(P, sm2[:, 0:1], sm2[:, 0:1], AF.Exp, [sm2], [sm2], scale=-0.5)
    stt(P, sm2[:, 1:2], mv[:, 0:1], -1.0, sm2[:, 0:1], ALU.mult, ALU.mult, [mv, sm2], [sm2])
    act(P, r[:], r[:], AF.Identity, [r, sm2], [r], scale=sm2[:, 0:1], bias=sm2[:, 1:2])
    tt(P, r[:], r[:], g_ap, ALU.mult, [r] + reads_rp, [r])
    tt(P, out_ap, r[:], b_ap, ALU.add, [r] + reads_rp, [out_buf])


def pass_out(P, Kx, T, l, hsrc):
    cst = Kx.cst
    A0, A1, B0, B1, C0, C1, D0, D1 = Kx.bank
    NB = T // 128
    with ExitStack() as st:
        P.stack = st
        try:
            Wo = P.sb("out_W", [128, 16, 1024], BF16)
            rp = P.sb("out_rp", [128, OUT_NRP], F32)
            wr = P.sb("out_wr", [128, 8, 20], F32)
            ycb = [P.sb(f"out_yc{i}", [128, 16, 128], BF16) for i in range(2)]
            hin = [P.sb(f"out_h{i}", [128, 1024], F32) for i in range(2)]
            r = [P.sb(f"out_r{i}", [128, 1024], F32) for i in range(2)]
            x1 = [P.sb(f"out_x1{i}", [128, 1024], F32) for i in range(2)]
            x1Tf = P.sb("out_x1Tf", [128, 8, 128], F32)
            x1Tb = [P.sb(f"out_x1Tb{i}", [128, 8, 128], BF16) for i in range(2)]
            stats = P.sb("out_stats", [128, 12], F32)
            mv = P.sb("out_mv", [128, 2], F32)
            sm2 = P.sb("out_sm2", [128, 2], F32)
            q = P.sb("out_q", [128, 96], F32)
            comb = [P.sb(f"out_comb{i}", [128, 16], F32) for i in range(2)]
            combT = [P.sb(f"out_combT{i}", [16, 128], F32) for i in range(2)]

            wo3 = Kx.w_out[l].rearrange("(k p) c -> p k c", p=128)
            for hf in range(2):
                for kh in range(2):
                    P.dma('pool', Wo[:, kh * 8:(kh + 1) * 8, hf * 512:(hf + 1) * 512], wo3[:, kh * 8:(kh + 1) * 8, hf * 512:(hf + 1) * 512],
                          writes=[Wo.sub((hf, kh))])
            P.dma('sp', rp[:], Kx.out_rp[l], writes=[rp])
            P.dma('sp', wr[:], Kx.wr[l], writes=[wr])
            g1 = rp[:, 0:1024]
            b1 = rp[:, 1024:2048]
            rb = rp[:, 2048:2068]
            chk(1)
            def stage_a(b):
                    tsl = slice(b * 128, (b + 1) * 128)
                    yc, h_, r_, x_ = ycb[b % 2], hin[b % 2], r[b % 2], x1[b % 2]
                    P.dma('sp', yc[:], Kx.ycT.rearrange("k p t -> p k t")[:, :, tsl], reads=[Kx.d_ycT], writes=[yc])
                    P.dma('act', h_[:], hsrc[tsl, :], reads=[Kx.d_hres], writes=[h_])
                    for hf in range(2):
                        bk = [A0, A1][hf]
                        for kc in range(16):
                            mm(P, bk[:, :], yc[:, kc, :], Wo[:, kc, hf * 512:(hf + 1) * 512], kc == 0, kc == 15, [yc, Wo.children[hf * 2 + kc // 8]], [bk])
                        stt(P, r_[:, hf * 512:(hf + 1) * 512], h_[:, hf * 512:(hf + 1) * 512], float(DN_ALPHA), bk[:, :], ALU.mult, ALU.add,
                            [h_, bk], [r_])
                    layer_norm_block(P, r_, stats, mv, sm2, g1, b1, [rp], x_[:], x_)
                    P.dma('sp', Kx.x1[tsl, :], x_[:], reads=[x_], writes=[Kx.d_x1])

            def stage_b(b):
                    tsl = slice(b * 128, (b + 1) * 128)
                    x_ = x1[b % 2]
                    for j in range(8):
                        bk = [B0, B1][j // 4]
                        tr(P, bk[:, (j % 4) * 128:(j % 4 + 1) * 128], x_[:, j * 128:(j + 1) * 128], cst[:, C_I, :], [x_, cst], [bk])
                    xb = x1Tb[b % 2]
                    for hf in range(2):
                        bk = [B0, B1][hf]
                        act(P, x1Tf[:, hf * 4:(hf + 1) * 4, :], bk[:, :].rearrange("p (j t) -> p j t", j=4), AF.Copy, [bk], [x1Tf])
                        P.op('dve', lambda e, hf=hf, bk=bk, xb=xb: e.tensor_copy(out=xb[:, hf * 4:(hf + 1) * 4, :], in_=bk[:, :].rearrange("p (j t) -> p j t", j=4)),
                             [bk], [xb])
                    P.dma('sp', Kx.x1T.rearrange("k p t -> p k t")[:, :, tsl], xb[:], reads=[xb], writes=[Kx.d_x1T])
                    for k in range(8):
                        mm(P, C0[:, 0:20], x1Tf[:, k, :], wr[:, k, :], k == 0, k == 7, [x1Tf, wr], [C0])
                    lgs = q[:, 0:20]
                    gm, ngm, gsum, gp, m1, m2, dlt, ed, w1, w2 = [q[:, 20 + i:21 + i] for i in range(10)]
                    ohg = q[:, 32:36]
                    egj = q[:, 36:40]
                    lsel = q[:, 40:44]
                    oh1 = q[:, 44:48]
                    msk = q[:, 48:52]
                    oh2 = q[:, 52:56]
                    wsel = q[:, 56:60]
                    tmp16 = q[:, 64:80]
                    tt(P, lgs, C0[:, 0:20], rb, ALU.add, [C0, rp], [q])
                    P.op('dve', lambda e: e.tensor_reduce(out=gm, in_=lgs[:, 0:4], axis=AX.X, op=ALU.max), [q], [q])
                    ts(P, ohg, lgs[:, 0:4], gm, None, ALU.is_equal, None, [q], [q])
                    ts(P, ngm, gm, -1.0, None, ALU.mult, None, [q], [q])
                    P.op('act', lambda e: e.activation(out=egj, in_=lgs[:, 0:4], func=AF.Exp, bias=ngm, accum_out=gsum), [q], [q])
                    P.op('dve', lambda e: e.reciprocal(out=gp, in_=gsum), [q], [q])
                    tt(P, tmp16.rearrange("p (g e) -> p g e", g=4), lgs[:, 4:20].rearrange("p (g e) -> p g e", g=4),
                       ohg.unsqueeze(2).to_broadcast([128, 4, 4]), ALU.mult, [q], [q])
                    P.op('dve', lambda e: e.tensor_reduce(out=lsel, in_=tmp16.rearrange("p (g e) -> p e g", g=4), axis=AX.X, op=ALU.add), [q], [q])
                    P.op('dve', lambda e: e.tensor_reduce(out=m1, in_=lsel, axis=AX.X, op=ALU.max), [q], [q])
                    ts(P, oh1, lsel, m1, None, ALU.is_equal, None, [q], [q])
                    stt(P, msk, oh1, -1e30, lsel, ALU.mult, ALU.add, [q], [q])
                    P.op('dve', lambda e: e.tensor_reduce(out=m2, in_=msk, axis=AX.X, op=ALU.max), [q], [q])
                    ts(P, oh2, msk, m2, None, ALU.is_equal, None, [q], [q])
                    tt(P, dlt, m2, m1, ALU.subtract, [q], [q])
                    act(P, ed, dlt, AF.Exp, [q], [q])
                    ts(P, w1, ed, 1.0, None, ALU.add, None, [q], [q])
                    P.op('dve', lambda e: e.reciprocal(out=w1, in_=w1), [q], [q])
                    tt(P, w2, ed, w1, ALU.mult, [q], [q])
                    tt(P, w1, w1, gp, ALU.mult, [q], [q])
                    tt(P, w2, w2, gp, ALU.mult, [q], [q])
                    ts(P, wsel, oh1, w1, None, ALU.mult, None, [q], [q])
                    stt(P, wsel, oh2, w2, wsel, ALU.mult, ALU.add, [q], [q])
                    cb_ = comb[b % 2]
                    tt(P, cb_[:].rearrange("p (g e) -> p g e", g=4), ohg.unsqueeze(2).to_broadcast([128, 4, 4]),
                       wsel.unsqueeze(1).to_broadcast([128, 4, 4]), ALU.mult, [q], [cb_])
                    P.dma('sp', Kx.comb[tsl, :], cb_[:], reads=[cb_], writes=[Kx.d_comb])

            stage_a(0)
            for b in range(NB):
                if b + 1 < NB:
                    stage_a(b + 1)
                stage_b(b)
        except StopPass:
            pass
        P.flush()


def pass_moe(P, Kx, T, l, dst, write_hT):
    cst = Kx.cst
    A0, A1, B0, B1, C0, C1, D0, D1 = Kx.bank
    ST = min(1024, T)
    NST = T // ST
    NBS = ST // 128
    with ExitStack() as st:
        P.stack = st
        try:
            Wgu = [P.sb(f"moe_Wgu{i}", [128, 8, 512], BF16) for i in range(8)]
            Wdn = [P.sb(f"moe_Wdn{i}", [128, 2, 1024], BF16) for i in range(8)]
            for w_ in Wgu:
                for k in range(8):
                    w_.sub(k)
            for w_ in Wdn:
                for k in range(2):
                    w_.sub(k)
            rp = P.sb("moe_rp", [128, 2048], F32)
            xT = P.sb("moe_xT", [128, 8, ST], BF16)
            cmb = P.sb("moe_cmb", [128, NBS, 16], F32)
            yacc = P.sb("moe_yacc", [128, NBS, 1024], F32)
            yaccb = [yacc.sub(i) for i in range(NBS)]
            sgb = [P.sb(f"moe_sg{i}", [128, 256], F32) for i in range(3)]
            hb_ = [P.sb(f"moe_h{i}", [128, 256], BF16) for i in range(3)]
            hT = [P.sb(f"moe_hT{i}", [128, 2, 128], BF16) for i in range(3)]
            identb = P.sb("moe_identb", [128, 128], BF16)
            x1b = [P.sb(f"moe_x1{i}", [128, 1024], F32) for i in range(2)]
            ob = [P.sb(f"moe_o{i}", [128, 1024], F32) for i in range(2)]
            oT = [P.sb(f"moe_oT{i}", [128, 8, 128], BF16) for i in range(2)]
            stats = P.sb("moe_stats", [128, 12], F32)
            mv = P.sb("moe_mv", [128, 2], F32)
            sm2 = P.sb("moe_sm2", [128, 2], F32)
            P.dma('sp', rp[:], Kx.moe_rp[l], writes=[rp])
            g2 = rp[:, 0:1024]
            b2 = rp[:, 1024:2048]
            act(P, identb[:], cst[:, C_I, :], AF.Copy, [cst], [identb])
            it = 0
            for sti in range(NST):
                s0 = sti * ST
                P.dma('sp', xT[:], Kx.x1T.rearrange("k p t -> p k t")[:, :, s0:s0 + ST], reads=[Kx.d_x1T], writes=[xT])
                P.dma('sp', cmb[:], Kx.comb[s0:s0 + ST, :].rearrange("(b p) e -> p b e", p=128), reads=[Kx.d_comb], writes=[cmb])
                for G in range(4):
                    slot0 = ((sti * 4 + G) % 2) * 4
                    for e4 in range(4):
                        e = G * 4 + e4
                        wg, wd = Wgu[slot0 + e4], Wdn[slot0 + e4]
                        for k in range(8):
                            P.dma('pool', wg[:, k, :], Kx.w_gu[l, e, k * 128:(k + 1) * 128, :], writes=[wg.children[k]])
                        for fc in range(2):
                            P.dma('pool', wd[:, fc, :], Kx.w_dn[l, e, fc * 128:(fc + 1) * 128, :], writes=[wd.children[fc]])
                    items = [(blk, e4) for blk in range(NBS) for e4 in range(4)]
                    NB3 = 3

                    def stage_a(i):
                        blk, e4 = items[i]
                        e = G * 4 + e4
                        tsl = slice(blk * 128, (blk + 1) * 128)
                        wg = Wgu[slot0 + e4]
                        gb = [C0, C1][i % 2]
                        sg_, h_ = sgb[i % NB3], hb_[i % NB3]
                        for k in range(8):
                            mm(P, gb[:, :], xT[:, k, tsl], wg[:, k, :], k == 0, k == 7, [xT, wg], [gb])
                        act(P, sg_[:], gb[:, 0:256], AF.Silu, [gb], [sg_])
                        stt(P, h_[:], gb[:, 256:512], cmb[:, blk, e:e + 1], sg_[:], ALU.mult, ALU.mult, [gb, cmb, sg_], [h_])

                    def stage_b(i):
                        tb = [D0, D1][i % 2]
                        h_, hT_ = hb_[i % NB3], hT[i % NB3]
                        tbb = tb[:, :].bitcast(BF16)
                        for fc in range(2):
                            tr(P, tbb[:, fc * 128:(fc + 1) * 128], h_[:, fc * 128:(fc + 1) * 128], identb[:], [h_, identb], [tb])
                        act(P, hT_[:], tbb[:, 0:256].rearrange("p (f t) -> p f t", f=2), AF.Copy, [tb], [hT_])

                    def stage_c(i):
                        blk, e4 = items[i]
                        wd = Wdn[slot0 + e4]
                        hT_ = hT[i % NB3]
                        ybk = [A0, A1] if blk % 2 == 0 else [B0, B1]
                        for hf in range(2):
                            for fc in range(2):
                                first = (e4 == 0 and fc == 0)
                                last = (e4 == 3 and fc == 1)
                                mm(P, ybk[hf][:, :], hT_[:, fc, :], wd[:, fc, hf * 512:(hf + 1) * 512], first, last, [hT_, wd], [ybk[hf]])
                        if e4 == 3:
                            for hf in range(2):
                                ya = yacc[:, blk, hf * 512:(hf + 1) * 512]
                                if G == 0:
                                    act(P, ya, ybk[hf][:, :], AF.Copy, [ybk[hf]], [yaccb[blk]])
                                else:
                                    tt(P, ya, ybk[hf][:, :], ya, ALU.add, [ybk[hf], yaccb[blk]], [yaccb[blk]])

                    n_it = len(items)
                    for step in range(n_it + 2):
                        if step < n_it:
                            stage_a(step)
                        if 0 <= step - 1 < n_it:
                            stage_b(step - 1)
                        if 0 <= step - 2 < n_it:
                            stage_c(step - 2)
                for blk in range(NBS):
                    tg = s0 + blk * 128
                    x_, o_ = x1b[blk % 2], ob[blk % 2]
                    P.dma('act', x_[:], Kx.x1[tg:tg + 128, :], reads=[Kx.d_x1], writes=[x_])
                    stt(P, x_[:], x_[:], float(DN_ALPHA), yacc[:, blk, :], ALU.mult, ALU.add, [x_, yaccb[blk]], [x_])
                    layer_norm_block(P, x_, stats, mv, sm2, g2, b2, [rp], o_[:], o_)
                    P.dma('sp', dst[tg:tg + 128, :], o_[:], reads=[o_], writes=[Kx.d_hres])
                    if write_hT:
                        c0b = C0[:, :].bitcast(BF16)
                        ot = oT[blk % 2]
                        for j in range(8):
                            bk = [C0, C1][j // 4]
                            tr(P, bk[:, (j % 4) * 128:(j % 4 + 1) * 128], o_[:, j * 128:(j + 1) * 128], cst[:, C_I, :], [o_, cst], [bk])
                        for hf in range(2):
                            act(P, ot[:, hf * 4:(hf + 1) * 4, :], [C0, C1][hf][:, :].rearrange("p (j t) -> p j t", j=4), AF.Copy,
                                [[C0, C1][hf]], [ot])
                        P.dma('sp', Kx.hT.rearrange("k p t -> p k t")[:, :, tg:tg + 128], ot[:], reads=[ot], writes=[Kx.d_hT])
        except StopPass:
            pass
        P.flush()


def build(T, NL, dbg=False, passes=("ssd", "gdn", "hg", "out", "moe"), layers=None):
    layers = list(range(NL)) if layers is None else layers
    nc = bass.Bass("TRN2", target_bir_lowering=False)
    Kx = K()
    ext_in = lambda name, shape: nc.dram_tensor(name, list(shape), F32, kind="ExternalInput").ap()
    Kx.x = ext_in("x", [T, D_MODEL])
    Kx.w_in = ext_in("w_in", [NL, D_MODEL, IN_COLS])
    Kx.cst_d = ext_in("cst", [128, NCONST, 128])
    Kx.ssd_pp = ext_in("ssd_pp", [NL, 128, SSD_NPP])
    Kx.ssd_rp = ext_in("ssd_rp", [NL, 128, SSD_NRP])
    Kx.gdn_pp = ext_in("gdn_pp", [NL, 128, GDN_NPP])
    Kx.gdn_rp = ext_in("gdn_rp", [NL, 128, GDN_NRP])
    Kx.hg_rp = ext_in("hg_rp", [NL, 128, HG_NRP])
    Kx.hg_lbl = ext_in("hg_lbl", [128, 4, 4])
    Kx.hg_lmask = ext_in("hg_lmask", [NL, 128, 4, 4])
    Kx.w_out = ext_in("w_out", [NL, 2048, 1024])
    Kx.out_rp = ext_in("out_rp", [NL, 128, OUT_NRP])
    Kx.wr = ext_in("wr", [NL, 128, 8, 20])
    Kx.moe_rp = ext_in("moe_rp", [NL, 128, 2048])
    Kx.w_gu = ext_in("w_gu", [NL, 16, 1024, 512])
    Kx.w_dn = ext_in("w_dn", [NL, 16, 256, 1024])
    Kx.out = nc.dram_tensor("out", [T, D_MODEL], F32, kind="ExternalOutput").ap()
    skind = "ExternalOutput" if dbg else "Internal"
    Kx.hT = nc.dram_tensor("hT", [8, 128, T], BF16, kind=skind).ap()
    Kx.ycT = nc.dram_tensor("ycT", [16, 128, T], BF16, kind=skind).ap()
    Kx.x1 = nc.dram_tensor("x1", [T, D_MODEL], F32, kind=skind).ap()
    Kx.x1T = nc.dram_tensor("x1T", [8, 128, T], BF16, kind=skind).ap()
    Kx.comb = nc.dram_tensor("comb", [T, 16], F32, kind=skind).ap()
    Kx.hres = nc.dram_tensor("hres", [T, D_MODEL], F32, kind="Internal").ap()
    Kx.d_hT = Buf("d_hT")
    Kx.d_ycT = Buf("d_ycT")
    Kx.d_x1 = Buf("d_x1")
    Kx.d_x1T = Buf("d_x1T")
    Kx.d_comb = Buf("d_comb")
    Kx.d_hres = Buf("d_hres")
    with ExitStack() as st0:
        P = Prog(nc, st0)
        Kx.bank = []
        for i in range(8):
            t = st0.enter_context(nc.psum_tensor(f"bank{i}", [128, 512], F32))
            Kx.bank.append(Buf(f"bank{i}", t))
            Kx.bank[-1].excl = True
        Kx.cst = P.sb("cst_sb", [128, NCONST, 128], F32)
        P.dma('sp', Kx.cst[:], Kx.cst_d, writes=[Kx.cst])
        for l in range(NL):
            if l == 0:
                pass_transpose_in(P, Kx, T, Kx.x, Kx.hT)
            if "ssd" in passes:
                pass_ssd(P, Kx, T, l)
            if "gdn" in passes:
                pass_gdn(P, Kx, T, l)
            if "hg" in passes:
                pass_hg(P, Kx, T, l, layers[l])
            if "out" in passes:
                pass_out(P, Kx, T, l, Kx.x if l == 0 else Kx.hres)
            if "moe" in passes:
                pass_moe(P, Kx, T, l, Kx.out if l == NL - 1 else Kx.hres, l < NL - 1)
        print("recorded ops", P.nops, "waits", P.nwaits)
    return nc


def host_params(inp, layers):
    out = {}
    NL = len(layers)
    pp = np.zeros((NL, 128, SSD_NPP), np.float32)
    rp = np.zeros((NL, 128, SSD_NRP), np.float32)
    for i, l in enumerate(layers):
        cw = inp['ssd_conv_w'][l]
        pp[i, :, 0:48] = cw.reshape(4, 12, 128).transpose(2, 1, 0).reshape(128, 48)
        pp[i, :, 48:60] = inp['ssd_conv_b'][l].reshape(12, 128).T
        rp[i, :, 0:16] = inp['ssd_dt_bias'][l][None, :]
        rp[i, :, 16:32] = inp['ssd_a_log'][l][None, :]
        rp[i, :, 32:48] = inp['ssd_d'][l][None, :]
        rp[i, :, 48:48 + 1024] = inp['ssd_norm_w'][l][None, :]
    out['ssd_pp'] = pp
    out['ssd_rp'] = rp
    gpp = np.zeros((NL, 128, GDN_NPP), np.float32)
    grp = np.zeros((NL, 128, GDN_NRP), np.float32)
    for i, l in enumerate(layers):
        gpp[i, :, 0:48] = inp['gdn_conv_w'][l].reshape(4, 12, 128).transpose(2, 1, 0).reshape(128, 48)
        grp[i, :, 0:4] = inp['gdn_dt_bias'][l][None, :]
        grp[i, :, 4:8] = inp['gdn_a_log'][l][None, :]
        grp[i, :, 8:136] = inp['gdn_norm_w'][l][None, :]
    out['gdn_pp'] = gpp
    out['gdn_rp'] = grp
    hrp = np.zeros((NL, 128, HG_NRP), np.float32)
    for i, l in enumerate(layers):
        hrp[i, :, 0:128] = inp['hg_norm_w'][l][None, :]
    out['hg_rp'] = hrp
    lmask = np.zeros((NL, 128, 4, 4), np.float32)
    for i, l in enumerate(layers):
        lmask[i, :, :, 1:l + 1] = 1.0
    out['hg_lmask'] = lmask
    out['hg_lbl'] = np.ascontiguousarray(inp['hg_lb_logits'].reshape(4, 4, 128).transpose(2, 1, 0))
    orp = np.zeros((NL, 128, OUT_NRP), np.float32)
    wr = np.zeros((NL, 128, 8, 20), np.float32)
    mrp = np.zeros((NL, 128, 2048), np.float32)
    for i, l in enumerate(layers):
        orp[i, :, 0:1024] = inp['ln1_g'][l][None, :]
        orp[i, :, 1024:2048] = inp['ln1_b'][l][None, :]
        orp[i, :, 2048:2052] = inp['b_router_group'][l][None, :]
        orp[i, :, 2052:2068] = inp['b_router_expert'][l][None, :]
        wcat = np.concatenate([inp['w_router_group'][l], inp['w_router_expert'][l]], axis=1)
        wr[i] = wcat.reshape(8, 128, 20).transpose(1, 0, 2)
        mrp[i, :, 0:1024] = inp['ln2_g'][l][None, :]
        mrp[i, :, 1024:2048] = inp['ln2_b'][l][None, :]
    out['out_rp'] = orp
    out['wr'] = wr
    out['moe_rp'] = mrp
    out['cst'] = make_consts()
    return out


def core_inputs(inp, b, T, layers):
    hp = host_params(inp, layers)
    ls = layers
    im = {
        "x": np.ascontiguousarray(inp['x'][b, :T]),
        "w_in": np.ascontiguousarray(inp['w_in'][ls]),
        "w_out": np.ascontiguousarray(inp['w_out'][ls]),
        "w_gu": np.ascontiguousarray(inp['w_expert_gate_up'][ls]),
        "w_dn": np.ascontiguousarray(inp['w_expert_down'][ls]),
    }
    im.update(hp)
    return im


_PROG = {}


def kernel(**inputs):
    inp = {k: np.asarray(v) for k, v in inputs.items()}
    B, T, _ = inp['x'].shape
    ncores = 8
    nc = build(T, DEPTH)
    base = core_inputs(inp, 0, T, list(range(DEPTH)))
    in_maps = []
    for c in range(ncores):
        m = dict(base)
        m["x"] = np.ascontiguousarray(inp['x'][c % B], dtype=np.float32)
        in_maps.append(m)
    res = run_bass_kernel_spmd(nc, in_maps, core_ids=list(range(ncores)))
    return np.stack([np.asarray(res.results[b]["out"]) for b in range(B)]).astype(np.float32)
```

```python
import numpy as np
from contextlib import ExitStack
import concourse.bass as bass
import concourse.mybir as mybir
from concourse.bass_utils import run_bass_kernel_spmd

F32 = mybir.dt.float32
BF16 = mybir.dt.bfloat16
F32R = mybir.dt.float32r
USE_F32R = True
AF = mybir.ActivationFunctionType
ALU = mybir.AluOpType
AX = mybir.AxisListType

COMPUTE = ('pe', 'dve', 'act', 'pool')
ALLENG = ('pe', 'dve', 'act', 'pool', 'sp')
QUEUES = ('sp', 'act', 'pool')
NDMA = 8

D_MODEL = 1024
IN_COLS = 6680
DEPTH = 4
DN_ALPHA = (2 * DEPTH) ** 0.25


class Buf:
    def __init__(self, name, t=None, parent=None):
        self.name = name
        self.t = t
        self.parent = parent
        self.children = []
        self.last_write = None
        self.reads = []
        self.excl = False

    def sub(self, key):
        c = Buf(f"{self.name}.{key}", self.t, self)
        self.children.append(c)
        return c

    def __getitem__(self, k):
        return self.t[k]


class Op:
    __slots__ = ('id', 'eng', 'dma', 'fn', 'deps', 'dur', 'tag', 'out', 'in_', 'kw', 'pos', 'tok', 'fin')

    def __init__(self, id, eng, dma, fn, deps, dur, tag):
        self.id = id
        self.eng = eng
        self.dma = dma
        self.fn = fn
        self.deps = deps
        self.dur = dur
        self.tag = tag
        self.tok = None
        self.fin = 0.0


SCHED_WINDOW = 64
ACT_SWITCH_US = 1.3
XLAT = 0.25
SCHED_EPS = 0.3
PASS_EPS = {'ssd': 0.3, 'gdn': 0.1, 'hg': 0.1, 'out': 0.05, 'moe': 0.0, 'p0': 0.0}


class Prog:
    def __init__(self, nc, stack):
        self.nc = nc
        self.stack = stack
        self.sem = {e: stack.enter_context(nc.semaphore(f"s_{e}")) for e in COMPUTE}
        self.cnt = {e: 0 for e in COMPUTE}
        self.known = {e: {} for e in ALLENG}
        self.dsem = {q: [stack.enter_context(nc.semaphore(f"d_{q}_{i}")) for i in range(NDMA)] for q in QUEUES}
        self.dcnt = {q: [0] * NDMA for q in QUEUES}
        self.dnext = {q: 0 for q in QUEUES}
        self.semobj = {}
        for e in COMPUTE:
            self.semobj[('c', e)] = self.sem[e]
        for q in QUEUES:
            for i in range(NDMA):
                self.semobj[('d', q, i)] = self.dsem[q][i]
        self.nops = 0
        self.nwaits = 0
        self.pend = []
        self.base = 0
        self.nid = 0
        self.nalloc = 0
        self.eps = 0.0

    def sb(self, name, shape, dtype=F32):
        self.nalloc += 1
        t = self.stack.enter_context(self.nc.sbuf_tensor(f"sb{self.nalloc}_{name}", list(shape), dtype))
        return Buf(name, t)

    def _collect(self, b, write):
        ids = []

        def add(x):
            if x.last_write is not None:
                ids.append(x.last_write)
            if write or x.excl:
                ids.extend(x.reads)
        add(b)
        p = b.parent
        while p is not None:
            add(p)
            p = p.parent

        def rec(x):
            for c in x.children:
                add(c)
                rec(c)
        rec(b)
        return ids

    def _mkdeps(self, eng, is_dma, reads, writes):
        deps = {}
        byid = self.pend_by_id
        for b in reads:
            wr = self._writers_of(b)
            for d in self._collect(b, False):
                if d < self.base:
                    continue
                D = byid[d]
                same = (D.eng == eng and not D.dma and not is_dma)
                if same and d not in wr:
                    deps[d] = deps.get(d, False)
                else:
                    deps[d] = True
        for b in writes:
            for d in self._collect(b, True):
                if d < self.base:
                    continue
                D = byid[d]
                same = (D.eng == eng and not D.dma and not is_dma)
                if same and eng == 'pe':
                    deps[d] = deps.get(d, False)
                else:
                    deps[d] = True
        return deps

    def _writers_of(self, b):
        w = set()

        def add(x):
            if x.last_write is not None:
                w.add(x.last_write)
        add(b)
        p = b.parent
        while p is not None:
            add(p)
            p = p.parent

        def rec(x):
            for c in x.children:
                add(c)
                rec(c)
        rec(b)
        return w

    @property
    def pend_by_id(self):
        return self._byid

    def _record(self, oid, reads, writes):
        for b in reads:
            b.reads.append(oid)
        for b in writes:
            b.last_write = oid
            b.reads = []

            def rec(x):
                for c in x.children:
                    c.last_write = None
                    c.reads = []
                    rec(c)
            rec(b)

    def _new(self, eng, dma, fn, reads, writes, dur, tag):
        if not hasattr(self, '_byid'):
            self._byid = {}
        deps = self._mkdeps(eng, dma, reads, writes)
        o = Op(self.nid, eng, dma, fn, deps, dur, tag)
        self.nid += 1
        self.pend.append(o)
        self._byid[o.id] = o
        self._record(o.id, reads, writes)
        self.nops += 1
        return o

    def op(self, eng, fn, reads=(), writes=(), dur=0.3, tag=None):
        return self._new(eng, False, fn, reads, writes, dur, tag)

    def dma(self, q, out, in_, reads=(), writes=(), dur=3.0, **kw):
        o = self._new(q, True, None, reads, writes, dur, None)
        o.out, o.in_, o.kw = out, in_, kw
        return o

    def _schedule(self):
        ops = self.pend
        if not ops:
            return {e: [] for e in ALLENG}
        queues = {e: [] for e in ALLENG}
        for o in ops:
            queues[o.eng].append(o)
        head = {e: 0 for e in ALLENG}
        done = set()
        free = {e: 0.0 for e in ALLENG}
        cur_tag = None
        order = {e: [] for e in ALLENG}
        scheduled = {}
        remaining = len(ops)
        base = self.base
        taken = {e: set() for e in ALLENG}
        tail = {o.id: o.dur for o in ops}
        byid_ = {o.id: o for o in ops}
        for o in reversed(ops):
            to = tail[o.id]
            for d, w in o.deps.items():
                if d < base:
                    continue
                t = byid_[d].dur + (XLAT if w else 0.0) + to
                if t > tail[d]:
                    tail[d] = t
        while remaining:
            best = None
            for e in ALLENG:
                q = queues[e]
                n = 0
                i = head[e]
                cands = []
                while i < len(q) and n < SCHED_WINDOW:
                    o = q[i]
                    i += 1
                    if o.id in done:
                        continue
                    n += 1
                    ok = True
                    rdy = 0.0
                    for d, w in o.deps.items():
                        if d < base:
                            continue
                        if d not in done:
                            ok = False
                            break
                        f = scheduled[d]
                        rdy = max(rdy, f + (XLAT if w else 0.0))
                    if not ok:
                        continue
                    st = max(rdy, free[e])
                    if e == 'act' and not o.dma and o.tag is not None and cur_tag is not None and o.tag != cur_tag:
                        st += ACT_SWITCH_US
                    cands.append((st, o))
                if not cands:
                    continue
                m = min(c[0] for c in cands)
                pick = None
                for st, o in cands:
                    if st <= m + self.eps:
                        if pick is None or tail[o.id] > tail[pick[1].id]:
                            pick = (st, o)
                key = (pick[0], pick[1].id)
                if best is None or key < best[0]:
                    best = (key, e, pick[1])
            assert best is not None, "scheduler deadlock (cyclic deps?)"
            (st, _), e, o = best
            if o.dma:
                free[e] = st + 0.15
                fin = st + o.dur
            else:
                free[e] = st + o.dur
                fin = st + o.dur
                if e == 'act' and o.tag is not None:
                    cur_tag = o.tag
            scheduled[o.id] = fin
            done.add(o.id)
            order[e].append(o)
            remaining -= 1
            q = queues[e]
            while head[e] < len(q) and q[head[e]].id in done:
                head[e] += 1
        self.est_us = max(scheduled.values()) if scheduled else 0.0
        return order

    def _wait(self, lst, eng, tok):
        key, val = tok
        if self.known[eng].get(key, 0) >= val:
            return
        self.known[eng][key] = val
        sem = self.semobj[key]
        lst.append(lambda e, sem=sem, val=val: e.wait_ge(sem, val))
        self.nwaits += 1

    def flush(self):
        order = self._schedule()
        for e in ALLENG:
            for o in order[e]:
                if o.dma:
                    i = self.dnext[e]
                    self.dnext[e] = (i + 1) % NDMA
                    o.pos = (i, self.dcnt[e][i])
                    self.dcnt[e][i] += 16
                    o.tok = (('d', e, i), self.dcnt[e][i])
                else:
                    self.cnt[e] += 1
                    o.tok = (('c', e), self.cnt[e])
        byid = self._byid
        lists = {e: [] for e in ALLENG}
        for e in ALLENG:
            lst = lists[e]
            for o in order[e]:
                for d, w in o.deps.items():
                    if d < self.base or not w:
                        continue
                    self._wait(lst, e, byid[d].tok)
                if o.dma:
                    i, prev = o.pos
                    if prev > 0:
                        self._wait(lst, e, (('d', e, i), prev))
                    sem = self.dsem[e][i]
                    lst.append(lambda en, o=o, sem=sem: en.dma_start(out=o.out, in_=o.in_, **o.kw).then_inc(sem, 16))
                else:
                    sem = self.sem[e]
                    lst.append(lambda en, o=o, sem=sem: o.fn(en).then_inc(sem, 1))
        for e in ALLENG:
            for c in COMPUTE:
                if self.cnt[c] > 0 and c != e:
                    self._wait(lists[e], e, (('c', c), self.cnt[c]))
            for q in QUEUES:
                for i in range(NDMA):
                    if self.dcnt[q][i] > 0:
                        self._wait(lists[e], e, (('d', q, i), self.dcnt[q][i]))
        self.pend = []
        self.base = self.nid
        self._byid = {}
        nc = self.nc
        with nc.Block() as block:
            @block.sync
            def _(e):
                for f in lists['sp']:
                    f(e)

            @block.tensor
            def _(e):
                for f in lists['pe']:
                    f(e)

            @block.vector
            def _(e):
                for f in lists['dve']:
                    f(e)

            @block.scalar
            def _(e):
                for f in lists['act']:
                    f(e)

            @block.gpsimd
            def _(e):
                for f in lists['pool']:
                    f(e)


C_I, C_LE, C_GT, C_ONES, C_LE64, C_GT64, C_LT64, C_SAME64, C_SEL0, C_SEL1 = range(10)
NCONST = 10


def make_consts():
    k = np.arange(128)[:, None]
    l = np.arange(128)[None, :]
    c = np.zeros((128, NCONST, 128), np.float32)
    c[:, C_I] = (k == l)
    c[:, C_LE] = (k <= l)
    c[:, C_GT] = (k > l)
    c[:, C_ONES] = 1.0
    same64 = (k // 64) == (l // 64)
    c[:, C_LE64] = (k <= l) & same64
    c[:, C_GT64] = (k > l) & same64
    c[:, C_LT64] = (k < l) & same64
    c[:, C_SAME64] = same64
    c[:, C_SEL0] = (k < 64) & (l >= 0)
    c[:, C_SEL1] = (k >= 64) & (l >= 0)
    return c


class K:
    pass


STAGE = 99


class StopPass(Exception):
    pass


def chk(n):
    if STAGE == n:
        raise StopPass()


def _fsz(ap):
    n = 1
    for d in ap.shape[1:]:
        n *= d
    return n


def _is32(ap):
    return ap.dtype == F32


ACT_TAG = {}


def _act_tag(func):
    if func == AF.Silu:
        return 'silu'
    if func == AF.Sigmoid:
        return 'sig'
    if func in (AF.Exp, AF.Ln):
        return 'lnexp'
    return None


def mm(P, out, lhsT, rhs, start, stop, reads, writes):
    passes = 4 if _is32(rhs) else 1
    dur = passes * max(_fsz(rhs), 64) / 2400.0 + passes * max(_fsz(lhsT), 32) / 2400.0 * 0.5 + 0.02
    return P.op('pe', lambda e: e.matmul(out, lhsT=lhsT, rhs=rhs, start=start, stop=stop), reads, writes, dur=dur)


def mmx(P, out, lhsT, rhs, start, stop, reads, writes):
    dur = max(_fsz(rhs), 64) / 2400.0 + max(_fsz(lhsT), 32) / 4800.0 + 0.02
    return P.op('pe', lambda e: e.matmul(out, lhsT=lhsT, rhs=rhs, start=start, stop=stop, skip_group_check=True), reads, writes, dur=dur)


def mmr(P, out, lhsT, rhs, start, stop, reads, writes):
    passes = 4
    if USE_F32R:
        lhsT = lhsT.bitcast(F32R)
        rhs = rhs.bitcast(F32R)
        passes = 1
    dur = passes * (max(_fsz(rhs), 64) / 2400.0 + max(_fsz(lhsT), 32) / 4800.0) + 0.02
    return P.op('pe', lambda e: e.matmul(out, lhsT=lhsT, rhs=rhs, start=start, stop=stop), reads, writes, dur=dur)


def rr(ap):
    return ap.bitcast(F32R) if USE_F32R else ap


def tr(P, out, in_, ident, reads, writes):
    passes = 4 if _is32(in_) else 1
    dur = passes * (max(_fsz(in_), 64) / 2400.0) * 1.5 + 0.02
    return P.op('pe', lambda e: e.transpose(out, in_, ident), reads, writes, dur=dur)


def act(P, out, in_, func, reads, writes, **kw):
    dur = _fsz(in_) / 1400.0 + 0.2 + (0.1 if ('scale' in kw and not isinstance(kw['scale'], float)) else 0.0)
    return P.op('act', lambda e: e.activation(out=out, in_=in_, func=func, **kw), reads, writes, dur=dur, tag=_act_tag(func))


def tt(P, out, in0, in1, op, reads, writes):
    dur = _fsz(out) / 960.0 + 0.12
    return P.op('dve', lambda e: e.tensor_tensor(out=out, in0=in0, in1=in1, op=op), reads, writes, dur=dur)


def ts(P, out, in0, s1, s2, op0, op1, reads, writes, **kw):
    dur = _fsz(out) / 1400.0 + 0.12
    if op1 is None:
        return P.op('dve', lambda e: e.tensor_scalar(out=out, in0=in0, scalar1=s1, scalar2=None, op0=op0, **kw), reads, writes, dur=dur)
    return P.op('dve', lambda e: e.tensor_scalar(out=out, in0=in0, scalar1=s1, scalar2=s2, op0=op0, op1=op1, **kw), reads, writes, dur=dur)


def stt(P, out, in0, scalar, in1, op0, op1, reads, writes):
    dur = _fsz(out) / 960.0 + 0.12
    return P.op('dve', lambda e: e.scalar_tensor_tensor(out=out, in0=in0, scalar=scalar, in1=in1, op0=op0, op1=op1), reads, writes, dur=dur)


def load_weights(P, dst, src2d, ncols, nk, order=None):
    nb = (ncols + 511) // 512
    if not dst.children:
        for i in range(nb):
            dst.sub(i)
    order = list(range(nb)) if order is None else order
    src3 = src2d.rearrange("(k p) c -> p k c", p=128)
    for i in order:
        c0, c1 = i * 512, min(ncols, (i + 1) * 512)
        P.dma('pool', dst[:, :, c0:c1], src3[:, :, c0:c1], writes=[dst.children[i]])


def wsub(W, col):
    return W.children[col // 512]


SSD_NPP = 12 * 4 + 12
SSD_NRP = 16 + 16 + 16 + 1024


def pass_transpose_in(P, Kx, T, src, dstT):
    nc = P.nc
    P.eps = PASS_EPS['p0']
    with ExitStack() as st:
        P.stack = st
        xt = [P.sb(f"p0_x{i}", [128, 1024], F32) for i in range(2)]
        xb = [P.sb(f"p0_b{i}", [128, 8, 128], BF16) for i in range(2)]
        for b in range(T // 128):
            x_ = xt[b % 2]
            o_ = xb[b % 2]
            P.dma('sp', x_[:], src[b * 128:(b + 1) * 128, :], writes=[x_])
            for j in range(8):
                bk = Kx.bank[j // 4]
                tr(P, bk[:, (j % 4) * 128:(j % 4 + 1) * 128], x_[:, j * 128:(j + 1) * 128], Kx.cst[:, C_I, :],
                   [x_, Kx.cst], [bk])
            for hlf in range(2):
                act(P, o_[:, hlf * 4:(hlf + 1) * 4, :], Kx.bank[hlf][:, :].rearrange("p (j t) -> p j t", j=4),
                    AF.Copy, [Kx.bank[hlf]], [o_])
            P.dma('sp', dstT.rearrange("k p t -> p k t")[:, :, b * 128:(b + 1) * 128], o_[:], reads=[o_], writes=[Kx.d_hT])
        P.flush()


def pass_ssd(P, Kx, T, l, dbg=None):
    nc = P.nc
    P.eps = PASS_EPS['ssd']
    SBT = min(512, T)
    NSB = T // SBT
    NBLK = SBT // 128
    cst = Kx.cst
    bank = Kx.bank
    A0, A1, B0, B1, C0, C1, D0, D1 = bank
    with ExitStack() as st:
        P.stack = st
        try:
            W = P.sb("ssd_W", [128, 8, 2576], BF16)
            pp = P.sb("ssd_pp", [128, SSD_NPP], F32)
            rp = P.sb("ssd_rp", [128, SSD_NRP], F32)
            hTb = [P.sb(f"ssd_hT{i}", [128, 8, SBT], BF16) for i in range(2)]
            xin = P.sb("ssd_xin", [128, 12, SBT + 3], F32)
            accs = [P.sb(f"ssd_acc{i}", [128, SBT], F32) for i in range(6)]
            xc = P.sb("ssd_xc", [128, 12, SBT], BF16)
            xcj = [xc.sub(j) for j in range(12)]
            xinj = [xin.sub(j) for j in range(12)]
            Dmat = P.sb("ssd_Dmat", [128, 16, 128], BF16)
            arep = P.sb("ssd_arep", [128, 16], F32)
            smP = [P.sb(f"ssd_sm{i}", [128, 16 * 12], F32) for i in range(2)]
            R = P.sb("ssd_R", [128, 16, 128], F32)
            DT = P.sb("ssd_DT", [128, 16, 128], BF16)
            GTP = [P.sb(f"ssd_GT{i}", [128, 16, 128], BF16) for i in range(2)]
            CBTm = P.sb("ssd_CBTm", [128, 2, 128], BF16)
            xsbP = [P.sb(f"ssd_xsb{i}", [128, 1024], BF16) for i in range(2)]
            xdtP = [P.sb(f"ssd_xdt{i}", [128, 1024], BF16) for i in range(2)]
            xendP = [P.sb(f"ssd_xend{i}", [128, 1024], BF16) for i in range(2)]
            BtmP = [P.sb(f"ssd_Btm{i}", [128, 256], BF16) for i in range(2)]
            szP = [P.sb(f"ssd_sz{i}", [128, 1024], F32) for i in range(2)]
            yoff = P.sb("ssd_yoff", [128, 1024], F32)
            y = P.sb("ssd_y", [128, 1024], F32)
            junk = P.sb("ssd_junk", [128, 512], F32)
            ybf = P.sb("ssd_ybf", [128, 1024], BF16)
            yT = [P.sb(f"ssd_yT{i}", [128, 8, 128], BF16) for i in range(2)]
            S32 = P.sb("ssd_S32", [128, 1024], F32)
            Sbf = P.sb("ssd_Sbf", [128, 1024], BF16)
            identb = P.sb("ssd_identb", [128, 128], BF16)

            load_weights(P, W, Kx.w_in[l, :, 0:2576], 2576, 8, order=[2, 3, 4, 0, 1, 5])
            P.dma('sp', pp[:], Kx.ssd_pp[l], writes=[pp])
            P.dma('sp', rp[:], Kx.ssd_rp[l], writes=[rp])
            dtb = rp[:, 0:16]
            alog = rp[:, 16:32]
            drep = rp[:, 32:48]
            normw = rp[:, 48:48 + 1024]
            cw = lambda j, k: pp[:, j * 4 + k: j * 4 + k + 1]
            cb = lambda j: pp[:, 48 + j: 48 + j + 1]
            act(P, identb[:], cst[:, C_I, :], AF.Copy, [cst], [identb])
            act(P, arep[:], alog, AF.Exp, [rp], [arep])
            ts(P, arep[:], arep[:], -1.0, None, ALU.mult, None, [arep], [arep])
            tt(P, Dmat[:], cst[:, C_I, :].unsqueeze(1).to_broadcast([128, 16, 128]),
               drep.unsqueeze(2).to_broadcast([128, 16, 128]), ALU.mult, [cst, rp], [Dmat])
            P.op('dve', lambda e: e.memset(S32[:], 0.0), [], [S32])
            P.op('dve', lambda e: e.memset(Sbf[:], 0.0), [], [Sbf])
            P.op('dve', lambda e: e.memset(xin[:], 0.0), [], [xin])
            chk(1)

            for sbi in range(NSB):
                hb = hTb[sbi % 2]
                P.dma('sp', hb[:], Kx.hT.rearrange("k p t -> p k t")[:, :, sbi * SBT:(sbi + 1) * SBT], reads=[Kx.d_hT], writes=[hb])
                banks4 = [D0, D1, C0, C1]
                for half in range(2):
                    js = range(half * 6, half * 6 + 6)
                    for j in js:
                        bk = banks4[j % 4]
                        for k in range(8):
                            mm(P, bk[:, 0:SBT], W[:, k, 1024 + j * 128: 1024 + (j + 1) * 128], hb[:, k, :], k == 0, k == 7,
                               [wsub(W, 1024 + j * 128), hb], [bk])
                        ac = accs[j % 6]
                        act(P, xin[:, j, 3:3 + SBT], bk[:, 0:SBT], AF.Copy, [bk], [xinj[j]])
                        act(P, ac[:], bk[:, 0:SBT], AF.Identity, [bk, pp], [ac], scale=cw(j, 3), bias=cb(j))
                    for j in js:
                        ac = accs[j % 6]
                        for k in (2, 1, 0):
                            stt(P, ac[:], xin[:, j, k:k + SBT], cw(j, k), ac[:], ALU.mult, ALU.add, [xinj[j], pp, ac], [ac])
                    for j in js:
                        ac = accs[j % 6]
                        act(P, xc[:, j, :], ac[:], AF.Silu, [ac], [xcj[j]])
                        act(P, xin[:, j, 0:3], xin[:, j, SBT:SBT + 3], AF.Copy, [xinj[j]], [xinj[j]])
                chk(2)
                for blk in range(NBLK):
                    t0 = blk * 128
                    tg = sbi * SBT + t0
                    tsl = slice(t0, t0 + 128)
                    par = blk % 2
                    sm = smP[par]
                    smv = lambda i, sm=sm: sm[:, i * 16:(i + 1) * 16]
                    GT, xsb, xdt, xend, Btm, sz = GTP[par], xsbP[par], xdtP[par], xendP[par], BtmP[par], szP[par]
                    for hf in range(2):
                        bk = [D0, D1][hf]
                        for k in range(8):
                            mm(P, bk[:, :], hb[:, k, t0:t0 + 128], W[:, k, hf * 512:(hf + 1) * 512], k == 0, k == 7, [hb, wsub(W, hf * 512)], [bk])
                        act(P, sz[:, hf * 512:(hf + 1) * 512], bk[:, :], AF.Silu, [bk], [sz])
                    for k in range(8):
                        mm(P, C0[:, 0:16], hb[:, k, t0:t0 + 128], W[:, k, 2560:2576], k == 0, k == 7, [hb, wsub(W, 2560)], [C0])
                    xr, xm, ex, lg, dt, dA, acs, te, dte, ea, cd = [smv(i) for i in range(11)]
                    tt(P, xr, C0[:, 0:16], dtb, ALU.add, [C0, rp], [sm])
                    ts(P, xm, xr, 30.0, None, ALU.min, None, [sm], [sm])
                    act(P, ex, xm, AF.Exp, [sm], [sm])
                    act(P, lg, ex, AF.Ln, [sm], [sm], bias=1.0)
                    tt(P, dt, lg, xr, ALU.max, [sm], [sm])
                    tt(P, dA, dt, arep[:], ALU.mult, [sm, arep], [sm])
                    mm(P, C0[:, 16:32], cst[:, C_LE, :], dA, True, True, [cst, sm], [C0])
                    mm(P, C0[:, 32:48], cst[:, C_ONES, :], dA, True, True, [cst, sm], [C0])
                    act(P, ea, C0[:, 16:32], AF.Exp, [C0], [sm])
                    act(P, cd, C0[:, 32:48], AF.Exp, [C0], [sm])
                    act(P, acs, C0[:, 16:32], AF.Copy, [C0], [sm])
                    tt(P, te, C0[:, 32:48], acs, ALU.subtract, [C0, sm], [sm])
                    act(P, te, te, AF.Exp, [sm], [sm])
                    tt(P, dte, dt, te, ALU.mult, [sm], [sm])
                    chk(3)
                    tt(P, R[:], cst[:, C_LE, :].unsqueeze(1).to_broadcast([128, 16, 128]),
                       dA.unsqueeze(2).to_broadcast([128, 16, 128]), ALU.mult, [cst, sm], [R])
                    for q in range(4):
                        bk = [D0, D1][q % 2]
                        mm(P, bk[:, :], cst[:, C_GT, :], R[:, 4 * q:4 * q + 4, :], True, True, [cst, R], [bk])
                        act(P, DT[:, 4 * q:4 * q + 4, :], bk[:, :].rearrange("p (h l) -> p h l", h=4), AF.Exp, [bk], [DT])
                    chk(4)
                    d1b = D1[:, :].bitcast(BF16)
                    for j in range(8):
                        tr(P, d1b[:, j * 128:(j + 1) * 128], xc[:, j, tsl], identb[:], [xcj[j], identb], [D1])
                    act(P, xsb[:], d1b[:, :], AF.Copy, [D1], [xsb])
                    pv = d1b[:, :].rearrange("p (h c) -> p h c", h=16)
                    tt(P, xdt[:].rearrange("p (h c) -> p h c", h=16), pv,
                       dt.unsqueeze(2).to_broadcast([128, 16, 64]), ALU.mult, [D1, sm], [xdt])
                    tt(P, xend[:].rearrange("p (h c) -> p h c", h=16), pv,
                       dte.unsqueeze(2).to_broadcast([128, 16, 64]), ALU.mult, [D1, sm], [xend])
                    chk(4.2)
                    c0b = C0[:, :].bitcast(BF16)
                    for g in range(2):
                        tr(P, c0b[:, 256 + g * 128:256 + (g + 1) * 128], xc[:, 8 + g, tsl], identb[:], [xcj[8 + g], identb], [C0])
                    act(P, Btm[:], c0b[:, 256:512], AF.Copy, [C0], [Btm])
                    chk(5)
                    for g in range(2):
                        mm(P, C1[:, g * 128:(g + 1) * 128], xc[:, 8 + g, tsl], xc[:, 10 + g, tsl], True, True, [xcj[8 + g], xcj[10 + g]], [C1])
                    tt(P, CBTm[:], C1[:, 0:256].rearrange("p (g l) -> p g l", g=2),
                       cst[:, C_LE, :].unsqueeze(1).to_broadcast([128, 2, 128]), ALU.mult, [C1, cst], [CBTm])
                    for g in range(2):
                        tt(P, GT[:, g * 8:(g + 1) * 8, :], DT[:, g * 8:(g + 1) * 8, :],
                           CBTm[:, g, :].unsqueeze(1).to_broadcast([128, 8, 128]), ALU.mult, [DT, CBTm], [GT])
                    chk(6)
                    for h in range(16):
                        bk = [A0, A1][h // 8]
                        o = bk[:, (h % 8) * 64:(h % 8 + 1) * 64]
                        mm(P, o, GT[:, h, :], xdt[:, h * 64:(h + 1) * 64], True, False, [GT, xdt], [bk])
                        mm(P, o, Dmat[:, h, :], xsb[:, h * 64:(h + 1) * 64], False, True, [Dmat, xsb], [bk])
                    for g in range(2):
                        bk = [B0, B1][g]
                        mm(P, bk[:, :], xc[:, 10 + g, tsl], Sbf[:, g * 512:(g + 1) * 512], True, True, [xcj[10 + g], Sbf], [bk])
                        tt(P, yoff[:, g * 512:(g + 1) * 512].rearrange("p (h c) -> p h c", h=8),
                           bk[:, :].rearrange("p (h c) -> p h c", h=8),
                           ea[:, g * 8:(g + 1) * 8].unsqueeze(2).to_broadcast([128, 8, 64]), ALU.mult, [bk, sm], [yoff])
                        tt(P, y[:, g * 512:(g + 1) * 512], [A0, A1][g][:, :], yoff[:, g * 512:(g + 1) * 512], ALU.add,
                           [[A0, A1][g], yoff], [y])
                    tt(P, y[:], y[:], sz[:], ALU.mult, [y, sz], [y])
                    chk(7)
                    ss = smv(11)
                    for g in range(2):
                        P.op('act', lambda e, g=g, sm=sm: e.activation(out=junk[:], in_=y[:, g * 512:(g + 1) * 512], func=AF.Square,
                                                               accum_out=sm[:, 176 + g:177 + g]), [y], [junk, sm])
                    act(P, sm[:, 178:180], sm[:, 176:178], AF.Ln, [sm], [sm], scale=1.0 / 512, bias=1e-6)
                    act(P, sm[:, 178:180], sm[:, 178:180], AF.Exp, [sm], [sm], scale=-0.5)
                    for g in range(2):
                        stt(P, ybf[:, g * 512:(g + 1) * 512], y[:, g * 512:(g + 1) * 512], sm[:, 178 + g:179 + g],
                            normw[:, g * 512:(g + 1) * 512], ALU.mult, ALU.mult, [y, sm, rp], [ybf])
                    a0b = A0[:, :].bitcast(BF16)
                    for j in range(8):
                        tr(P, a0b[:, j * 128:(j + 1) * 128], ybf[:, j * 128:(j + 1) * 128], identb[:], [ybf, identb], [A0])
                    yt = yT[(sbi * NBLK + blk) % 2]
                    act(P, yt[:], a0b.rearrange("p (j t) -> p j t", j=8), AF.Copy, [A0], [yt])
                    P.dma('sp', Kx.ycT.rearrange("k p t -> p k t")[:, 0:8, tg:tg + 128], yt[:], reads=[yt], writes=[Kx.d_ycT])
                    chk(8)
                    for g in range(2):
                        bk = [B0, B1][g]
                        mm(P, bk[:, :], Btm[:, g * 128:(g + 1) * 128], xend[:, g * 512:(g + 1) * 512], True, True, [Btm, xend], [bk])
                    tt(P, S32[:].rearrange("p (h c) -> p h c", h=16), S32[:].rearrange("p (h c) -> p h c", h=16),
                       cd.unsqueeze(2).to_broadcast([128, 16, 64]), ALU.mult, [S32, sm], [S32])
                    for g in range(2):
                        tt(P, S32[:, g * 512:(g + 1) * 512], [B0, B1][g][:, :], S32[:, g * 512:(g + 1) * 512], ALU.add,
                           [[B0, B1][g], S32], [S32])
                    act(P, Sbf[:], S32[:], AF.Copy, [S32], [Sbf])
        except StopPass:
            pass
        P.flush()


GDN_NPP = 48
GDN_NRP = 4 + 4 + 128
GDN_BASE = 2576


def pass_gdn(P, Kx, T, l):
    P.eps = PASS_EPS['gdn']
    SBT = min(512, T)
    NSB = T // SBT
    NBLK = SBT // 128
    cst = Kx.cst
    A0, A1, B0, B1, C0, C1, D0, D1 = Kx.bank
    b4 = lambda ap: ap.rearrange("p (h c) -> p h c", h=4)
    with ExitStack() as st:
        P.stack = st
        try:
            W = P.sb("gdn_W", [128, 8, 2056], BF16)
            pp = P.sb("gdn_pp", [128, GDN_NPP], F32)
            rp = P.sb("gdn_rp", [128, GDN_NRP], F32)
            hTb = [P.sb(f"gdn_hT{i}", [128, 8, SBT], BF16) for i in range(2)]
            xin = P.sb("gdn_xin", [128, 12, SBT + 3], F32)
            xinj = [xin.sub(j) for j in range(12)]
            accs = [P.sb(f"gdn_acc{i}", [128, SBT], F32) for i in range(8)]
            sgS = P.sb("gdn_sgS", [128, NBLK, 512], F32)
            xc = P.sb("gdn_xc", [128, 12, SBT], F32)
            xcj = [xc.sub(j) for j in range(12)]
            qT = P.sb("gdn_qT", [128, 4, SBT], BF16)
            kT = P.sb("gdn_kT", [128, 4, SBT], BF16)
            identb = P.sb("gdn_identb", [128, 128], BF16)
            arep = P.sb("gdn_arep", [128, 4], F32)
            smP = [P.sb(f"gdn_sm{i}", [128, 4 * 20], F32) for i in range(2)]
            RP = [P.sb(f"gdn_R{i}", [128, 4, 128], F32) for i in range(2)]
            DecP = [P.sb(f"gdn_Dec{i}", [128, 4, 128], F32) for i in range(2)]
            DecUP = [P.sb(f"gdn_DecU{i}", [128, 4, 128], F32) for i in range(2)]
            t1P = [P.sb(f"gdn_t1{i}", [128, 4, 128], F32) for i in range(2)]
            qkTmP = [P.sb(f"gdn_qkTm{i}", [128, 4, 128], BF16) for i in range(2)]
            YaP = [[P.sb(f"gdn_Y{p}{i}", [128, 4, 128], F32) for i in range(2)] for p in range(2)]
            ZaP = [[P.sb(f"gdn_Z{p}{i}", [128, 4, 128], F32) for i in range(2)] for p in range(2)]
            VP = [P.sb(f"gdn_V{i}", [128, 4, 128], F32) for i in range(2)]
            VbfP = [P.sb(f"gdn_Vbf{i}", [128, 4, 128], BF16) for i in range(2)]
            vtmP = [P.sb(f"gdn_vtm{i}", [128, 4, 128], F32) for i in range(2)]
            kdP = [P.sb(f"gdn_kd{i}", [128, 4, 128], BF16) for i in range(2)]
            rhs2 = P.sb("gdn_rhs2", [128, 4, 128], BF16)
            vnew = P.sb("gdn_vnew", [128, 4, 128], BF16)
            As = P.sb("gdn_As", [128, 4, 128], F32)
            o = P.sb("gdn_o", [128, 4, 128], F32)
            junk = P.sb("gdn_junk", [128, 128], F32)
            ybf = P.sb("gdn_ybf", [128, 4, 128], BF16)
            yT = [P.sb(f"gdn_yT{i}", [128, 4, 128], BF16) for i in range(2)]
            S32 = P.sb("gdn_S32", [128, 4, 128], F32)
            Sbf = P.sb("gdn_Sbf", [128, 4, 128], BF16)

            load_weights(P, W, Kx.w_in[l, :, GDN_BASE:GDN_BASE + 2056], 2056, 8)
            P.dma('sp', pp[:], Kx.gdn_pp[l], writes=[pp])
            P.dma('sp', rp[:], Kx.gdn_rp[l], writes=[rp])
            dtb = rp[:, 0:4]
            alog = rp[:, 4:8]
            normw = rp[:, 8:136]
            cw = lambda j, k: pp[:, j * 4 + k: j * 4 + k + 1]
            act(P, identb[:], cst[:, C_I, :], AF.Copy, [cst], [identb])
            act(P, arep[:], alog, AF.Exp, [rp], [arep])
            ts(P, arep[:], arep[:], -1.0, None, ALU.mult, None, [arep], [arep])
            P.op('dve', lambda e: e.memset(S32[:], 0.0), [], [S32])
            P.op('dve', lambda e: e.memset(Sbf[:], 0.0), [], [Sbf])
            P.op('dve', lambda e: e.memset(xin[:], 0.0), [], [xin])
            chk(1)
            for sbi in range(NSB):
                hb = hTb[sbi % 2]
                P.dma('sp', hb[:], Kx.hT.rearrange("k p t -> p k t")[:, :, sbi * SBT:(sbi + 1) * SBT], reads=[Kx.d_hT], writes=[hb])
                banks4 = [D0, D1, C0, C1]

                def conv_tail(js):
                    for j in js:
                        ac = accs[j % 8]
                        for k in (2, 1, 0):
                            stt(P, ac[:], xin[:, j, k:k + SBT], cw(j, k), ac[:], ALU.mult, ALU.add, [xinj[j], pp, ac], [ac])
                    for j in js:
                        ac = accs[j % 8]
                        act(P, xc[:, j, :], ac[:], AF.Silu, [ac], [xcj[j]])
                        act(P, xin[:, j, 0:3], xin[:, j, SBT:SBT + 3], AF.Copy, [xinj[j]], [xinj[j]])

                for j in range(12):
                    bk = banks4[j % 4]
                    for k in range(8):
                        mm(P, bk[:, 0:SBT], W[:, k, j * 128:(j + 1) * 128], hb[:, k, :], k == 0, k == 7, [wsub(W, j * 128), hb], [bk])
                    ac = accs[j % 8]
                    act(P, xin[:, j, 3:3 + SBT], bk[:, 0:SBT], AF.Copy, [bk], [xinj[j]])
                    act(P, ac[:], bk[:, 0:SBT], AF.Copy, [bk, pp], [ac], scale=cw(j, 3))
                    if j == 7:
                        conv_tail(range(0, 8))
                conv_tail(range(8, 12))
                for j in range(0):
                    ac = accs[j]
                    for k in (2, 1, 0):
                        stt(P, ac[:], xin[:, j, k:k + SBT], cw(j, k), ac[:], ALU.mult, ALU.add, [xinj[j], pp, ac], [ac])
                for blk in range(NBLK):
                    bk = banks4[blk % 4]
                    for k in range(8):
                        mm(P, bk[:, :], hb[:, k, blk * 128:(blk + 1) * 128], W[:, k, 1536:2048], k == 0, k == 7, [hb, wsub(W, 1536)], [bk])
                    act(P, sgS[:, blk, :], bk[:, :], AF.Silu, [bk], [sgS])
                for j in range(8):
                    act(P, accs[j][:], xc[:, j, :], AF.Square, [xcj[j]], [accs[j]])
                for j in range(8):
                    bk2 = banks4[j % 4]
                    r_ = accs[j]
                    mm(P, bk2[:, 0:SBT], cst[:, C_ONES, :], r_[:], True, True, [cst, r_], [bk2])
                    act(P, r_[:], bk2[:, 0:SBT], AF.Ln, [bk2], [r_], bias=1e-6)
                for j in range(8):
                    r_ = accs[j]
                    act(P, r_[:], r_[:], AF.Exp, [r_], [r_], scale=-0.5,
                        bias=(-0.5 * float(np.log(128.0))) if j < 4 else 0.0)
                    dst = qT if j < 4 else kT
                    tt(P, dst[:, j % 4, :], xc[:, j, :], r_[:], ALU.mult, [xcj[j], r_], [dst])
                chk(2)
                def h1(blk, par):
                    t0 = blk * 128
                    tsl = slice(t0, t0 + 128)
                    sm = smP[par]
                    smv = lambda i: sm[:, i * 4:(i + 1) * 4]
                    Vbf, qkTm, kd, v_tm = VbfP[par], qkTmP[par], kdP[par], vtmP[par]
                    R, Dec, DecU, t1, Ya, Za, V = RP[par], DecP[par], DecUP[par], t1P[par], YaP[par], ZaP[par], VP[par]
                    for k in range(8):
                        mm(P, C0[:, 0:8], hb[:, k, tsl], W[:, k, 2048:2056], k == 0, k == 7, [hb, wsub(W, 2048)], [C0])
                    eb, beta, xr, xm, ex, lg, sp, la, eg, gs, ekd, negeg = [smv(i) for i in range(12)]
                    glrep = sm[:, 48:56]
                    act(P, eb, C0[:, 0:4], AF.Exp, [C0], [sm], scale=-1.0)
                    ts(P, eb, eb, 1.0, None, ALU.add, None, [sm], [sm])
                    P.op('dve', lambda e: e.reciprocal(out=beta, in_=eb), [sm], [sm])
                    tt(P, xr, C0[:, 4:8], dtb, ALU.add, [C0, rp], [sm])
                    ts(P, xm, xr, 30.0, None, ALU.min, None, [sm], [sm])
                    yield
                    act(P, ex, xm, AF.Exp, [sm], [sm])
                    act(P, lg, ex, AF.Ln, [sm], [sm], bias=1.0)
                    tt(P, sp, lg, xr, ALU.max, [sm], [sm])
                    tt(P, la, sp, arep[:], ALU.mult, [sm, arep], [sm])
                    yield
                    mm(P, C0[:, 8:12], cst[:, C_LE64, :], la, True, True, [cst, sm], [C0])
                    mm(P, C0[:, 12:16], cst[:, C_SAME64, :], la, True, True, [cst, sm], [C0])
                    mm(P, C0[:, 16:20], cst[:, C_SEL0, :], la, True, True, [cst, sm], [C0])
                    mm(P, C0[:, 20:24], cst[:, C_SEL1, :], la, True, True, [cst, sm], [C0])
                    act(P, eg, C0[:, 8:12], AF.Exp, [C0], [sm])
                    act(P, gs, C0[:, 8:12], AF.Copy, [C0], [sm])
                    act(P, glrep, C0[:, 16:24], AF.Exp, [C0], [sm])
                    tt(P, ekd, C0[:, 12:16], gs, ALU.subtract, [C0, sm], [sm])
                    yield
                    act(P, ekd, ekd, AF.Exp, [sm], [sm])
                    ts(P, negeg, eg, -1.0, None, ALU.mult, None, [sm], [sm])
                    tt(P, R[:], cst[:, C_LE64, :].unsqueeze(1).to_broadcast([128, 4, 128]),
                       la.unsqueeze(2).to_broadcast([128, 4, 128]), ALU.mult, [cst, sm], [R])
                    yield
                    mm(P, D1[:, :], cst[:, C_GT64, :], R[:], True, True, [cst, R], [D1])
                    act(P, Dec[:], b4(D1[:, :]), AF.Exp, [D1], [Dec])
                    tt(P, DecU[:], Dec[:], cst[:, C_LE64, :].unsqueeze(1).to_broadcast([128, 4, 128]), ALU.mult, [Dec, cst], [DecU])
                    yield
                    for h in range(4):
                        mm(P, D0[:, h * 128:(h + 1) * 128], kT[:, h, tsl], kT[:, h, tsl], True, True, [kT], [D0])
                    for h in range(4):
                        mm(P, D1[:, h * 128:(h + 1) * 128], kT[:, h, tsl], qT[:, h, tsl], True, True, [kT, qT], [D1])
                    yield
                    tt(P, qkTm[:], b4(D1[:, :]), DecU[:], ALU.mult, [D1, DecU], [qkTm])
                    tt(P, t1[:], b4(D0[:, :]), DecU[:], ALU.mult, [D0, DecU], [t1])
                    yield
                    X = Ya[0]
                    for h in range(4):
                        stt(P, rr(X[:, h, :]), t1[:, h, :], beta[:, h:h + 1], cst[:, C_LT64, :], ALU.mult, ALU.mult, [t1, sm, cst], [X])
                    yield
                    for h in range(4):
                        tr(P, D0[:, h * 128:(h + 1) * 128], X[:, h, :], cst[:, C_I, :], [X, cst], [D0])
                    act(P, rr(Za[0][:]), b4(D0[:, :]), AF.Copy, [D0], [Za[0]])
                    tt(P, rr(V[:]), cst[:, C_I, :].unsqueeze(1).to_broadcast([128, 4, 128]), X[:], ALU.subtract, [cst, X], [V])
                    yield
                    for lev in range(5):
                        Yc, Zc = Ya[lev % 2], Za[lev % 2]
                        Yn, Zn = Ya[(lev + 1) % 2], Za[(lev + 1) % 2]
                        for h in range(4):
                            mmr(P, D1[:, h * 128:(h + 1) * 128], Yc[:, h, :], Zc[:, h, :], True, True, [Yc, Zc], [D1])
                        act(P, rr(Zn[:]), b4(D1[:, :]), AF.Copy, [D1], [Zn])
                        yield
                        if lev < 4:
                            for h in range(4):
                                mmr(P, D0[:, h * 128:(h + 1) * 128], Zc[:, h, :], Yc[:, h, :], True, True, [Yc, Zc], [D0])
                            act(P, rr(Yn[:]), b4(D0[:, :]), AF.Copy, [D0], [Yn])
                            yield
                        for h in range(4):
                            mmr(P, C1[:, h * 128:(h + 1) * 128], Zn[:, h, :], V[:, h, :], True, True, [Zn, V], [C1])
                        tt(P, rr(V[:]), b4(C1[:, :]), V[:], ALU.add, [C1, V], [V])
                        yield
                    act(P, Vbf[:], V[:], AF.Copy, [V], [Vbf])
                    for h in range(4):
                        tr(P, D0[:, h * 128:(h + 1) * 128], xc[:, 8 + h, tsl], cst[:, C_I, :], [xcj[8 + h], cst], [D0])
                    act(P, v_tm[:], b4(D0[:, :]), AF.Copy, [D0], [v_tm])
                    yield
                    d1b = D1[:, :].bitcast(BF16)
                    for h in range(4):
                        tr(P, d1b[:, h * 128:(h + 1) * 128], kT[:, h, tsl], identb[:], [kT, identb], [D1])
                    tt(P, kd[:], b4(d1b[:, 0:512]), ekd.unsqueeze(2).to_broadcast([128, 4, 128]), ALU.mult, [D1, sm], [kd])
                    yield

                def h2(blk, par):
                    t0 = blk * 128
                    tg = sbi * SBT + t0
                    tsl = slice(t0, t0 + 128)
                    sm = smP[par]
                    smv = lambda i: sm[:, i * 4:(i + 1) * 4]
                    Vbf, qkTm, kd, v_tm = VbfP[par], qkTmP[par], kdP[par], vtmP[par]
                    eb, beta, xr, xm, ex, lg, sp, la, eg, gs, ekd, negeg = [smv(i) for i in range(12)]
                    glrep = sm[:, 48:56]
                    for c in range(2):
                        sl = slice(64 * c, 64 * c + 64)
                        for h in range(4):
                            mm(P, A0[:, h * 128:(h + 1) * 128], kT[:, h, tsl], Sbf[:, h, :], True, True, [kT, Sbf], [A0])
                        for h in range(4):
                            mm(P, B0[:, h * 128:(h + 1) * 128], qT[:, h, tsl], Sbf[:, h, :], True, True, [qT, Sbf], [B0])
                        for h in range(4):
                            stt(P, rhs2[sl, h, :], A0[sl, h * 128:(h + 1) * 128], negeg[sl, h:h + 1], v_tm[sl, h, :],
                                ALU.mult, ALU.add, [A0, sm, v_tm], [rhs2])
                        yield
                        for h in range(4):
                            mm(P, A0[:, h * 128:(h + 1) * 128], Vbf[sl, h, :], rhs2[sl, h, :], True, True, [Vbf, rhs2], [A0])
                        tt(P, vnew[sl], b4(A0[sl, :]), beta[sl].unsqueeze(2).to_broadcast([64, 4, 128]), ALU.mult, [A0, sm], [vnew])
                        yield
                        for h in range(4):
                            mm(P, A1[:, h * 128:(h + 1) * 128], kd[sl, h, :], vnew[sl, h, :], True, True, [kd, vnew], [A1])
                        for h in range(4):
                            mmx(P, B1[:, h * 128:(h + 1) * 128], qkTm[sl, h, :], vnew[sl, h, :], (c == 0 and h == 0), (c == 1),
                                [qkTm, vnew], [B1])
                        for h in range(4):
                            stt(P, S32[:, h, :], S32[:, h, :], glrep[:, c * 4 + h:c * 4 + h + 1], A1[:, h * 128:(h + 1) * 128],
                                ALU.mult, ALU.add, [S32, sm, A1], [S32])
                        act(P, Sbf[:], S32[:], AF.Copy, [S32], [Sbf])
                        yield
                        tt(P, As[sl], b4(B0[sl, :]), eg[sl].unsqueeze(2).to_broadcast([64, 4, 128]), ALU.mult, [B0, sm], [As])
                        yield
                    tt(P, o[:], b4(B1[:, :]), As[:], ALU.add, [B1, As], [o])
                    for h in range(4):
                        P.op('act', lambda e, h=h, sm=sm: e.activation(out=junk[:], in_=o[:, h, :], func=AF.Square,
                                                                      accum_out=sm[:, 56 + h:57 + h]), [o], [junk, sm])
                    yield
                    act(P, sm[:, 60:64], sm[:, 56:60], AF.Ln, [sm], [sm], scale=1.0 / 128, bias=1e-6)
                    act(P, sm[:, 60:64], sm[:, 60:64], AF.Exp, [sm], [sm], scale=-0.5)
                    for h in range(4):
                        stt(P, o[:, h, :], o[:, h, :], sm[:, 60 + h:61 + h], normw, ALU.mult, ALU.mult, [o, sm, rp], [o])
                    yield
                    tt(P, ybf[:], o[:], b4(sgS[:, blk, :]), ALU.mult, [o, sgS], [ybf])
                    c1b = A1[:, :].bitcast(BF16)
                    for h in range(4):
                        tr(P, c1b[:, h * 128:(h + 1) * 128], ybf[:, h, :], identb[:], [ybf, identb], [A1])
                    yt = yT[(sbi * NBLK + blk) % 2]
                    act(P, yt[:], b4(c1b[:, 0:512]), AF.Copy, [A1], [yt])
                    P.dma('sp', Kx.ycT.rearrange("k p t -> p k t")[:, 8:12, tg:tg + 128], yt[:], reads=[yt], writes=[Kx.d_ycT])
                    yield

                for _ in h1(0, 0):
                    pass
                for blk in range(NBLK):
                    g2 = h2(blk, blk % 2)
                    g1 = h1(blk + 1, (blk + 1) % 2) if blk + 1 < NBLK else iter(())
                    d1 = d2 = False
                    while not (d1 and d2):
                        if not d2:
                            try:
                                next(g2)
                            except StopIteration:
                                d2 = True
                        for _r in range(2):
                            if not d1:
                                try:
                                    next(g1)
                                except StopIteration:
                                    d1 = True
        except StopPass:
            pass
        P.flush()


HG_BASE = 4632
HG_NRP = 128


def pass_hg(P, Kx, T, l, labs):
    P.eps = PASS_EPS['hg']
    SBT = min(512, T)
    NSB = T // SBT
    NBLK = SBT // 128
    cst = Kx.cst
    A0, A1, B0, B1, C0, C1, D0, D1 = Kx.bank
    b4 = lambda ap: ap.rearrange("p (h c) -> p h c", h=4)
    with ExitStack() as st:
        P.stack = st
        try:
            W = P.sb("hg_W", [128, 8, 2048], BF16)
            rp = P.sb("hg_rp", [128, HG_NRP], F32)
            lbl = P.sb("hg_lbl", [128, 4, 4], F32)
            lbw = P.sb("hg_lbw", [128, 4 * 6], F32)
            lmk = P.sb("hg_lmk", [128, 4, 4], F32)
            hTb = [P.sb(f"hg_hT{i}", [128, 8, SBT], BF16) for i in range(2)]
            qTf = P.sb("hg_qTf", [128, 4, SBT], F32)
            kTf = P.sb("hg_kTf", [128, 4, SBT], F32)
            lgf = P.sb("hg_lgf", [128, 4, SBT], F32)
            ftmp = [P.sb(f"hg_ft{i}", [128, SBT], F32) for i in range(4)]
            sgS = P.sb("hg_sgS", [128, NBLK, 512], F32)
            ones = P.sb("hg_ones", [128, 128], F32)
            identb = P.sb("hg_identb", [128, 128], BF16)
            Bt = P.sb("hg_Bt", [128, 4, 132], F32)
            D1t = [P.sb(f"hg_D1{i}", [128, 8, 128], F32) for i in range(2)]
            Et = [P.sb(f"hg_E{i}", [128, 8, 128], F32) for i in range(2)]
            kfac = [P.sb(f"hg_kfac{i}", [128, 8, 128], BF16) for i in range(2)]
            Eq = P.sb("hg_Eq", [128, 4, 128], F32)
            EB = P.sb("hg_EB", [128, 4, 128], F32)
            Ek = P.sb("hg_Ek", [128, 4, 128], F32)
            qg = P.sb("hg_qg", [128, 4, 128], BF16)
            qG = P.sb("hg_qG", [128, 4, 128], BF16)
            kdT = P.sb("hg_kdT", [128, 4, 128], BF16)
            kdtm = P.sb("hg_kdtm", [128, 4, 128], BF16)
            scT = P.sb("hg_scT", [128, 4, 128], BF16)
            vbf = P.sb("hg_vbf", [128, 4, 128], BF16)
            sm = P.sb("hg_sm", [128, 16], F32)
            junk = P.sb("hg_junk", [128, 128], F32)
            o = P.sb("hg_o", [128, 4, 128], F32)
            ybf = P.sb("hg_ybf", [128, 4, 128], BF16)
            yT = [P.sb(f"hg_yT{i}", [128, 4, 128], BF16) for i in range(2)]
            S32 = P.sb("hg_S32", [128, 4, 128], F32)
            Sbf = P.sb("hg_Sbf", [128, 4, 128], BF16)

            load_weights(P, W, Kx.w_in[l, :, HG_BASE:HG_BASE + 2048], 2048, 8, order=[0, 3, 1, 2])
            P.dma('sp', rp[:], Kx.hg_rp[l], writes=[rp])
            P.dma('sp', lbl[:], Kx.hg_lbl, writes=[lbl])
            normw = rp[:, 0:128]
            act(P, identb[:], cst[:, C_I, :], AF.Copy, [cst], [identb])
            P.op('dve', lambda e: e.memset(ones[:], 1.0), [], [ones])
            P.op('dve', lambda e: e.memset(S32[:], 0.0), [], [S32])
            P.op('dve', lambda e: e.memset(Sbf[:], 0.0), [], [Sbf])
            P.op('dve', lambda e: e.memset(Bt[:], 0.0), [], [Bt])
            mx, sme, rs, lb, oml = [lbw[:, i * 4:(i + 1) * 4] for i in range(5)]
            P.op('dve', lambda e: e.tensor_reduce(out=mx, in_=lbl[:], axis=AX.X, op=ALU.max), [lbl], [lbw])
            tt(P, lbl[:], lbl[:], mx.unsqueeze(2).to_broadcast([128, 4, 4]), ALU.subtract, [lbl, lbw], [lbl])
            act(P, lbl[:], lbl[:], AF.Exp, [lbl], [lbl])
            P.op('dve', lambda e: e.tensor_reduce(out=sme, in_=lbl[:], axis=AX.X, op=ALU.add), [lbl], [lbw])
            P.op('dve', lambda e: e.reciprocal(out=rs, in_=sme), [lbw], [lbw])
            P.dma('sp', lmk[:], Kx.hg_lmask[l], writes=[lmk])
            tt(P, lbl[:], lbl[:], lmk[:], ALU.mult, [lbl, lmk], [lbl])
            P.op('dve', lambda e: e.tensor_reduce(out=lb, in_=lbl[:], axis=AX.X, op=ALU.add), [lbl], [lbw])
            tt(P, lb, lb, rs, ALU.mult, [lbw], [lbw])
            ts(P, oml, lb, -1.0, 1.0, ALU.mult, ALU.add, [lbw], [lbw])
            chk(1)
            for sbi in range(NSB):
                hb = hTb[sbi % 2]
                P.dma('sp', hb[:], Kx.hT.rearrange("k p t -> p k t")[:, :, sbi * SBT:(sbi + 1) * SBT], reads=[Kx.d_hT], writes=[hb])
                banks4 = [D0, D1, C0, C1]
                for j in range(4):
                    bk = banks4[j % 4]
                    for k in range(8):
                        mm(P, bk[:, 0:SBT], W[:, k, j * 128:(j + 1) * 128], hb[:, k, :], k == 0, k == 7, [wsub(W, 0), hb], [bk])
                    act(P, qTf[:, j, :], bk[:, 0:SBT], AF.Silu, [bk], [qTf])
                for blk in range(NBLK):
                    bk = banks4[blk % 4]
                    for k in range(8):
                        mm(P, bk[:, :], hb[:, k, blk * 128:(blk + 1) * 128], W[:, k, 1536:2048], k == 0, k == 7, [hb, wsub(W, 1536)], [bk])
                    act(P, sgS[:, blk, :], bk[:, :], AF.Silu, [bk], [sgS])
                for h in range(4):
                    bk = banks4[h % 4]
                    for k in range(8):
                        mm(P, bk[:, 0:SBT], W[:, k, (4 + h) * 128:(5 + h) * 128], hb[:, k, :], k == 0, k == 7, [wsub(W, 512), hb], [bk])
                    act(P, ftmp[h][:], bk[:, 0:SBT], AF.Sigmoid, [bk], [ftmp[h]])
                for h in range(4):
                    f_ = ftmp[h]
                    ts(P, f_[:], f_[:], oml[:, h:h + 1], lb[:, h:h + 1], ALU.mult, ALU.add, [f_, lbw], [f_])
                    ts(P, kTf[:, h, :], f_[:], -1.0, 1.0, ALU.mult, ALU.add, [f_], [kTf])
                for h in range(4):
                    act(P, lgf[:, h, :], ftmp[h][:], AF.Ln, [ftmp[h]], [lgf])
                chk(2)
                for blk in range(NBLK):
                    t0 = blk * 128
                    tg = sbi * SBT + t0
                    tsl = slice(t0, t0 + 128)
                    for k in range(8):
                        mm(P, A0[:, :], hb[:, k, tsl], W[:, k, 1024:1536], k == 0, k == 7, [hb, wsub(W, 1024)], [A0])
                    act(P, vbf[:], b4(A0[:, :]), AF.Copy, [A0], [vbf])
                    for h in range(4):
                        i2 = h % 2
                        D1_, E_, kf_ = D1t[i2], Et[i2], kfac[i2]
                        P.op('dve', lambda e, h=h, tsl=tsl: e.tensor_tensor_scan(out=Bt[:, h, 1:129], data0=ones[:, :], data1=lgf[:, h, tsl],
                                                                       initial=0.0, op0=ALU.mult, op1=ALU.add), [ones, lgf], [Bt])
                        for c in range(8):
                            ts(P, D1_[:, c, :], Bt[:, h, 1:129], Bt[:, h, 16 * c:16 * c + 1], -60.0, ALU.subtract, ALU.max, [Bt], [D1_])
                        act(P, E_[:], D1_[:], AF.Exp, [D1_], [E_], scale=-1.0)
                        tt(P, kf_[:], E_[:], kTf[:, h, tsl].unsqueeze(1).to_broadcast([128, 8, 128]), ALU.mult, [E_, kTf], [kf_])
                        base = D1_[:, :, :]
                        dg = bass.AP(base.tensor, base.offset, [list(base.ap[0]), [144, 8], [1, 16]])
                        act(P, Eq[:, h, :].rearrange("p (c j) -> p c j", c=8), dg, AF.Exp, [D1_], [Eq])
                        tt(P, qg[:, h, :], qTf[:, h, tsl], Eq[:, h, :], ALU.mult, [qTf, Eq], [qg])
                        act(P, EB[:, h, :], Bt[:, h, 1:129], AF.Exp, [Bt], [EB])
                        tt(P, qG[:, h, :], qTf[:, h, tsl], EB[:, h, :], ALU.mult, [qTf, EB], [qG])
                        act(P, Ek[:, h, :], Bt[:, h, 1:129], AF.Exp, [Bt], [Ek], scale=-1.0, bias=Bt[:, h, 128:129])
                        tt(P, kdT[:, h, :], kTf[:, h, tsl], Ek[:, h, :], ALU.mult, [kTf, Ek], [kdT])
                        for c in range(8):
                            mm(P, B0[:, h * 128 + 16 * c:h * 128 + 16 * c + 16], kf_[:, c, :], qg[:, h, 16 * c:16 * c + 16], True, True,
                               [kf_, qg], [B0])
                    tt(P, scT[:], b4(B0[:, :]), cst[:, C_LE, :].unsqueeze(1).to_broadcast([128, 4, 128]), ALU.mult, [B0, cst], [scT])
                    chk(3)
                    for h in range(4):
                        mm(P, B1[:, h * 128:(h + 1) * 128], scT[:, h, :], vbf[:, h, :], True, False, [scT, vbf], [B1])
                        mm(P, B1[:, h * 128:(h + 1) * 128], qG[:, h, :], Sbf[:, h, :], False, True, [qG, Sbf], [B1])
                    c0b = C0[:, :].bitcast(BF16)
                    for h in range(4):
                        tr(P, c0b[:, h * 128:(h + 1) * 128], kdT[:, h, :], identb[:], [kdT, identb], [C0])
                    act(P, kdtm[:], b4(c0b[:, 0:512]), AF.Copy, [C0], [kdtm])
                    for h in range(4):
                        mm(P, C1[:, h * 128:(h + 1) * 128], kdtm[:, h, :], vbf[:, h, :], True, True, [kdtm, vbf], [C1])
                    for h in range(4):
                        stt(P, S32[:, h, :], S32[:, h, :], EB[:, h, 127:128], C1[:, h * 128:(h + 1) * 128], ALU.mult, ALU.add,
                            [S32, EB, C1], [S32])
                    act(P, Sbf[:], S32[:], AF.Copy, [S32], [Sbf])
                    chk(4)
                    for h in range(4):
                        P.op('act', lambda e, h=h: e.activation(out=junk[:], in_=B1[:, h * 128:(h + 1) * 128], func=AF.Square,
                                                               accum_out=sm[:, h:h + 1]), [B1], [junk, sm])
                    act(P, sm[:, 4:8], sm[:, 0:4], AF.Ln, [sm], [sm], scale=1.0 / 128, bias=1e-6)
                    act(P, sm[:, 4:8], sm[:, 4:8], AF.Exp, [sm], [sm], scale=-0.5)
                    for h in range(4):
                        stt(P, o[:, h, :], B1[:, h * 128:(h + 1) * 128], sm[:, 4 + h:5 + h], normw, ALU.mult, ALU.mult, [B1, sm, rp], [o])
                    tt(P, ybf[:], o[:], b4(sgS[:, blk, :]), ALU.mult, [o, sgS], [ybf])
                    c1b = C1[:, :].bitcast(BF16)
                    for h in range(4):
                        tr(P, c1b[:, h * 128:(h + 1) * 128], ybf[:, h, :], identb[:], [ybf, identb], [C1])
                    yt = yT[(sbi * NBLK + blk) % 2]
                    act(P, yt[:], b4(c1b[:, 0:512]), AF.Copy, [C1], [yt])
                    P.dma('sp', Kx.ycT.rearrange("k p t -> p k t")[:, 12:16, tg:tg + 128], yt[:], reads=[yt], writes=[Kx.d_ycT])
        except StopPass:
            pass
        P.flush()


OUT_NRP = 1024 + 1024 + 20
NSEL = 16


def layer_norm_block(P, r, stats, mv, sm2, g_ap, b_ap, reads_rp, out_ap, out_buf):
    for hf in range(2):
        P.op('dve', lambda e, hf=hf: e.bn_stats(out=stats[:, hf * 6:(hf + 1) * 6], in_=r[:, hf * 512:(hf + 1) * 512]), [r], [stats])
    P.op('dve', lambda e: e.bn_aggr(out=mv[:, 0:2], in_=stats[:, 0:12]), [stats], [mv])
    act(P, sm2[:, 0:1], mv[:, 1:2], AF.Ln, [mv], [sm2], bias=1e-5)
    act(P, sm2[:, 0:1], sm2[:, 0:1], AF.Exp, [sm2], [sm2], scale=-0.5)
    stt(P, sm2[:, 1:2], mv[:, 0:1], -1.0, sm2[:, 0:1], ALU.mult, ALU.mult, [mv, sm2], [sm2])
    act(P, r[:], r[:], AF.Identity, [r, sm2], [r], scale=sm2[:, 0:1], bias=sm2[:, 1:2])
    tt(P, r[:], r[:], g_ap, ALU.mult, [r] + reads_rp, [r])
    tt(P, out_ap, r[:], b_ap, ALU.add, [r] + reads_rp, [out_buf])


def pass_out(P, Kx, T, l, hsrc):
    P.eps = PASS_EPS['out']
    cst = Kx.cst
    A0, A1, B0, B1, C0, C1, D0, D1 = Kx.bank
    NB = T // 128
    with ExitStack() as st:
        P.stack = st
        try:
            Wo = P.sb("out_W", [128, 16, 1024], BF16)
            rp = P.sb("out_rp", [128, OUT_NRP], F32)
            wr = P.sb("out_wr", [128, 8, 20], F32)
            ycb = [P.sb(f"out_yc{i}", [128, 16, 128], BF16) for i in range(2)]
            hin = [P.sb(f"out_h{i}", [128, 1024], F32) for i in range(2)]
            r = [P.sb(f"out_r{i}", [128, 1024], F32) for i in range(2)]
            x1 = [P.sb(f"out_x1{i}", [128, 1024], F32) for i in range(2)]
            x1Tf = P.sb("out_x1Tf", [128, 8, 128], F32)
            x1Tb = [P.sb(f"out_x1Tb{i}", [128, 8, 128], BF16) for i in range(2)]
            stats = P.sb("out_stats", [128, 12], F32)
            mv = P.sb("out_mv", [128, 2], F32)
            sm2 = P.sb("out_sm2", [128, 2], F32)
            q = P.sb("out_q", [128, 96], F32)
            comb = [P.sb(f"out_comb{i}", [128, 16], F32) for i in range(2)]
            combT = [P.sb(f"out_combT{i}", [16, 128], F32) for i in range(2)]

            wo3 = Kx.w_out[l].rearrange("(k p) c -> p k c", p=128)
            for hf in range(2):
                for kh in range(2):
                    P.dma('pool', Wo[:, kh * 8:(kh + 1) * 8, hf * 512:(hf + 1) * 512], wo3[:, kh * 8:(kh + 1) * 8, hf * 512:(hf + 1) * 512],
                          writes=[Wo.sub((hf, kh))])
            P.dma('sp', rp[:], Kx.out_rp[l], writes=[rp])
            P.dma('sp', wr[:], Kx.wr[l], writes=[wr])
            g1 = rp[:, 0:1024]
            b1 = rp[:, 1024:2048]
            rb = rp[:, 2048:2068]
            chk(1)
            def stage_a(b):
                    tsl = slice(b * 128, (b + 1) * 128)
                    yc, h_, r_, x_ = ycb[b % 2], hin[b % 2], r[b % 2], x1[b % 2]
                    P.dma('sp', yc[:], Kx.ycT.rearrange("k p t -> p k t")[:, :, tsl], reads=[Kx.d_ycT], writes=[yc])
                    P.dma('act', h_[:], hsrc[tsl, :], reads=[Kx.d_hres], writes=[h_])
                    for hf in range(2):
                        bk = [A0, A1][hf]
                        for kc in range(16):
                            mm(P, bk[:, :], yc[:, kc, :], Wo[:, kc, hf * 512:(hf + 1) * 512], kc == 0, kc == 15, [yc, Wo.children[hf * 2 + kc // 8]], [bk])
                        stt(P, r_[:, hf * 512:(hf + 1) * 512], h_[:, hf * 512:(hf + 1) * 512], float(DN_ALPHA), bk[:, :], ALU.mult, ALU.add,
                            [h_, bk], [r_])
                    layer_norm_block(P, r_, stats, mv, sm2, g1, b1, [rp], x_[:], x_)
                    P.dma('sp', Kx.x1[tsl, :], x_[:], reads=[x_], writes=[Kx.d_x1])

            def stage_b(b):
                    tsl = slice(b * 128, (b + 1) * 128)
                    x_ = x1[b % 2]
                    for j in range(8):
                        bk = [B0, B1][j // 4]
                        tr(P, bk[:, (j % 4) * 128:(j % 4 + 1) * 128], x_[:, j * 128:(j + 1) * 128], cst[:, C_I, :], [x_, cst], [bk])
                    xb = x1Tb[b % 2]
                    for hf in range(2):
                        bk = [B0, B1][hf]
                        act(P, x1Tf[:, hf * 4:(hf + 1) * 4, :], bk[:, :].rearrange("p (j t) -> p j t", j=4), AF.Copy, [bk], [x1Tf])
                        P.op('dve', lambda e, hf=hf, bk=bk, xb=xb: e.tensor_copy(out=xb[:, hf * 4:(hf + 1) * 4, :], in_=bk[:, :].rearrange("p (j t) -> p j t", j=4)),
                             [bk], [xb])
                    P.dma('sp', Kx.x1T.rearrange("k p t -> p k t")[:, :, tsl], xb[:], reads=[xb], writes=[Kx.d_x1T])
                    for k in range(8):
                        mm(P, C0[:, 0:20], x1Tf[:, k, :], wr[:, k, :], k == 0, k == 7, [x1Tf, wr], [C0])
                    lgs = q[:, 0:20]
                    gm, ngm, gsum, gp, m1, m2, dlt, ed, w1, w2 = [q[:, 20 + i:21 + i] for i in range(10)]
                    ohg = q[:, 32:36]
                    egj = q[:, 36:40]
                    lsel = q[:, 40:44]
                    oh1 = q[:, 44:48]
                    msk = q[:, 48:52]
                    oh2 = q[:, 52:56]
                    wsel = q[:, 56:60]
                    tmp16 = q[:, 64:80]
                    tt(P, lgs, C0[:, 0:20], rb, ALU.add, [C0, rp], [q])
                    P.op('dve', lambda e: e.tensor_reduce(out=gm, in_=lgs[:, 0:4], axis=AX.X, op=ALU.max), [q], [q])
                    ts(P, ohg, lgs[:, 0:4], gm, None, ALU.is_equal, None, [q], [q])
                    ts(P, ngm, gm, -1.0, None, ALU.mult, None, [q], [q])
                    P.op('act', lambda e: e.activation(out=egj, in_=lgs[:, 0:4], func=AF.Exp, bias=ngm, accum_out=gsum), [q], [q])
                    P.op('dve', lambda e: e.reciprocal(out=gp, in_=gsum), [q], [q])
                    tt(P, tmp16.rearrange("p (g e) -> p g e", g=4), lgs[:, 4:20].rearrange("p (g e) -> p g e", g=4),
                       ohg.unsqueeze(2).to_broadcast([128, 4, 4]), ALU.mult, [q], [q])
                    P.op('dve', lambda e: e.tensor_reduce(out=lsel, in_=tmp16.rearrange("p (g e) -> p e g", g=4), axis=AX.X, op=ALU.add), [q], [q])
                    P.op('dve', lambda e: e.tensor_reduce(out=m1, in_=lsel, axis=AX.X, op=ALU.max), [q], [q])
                    ts(P, oh1, lsel, m1, None, ALU.is_equal, None, [q], [q])
                    stt(P, msk, oh1, -1e30, lsel, ALU.mult, ALU.add, [q], [q])
                    P.op('dve', lambda e: e.tensor_reduce(out=m2, in_=msk, axis=AX.X, op=ALU.max), [q], [q])
                    ts(P, oh2, msk, m2, None, ALU.is_equal, None, [q], [q])
                    tt(P, dlt, m2, m1, ALU.subtract, [q], [q])
                    act(P, ed, dlt, AF.Exp, [q], [q])
                    ts(P, w1, ed, 1.0, None, ALU.add, None, [q], [q])
                    P.op('dve', lambda e: e.reciprocal(out=w1, in_=w1), [q], [q])
                    tt(P, w2, ed, w1, ALU.mult, [q], [q])
                    tt(P, w1, w1, gp, ALU.mult, [q], [q])
                    tt(P, w2, w2, gp, ALU.mult, [q], [q])
                    ts(P, wsel, oh1, w1, None, ALU.mult, None, [q], [q])
                    stt(P, wsel, oh2, w2, wsel, ALU.mult, ALU.add, [q], [q])
                    cb_ = comb[b % 2]
                    tt(P, cb_[:].rearrange("p (g e) -> p g e", g=4), ohg.unsqueeze(2).to_broadcast([128, 4, 4]),
                       wsel.unsqueeze(1).to_broadcast([128, 4, 4]), ALU.mult, [q], [cb_])
                    P.dma('sp', Kx.comb[tsl, :], cb_[:], reads=[cb_], writes=[Kx.d_comb])

            stage_a(0)
            for b in range(NB):
                if b + 1 < NB:
                    stage_a(b + 1)
                stage_b(b)
        except StopPass:
            pass
        P.flush()


def pass_moe(P, Kx, T, l, dst, write_hT):
    P.eps = PASS_EPS['moe']
    cst = Kx.cst
    A0, A1, B0, B1, C0, C1, D0, D1 = Kx.bank
    ST = min(1024, T)
    NST = T // ST
    NBS = ST // 128
    with ExitStack() as st:
        P.stack = st
        try:
            Wgu = [P.sb(f"moe_Wgu{i}", [128, 8, 512], BF16) for i in range(8)]
            Wdn = [P.sb(f"moe_Wdn{i}", [128, 2, 1024], BF16) for i in range(8)]
            for w_ in Wgu:
                for k in range(8):
                    w_.sub(k)
            for w_ in Wdn:
                for k in range(2):
                    w_.sub(k)
            rp = P.sb("moe_rp", [128, 2048], F32)
            xT = P.sb("moe_xT", [128, 8, ST], BF16)
            cmb = P.sb("moe_cmb", [128, NBS, 16], F32)
            yacc = P.sb("moe_yacc", [128, NBS, 1024], F32)
            yaccb = [yacc.sub(i) for i in range(NBS)]
            sgb = [P.sb(f"moe_sg{i}", [128, 256], F32) for i in range(3)]
            hb_ = [P.sb(f"moe_h{i}", [128, 256], BF16) for i in range(3)]
            hT = [P.sb(f"moe_hT{i}", [128, 2, 128], BF16) for i in range(3)]
            identb = P.sb("moe_identb", [128, 128], BF16)
            x1b = [P.sb(f"moe_x1{i}", [128, 1024], F32) for i in range(2)]
            ob = [P.sb(f"moe_o{i}", [128, 1024], F32) for i in range(2)]
            oT = [P.sb(f"moe_oT{i}", [128, 8, 128], BF16) for i in range(2)]
            stats = P.sb("moe_stats", [128, 12], F32)
            mv = P.sb("moe_mv", [128, 2], F32)
            sm2 = P.sb("moe_sm2", [128, 2], F32)
            P.dma('sp', rp[:], Kx.moe_rp[l], writes=[rp])
            g2 = rp[:, 0:1024]
            b2 = rp[:, 1024:2048]
            act(P, identb[:], cst[:, C_I, :], AF.Copy, [cst], [identb])
            it = 0
            for sti in range(NST):
                s0 = sti * ST
                P.dma('sp', xT[:], Kx.x1T.rearrange("k p t -> p k t")[:, :, s0:s0 + ST], reads=[Kx.d_x1T], writes=[xT])
                P.dma('sp', cmb[:], Kx.comb[s0:s0 + ST, :].rearrange("(b p) e -> p b e", p=128), reads=[Kx.d_comb], writes=[cmb])
                for G in range(4):
                    slot0 = ((sti * 4 + G) % 2) * 4
                    for e4 in range(4):
                        e = G * 4 + e4
                        wg, wd = Wgu[slot0 + e4], Wdn[slot0 + e4]
                        for k in range(8):
                            P.dma('pool', wg[:, k, :], Kx.w_gu[l, e, k * 128:(k + 1) * 128, :], writes=[wg.children[k]])
                        for fc in range(2):
                            P.dma('pool', wd[:, fc, :], Kx.w_dn[l, e, fc * 128:(fc + 1) * 128, :], writes=[wd.children[fc]])
                    items = [(blk, e4) for blk in range(NBS) for e4 in range(4)]
                    NB3 = 3

                    def stage_a(i):
                        blk, e4 = items[i]
                        e = G * 4 + e4
                        tsl = slice(blk * 128, (blk + 1) * 128)
                        wg = Wgu[slot0 + e4]
                        gb = [C0, C1][i % 2]
                        sg_, h_ = sgb[i % NB3], hb_[i % NB3]
                        for k in range(8):
                            mm(P, gb[:, :], xT[:, k, tsl], wg[:, k, :], k == 0, k == 7, [xT, wg], [gb])
                        act(P, sg_[:], gb[:, 0:256], AF.Silu, [gb], [sg_])
                        stt(P, h_[:], gb[:, 256:512], cmb[:, blk, e:e + 1], sg_[:], ALU.mult, ALU.mult, [gb, cmb, sg_], [h_])

                    def stage_b(i):
                        tb = [D0, D1][i % 2]
                        h_, hT_ = hb_[i % NB3], hT[i % NB3]
                        tbb = tb[:, :].bitcast(BF16)
                        for fc in range(2):
                            tr(P, tbb[:, fc * 128:(fc + 1) * 128], h_[:, fc * 128:(fc + 1) * 128], identb[:], [h_, identb], [tb])
                        act(P, hT_[:], tbb[:, 0:256].rearrange("p (f t) -> p f t", f=2), AF.Copy, [tb], [hT_])

                    def stage_c(i):
                        blk, e4 = items[i]
                        wd = Wdn[slot0 + e4]
                        hT_ = hT[i % NB3]
                        ybk = [A0, A1] if blk % 2 == 0 else [B0, B1]
                        for hf in range(2):
                            for fc in range(2):
                                first = (e4 == 0 and fc == 0)
                                last = (e4 == 3 and fc == 1)
                                mm(P, ybk[hf][:, :], hT_[:, fc, :], wd[:, fc, hf * 512:(hf + 1) * 512], first, last, [hT_, wd], [ybk[hf]])
                        if e4 == 3:
                            for hf in range(2):
                                ya = yacc[:, blk, hf * 512:(hf + 1) * 512]
                                if G == 0:
                                    act(P, ya, ybk[hf][:, :], AF.Copy, [ybk[hf]], [yaccb[blk]])
                                else:
                                    tt(P, ya, ybk[hf][:, :], ya, ALU.add, [ybk[hf], yaccb[blk]], [yaccb[blk]])

                    n_it = len(items)
                    for step in range(n_it + 2):
                        if step < n_it:
                            stage_a(step)
                        if 0 <= step - 1 < n_it:
                            stage_b(step - 1)
                        if 0 <= step - 2 < n_it:
                            stage_c(step - 2)
                for blk in range(NBS):
                    tg = s0 + blk * 128
                    x_, o_ = x1b[blk % 2], ob[blk % 2]
                    P.dma('act', x_[:], Kx.x1[tg:tg + 128, :], reads=[Kx.d_x1], writes=[x_])
                    stt(P, x_[:], x_[:], float(DN_ALPHA), yacc[:, blk, :], ALU.mult, ALU.add, [x_, yaccb[blk]], [x_])
                    layer_norm_block(P, x_, stats, mv, sm2, g2, b2, [rp], o_[:], o_)
                    P.dma('sp', dst[tg:tg + 128, :], o_[:], reads=[o_], writes=[Kx.d_hres])
                    if write_hT:
                        c0b = C0[:, :].bitcast(BF16)
                        ot = oT[blk % 2]
                        for j in range(8):
                            bk = [C0, C1][j // 4]
                            tr(P, bk[:, (j % 4) * 128:(j % 4 + 1) * 128], o_[:, j * 128:(j + 1) * 128], cst[:, C_I, :], [o_, cst], [bk])
                        for hf in range(2):
                            act(P, ot[:, hf * 4:(hf + 1) * 4, :], [C0, C1][hf][:, :].rearrange("p (j t) -> p j t", j=4), AF.Copy,
                                [[C0, C1][hf]], [ot])
                        P.dma('sp', Kx.hT.rearrange("k p t -> p k t")[:, :, tg:tg + 128], ot[:], reads=[ot], writes=[Kx.d_hT])
        except StopPass:
            pass
        P.flush()


def build(T, NL, dbg=False, passes=("ssd", "gdn", "hg", "out", "moe"), layers=None):
    layers = list(range(NL)) if layers is None else layers
    nc = bass.Bass("TRN2", target_bir_lowering=False)
    Kx = K()
    ext_in = lambda name, shape: nc.dram_tensor(name, list(shape), F32, kind="ExternalInput").ap()
    Kx.x = ext_in("x", [T, D_MODEL])
    Kx.w_in = ext_in("w_in", [NL, D_MODEL, IN_COLS])
    Kx.cst_d = ext_in("cst", [128, NCONST, 128])
    Kx.ssd_pp = ext_in("ssd_pp", [NL, 128, SSD_NPP])
    Kx.ssd_rp = ext_in("ssd_rp", [NL, 128, SSD_NRP])
    Kx.gdn_pp = ext_in("gdn_pp", [NL, 128, GDN_NPP])
    Kx.gdn_rp = ext_in("gdn_rp", [NL, 128, GDN_NRP])
    Kx.hg_rp = ext_in("hg_rp", [NL, 128, HG_NRP])
    Kx.hg_lbl = ext_in("hg_lbl", [128, 4, 4])
    Kx.hg_lmask = ext_in("hg_lmask", [NL, 128, 4, 4])
    Kx.w_out = ext_in("w_out", [NL, 2048, 1024])
    Kx.out_rp = ext_in("out_rp", [NL, 128, OUT_NRP])
    Kx.wr = ext_in("wr", [NL, 128, 8, 20])
    Kx.moe_rp = ext_in("moe_rp", [NL, 128, 2048])
    Kx.w_gu = ext_in("w_gu", [NL, 16, 1024, 512])
    Kx.w_dn = ext_in("w_dn", [NL, 16, 256, 1024])
    Kx.out = nc.dram_tensor("out", [T, D_MODEL], F32, kind="ExternalOutput").ap()
    skind = "ExternalOutput" if dbg else "Internal"
    Kx.hT = nc.dram_tensor("hT", [8, 128, T], BF16, kind=skind).ap()
    Kx.ycT = nc.dram_tensor("ycT", [16, 128, T], BF16, kind=skind).ap()
    Kx.x1 = nc.dram_tensor("x1", [T, D_MODEL], F32, kind=skind).ap()
    Kx.x1T = nc.dram_tensor("x1T", [8, 128, T], BF16, kind=skind).ap()
    Kx.comb = nc.dram_tensor("comb", [T, 16], F32, kind=skind).ap()
    Kx.hres = nc.dram_tensor("hres", [T, D_MODEL], F32, kind="Internal").ap()
    Kx.d_hT = Buf("d_hT")
    Kx.d_ycT = Buf("d_ycT")
    Kx.d_x1 = Buf("d_x1")
    Kx.d_x1T = Buf("d_x1T")
    Kx.d_comb = Buf("d_comb")
    Kx.d_hres = Buf("d_hres")
    with ExitStack() as st0:
        P = Prog(nc, st0)
        Kx.bank = []
        for i in range(8):
            t = st0.enter_context(nc.psum_tensor(f"bank{i}", [128, 512], F32))
            Kx.bank.append(Buf(f"bank{i}", t))
            Kx.bank[-1].excl = True
        Kx.cst = P.sb("cst_sb", [128, NCONST, 128], F32)
        P.dma('sp', Kx.cst[:], Kx.cst_d, writes=[Kx.cst])
        for l in range(NL):
            if l == 0:
                pass_transpose_in(P, Kx, T, Kx.x, Kx.hT)
            if "ssd" in passes:
                pass_ssd(P, Kx, T, l)
            if "gdn" in passes:
                pass_gdn(P, Kx, T, l)
            if "hg" in passes:
                pass_hg(P, Kx, T, l, layers[l])
            if "out" in passes:
                pass_out(P, Kx, T, l, Kx.x if l == 0 else Kx.hres)
            if "moe" in passes:
                pass_moe(P, Kx, T, l, Kx.out if l == NL - 1 else Kx.hres, l < NL - 1)
        print("recorded ops", P.nops, "waits", P.nwaits)
    return nc


def host_params(inp, layers):
    out = {}
    NL = len(layers)
    pp = np.zeros((NL, 128, SSD_NPP), np.float32)
    rp = np.zeros((NL, 128, SSD_NRP), np.float32)
    for i, l in enumerate(layers):
        cw = inp['ssd_conv_w'][l]
        pp[i, :, 0:48] = cw.reshape(4, 12, 128).transpose(2, 1, 0).reshape(128, 48)
        pp[i, :, 48:60] = inp['ssd_conv_b'][l].reshape(12, 128).T
        rp[i, :, 0:16] = inp['ssd_dt_bias'][l][None, :]
        rp[i, :, 16:32] = inp['ssd_a_log'][l][None, :]
        rp[i, :, 32:48] = inp['ssd_d'][l][None, :]
        rp[i, :, 48:48 + 1024] = inp['ssd_norm_w'][l][None, :]
    out['ssd_pp'] = pp
    out['ssd_rp'] = rp
    gpp = np.zeros((NL, 128, GDN_NPP), np.float32)
    grp = np.zeros((NL, 128, GDN_NRP), np.float32)
    for i, l in enumerate(layers):
        gpp[i, :, 0:48] = inp['gdn_conv_w'][l].reshape(4, 12, 128).transpose(2, 1, 0).reshape(128, 48)
        grp[i, :, 0:4] = inp['gdn_dt_bias'][l][None, :]
        grp[i, :, 4:8] = inp['gdn_a_log'][l][None, :]
        grp[i, :, 8:136] = inp['gdn_norm_w'][l][None, :]
    out['gdn_pp'] = gpp
    out['gdn_rp'] = grp
    hrp = np.zeros((NL, 128, HG_NRP), np.float32)
    for i, l in enumerate(layers):
        hrp[i, :, 0:128] = inp['hg_norm_w'][l][None, :]
    out['hg_rp'] = hrp
    lmask = np.zeros((NL, 128, 4, 4), np.float32)
    for i, l in enumerate(layers):
        lmask[i, :, :, 1:l + 1] = 1.0
    out['hg_lmask'] = lmask
    out['hg_lbl'] = np.ascontiguousarray(inp['hg_lb_logits'].reshape(4, 4, 128).transpose(2, 1, 0))
    orp = np.zeros((NL, 128, OUT_NRP), np.float32)
    wr = np.zeros((NL, 128, 8, 20), np.float32)
    mrp = np.zeros((NL, 128, 2048), np.float32)
    for i, l in enumerate(layers):
        orp[i, :, 0:1024] = inp['ln1_g'][l][None, :]
        orp[i, :, 1024:2048] = inp['ln1_b'][l][None, :]
        orp[i, :, 2048:2052] = inp['b_router_group'][l][None, :]
        orp[i, :, 2052:2068] = inp['b_router_expert'][l][None, :]
        wcat = np.concatenate([inp['w_router_group'][l], inp['w_router_expert'][l]], axis=1)
        wr[i] = wcat.reshape(8, 128, 20).transpose(1, 0, 2)
        mrp[i, :, 0:1024] = inp['ln2_g'][l][None, :]
        mrp[i, :, 1024:2048] = inp['ln2_b'][l][None, :]
    out['out_rp'] = orp
    out['wr'] = wr
    out['moe_rp'] = mrp
    out['cst'] = make_consts()
    return out


def core_inputs(inp, b, T, layers):
    hp = host_params(inp, layers)
    ls = layers
    im = {
        "x": np.ascontiguousarray(inp['x'][b, :T]),
        "w_in": np.ascontiguousarray(inp['w_in'][ls]),
        "w_out": np.ascontiguousarray(inp['w_out'][ls]),
        "w_gu": np.ascontiguousarray(inp['w_expert_gate_up'][ls]),
        "w_dn": np.ascontiguousarray(inp['w_expert_down'][ls]),
    }
    im.update(hp)
    return im


_PROG = {}


def kernel(**inputs):
    inp = {k: np.asarray(v) for k, v in inputs.items()}
    B, T, _ = inp['x'].shape
    ncores = 8
    nc = build(T, DEPTH)
    base = core_inputs(inp, 0, T, list(range(DEPTH)))
    in_maps = []
    for c in range(ncores):
        m = dict(base)
        m["x"] = np.ascontiguousarray(inp['x'][c % B], dtype=np.float32)
        in_maps.append(m)
    res = run_bass_kernel_spmd(nc, in_maps, core_ids=list(range(ncores)))
    return np.stack([np.asarray(res.results[b]["out"]) for b in range(B)]).astype(np.float32)
```
